# Optimizing a Trainium2 kernel written in Bass

```python
import math
import jax, jax.numpy as jnp
from jax import lax
import numpy as np

D_MODEL = 1024
BATCH = 32
SEQ = 2048
DEPTH = 2

GRID_W = 64
CTX_LEN = 256

GM_DIM = 256
GM_GROUPS = 4
GM_CHUNK = 128
HY_DIM = 256
HY_EMB = 33
HY_BANDS = (HY_EMB - 1) // 2
HY_FFN = 64
HY_DECAY_FAST = 0.3
HY_DECAY_SLOW = 1.5
HY_DECAY_TARGET = 1e-2
HY_DECAY_SHIFT = 0.05
DA_HEADS = 4
DA_HEAD_DIM = 64
DA_V_DIM = 2 * DA_HEAD_DIM
DA_Q_BLOCK = 128
DA_QK_W = DA_HEADS * 2 * DA_HEAD_DIM
DA_V_W = DA_HEADS * DA_V_DIM
ROPE_BASE = 10000.0
N_BRANCH = 3
OFF_GM = 0
OFF_HY = OFF_GM + 2 * GM_DIM
OFF_Q = OFF_HY + 3 * HY_DIM
OFF_K = OFF_Q + DA_QK_W
OFF_GATE = OFF_K + DA_QK_W + DA_V_W
N_IN = OFF_GATE + N_BRANCH * D_MODEL
MOE_GROUPS = 4
MOE_EXPERTS_PER_GROUP = 8
MOE_N_EXPERTS = MOE_GROUPS * MOE_EXPERTS_PER_GROUP
MOE_TOP_K = 2
MOE_HIDDEN = 512
MOE_BLOCK = 256
DEEPNORM_ALPHA = (2.0 * DEPTH) ** 0.25
DEEPNORM_BETA = (8.0 * DEPTH) ** -0.25
LN_EPS = 1e-5

kernel_name = 'hybrid_gmlp_hyena_diffattn_hmoe_block'


def layer_norm(x, g, b):
    xf = x.astype(jnp.float32)
    mu = jnp.mean(xf, axis=-1, keepdims=True)
    var = jnp.mean(jnp.square(xf - mu), axis=-1, keepdims=True)
    y = (xf - mu) * lax.rsqrt(var + LN_EPS) * g.astype(jnp.float32) + b.astype(jnp.float32)
    return y.astype(x.dtype)


def modulate(x, shift, scale):
    return x * (1 + scale) + shift


def chunk_gmlp(z, ln_g, ln_b, ws, bs):
    u, v = jnp.split(jax.nn.gelu(z), 2, axis=-1)
    v = layer_norm(v, ln_g, ln_b)
    B, L, _ = v.shape
    v = v.reshape(B, L // GM_CHUNK, GM_CHUNK, GM_GROUPS, GM_DIM // GM_GROUPS)
    v = jnp.einsum('gpq,bnqgc->bnpgc', ws, v) + jnp.transpose(bs)[:, :, None]
    return u * v.reshape(B, L, GM_DIM)


def short_conv(z, w, b):
    L = z.shape[1]
    zp = jnp.pad(z, ((0, 0), (1, 1), (0, 0)))
    return zp[:, :L] * w[0] + zp[:, 1:L + 1] * w[1] + zp[:, 2:] * w[2] + b


def hyena_filters(L, w1, b1, w2, b2, w3, b3):
    f32 = jnp.float32
    t = jnp.linspace(0.0, 1.0, L, dtype=f32)[:, None]
    w = 2.0 * math.pi * jnp.arange(L, dtype=f32)[:, None] / L
    f = jnp.linspace(1e-4, HY_BANDS - 1, HY_BANDS, dtype=f32)[None, :]
    emb = jnp.concatenate([t, jnp.cos(f * w), -jnp.sin(f * w)], axis=-1)
    h = jnp.sin(emb @ w1.astype(f32) + b1.astype(f32))
    h = jnp.sin(h @ w2.astype(f32) + b2.astype(f32))
    h = h @ w3.astype(f32) + b3.astype(f32)
    max_decay = math.log(HY_DECAY_TARGET) / HY_DECAY_FAST
    min_decay = math.log(HY_DECAY_TARGET) / HY_DECAY_SLOW
    deltas = jnp.abs(jnp.linspace(min_decay, max_decay, HY_DIM, dtype=f32))
    window = jnp.exp(-t * deltas[None, :]) + HY_DECAY_SHIFT
    h = h.reshape(L, 2, HY_DIM) * window[:, None, :]
    return h[:, 0], h[:, 1]


def bidirectional_long_conv(v, h_fwd, h_bwd):
    L = v.shape[1]
    k = jnp.concatenate([h_fwd, jnp.zeros((1, HY_DIM), jnp.float32), jnp.flip(h_bwd[1:], axis=0)], axis=0)
    kf = jnp.fft.rfft(k, axis=0)
    vf = jnp.fft.rfft(v.astype(jnp.float32), n=2 * L, axis=1)
    return jnp.fft.irfft(vf * kf[None], n=2 * L, axis=1)[:, :L]


def hyena_mixer(z, conv_w, conv_b, h_fwd, h_bwd, skip):
    zc = short_conv(z, conv_w, conv_b)
    x0, x1, v = jnp.split(zc, 3, axis=-1)
    v = v * x1
    y = bidirectional_long_conv(v, h_fwd, h_bwd) + v.astype(jnp.float32) * skip.astype(jnp.float32)
    return y.astype(z.dtype) * x0


def axial_rope(rows):
    n_freq = DA_HEAD_DIM // 4
    row = jnp.broadcast_to(jnp.arange(rows)[:, None], (rows, GRID_W)).reshape(-1).astype(jnp.float32)
    col = jnp.broadcast_to(jnp.arange(GRID_W)[None, :], (rows, GRID_W)).reshape(-1).astype(jnp.float32)
    inv = ROPE_BASE ** (-jnp.arange(n_freq, dtype=jnp.float32) / n_freq)
    ang = jnp.stack([row[:, None] * inv, col[:, None] * inv], axis=1)
    return jnp.cos(ang), jnp.sin(ang)


def apply_rope(x, cos, sin):
    n_freq = DA_HEAD_DIM // 4
    xr = x.astype(jnp.float32).reshape(x.shape[:-1] + (2, 2, n_freq))
    x1, x2 = xr[..., 0, :], xr[..., 1, :]
    c = cos[:, None, None]
    s = sin[:, None, None]
    out = jnp.stack([x1 * c - x2 * s, x2 * c + x1 * s], axis=-2)
    return out.reshape(x.shape).astype(x.dtype)


def split_q(zq):
    B, L, _ = zq.shape
    return zq.reshape(B, L, DA_HEADS, 2, DA_HEAD_DIM)


def split_kv(zkv):
    B, L, _ = zkv.shape
    k = zkv[..., :DA_QK_W].reshape(B, L, DA_HEADS, 2, DA_HEAD_DIM)
    v = zkv[..., DA_QK_W:].reshape(B, L, DA_HEADS, DA_V_DIM)
    return k, v


def diff_softmax_attend(q, k, v, lam):
    s = jnp.einsum('bqhmd,bkhmd->bhmqk', q, k, preferred_element_type=jnp.float32) * (DA_HEAD_DIM ** -0.5)
    p = jax.nn.softmax(s, axis=-1)
    a = p[:, :, 0] - lam * p[:, :, 1]
    return jnp.einsum('bhqk,bkhe->bqhe', a.astype(v.dtype), v)


def latent_diff_attention(q, k, v, lam):
    B, L = q.shape[:2]
    nb = L // DA_Q_BLOCK
    qb = jnp.moveaxis(q.reshape((B, nb, DA_Q_BLOCK) + q.shape[2:]), 1, 0)
    ob = lax.map(lambda qq: diff_softmax_attend(qq, k, v, lam), qb)
    return jnp.moveaxis(ob, 0, 1).reshape(B, L, DA_HEADS, DA_V_DIM)


def head_rms(o, g, lam_init):
    B, L = o.shape[:2]
    of = o.astype(jnp.float32)
    of = of * lax.rsqrt(jnp.mean(jnp.square(of), axis=-1, keepdims=True) + LN_EPS) * g.astype(jnp.float32)
    return (of * (1.0 - lam_init)).reshape(B, L, DA_V_W).astype(o.dtype)


def merge_branches(z, y_c, lp):
    L = z.shape[1]
    y_a = chunk_gmlp(z[..., OFF_GM:OFF_HY], lp['gm_ln_g'], lp['gm_ln_b'], lp['gm_ws'], lp['gm_bs'])
    h_fwd, h_bwd = hyena_filters(L, lp['hy_f_w1'], lp['hy_f_b1'], lp['hy_f_w2'], lp['hy_f_b2'],
                                 lp['hy_f_w3'], lp['hy_f_b3'])
    y_b = hyena_mixer(z[..., OFF_HY:OFF_Q], lp['hy_conv_w'], lp['hy_conv_b'], h_fwd, h_bwd, lp['hy_skip'])
    g_a, g_b, g_c = jnp.split(jax.nn.sigmoid(z[..., OFF_GATE:]), N_BRANCH, axis=-1)
    merged = g_a * (y_a @ lp['p_a']) + g_b * (y_b @ lp['p_b']) + g_c * (y_c @ lp['p_c'])
    return merged @ lp['w_out']


def hierarchical_moe(xt, wg, bg, we, be, w_gate, w_up, w_down):
    T, D = xt.shape
    A = T * MOE_TOP_K
    g_logits = (xt @ wg + bg).astype(jnp.float32)
    g_idx = jnp.argmax(g_logits, axis=-1)
    g_prob = jnp.take_along_axis(jax.nn.softmax(g_logits, axis=-1), g_idx[:, None], axis=-1)
    e_logits = (xt @ we + be).astype(jnp.float32).reshape(T, MOE_GROUPS, MOE_EXPERTS_PER_GROUP)
    e_logits = jnp.take_along_axis(e_logits, g_idx[:, None, None], axis=1)[:, 0]
    top_v, top_i = lax.top_k(e_logits, MOE_TOP_K)
    weights = g_prob * jax.nn.softmax(top_v, axis=-1)
    eid = (g_idx[:, None] * MOE_EXPERTS_PER_GROUP + top_i).reshape(-1).astype(jnp.int32)
    w_flat = weights.reshape(-1)
    order = jnp.argsort(eid)
    e_s = eid[order]
    tok_s = (order // MOE_TOP_K).astype(jnp.int32)
    w_s = w_flat[order]
    counts = jnp.bincount(eid, length=MOE_N_EXPERTS)
    starts = jnp.cumsum(counts) - counts
    pcounts = (counts + MOE_BLOCK - 1) // MOE_BLOCK * MOE_BLOCK
    pends = jnp.cumsum(pcounts)
    pstarts = pends - pcounts
    dest = pstarts[e_s] + jnp.arange(A) - starts[e_s]
    n_blk = -(-A // MOE_BLOCK) + MOE_N_EXPERTS
    P = n_blk * MOE_BLOCK
    src = jnp.zeros((P,), jnp.int32).at[dest].set(tok_s)
    wbuf = jnp.zeros((P,), jnp.float32).at[dest].set(w_s)
    valid = jnp.zeros((P,), bool).at[dest].set(True)
    xbuf = jnp.where(valid[:, None], xt[src], 0).reshape(n_blk, MOE_BLOCK, D)
    blk_e = jnp.minimum(jnp.searchsorted(pends, jnp.arange(n_blk) * MOE_BLOCK, side='right'),
                        MOE_N_EXPERTS - 1)

    def expert_block(args):
        xb, e = args
        return (jax.nn.silu(xb @ w_gate[e]) * (xb @ w_up[e])) @ w_down[e]

    ybuf = lax.map(expert_block, (xbuf, blk_e)).reshape(P, D)
    return jax.ops.segment_sum(ybuf * wbuf[:, None].astype(ybuf.dtype), src, num_segments=T)


def setup_inputs(seed: int = 0) -> dict:
    key = jax.random.key(seed)
    ks = iter(jax.random.split(key, 64))
    D = D_MODEL
    L_ = DEPTH

    def nrm(shape, scale):
        return jax.random.normal(next(ks), shape, jnp.float32) * scale

    beta = DEEPNORM_BETA
    return {
        'x': nrm((BATCH, SEQ, D), 1.0),
        'c': nrm((BATCH, D), 1.0),
        'ctx': nrm((BATCH, CTX_LEN, D), 1.0),
        'c_ctx': nrm((D,), 1.0),
        'ada_w': nrm((L_, D, 6 * D), 0.5 * D ** -0.5),
        'ada_b': nrm((L_, 6 * D), 0.02),
        'w_in': nrm((L_, D, N_IN), D ** -0.5),
        'gm_ln_g': 1.0 + nrm((L_, GM_DIM), 0.05),
        'gm_ln_b': nrm((L_, GM_DIM), 0.02),
        'gm_ws': nrm((L_, GM_GROUPS, GM_CHUNK, GM_CHUNK), GM_CHUNK ** -0.5),
        'gm_bs': 1.0 + nrm((L_, GM_GROUPS, GM_CHUNK), 0.1),
        'hy_conv_w': nrm((L_, 3, 3 * HY_DIM), 3 ** -0.5),
        'hy_conv_b': nrm((L_, 3 * HY_DIM), 0.02),
        'hy_f_w1': nrm((L_, HY_EMB, HY_FFN), HY_EMB ** -0.5),
        'hy_f_b1': nrm((L_, HY_FFN), 0.1),
        'hy_f_w2': nrm((L_, HY_FFN, HY_FFN), HY_FFN ** -0.5),
        'hy_f_b2': nrm((L_, HY_FFN), 0.1),
        'hy_f_w3': nrm((L_, HY_FFN, 2 * HY_DIM), HY_FFN ** -0.5),
        'hy_f_b3': nrm((L_, 2 * HY_DIM), 0.02),
        'hy_skip': nrm((L_, HY_DIM), 1.0),
        'da_lq1': nrm((L_, DA_HEAD_DIM), 0.1),
        'da_lk1': nrm((L_, DA_HEAD_DIM), 0.1),
        'da_lq2': nrm((L_, DA_HEAD_DIM), 0.1),
        'da_lk2': nrm((L_, DA_HEAD_DIM), 0.1),
        'da_norm_g': 1.0 + nrm((L_, DA_V_DIM), 0.05),
        'p_a': nrm((L_, GM_DIM, D), beta * GM_DIM ** -0.5),
        'p_b': nrm((L_, HY_DIM, D), beta * HY_DIM ** -0.5),
        'p_c': nrm((L_, DA_V_W, D), beta * DA_V_W ** -0.5),
        'w_out': nrm((L_, D, D), beta * D ** -0.5),
        'ln1_g': 1.0 + nrm((L_, D), 0.05),
        'ln1_b': nrm((L_, D), 0.02),
        'moe_wg': nrm((L_, D, MOE_GROUPS), D ** -0.5),
        'moe_bg': nrm((L_, MOE_GROUPS), 0.01),
        'moe_we': nrm((L_, D, MOE_N_EXPERTS), D ** -0.5),
        'moe_be': nrm((L_, MOE_N_EXPERTS), 0.01),
        'ex_w_gate': nrm((L_, MOE_N_EXPERTS, D, MOE_HIDDEN), D ** -0.5),
        'ex_w_up': nrm((L_, MOE_N_EXPERTS, D, MOE_HIDDEN), D ** -0.5),
        'ex_w_down': nrm((L_, MOE_N_EXPERTS, MOE_HIDDEN, D), beta * MOE_HIDDEN ** -0.5),
        'ln2_g': 1.0 + nrm((L_, D), 0.05),
        'ln2_b': nrm((L_, D), 0.02),
    }


def reference(x, c, ctx, c_ctx, ada_w, ada_b, w_in, gm_ln_g, gm_ln_b, gm_ws, gm_bs,
              hy_conv_w, hy_conv_b, hy_f_w1, hy_f_b1, hy_f_w2, hy_f_b2, hy_f_w3, hy_f_b3, hy_skip,
              da_lq1, da_lk1, da_lq2, da_lk2, da_norm_g, p_a, p_b, p_c, w_out, ln1_g, ln1_b,
              moe_wg, moe_bg, moe_we, moe_be, ex_w_gate, ex_w_up, ex_w_down, ln2_g, ln2_b):
    B, L, D = x.shape
    rows = L // GRID_W
    cos, sin = axial_rope(rows)
    xc = ctx
    for l in range(DEPTH):
        last = l == DEPTH - 1
        lp = {
            'gm_ln_g': gm_ln_g[l], 'gm_ln_b': gm_ln_b[l], 'gm_ws': gm_ws[l], 'gm_bs': gm_bs[l],
            'hy_conv_w': hy_conv_w[l], 'hy_conv_b': hy_conv_b[l],
            'hy_f_w1': hy_f_w1[l], 'hy_f_b1': hy_f_b1[l], 'hy_f_w2': hy_f_w2[l], 'hy_f_b2': hy_f_b2[l],
            'hy_f_w3': hy_f_w3[l], 'hy_f_b3': hy_f_b3[l], 'hy_skip': hy_skip[l],
            'p_a': p_a[l], 'p_b': p_b[l], 'p_c': p_c[l], 'w_out': w_out[l],
        }
        lam_init = 0.8 - 0.6 * math.exp(-0.3 * l)
        lam = (jnp.exp(jnp.sum(da_lq1[l].astype(jnp.float32) * da_lk1[l].astype(jnp.float32)))
               - jnp.exp(jnp.sum(da_lq2[l].astype(jnp.float32) * da_lk2[l].astype(jnp.float32)))
               + lam_init)
        mod = jax.nn.silu(c) @ ada_w[l] + ada_b[l]
        mod_c = jax.nn.silu(c_ctx) @ ada_w[l] + ada_b[l]
        sh1, sc1, g1, sh2, sc2, g2 = jnp.split(mod[:, None, :], 6, axis=-1)
        csh1, csc1, cg1, csh2, csc2, cg2 = jnp.split(mod_c, 6, axis=-1)

        h = modulate(x, sh1, sc1)
        hc = modulate(xc, csh1, csc1)
        z = h @ w_in[l]
        if last:
            k_c, v_c = split_kv(hc @ w_in[l][:, OFF_K:OFF_GATE])
        else:
            zc = hc @ w_in[l]
            k_c, v_c = split_kv(zc[..., OFF_K:OFF_GATE])
        q_l = apply_rope(split_q(z[..., OFF_Q:OFF_K]), cos, sin)
        k_l, v_l = split_kv(z[..., OFF_K:OFF_GATE])
        k_l = apply_rope(k_l, cos, sin)
        k_all = jnp.concatenate([k_l, k_c], axis=1)
        v_all = jnp.concatenate([v_l, v_c], axis=1)
        y_c = head_rms(latent_diff_attention(q_l, k_all, v_all, lam), da_norm_g[l], lam_init)
        out = merge_branches(z, y_c, lp)
        x = layer_norm(DEEPNORM_ALPHA * x + g1 * out, ln1_g[l], ln1_b[l])
        if not last:
            q_c = split_q(zc[..., OFF_Q:OFF_K])
            yc_c = head_rms(diff_softmax_attend(q_c, k_c, v_c, lam), da_norm_g[l], lam_init)
            out_c = merge_branches(zc, yc_c, lp)
            xc = layer_norm(DEEPNORM_ALPHA * xc + cg1 * out_c, ln1_g[l], ln1_b[l])

        h2 = modulate(x, sh2, sc2).reshape(B * L, D)
        if last:
            y = hierarchical_moe(h2, moe_wg[l], moe_bg[l], moe_we[l], moe_be[l],
                                 ex_w_gate[l], ex_w_up[l], ex_w_down[l]).reshape(B, L, D)
        else:
            Lc = xc.shape[1]
            h2c = modulate(xc, csh2, csc2).reshape(B * Lc, D)
            y_all = hierarchical_moe(jnp.concatenate([h2, h2c], axis=0), moe_wg[l], moe_bg[l],
                                     moe_we[l], moe_be[l], ex_w_gate[l], ex_w_up[l], ex_w_down[l])
            y = y_all[:B * L].reshape(B, L, D)
            y_c2 = y_all[B * L:].reshape(B, Lc, D)
            xc = layer_norm(DEEPNORM_ALPHA * xc + cg2 * y_c2, ln2_g[l], ln2_b[l])
        x = layer_norm(DEEPNORM_ALPHA * x + g2 * y, ln2_g[l], ln2_b[l])
    return x
```

```python
import math
from contextlib import ExitStack

import numpy as np
import concourse.bass as bass
import concourse.mybir as mybir
from concourse.bass_utils import run_bass_kernel_spmd

F32 = mybir.dt.float32
BF16 = mybir.dt.bfloat16
I32 = mybir.dt.int32
AF = mybir.ActivationFunctionType
ALU = mybir.AluOpType
AX = mybir.AxisListType

D = 1024
DEPTH = 2
GRID_W = 64
N_IN = 5888
OFF_HY, OFF_Q, OFF_K, OFF_V, OFF_GATE = 512, 1280, 1792, 2304, 2816
NWB = N_IN + 1024
HY_EMB, HY_FFN, HY_BANDS = 33, 64, 16
NE, NG, EPG, HID = 32, 4, 8, 512
ALPHA = (2.0 * DEPTH) ** 0.25
EPS = 1e-5
BIG = 1.0e30

ENGINES = ("tensor", "vector", "scalar", "gpsimd", "sync")
N_DMA_SEMS = 40


class Prog:
    def __init__(self, nc, stack):
        self.nc = nc
        self.ops = []
        self.esem = {e: stack.enter_context(nc.semaphore("s_" + e)) for e in ENGINES}
        self.dsem = [stack.enter_context(nc.semaphore("d%d" % i)) for i in range(N_DMA_SEMS)]
        self.ecount = {e: 0 for e in ENGINES}
        self.dcount = [0] * N_DMA_SEMS
        self.dnext = 0
        self.lastw = {}
        self.readers = {}
        self.known = {}
        self.nops = 0

    def add(self, eng, fn, r=(), w=(), dma=False):
        self.ops.append((eng, fn, tuple(r), tuple(w), dma))

    def flush(self, final=False):
        nc = self.nc
        esem, dsem, ecount, dcount = self.esem, self.dsem, self.ecount, self.dcount
        lastw, readers, known = self.lastw, self.readers, self.known
        plan = {e: [] for e in ENGINES}
        fence = [(("d", i), dcount[i]) for i in range(N_DMA_SEMS) if dcount[i] > 0]
        fence += [(("e", e), ecount[e]) for e in ENGINES if ecount[e] > 0]
        for (eng, fn, r, w, dma) in self.ops:
            deps = []
            for k in r:
                t = lastw.get(k)
                if t is not None:
                    deps.append(t)
            for k in w:
                t = lastw.get(k)
                if t is not None:
                    deps.append(t)
                for tk, tv in readers.get(k, {}).items():
                    deps.append((tk[0], tk[1], tv))
            if dma:
                si = self.dnext
                self.dnext = (self.dnext + 1) % N_DMA_SEMS
                if dcount[si] > 0:
                    deps.append(("d", si, dcount[si]))
                dcount[si] += 16
                tok = ("d", si, dcount[si])
                inc = (dsem[si], 16)
            else:
                ecount[eng] += 1
                tok = ("e", eng, ecount[eng])
                inc = (esem[eng], 1)
            waits = {}
            for (kind, key, val) in deps:
                if kind == "e" and key == eng and eng == "tensor":
                    continue
                sk = (kind, key)
                if known.get((eng, sk), 0) >= val:
                    continue
                if waits.get(sk, 0) < val:
                    waits[sk] = val
            wl = []
            for sk, val in waits.items():
                known[(eng, sk)] = val
                wl.append((esem[sk[1]] if sk[0] == "e" else dsem[sk[1]], val))
            plan[eng].append((wl, fn, inc))
            for k in r:
                d = readers.setdefault(k, {})
                if d.get(tok[:2], 0) < tok[2]:
                    d[tok[:2]] = tok[2]
            for k in w:
                lastw[k] = tok
                readers[k] = {}
        self.nops += len(self.ops)
        self.ops = []
        endw = []
        if final:
            endw = [(dsem[i], dcount[i]) for i in range(N_DMA_SEMS) if dcount[i] > 0]
            endw += [(esem[e], ecount[e]) for e in ENGINES if ecount[e] > 0]

        def runner(ename):
            def body(eng):
                for sk, val in fence:
                    if sk == ("e", ename):
                        continue
                    if known.get((ename, sk), 0) >= val:
                        continue
                    known[(ename, sk)] = val
                    eng.wait_ge(esem[sk[1]] if sk[0] == "e" else dsem[sk[1]], val)
                for (wl, fn, inc) in plan[ename]:
                    for (sem, val) in wl:
                        eng.wait_ge(sem, val)
                    ins = fn(eng)
                    ins.then_inc(inc[0], inc[1])
                for (sem, val) in endw:
                    eng.wait_ge(sem, val)
            return body

        with nc.Block() as block:
            block.tensor(runner("tensor"))
            block.vector(runner("vector"))
            block.scalar(runner("scalar"))
            block.gpsimd(runner("gpsimd"))
            block.sync(runner("sync"))


class Cfg:
    def __init__(self, NB=4, L=2048, LC=256, BLK=512, stop=None):
        self.NB, self.L, self.LC, self.BLK, self.stop = NB, L, LC, BLK, stop


class _Stop(Exception):
    pass


def _ap(base, off, dims, part=None):
    p = list(base.ap[0]) if part is None else [base.ap[0][0], part]
    return bass.AP(tensor=base.tensor, offset=base.offset + off, ap=[p] + [list(d) for d in dims])


class Rot:
    def __init__(self, tiles, name):
        self.tiles, self.name, self.i = tiles, name, 0

    def next(self):
        t = self.tiles[self.i % len(self.tiles)]
        k = "%s%d" % (self.name, self.i % len(self.tiles))
        self.i += 1
        return t, k


def build(cfg, debug=False):
    holder = {}
    try:
        _build(cfg, debug, holder)
    except _Stop:
        pass
    return holder["nc"]


def _build(cfg, debug, holder):
    NB, L, LC, BLK = cfg.NB, cfg.L, cfg.LC, cfg.BLK
    nc = bass.Bass("TRN2", target_bir_lowering=False)
    holder["nc"] = nc
    T = NB * (L + LC)
    NT = T // 128
    NTL = NB * L // 128
    RB = BLK // 128

    def din(name, shape, dt=F32):
        return nc.dram_tensor(name, list(shape), dt, kind="ExternalInput").ap()

    def dscr(name, shape, dt):
        return nc.dram_tensor(name, list(shape), dt, kind="ExternalOutput" if debug else "Internal").ap()

    x_in = din("x", [NB * L, D])
    ctx_in = din("ctx", [NB * LC, D])
    cT_in = din("cT", [128, 8, NB + 1])
    ada_w = din("ada_w", [DEPTH, D, 6 * D])
    ada_b = din("ada_b", [DEPTH, 6 * D])
    w_in = din("w_in", [DEPTH, D, N_IN])
    gm_ln_g = din("gm_ln_g", [DEPTH, 256])
    gm_ln_b = din("gm_ln_b", [DEPTH, 256])
    gm_wsT = din("gm_wsT", [DEPTH, 128, 4, 128])
    gm_bsT = din("gm_bsT", [DEPTH, 128, 4])
    hy_cw = din("hy_cw", [DEPTH, 128, 6, 4])
    hy_w1 = din("hy_f_w1", [DEPTH, HY_EMB, HY_FFN])
    hy_b1 = din("hy_f_b1", [DEPTH, HY_FFN, 1])
    hy_w2 = din("hy_f_w2", [DEPTH, HY_FFN, HY_FFN])
    hy_b2 = din("hy_f_b2", [DEPTH, HY_FFN, 1])
    hy_w3 = din("hy_f_w3", [DEPTH, HY_FFN, 512])
    hy_b3T = din("hy_b3T", [DEPTH, 128, 4])
    hy_skipT = din("hy_skipT", [DEPTH, 128, 2])
    da_l = din("da_l", [DEPTH, 4, 64])
    da_g = din("da_norm_g", [DEPTH, 128])
    p_a = din("p_a", [DEPTH, 256, D])
    p_b = din("p_b", [DEPTH, 256, D])
    p_c = din("p_c", [DEPTH, 512, D])
    w_out = din("w_out", [DEPTH, D, D])
    ln1_g = din("ln1_g", [DEPTH, D])
    ln1_b = din("ln1_b", [DEPTH, D])
    moe_wr = din("moe_wr", [DEPTH, D, 36])
    moe_br = din("moe_br", [DEPTH, 36])
    ex_g = din("ex_w_gate", [DEPTH * NE * D, HID])
    ex_u = din("ex_w_up", [DEPTH * NE * D, HID])
    ex_d = din("ex_w_down", [DEPTH * NE * HID, D])
    ln2_g = din("ln2_g", [DEPTH, D])
    ln2_b = din("ln2_b", [DEPTH, D])
    ident_in = din("c_ident", [128, 128])
    rope_in = din("c_rope", [2, 128, L])
    emb_in = {L: din("c_emb_L", [2, HY_EMB, L]), LC: din("c_emb_C", [2, HY_EMB, LC])}
    win_in = {L: din("c_win_L", [2, 256, L]), LC: din("c_win_C", [2, 256, LC])}
    NBLK = -(-(2 * T) // BLK) + NE
    MMAX = -(-(2 * T) // BLK) + 1
    cmoe_in = din("c_moe", [128, 128 + 8 + 4 + MMAX + NBLK + 32])
    out = nc.dram_tensor("out", [NB * L, D], F32, kind="ExternalOutput").ap()

    Wb = dscr("s_wb", [8, 128, NWB], BF16)
    MODS = dscr("s_mods", [DEPTH, NB + 1, 6 * D], F32)
    KREV = {L: dscr("s_krevL", [256, 2 * L], BF16), LC: dscr("s_krevC", [256, 2 * LC], BF16)}
    QTd = dscr("s_qt", [4, 128, T], BF16)
    KTd = dscr("s_kt", [4, 128, T], BF16)
    Vd = dscr("s_v", [T, 512], BF16)
    Gd = dscr("s_g", [24, 128, T], BF16)
    YATd = dscr("s_yat", [2, 128, T], BF16)
    X0Td = dscr("s_x0t", [2, 128, T], BF16)
    YBTd = dscr("s_ybt", [2, 128, T], BF16)
    YCTd = dscr("s_yct", [4, 128, T], BF16)
    X1d = dscr("s_x1", [T, D], F32)
    X2d = dscr("s_x2", [T, D], F32)
    H2d = dscr("s_h2", [T, D], BF16)
    XBUF = dscr("s_xbuf", [NBLK * BLK, D], BF16)
    YBUF = dscr("s_ybuf", [NBLK * BLK, D], BF16)

    seqs = [("lat", b, b * L, L) for b in range(NB)] + [("ctx", b, NB * L + b * LC, LC) for b in range(NB)]

    with ExitStack() as top:
        p = Prog(nc, top)

        uniq = [0]

        def sbt(st, name, shape, dt):
            uniq[0] += 1
            return st.enter_context(nc.sbuf_tensor("%s_%d" % (name, uniq[0]), list(shape), dt))

        def pst(st, name, shape, dt=F32):
            uniq[0] += 1
            return st.enter_context(nc.psum_tensor("%s_%d" % (name, uniq[0]), list(shape), dt))

        def dma(eng, out_, in_, r, w, **kw):
            p.add(eng, lambda e: e.dma_start(out=out_, in_=in_, **kw), r, w, dma=True)

        def mm(out_, lhsT, rhs, start, stop, r, w):
            p.add("tensor", lambda e: e.matmul(out_, lhsT=lhsT, rhs=rhs, start=start, stop=stop,
                                               skip_group_check=True), r, w)

        def tr(out_, in_, ident, r, w):
            p.add("tensor", lambda e: e.transpose(out=out_, in_=in_, identity=ident), r, w)

        def act(out_, in_, func, r, w, **kw):
            p.add("scalar", lambda e: e.activation(out=out_, in_=in_, func=func, **kw), r, w)

        def tt(eng, out_, a, b, op, r, w):
            p.add(eng, lambda e: e.tensor_tensor(out=out_, in0=a, in1=b, op=op), r, w)

        def ts(eng, out_, a, s1, op0, r, w, s2=None, op1=None):
            if op1 is None:
                p.add(eng, lambda e: e.tensor_scalar(out=out_, in0=a, scalar1=s1, scalar2=None, op0=op0), r, w)
            else:
                p.add(eng, lambda e: e.tensor_scalar(out=out_, in0=a, scalar1=s1, scalar2=s2, op0=op0, op1=op1), r, w)

        def stt(out_, a, s, b, op0, op1, r, w):
            p.add("vector", lambda e: e.scalar_tensor_tensor(out=out_, in0=a, scalar=s, in1=b, op0=op0, op1=op1), r, w)

        def cp(eng, out_, in_, r, w):
            if eng == "scalar":
                p.add(eng, lambda e: e.copy(out=out_, in_=in_), r, w)
            else:
                p.add(eng, lambda e: e.tensor_copy(out=out_, in_=in_), r, w)

        def red(out_, in_, op, axis, r, w):
            p.add("vector", lambda e: e.tensor_reduce(out=out_, in_=in_, axis=axis, op=op), r, w)

        def memset(eng, ap_, val, w):
            p.add(eng, lambda e: e.memset(ap_, val), (), w)

        phase_no = [0]

        def ph_end(final=False):
            phase_no[0] += 1
            stop = cfg.stop is not None and phase_no[0] >= cfg.stop
            p.flush(final=final or stop)
            if stop and not final:
                raise _Stop()

        identf = sbt(top, "identf", [128, 128], F32)
        identb = sbt(top, "identb", [128, 128], BF16)
        ropeT = sbt(top, "ropeT", [128, 2, L], BF16)
        epsc = sbt(top, "epsc", [128, 1], F32)
        dma("sync", identf[:], ident_in[:, :], [], ["identf"])
        cp("vector", identb[:], identf[:], ["identf"], ["identb"])
        with ExitStack() as ph:
            ropeF = sbt(ph, "ropeF", [128, 2, L], F32)
            dma("sync", ropeF[:, 0, :], rope_in[0, :, :], [], ["ropeF"])
            dma("sync", ropeF[:, 1, :], rope_in[1, :, :], [], ["ropeF"])
            cp("vector", ropeT[:], ropeF[:], ["ropeF"], ["ropeT"])
            memset("vector", epsc[:], EPS, ["epsc"])
            ph_end()

        def layer_norm_rows(st_tag, r_t, rk, g_b, b_b, out_t, ok, tmp):
            stats, mv, rstd = tmp["stats"], tmp["mv"], tmp["rstd"]
            for hh in range(2):
                p.add("vector", lambda e, hh=hh: e.bn_stats(out=stats[:, hh, :], in_=r_t[:, hh * 512:(hh + 1) * 512]),
                      [rk], [st_tag + "stats"])
            p.add("vector", lambda e: e.bn_aggr(out=mv[:], in_=stats[:].rearrange("p a b -> p (a b)")),
                  [st_tag + "stats"], [st_tag + "mv"])
            act(rstd[:], mv[:, 1:2], AF.Sqrt, [st_tag + "mv", "epsc"], [st_tag + "rstd"], bias=epsc[:], scale=1.0)
            p.add("vector", lambda e: e.reciprocal(out=rstd[:], in_=rstd[:]), [st_tag + "rstd"], [st_tag + "rstd"])
            ts("vector", out_t[:], r_t[:], mv[:, 0:1], ALU.subtract, [rk, st_tag + "mv", st_tag + "rstd"], [ok],
               s2=rstd[:, 0:1], op1=ALU.mult)
            tt("gpsimd", out_t[:], out_t[:], g_b[:], ALU.mult, [ok, "lnconst"], [ok])
            tt("gpsimd", out_t[:], out_t[:], b_b[:], ALU.add, [ok, "lnconst"], [ok])

        for l in range(DEPTH):
            last = l == DEPTH - 1
            lam_init = 0.8 - 0.6 * math.exp(-0.3 * l)
            Xsrc = (lambda tok0, n: (x_in[tok0:tok0 + n, :] if tok0 < NB * L else ctx_in[tok0 - NB * L:tok0 - NB * L + n, :])) \
                if l == 0 else (lambda tok0, n: X2d[tok0:tok0 + n, :])
            act_seqs = [s for s in seqs if not (last and s[0] == "ctx")]
            NTm = (NTL if last else NT)
            with ExitStack() as lay:
                lamt = sbt(lay, "lamt", [128, 4], F32)
                gsc = sbt(lay, "gsc", [128, 128], F32)

                with ExitStack() as ph:
                    wf = [sbt(ph, "wf%d" % i, [128, N_IN], F32) for i in range(2)]
                    wbt = [sbt(ph, "wbt%d" % i, [128, NWB], BF16) for i in range(2)]
                    for kt in range(8):
                        a, b_ = wf[kt % 2], wbt[kt % 2]
                        ka, kb = "wf%d" % (kt % 2), "wbt%d" % (kt % 2)
                        dma("sync", a[:], w_in[l, kt * 128:(kt + 1) * 128, :], [], [ka])
                        cp("vector", b_[:, 0:2048], a[:, 0:2048], [ka], [kb + "a"])
                        cp("gpsimd", b_[:, 2048:4096], a[:, 2048:4096], [ka], [kb + "b"])
                        cp("scalar", b_[:, 4096:N_IN], a[:, 4096:N_IN], [ka], [kb + "c"])
                        for qi, off in enumerate((OFF_Q, OFF_K)):
                            for rr in range(2):
                                o_ = _ap(b_[:], N_IN + qi * 512 + rr * 16, [[32, 16], [1, 16]])
                                i_ = _ap(a[:], off + (1 - rr) * 16, [[32, 16], [1, 16]])
                                cp("vector", o_, i_, [ka], [kb + "d%d%d" % (qi, rr)])
                        dma("sync", Wb[kt, :, :], b_[:], [kb + "a", kb + "b", kb + "c", kb + "d00", kb + "d01", kb + "d10", kb + "d11"], ["Wb"])
                    dl = sbt(ph, "dl", [128, 4, 64], F32)
                    dg = sbt(ph, "dg", [128, 128], F32)
                    pr = sbt(ph, "pr", [128, 2, 64], F32)
                    dma("sync", dl[:], bass.AP(tensor=da_l.tensor, offset=da_l.offset + l * 256, ap=[[0, 128], [64, 4], [1, 64]]), [], ["dl"])
                    dma("sync", dg[:], bass.AP(tensor=da_g.tensor, offset=da_g.offset + l * 128, ap=[[0, 128], [1, 128]]), [], ["dg"])
                    tt("vector", pr[:, 0, :], dl[:, 0, :], dl[:, 1, :], ALU.mult, ["dl"], ["pr"])
                    tt("vector", pr[:, 1, :], dl[:, 2, :], dl[:, 3, :], ALU.mult, ["dl"], ["pr"])
                    red(lamt[:, 1:3], pr[:], ALU.add, AX.X, ["pr"], ["lamt"])
                    act(lamt[:, 1:3], lamt[:, 1:3], AF.Exp, ["lamt"], ["lamt"])
                    tt("vector", lamt[:, 0:1], lamt[:, 2:3], lamt[:, 1:2], ALU.subtract, ["lamt"], ["lamt"])
                    ts("vector", lamt[:, 0:1], lamt[:, 0:1], -lam_init, ALU.add, ["lamt"], ["lamt"])
                    ts("vector", gsc[:], dg[:], 1.0 - lam_init, ALU.mult, ["dg"], ["gsc"])
                    ph_end()

                with ExitStack() as ph:
                    cTt = sbt(ph, "cTt", [128, 8, NB + 1], F32)
                    sct = sbt(ph, "sct", [128, 8, NB + 1], F32)
                    adb = sbt(ph, "adb", [NB + 1, 6 * D], F32)
                    modt = sbt(ph, "modt", [NB + 1, 6 * D], F32)
                    awt = [sbt(ph, "awt%d" % i, [128, 3072], F32) for i in range(2)]
                    psm = pst(ph, "psm", [128, 3072])
                    dma("sync", cTt[:], cT_in[:, :, :], [], ["cTt"])
                    dma("sync", adb[:], bass.AP(tensor=ada_b.tensor, offset=ada_b.offset + l * 6 * D,
                                                ap=[[0, NB + 1], [1, 6 * D]]), [], ["adb"])
                    act(sct[:], cTt[:], AF.Silu, ["cTt"], ["sct"])
                    i = 0
                    for half in range(2):
                        for kt in range(8):
                            a, ka = awt[i % 2], "awt%d" % (i % 2)
                            i += 1
                            dma("sync" if kt % 2 == 0 else "gpsimd", a[:],
                                ada_w[l, kt * 128:(kt + 1) * 128, half * 3072:(half + 1) * 3072], [], [ka])
                            for ng in range(6):
                                mm(psm[0:NB + 1, ng * 512:(ng + 1) * 512], sct[:, kt, :], a[:, ng * 512:(ng + 1) * 512],
                                   kt == 0, kt == 7, [ka, "sct"], ["psm"])
                        tt("vector", modt[:, half * 3072:(half + 1) * 3072], psm[0:NB + 1, :],
                           adb[:, half * 3072:(half + 1) * 3072], ALU.add, ["psm", "adb"], ["modt"])
                    dma("sync", MODS[l, :, :], modt[:], ["modt"], ["MODS"])
                    ph_end()

                with ExitStack() as ph:
                    w1f = sbt(ph, "w1f", [HY_EMB, HY_FFN], F32)
                    w2f = sbt(ph, "w2f", [HY_FFN, HY_FFN], F32)
                    w3f = sbt(ph, "w3f", [HY_FFN, 512], F32)
                    b1t = sbt(ph, "b1t", [HY_FFN, 1], F32)
                    b2t = sbt(ph, "b2t", [HY_FFN, 1], F32)
                    b3t = sbt(ph, "b3t", [128, 4], F32)
                    skt = sbt(ph, "skt", [128, 2], F32)
                    dma("sync", w1f[:], hy_w1[l, :, :], [], ["hyw"])
                    dma("sync", w2f[:], hy_w2[l, :, :], [], ["hyw"])
                    dma("sync", w3f[:], hy_w3[l, :, :], [], ["hyw"])
                    dma("sync", b1t[:], hy_b1[l, :, :], [], ["hyw"])
                    dma("sync", b2t[:], hy_b2[l, :, :], [], ["hyw"])
                    dma("sync", b3t[:], hy_b3T[l, :, :], [], ["hyw"])
                    dma("sync", skt[:], hy_skipT[l, :, :], [], ["hyw"])
                    ps1 = pst(ph, "ps1", [128, 512])
                    ps2 = pst(ph, "ps2", [128, 512])
                    ps3 = pst(ph, "ps3", [128, 512])
                    for Lf in ([L] if last else [L, LC]):
                        with ExitStack() as ph2:
                            CHF = min(512, Lf)
                            embt = sbt(ph2, "embt", [HY_EMB, 2, Lf], F32)
                            wint = sbt(ph2, "wint", [128, 2, 2, Lf], F32)
                            krf = sbt(ph2, "krf", [128, 2, 2 * Lf], F32)
                            krb = sbt(ph2, "krb", [128, 2, 2 * Lf], BF16)
                            h1 = sbt(ph2, "h1", [HY_FFN, 512], F32)
                            h2 = sbt(ph2, "h2", [HY_FFN, 512], F32)
                            wr1 = sbt(ph2, "wr1", [HY_FFN, 512], F32)
                            wr2 = sbt(ph2, "wr2", [HY_FFN, 512], F32)
                            tg = "f%d" % Lf
                            for dr in range(2):
                                dma("sync", embt[:, dr, :], emb_in[Lf][dr, :, :], [], [tg + "emb"])
                                for cc in range(2):
                                    dma("gpsimd", wint[:, dr, cc, :], win_in[Lf][dr, cc * 128:(cc + 1) * 128, :], [], [tg + "win"])
                            memset("gpsimd", krf[:], 0.0, [tg + "krf"])
                            for dr in range(2):
                                for ch in range(Lf // CHF):
                                    cs = slice(ch * CHF, (ch + 1) * CHF)
                                    mm(ps1[0:HY_FFN, 0:CHF], w1f[:], embt[:, dr, cs], True, True, ["hyw", tg + "emb"], ["ps1"])
                                    ts("vector", h1[:, 0:CHF], ps1[0:HY_FFN, 0:CHF], b1t[:, 0:1], ALU.add, ["ps1", "hyw"], ["h1"])
                                    ts("vector", wr1[:, 0:CHF], h1[:, 0:CHF], math.pi, ALU.is_gt, ["h1"], ["wr1"], s2=-2 * math.pi, op1=ALU.mult)
                                    ts("vector", wr2[:, 0:CHF], h1[:, 0:CHF], -math.pi, ALU.is_lt, ["h1"], ["wr2"], s2=2 * math.pi, op1=ALU.mult)
                                    tt("vector", h1[:, 0:CHF], h1[:, 0:CHF], wr1[:, 0:CHF], ALU.add, ["h1", "wr1"], ["h1"])
                                    tt("vector", h1[:, 0:CHF], h1[:, 0:CHF], wr2[:, 0:CHF], ALU.add, ["h1", "wr2"], ["h1"])
                                    act(h1[:, 0:CHF], h1[:, 0:CHF], AF.Sin, ["h1"], ["h1"])
                                    mm(ps2[0:HY_FFN, 0:CHF], w2f[:], h1[:, 0:CHF], True, True, ["hyw", "h1"], ["ps2"])
                                    ts("vector", h2[:, 0:CHF], ps2[0:HY_FFN, 0:CHF], b2t[:, 0:1], ALU.add, ["ps2", "hyw"], ["h2"])
                                    ts("vector", wr1[:, 0:CHF], h2[:, 0:CHF], math.pi, ALU.is_gt, ["h2"], ["wr1"], s2=-2 * math.pi, op1=ALU.mult)
                                    ts("vector", wr2[:, 0:CHF], h2[:, 0:CHF], -math.pi, ALU.is_lt, ["h2"], ["wr2"], s2=2 * math.pi, op1=ALU.mult)
                                    tt("vector", h2[:, 0:CHF], h2[:, 0:CHF], wr1[:, 0:CHF], ALU.add, ["h2", "wr1"], ["h2"])
                                    tt("vector", h2[:, 0:CHF], h2[:, 0:CHF], wr2[:, 0:CHF], ALU.add, ["h2", "wr2"], ["h2"])
                                    act(h2[:, 0:CHF], h2[:, 0:CHF], AF.Sin, ["h2"], ["h2"])
                                    for cc in range(2):
                                        mm(ps3[:, 0:CHF], w3f[:, dr * 256 + cc * 128:dr * 256 + (cc + 1) * 128], h2[:, 0:CHF],
                                           True, True, ["hyw", "h2"], ["ps3"])
                                        o0 = dr * Lf + ch * CHF
                                        stt(krf[:, cc, o0:o0 + CHF], ps3[:, 0:CHF], b3t[:, dr * 2 + cc:dr * 2 + cc + 1],
                                            wint[:, dr, cc, cs], ALU.add, ALU.mult, ["ps3", "hyw", tg + "win", tg + "krf"], [tg + "krf"])
                            for cc in range(2):
                                ts("vector", krf[:, cc, Lf - 1:Lf], krf[:, cc, Lf - 1:Lf], skt[:, cc:cc + 1], ALU.add,
                                   [tg + "krf", "hyw"], [tg + "krf"])
                            cp("vector", krb[:, 0, :], krf[:, 0, :], [tg + "krf"], [tg + "krb"])
                            cp("gpsimd", krb[:, 1, :], krf[:, 1, :], [tg + "krf"], [tg + "krb"])
                            for cc in range(2):
                                dma("sync", KREV[Lf][cc * 128:(cc + 1) * 128, :], krb[:, cc, :], [tg + "krb"], ["KREV%d" % Lf])
                            ph_end()

                with ExitStack() as ph45:
                    nbL, nbC = L // 128, LC // 128
                    VXs = {"lat": sbt(ph45, "VXsL", [128, 256, nbL, NB], BF16)}
                    if not last:
                        VXs["ctx"] = sbt(ph45, "VXsC", [128, 256, nbC, NB], BF16)
                    with ExitStack() as ph:
                        LMAX = L
                        hT = sbt(ph, "hT", [128, 8, LMAX], BF16)
                        zhs = Rot([sbt(ph, "zh%d" % i, [128, LMAX + 2], F32) for i in range(2)], "zh")
                        VT2 = sbt(ph, "VT2", [128, 2, LMAX], F32)
                        tmpA = sbt(ph, "tmpA", [128, LMAX], F32)
                        x0bs = Rot([sbt(ph, "x0b%d" % i, [128, LMAX], BF16) for i in range(2)], "x0b")
                        wgs = Rot([sbt(ph, "wg%d" % i, [128, 8, 512], BF16) for i in range(3)], "wg")
                        scb = sbt(ph, "scb", [128, D], F32)
                        shb = sbt(ph, "shb", [128, D], F32)
                        xts = Rot([sbt(ph, "xt%d" % i, [128, D], F32) for i in range(2)], "xt")
                        hbs = Rot([sbt(ph, "hb%d" % i, [128, D], BF16) for i in range(2)], "hb")
                        lngb = sbt(ph, "lngb", [128, 256], F32)
                        lnbb = sbt(ph, "lnbb", [128, 256], F32)
                        wsf = sbt(ph, "wsf", [128, 4, 128], F32)
                        wsb = sbt(ph, "wsb", [128, 4, 128], BF16)
                        bst = sbt(ph, "bst", [128, 4], F32)
                        cwt = sbt(ph, "cwt", [128, 6, 4], F32)
                        gmf = sbt(ph, "gmf", [128, 512], F32)
                        vnb = sbt(ph, "vnb", [128, 256], BF16)
                        vnf = sbt(ph, "vnf", [128, 256], F32)
                        yab = sbt(ph, "yab", [128, 256], BF16)
                        yaT = sbt(ph, "yaT", [128, 2, 128], BF16)
                        gst = sbt(ph, "gst", [128, 6], F32)
                        gmv = sbt(ph, "gmv", [128, 2], F32)
                        grs = sbt(ph, "grs", [128, 1], F32)
                        vts = Rot([sbt(ph, "vt%d" % i, [128, 512], BF16) for i in range(2)], "vt")
                        fos = Rot([sbt(ph, "fo%d" % i, [128, 512], BF16) for i in range(3)], "fo")
                        t1s = Rot([sbt(ph, "t1_%d" % i, [128, 512], F32) for i in range(2)], "t1_")
                        t2s = Rot([sbt(ph, "t2_%d" % i, [128, 512], F32) for i in range(2)], "t2_")
                        VXT = sbt(ph, "VXT", [128, 2, LMAX], BF16)
                        psH = pst(ph, "psH", [128, 8, 128], BF16)
                        psFs = Rot([pst(ph, "psF%d" % i, [128, 512]) for i in range(2)], "psF")
                        psPs = Rot([pst(ph, "psP%d" % i, [128, 512]) for i in range(2)], "psP")
                        psT = pst(ph, "psT", [128, 512])
                        psS = pst(ph, "psS", [128, 256])
                        psYA = pst(ph, "psYA", [128, 2, 128], BF16)
                        dma("sync", lngb[:], bass.AP(tensor=gm_ln_g.tensor, offset=gm_ln_g.offset + l * 256, ap=[[0, 128], [1, 256]]), [], ["gmc"])
                        dma("sync", lnbb[:], bass.AP(tensor=gm_ln_b.tensor, offset=gm_ln_b.offset + l * 256, ap=[[0, 128], [1, 256]]), [], ["gmc"])
                        dma("sync", wsf[:], gm_wsT[l, :, :, :], [], ["wsf"])
                        cp("vector", wsb[:], wsf[:], ["wsf"], ["gmc"])
                        dma("sync", bst[:], gm_bsT[l, :, :], [], ["gmc"])
                        dma("sync", cwt[:], hy_cw[l, :, :, :], [], ["gmc"])
                        for zt in zhs.tiles:
                            memset("vector", zt[:, 0:1], 0.0, ["zh0", "zh1"])

                        for (kind, b, tok0, Ls) in seqs:
                            full = not (last and kind == "ctx")
                            CH = min(512, Ls)
                            nch = Ls // CH
                            ntile = Ls // 128
                            mrow = b if kind == "lat" else NB
                            rope = kind == "lat"
                            dma("sync", shb[:], bass.AP(tensor=MODS.tensor, offset=MODS.offset + (l * (NB + 1) + mrow) * 6 * D,
                                                        ap=[[0, 128], [1, D]]), ["MODS"], ["shb"])
                            dma("sync", scb[:], bass.AP(tensor=MODS.tensor, offset=MODS.offset + (l * (NB + 1) + mrow) * 6 * D + D,
                                                        ap=[[0, 128], [1, D]]), ["MODS"], ["scb"])
                            ts("gpsimd", scb[:], scb[:], 1.0, ALU.add, ["scb"], ["scb"])
                            for zt in zhs.tiles:
                                memset("vector", zt[:, Ls + 1:Ls + 2], 0.0, ["zh0", "zh1"])
                            for tI in range(ntile):
                                xt, xk = xts.next()
                                hb, hk = hbs.next()
                                dma("sync" if tI % 2 == 0 else "gpsimd", xt[:], Xsrc(tok0 + tI * 128, 128), ["X2d"] if l else [], [xk])
                                tt("vector", xt[:], xt[:], scb[:], ALU.mult, [xk, "scb"], [xk])
                                tt("gpsimd", hb[:], xt[:], shb[:], ALU.add, [xk, "shb"], [hk])
                                for kt in range(8):
                                    tr(psH[:, kt, :], hb[:, kt * 128:(kt + 1) * 128], identb[:], [hk, "identb"], ["psH"])
                                cp("scalar", hT[:, :, tI * 128:(tI + 1) * 128], psH[:], ["psH"], ["hT"])

                            def load_group(col0):
                                wg, wk = wgs.next()
                                dma("sync", wg[:], Wb[:, :, col0:col0 + 512].rearrange("k p c -> p k c"), ["Wb"], [wk])
                                return wg, wk

                            def fm_chunk(wg, wk, ci, tc, ps, pk):
                                for kt in range(8):
                                    mm(ps[:, 0:CH], wg[:, kt, ci * 128:(ci + 1) * 128], hT[:, kt, tc * CH:(tc + 1) * CH],
                                       kt == 0, kt == 7, [wk, "hT"], [pk])

                            if full:
                                wg, wk = load_group(0)
                                for tI in range(ntile):
                                    for kt in range(8):
                                        mm(psT[:], hT[:, kt, tI * 128:(tI + 1) * 128], wg[:, kt, :], kt == 0, kt == 7, [wk, "hT"], ["psT"])
                                    act(gmf[:], psT[:], AF.Gelu, ["psT"], ["gmf"])
                                    p.add("vector", lambda e: e.bn_stats(out=gst[:], in_=gmf[:, 256:512]), ["gmf"], ["gst"])
                                    p.add("vector", lambda e: e.bn_aggr(out=gmv[:], in_=gst[:]), ["gst"], ["gmv"])
                                    act(grs[:], gmv[:, 1:2], AF.Sqrt, ["gmv", "epsc"], ["grs"], bias=epsc[:], scale=1.0)
                                    p.add("vector", lambda e: e.reciprocal(out=grs[:], in_=grs[:]), ["grs"], ["grs"])
                                    ts("vector", vnf[:], gmf[:, 256:512], gmv[:, 0:1], ALU.subtract, ["gmf", "gmv", "grs"], ["vnf"],
                                       s2=grs[:, 0:1], op1=ALU.mult)
                                    tt("gpsimd", vnf[:], vnf[:], lngb[:], ALU.mult, ["vnf", "gmc"], ["vnf"])
                                    tt("gpsimd", vnb[:], vnf[:], lnbb[:], ALU.add, ["vnf", "gmc"], ["vnb"])
                                    for g in range(4):
                                        mm(psS[:, g * 64:(g + 1) * 64], wsb[:, g, :], vnb[:, g * 64:(g + 1) * 64], True, True, ["gmc", "vnb"], ["psS"])
                                    for g in range(4):
                                        stt(yab[:, g * 64:(g + 1) * 64], psS[:, g * 64:(g + 1) * 64], bst[:, g:g + 1],
                                            gmf[:, g * 64:(g + 1) * 64], ALU.add, ALU.mult, ["psS", "gmc", "gmf"], ["yab"])
                                    for cc in range(2):
                                        tr(psYA[:, cc, :], yab[:, cc * 128:(cc + 1) * 128], identb[:], ["yab", "identb"], ["psYA"])
                                    cp("scalar", yaT[:], psYA[:], ["psYA"], ["yaT"])
                                    t0 = tok0 + tI * 128
                                    dma("gpsimd", YATd[:, :, t0:t0 + 128].rearrange("c p t -> p c t"), yaT[:], ["yaT"], ["YATd"])
                                for (col0, cis) in ((OFF_HY + 512, (1, 0)), (OFF_HY, (3, 2, 1, 0))):
                                    wg, wk = load_group(col0)
                                    for ci in cis:
                                        r_ = (col0 - OFF_HY) // 128 + ci
                                        zh, zk = zhs.next()
                                        for tc in range(nch):
                                            ps, pk = psFs.next()
                                            fm_chunk(wg, wk, ci, tc, ps, pk)
                                            cp("scalar", zh[:, 1 + tc * CH:1 + (tc + 1) * CH], ps[:, 0:CH], [pk], [zk])
                                        if r_ >= 4:
                                            acc, ak = VT2[:, r_ - 4, 0:Ls], "VT2"
                                        else:
                                            acc, ak = tmpA[:, 0:Ls], "tmpA"
                                        ts("vector", acc, zh[:, 0:Ls], cwt[:, r_, 0:1], ALU.mult, [zk, "gmc"], [ak],
                                           s2=cwt[:, r_, 3:4], op1=ALU.add)
                                        stt(acc, zh[:, 1:Ls + 1], cwt[:, r_, 1:2], acc, ALU.mult, ALU.add, [zk, "gmc", ak], [ak])
                                        if r_ >= 4:
                                            stt(acc, zh[:, 2:Ls + 2], cwt[:, r_, 2:3], acc, ALU.mult, ALU.add, [zk, "gmc", ak], [ak])
                                        elif r_ >= 2:
                                            stt(acc, zh[:, 2:Ls + 2], cwt[:, r_, 2:3], acc, ALU.mult, ALU.add, [zk, "gmc", ak], [ak])
                                            tt("gpsimd", VXT[:, r_ - 2, 0:Ls], acc, VT2[:, r_ - 2, 0:Ls], ALU.mult, [ak, "VT2"], ["VXT"])
                                        else:
                                            x0b, x0k = x0bs.next()
                                            stt(x0b[:, 0:Ls], zh[:, 2:Ls + 2], cwt[:, r_, 2:3], acc, ALU.mult, ALU.add, [zk, "gmc", ak], [x0k])
                                            dma("gpsimd", X0Td[r_, :, tok0:tok0 + Ls], x0b[:, 0:Ls], [x0k], ["X0Td"])
                                vx = VXs[kind]
                                for tI in range(ntile):
                                    for cc in range(2):
                                        tr(psYA[:, cc, :], VXT[:, cc, tI * 128:(tI + 1) * 128], identb[:], ["VXT", "identb"], ["psYA"])
                                    cp("scalar", vx[:, :, tI, b], psYA[:].rearrange("p a b -> p (a b)"), ["psYA"], ["VXs" + kind])
                            for (nm, colA, colB, dst) in (("q", OFF_Q, N_IN, QTd), ("k", OFF_K, N_IN + 512, KTd)):
                                if nm == "q" and not full:
                                    continue
                                wgA, wkA = load_group(colA)
                                if rope:
                                    wgB, wkB = load_group(colB)
                                for h in range(4):
                                    for tc in range(nch):
                                        ps, pk = psFs.next()
                                        fm_chunk(wgA, wkA, h, tc, ps, pk)
                                        fo, fk = fos.next()
                                        if rope:
                                            ps2_, pk2 = psPs.next()
                                            fm_chunk(wgB, wkB, h, tc, ps2_, pk2)
                                            t1, k1 = t1s.next()
                                            t2, k2 = t2s.next()
                                            tt("vector", t1[:, 0:CH], ps[:, 0:CH], ropeT[:, 0, tc * CH:(tc + 1) * CH], ALU.mult, [pk, "ropeT"], [k1])
                                            tt("vector", t2[:, 0:CH], ps2_[:, 0:CH], ropeT[:, 1, tc * CH:(tc + 1) * CH], ALU.mult, [pk2, "ropeT"], [k2])
                                            tt("gpsimd", fo[:, 0:CH], t1[:, 0:CH], t2[:, 0:CH], ALU.add, [k1, k2], [fk])
                                        else:
                                            cp("scalar", fo[:, 0:CH], ps[:, 0:CH], [pk], [fk])
                                        t0 = tok0 + tc * CH
                                        dma("gpsimd", dst[h, :, t0:t0 + CH], fo[:, 0:CH], [fk], [nm + "Td"])
                            wg, wk = load_group(OFF_V)
                            for tI in range(ntile):
                                for kt in range(8):
                                    mm(psT[:], hT[:, kt, tI * 128:(tI + 1) * 128], wg[:, kt, :], kt == 0, kt == 7, [wk, "hT"], ["psT"])
                                vt, vk = vts.next()
                                cp("scalar", vt[:], psT[:], ["psT"], [vk])
                                t0 = tok0 + tI * 128
                                dma("gpsimd", Vd[t0:t0 + 128, :], vt[:], [vk], ["Vd"])
                            if full:
                                for gi in range(6):
                                    wg, wk = load_group(OFF_GATE + gi * 512)
                                    for ci in range(4):
                                        for tc in range(nch):
                                            ps, pk = psFs.next()
                                            fm_chunk(wg, wk, ci, tc, ps, pk)
                                            fo, fk = fos.next()
                                            act(fo[:, 0:CH], ps[:, 0:CH], AF.Sigmoid, [pk], [fk])
                                            t0 = tok0 + tc * CH
                                            dma("gpsimd", Gd[gi * 4 + ci, :, t0:t0 + CH], fo[:, 0:CH], [fk], ["Gd"])
                        ph_end()

                    with ExitStack() as ph:
                        hsk = Rot([sbt(ph, "hsk%d" % i, [128, 128 * (2 * nbL - 1)], BF16) for i in range(3)], "hsk")
                        psYs = Rot([pst(ph, "psY%d" % i, [128, 8, nbL * NB]) for i in range(2)], "psY")
                        psTt = pst(ph, "psTt", [128, 2, 128], BF16)
                        YBT = sbt(ph, "YBT", [128, 2, T], BF16)
                        x0ls = Rot([sbt(ph, "x0l%d" % i, [128, 2, L], BF16) for i in range(2)], "x0l")
                        for kind in (["lat"] if last else ["lat", "ctx"]):
                            Lf = L if kind == "lat" else LC
                            nb = Lf // 128
                            W = 128 * (2 * nb - 1)
                            with ExitStack() as ph2:
                                Yr = sbt(ph2, "Yr" + kind, [128, nb, NB, 256], BF16)
                                vx = VXs[kind]
                                kdr = KREV[Lf]
                                for cg in range(32):
                                    psY, pyk = psYs.next()
                                    for c8 in range(8):
                                        c = cg * 8 + c8
                                        hk_, hkk = hsk.next()
                                        dma("sync" if c % 2 == 0 else "gpsimd", hk_[:, 0:W],
                                            bass.AP(tensor=kdr.tensor, offset=kdr.offset + c * 2 * Lf, ap=[[1, 128], [1, W]]),
                                            ["KREV%d" % Lf], [hkk])
                                        lags = [0] + [d for d in range(-(nb - 1), nb) if d != 0]
                                        for di, d in enumerate(lags):
                                            j0, j1 = max(0, -d), min(nb, nb - d)
                                            m0 = 128 * (nb - 1 - d)
                                            o_ = _ap(psY[:], c8 * nbL * NB + (j0 + d) * NB, [[1, (j1 - j0) * NB]])
                                            mm(o_, hk_[:, m0:m0 + 128], vx[:, c, j0:j1, :].rearrange("p j b -> p (j b)"),
                                               di == 0, di == len(lags) - 1, [hkk, "VXs" + kind], [pyk])
                                    src = _ap(psY[:], 0, [[nbL * NB, 8], [NB, nb], [1, NB]])
                                    dst_ = _ap(Yr[:], cg * 8, [[1, 8], [NB * 256, nb], [256, NB]])
                                    cp("scalar" if cg % 2 == 0 else "vector", dst_, src, [pyk], ["Yr"])
                                for b in range(NB):
                                    tok0 = b * L if kind == "lat" else NB * L + b * LC
                                    x0l, x0lk = x0ls.next()
                                    dma("sync", x0l[:, :, 0:Lf], X0Td[:, :, tok0:tok0 + Lf].rearrange("c p t -> p c t"), ["X0Td"], [x0lk])
                                    for i_ in range(nb):
                                        for cc in range(2):
                                            tr(psTt[:, cc, :], Yr[:, i_, b, cc * 128:(cc + 1) * 128], identb[:], ["Yr", "identb"], ["psTt"])
                                        t0 = tok0 + i_ * 128
                                        for cc in range(2):
                                            rev = _ap(psTt[:], cc * 128 + 127, [[-1, 128]])
                                            tt("vector", YBT[:, cc, t0:t0 + 128], rev, x0l[:, cc, i_ * 128:(i_ + 1) * 128], ALU.mult,
                                               ["psTt", x0lk], ["YBT"])
                        ntok = NB * L if last else T
                        for cc in range(2):
                            dma("sync", YBTd[cc, :, 0:ntok], YBT[:, cc, 0:ntok], ["YBT"], ["YBTd"])
                        ph_end()

                with ExitStack() as ph:
                    NKmax = (L + LC) // 128
                    QT = sbt(ph, "QT", [128, 4, L], BF16)
                    KT = sbt(ph, "KT", [128, 4, L + LC], BF16)
                    Vone = sbt(ph, "Vone", [128, NKmax, 4, 129], BF16)
                    Es = Rot([sbt(ph, "E%d" % i, [128, 512], BF16) for i in range(3)], "E")
                    psSs = Rot([pst(ph, "psA%d" % i, [128, 512]) for i in range(2)], "psA")
                    acc = pst(ph, "acc", [128, 4, 2, 256])
                    psC = pst(ph, "psC", [128, 4, 128], BF16)
                    rec = sbt(ph, "rec", [128, 4, 2], F32)
                    o1 = sbt(ph, "o1", [128, 128], F32)
                    o2 = sbt(ph, "o2", [128, 128], F32)
                    sq = sbt(ph, "sq", [128, 128], F32)
                    ss = sbt(ph, "ss", [128, 1], F32)
                    YC = sbt(ph, "YC", [128, 4, 512], BF16)
                    ycts = Rot([sbt(ph, "yct%d" % i, [128, 4, 512], BF16) for i in range(2)], "yct")
                    memset("vector", Vone[:], 1.0, ["Vone"])
                    for (kind, b, tok0, Lq) in act_seqs:
                        if kind == "lat":
                            ksegs = [(tok0, L), (NB * L + b * LC, LC)]
                        else:
                            ksegs = [(tok0, LC)]
                        NK = sum(s[1] for s in ksegs) // 128
                        dma("sync", QT[:, :, 0:Lq], QTd[:, :, tok0:tok0 + Lq].rearrange("h p t -> p h t"), ["qTd"], ["QT"])
                        ko = 0
                        for (kt0, kl) in ksegs:
                            dma("gpsimd", KT[:, :, ko:ko + kl], KTd[:, :, kt0:kt0 + kl].rearrange("h p t -> p h t"), ["kTd"], ["KT"])
                            for j in range(kl // 128):
                                dma("sync" if j % 2 else "gpsimd", Vone[:, ko // 128 + j, :, 0:128],
                                    Vd[kt0 + j * 128:kt0 + (j + 1) * 128, :].rearrange("t (h e) -> t h e", h=4), ["Vd"], ["Vone"])
                            ko += kl
                        QC = min(512, Lq)
                        nsub = QC // 128
                        for qc in range(Lq // QC):
                            for h in range(4):
                                for m in range(2):
                                    ms = slice(m * 64, (m + 1) * 64)
                                    for kt in range(NK):
                                        psA, pak = psSs.next()
                                        mm(psA[:, 0:QC], KT[ms, h, kt * 128:(kt + 1) * 128], QT[ms, h, qc * QC:(qc + 1) * QC],
                                           True, True, ["KT", "QT"], [pak])
                                        E, ek = Es.next()
                                        act(E[:, 0:QC], psA[:, 0:QC], AF.Exp, [pak], [ek], scale=0.125)
                                        for qs in range(nsub):
                                            mm(acc[:, qs, m, 0:129], E[:, qs * 128:(qs + 1) * 128], Vone[:, kt, h, :],
                                               kt == 0, kt == NK - 1, [ek, "Vone"], ["acc"])
                                p.add("vector", lambda e, nsub=nsub: e.reciprocal(out=rec[:, 0:nsub, :], in_=acc[:, 0:nsub, :, 128]), ["acc"], ["rec"])
                                ts("vector", rec[:, 0:nsub, 1], rec[:, 0:nsub, 1], lamt[:, 0:1], ALU.mult, ["rec", "lamt"], ["rec"])
                                for qs in range(nsub):
                                    ts("vector", o1[:], acc[:, qs, 0, 0:128], rec[:, qs, 0:1], ALU.mult, ["acc", "rec"], ["o1"])
                                    stt(o2[:], acc[:, qs, 1, 0:128], rec[:, qs, 1:2], o1[:], ALU.mult, ALU.add, ["acc", "rec", "o1"], ["o2"])
                                    tt("gpsimd", sq[:], o2[:], o2[:], ALU.mult, ["o2"], ["sq"])
                                    red(ss[:], sq[:], ALU.add, AX.X, ["sq"], ["ss"])
                                    act(ss[:], ss[:], AF.Sqrt, ["ss", "epsc"], ["ss"], bias=epsc[:], scale=1.0 / 128.0)
                                    p.add("vector", lambda e: e.reciprocal(out=ss[:], in_=ss[:]), ["ss"], ["ss"])
                                    stt(YC[:, qs, h * 128:(h + 1) * 128], o2[:], ss[:, 0:1], gsc[:], ALU.mult, ALU.mult,
                                        ["o2", "ss", "gsc"], ["YC"])
                            yct, yk = ycts.next()
                            for qs in range(nsub):
                                for h in range(4):
                                    tr(psC[:, h, :], YC[:, qs, h * 128:(h + 1) * 128], identb[:], ["YC", "identb"], ["psC"])
                                cp("scalar", yct[:, :, qs * 128:(qs + 1) * 128], psC[:], ["psC"], [yk])
                            t0 = tok0 + qc * QC
                            dma("sync", YCTd[:, :, t0:t0 + QC].rearrange("h p t -> p h t"), yct[:, :, 0:QC], [yk], ["YCTd"])
                    ph_end()

                LOG = sbt(lay, "LOG", [128, NT, 36], F32)
                WTS = sbt(lay, "WTS", [128, NT, 2], F32)
                DEST = sbt(lay, "DEST", [128, NT, 2], I32)
                IDXG = sbt(lay, "IDXG", [128, NBLK, 8], I32)
                IDXD = sbt(lay, "IDXD", [128, NBLK, 4], I32)
                with ExitStack() as ph:
                    pab = sbt(ph, "pab", [128, 2, D], BF16)
                    pbb = sbt(ph, "pbb", [128, 2, D], BF16)
                    pcb = sbt(ph, "pcb", [128, 4, D], BF16)
                    wob = sbt(ph, "wob", [128, 8, D], BF16)
                    wrb = sbt(ph, "wrb", [128, 8, 36], BF16)
                    wrf = sbt(ph, "wrf", [128, 8, 36], F32)
                    rbb = sbt(ph, "rbb", [128, 36], F32)
                    stg = Rot([sbt(ph, "stg%d" % i, [128, 2, D], F32) for i in range(2)], "stg")
                    lg = sbt(ph, "lg", [128, D], F32)
                    lb = sbt(ph, "lb", [128, D], F32)
                    g1b = sbt(ph, "g1b", [128, D], F32)
                    sc2b = sbt(ph, "sc2b", [128, D], F32)
                    sh2b = sbt(ph, "sh2b", [128, D], F32)
                    si = 0
                    for (src, nk, dstw) in ((p_a, 2, pab), (p_b, 2, pbb), (p_c, 4, pcb), (w_out, 8, wob)):
                        for k2 in range(0, nk, 2):
                            s_, sk_ = stg.next()
                            dma("sync", s_[:], src[l, k2 * 128:(k2 + 2) * 128, :].rearrange("(k p) d -> p k d", p=128), [], [sk_])
                            cp("vector" if si % 2 == 0 else "gpsimd", dstw[:, k2:k2 + 2, :], s_[:], [sk_], ["mw"])
                            si += 1
                    dma("sync", wrf[:], moe_wr[l, :, :].rearrange("(k p) e -> p k e", p=128), [], ["wrf"])
                    cp("vector", wrb[:], wrf[:], ["wrf"], ["mw"])
                    dma("sync", rbb[:], bass.AP(tensor=moe_br.tensor, offset=moe_br.offset + l * 36, ap=[[0, 128], [1, 36]]), [], ["mw"])
                    dma("sync", lg[:], bass.AP(tensor=ln1_g.tensor, offset=ln1_g.offset + l * D, ap=[[0, 128], [1, D]]), [], ["lnconst"])
                    dma("sync", lb[:], bass.AP(tensor=ln1_b.tensor, offset=ln1_b.offset + l * D, ap=[[0, 128], [1, D]]), [], ["lnconst"])
                    yaTs = sbt(ph, "yaTs", [128, 2, 512], BF16)
                    ybTs = sbt(ph, "ybTs", [128, 2, 512], BF16)
                    ycTs = sbt(ph, "ycTs", [128, 4, 512], BF16)
                    Gs = sbt(ph, "Gs", [128, 24, 512], BF16)
                    mT = sbt(ph, "mT", [128, 8, 512], BF16)
                    ta = sbt(ph, "ta", [128, 512], F32)
                    tb = sbt(ph, "tb", [128, 512], F32)
                    tcx = sbt(ph, "tcx", [128, 512], F32)
                    xr = Rot([sbt(ph, "xr%d" % i, [128, D], F32) for i in range(2)], "xr")
                    rr_ = sbt(ph, "rr_", [128, D], F32)
                    x1t = Rot([sbt(ph, "x1t%d" % i, [128, D], F32) for i in range(2)], "x1t")
                    h2f = sbt(ph, "h2f", [128, D], F32)
                    h2b = Rot([sbt(ph, "h2b%d" % i, [128, D], BF16) for i in range(2)], "h2b")
                    h2T = sbt(ph, "h2T", [128, 8, 128], BF16)
                    lntmp = {"stats": sbt(ph, "lnst", [128, 2, 6], F32), "mv": sbt(ph, "lnmv", [128, 2], F32),
                             "rstd": sbt(ph, "lnrs", [128, 1], F32)}
                    psa = pst(ph, "psa", [128, 512])
                    psb = pst(ph, "psb", [128, 512])
                    psc = pst(ph, "psc", [128, 512])
                    psO = Rot([pst(ph, "psO%d" % i, [128, 512]) for i in range(2)], "psO")
                    psh = pst(ph, "psh", [128, 8, 128], BF16)
                    psl = pst(ph, "psl", [128, 36])
                    for (kind, b, tok0, Ls) in act_seqs:
                        mrow = b if kind == "lat" else NB
                        mo = MODS.offset + (l * (NB + 1) + mrow) * 6 * D
                        dma("sync", g1b[:], bass.AP(tensor=MODS.tensor, offset=mo + 2 * D, ap=[[0, 128], [1, D]]), ["MODS"], ["g1b"])
                        dma("sync", sh2b[:], bass.AP(tensor=MODS.tensor, offset=mo + 3 * D, ap=[[0, 128], [1, D]]), ["MODS"], ["sh2b"])
                        dma("sync", sc2b[:], bass.AP(tensor=MODS.tensor, offset=mo + 4 * D, ap=[[0, 128], [1, D]]), ["MODS"], ["sc2b"])
                        ts("gpsimd", sc2b[:], sc2b[:], 1.0, ALU.add, ["sc2b"], ["sc2b"])
                        CH = min(512, Ls)
                        for tc in range(Ls // CH):
                            t0 = tok0 + tc * CH
                            dma("sync", yaTs[:, :, 0:CH], YATd[:, :, t0:t0 + CH].rearrange("c p t -> p c t"), ["YATd"], ["yaTs"])
                            dma("gpsimd", ybTs[:, :, 0:CH], YBTd[:, :, t0:t0 + CH].rearrange("c p t -> p c t"), ["YBTd"], ["ybTs"])
                            dma("sync", ycTs[:, :, 0:CH], YCTd[:, :, t0:t0 + CH].rearrange("c p t -> p c t"), ["YCTd"], ["ycTs"])
                            dma("gpsimd", Gs[:, :, 0:CH], Gd[:, :, t0:t0 + CH].rearrange("c p t -> p c t"), ["Gd"], ["Gs"])
                            for dc in range(8):
                                ds_ = slice(dc * 128, (dc + 1) * 128)
                                for kt in range(2):
                                    mm(psa[:, 0:CH], pab[:, kt, ds_], yaTs[:, kt, 0:CH], kt == 0, kt == 1, ["mw", "yaTs"], ["psa"])
                                for kt in range(2):
                                    mm(psb[:, 0:CH], pbb[:, kt, ds_], ybTs[:, kt, 0:CH], kt == 0, kt == 1, ["mw", "ybTs"], ["psb"])
                                for kt in range(4):
                                    mm(psc[:, 0:CH], pcb[:, kt, ds_], ycTs[:, kt, 0:CH], kt == 0, kt == 3, ["mw", "ycTs"], ["psc"])
                                tt("vector", ta[:, 0:CH], psa[:, 0:CH], Gs[:, dc, 0:CH], ALU.mult, ["psa", "Gs"], ["ta"])
                                tt("vector", tb[:, 0:CH], psb[:, 0:CH], Gs[:, 8 + dc, 0:CH], ALU.mult, ["psb", "Gs"], ["tb"])
                                tt("vector", tcx[:, 0:CH], psc[:, 0:CH], Gs[:, 16 + dc, 0:CH], ALU.mult, ["psc", "Gs"], ["tcx"])
                                tt("gpsimd", ta[:, 0:CH], ta[:, 0:CH], tb[:, 0:CH], ALU.add, ["ta", "tb"], ["ta"])
                                tt("gpsimd", mT[:, dc, 0:CH], ta[:, 0:CH], tcx[:, 0:CH], ALU.add, ["ta", "tcx"], ["mT"])
                            for tsb in range(CH // 128):
                                tk0 = t0 + tsb * 128
                                gt = tk0 // 128
                                xt, xk = xr.next()
                                dma("sync", xt[:], Xsrc(tk0, 128), ["X2d"] if l else [], [xk])
                                for half in range(2):
                                    hs = slice(half * 512, (half + 1) * 512)
                                    pso, pok = psO.next()
                                    for kt in range(8):
                                        mm(pso[:], mT[:, kt, tsb * 128:(tsb + 1) * 128], wob[:, kt, hs], kt == 0, kt == 7, ["mw", "mT"], [pok])
                                    tt("vector", rr_[:, hs], pso[:], g1b[:, hs], ALU.mult, [pok, "g1b"], ["rr_"])
                                stt(rr_[:], xt[:], ALPHA, rr_[:], ALU.mult, ALU.add, [xk, "rr_"], ["rr_"])
                                x1, x1k = x1t.next()
                                layer_norm_rows("ln1", rr_, "rr_", lg, lb, x1, x1k, lntmp)
                                dma("sync", X1d[tk0:tk0 + 128, :], x1[:], [x1k], ["X1d"])
                                tt("vector", h2f[:], x1[:], sc2b[:], ALU.mult, [x1k, "sc2b"], ["h2f"])
                                hb_, hbk = h2b.next()
                                tt("gpsimd", hb_[:], h2f[:], sh2b[:], ALU.add, ["h2f", "sh2b"], [hbk])
                                dma("gpsimd", H2d[tk0:tk0 + 128, :], hb_[:], [hbk], ["H2d"])
                                for kt in range(8):
                                    tr(psh[:, kt, :], hb_[:, kt * 128:(kt + 1) * 128], identb[:], [hbk, "identb"], ["psh"])
                                cp("scalar", h2T[:], psh[:], ["psh"], ["h2T"])
                                for kt in range(8):
                                    mm(psl[:], h2T[:, kt, :], wrb[:, kt, :], kt == 0, kt == 7, ["h2T", "mw"], ["psl"])
                                tt("vector", LOG[:, gt, :], psl[:], rbb[:], ALU.add, ["psl", "mw"], ["LOG"])
                    ph_end()

                with ExitStack() as ph:
                    NC_ = NTm * 32
                    cm = sbt(ph, "cm", [128, 128 + 8 + 4 + MMAX + NBLK + 32], F32)
                    dma("sync", cm[:], cmoe_in[:, :], [], ["cm"])
                    ustr = sbt(ph, "ustr", [128, 128], BF16)
                    cp("vector", ustr[:], cm[:, 0:128], ["cm"], ["ustr"])
                    o_kp8, o_kp4, o_mrow, o_nrow, o_i32 = 128, 136, 140, 140 + MMAX, 140 + MMAX + NBLK
                    onesb = sbt(ph, "onesb", [128, 1], BF16)
                    onesf = sbt(ph, "onesf", [32, 128], F32)
                    memset("vector", onesb[:], 1.0, ["onesb"])
                    memset("vector", onesf[:], 1.0, ["onesf"])
                    gmax = sbt(ph, "gmax", [128, NTm], F32)
                    goh = sbt(ph, "goh", [128, NTm, 4], F32)
                    gex = sbt(ph, "gex", [128, NTm, 4], F32)
                    gsum = sbt(ph, "gsum", [128, NTm], F32)
                    EM = sbt(ph, "EM", [128, NTm, 32], F32)
                    oh1 = sbt(ph, "oh1", [128, NTm, 32], F32)
                    oh2 = sbt(ph, "oh2", [128, NTm, 32], F32)
                    m1 = sbt(ph, "m1", [128, NTm], F32)
                    m2 = sbt(ph, "m2", [128, NTm], F32)
                    dd = sbt(ph, "dd", [128, NTm], F32)
                    Mb = sbt(ph, "Mb", [128, NTm, 32], BF16)
                    POS = sbt(ph, "POS", [128, NTm, 32], F32)
                    tmpP = sbt(ph, "tmpP", [128, NTm, 32], F32)
                    dstf = sbt(ph, "dstf", [128, NTm, 2], F32)
                    tot = sbt(ph, "tot", [32, NTm], F32)
                    cum = sbt(ph, "cum", [32, NTm], F32)
                    ones32 = sbt(ph, "ones32", [32, NTm], F32)
                    cnt = sbt(ph, "cnt", [32, 4], F32)
                    cmpm = sbt(ph, "cmpm", [32, MMAX], F32)
                    base = sbt(ph, "base", [32, NTm], F32)
                    R = sbt(ph, "R", [32, NTm, 32], F32)
                    dg32 = sbt(ph, "dg32", [32, 32], F32)
                    pendr = sbt(ph, "pendr", [128, 32], F32)
                    cmpb = sbt(ph, "cmpb", [128, NBLK, 32], F32)
                    be = sbt(ph, "be", [128, NBLK], F32)
                    idf = sbt(ph, "idf", [128, NBLK, 8], F32)
                    NPS = -(-NC_ // 512)
                    psR = pst(ph, "psR", [128, NPS * 512])
                    psTot = pst(ph, "psTot", [128, 512])
                    psSm = pst(ph, "psSm", [128, 512])
                    Lg = LOG[:, 0:NTm, :]
                    G4 = LOG[:, 0:NTm, 0:4]
                    E32 = LOG[:, 0:NTm, 4:36]

                    def bc_last(t2d, n):
                        a = t2d
                        return bass.AP(tensor=a.tensor, offset=a.offset, ap=[list(a.ap[0]), list(a.ap[1]), [0, n]])

                    red(gmax[:], G4, ALU.max, AX.X, ["LOG"], ["gmax"])
                    tt("vector", goh[:], G4, bc_last(gmax[:], 4), ALU.is_equal, ["LOG", "gmax"], ["goh"])
                    tt("vector", gex[:], G4, bc_last(gmax[:], 4), ALU.subtract, ["LOG", "gmax"], ["gex"])
                    act(gex[:], gex[:], AF.Exp, ["gex"], ["gex"])
                    red(gsum[:], gex[:], ALU.add, AX.X, ["gex"], ["gsum"])
                    p.add("vector", lambda e: e.reciprocal(out=gsum[:], in_=gsum[:]), ["gsum"], ["gsum"])
                    ts("vector", goh[:], goh[:], 1.0, ALU.subtract, ["goh"], ["goh"], s2=BIG, op1=ALU.mult)
                    gb = goh[:]
                    p.add("vector", lambda e: e.tensor_tensor(
                        out=bass.AP(tensor=EM[:].tensor, offset=EM[:].offset, ap=[list(EM[:].ap[0]), [32, NTm], [8, 4], [1, 8]]),
                        in0=bass.AP(tensor=LOG[:].tensor, offset=LOG[:].offset + 4, ap=[list(LOG[:].ap[0]), [36, NTm], [8, 4], [1, 8]]),
                        in1=bass.AP(tensor=gb.tensor, offset=gb.offset, ap=[list(gb.ap[0]), [4, NTm], [1, 4], [0, 8]]),
                        op=ALU.add), ["LOG", "goh"], ["EM"])
                    red(m1[:], EM[:], ALU.max, AX.X, ["EM"], ["m1"])
                    tt("vector", oh1[:], EM[:], bc_last(m1[:], 32), ALU.is_equal, ["EM", "m1"], ["oh1"])
                    stt(EM[:], oh1[:], -BIG, EM[:], ALU.mult, ALU.add, ["oh1", "EM"], ["EM"])
                    red(m2[:], EM[:], ALU.max, AX.X, ["EM"], ["m2"])
                    tt("vector", oh2[:], EM[:], bc_last(m2[:], 32), ALU.is_equal, ["EM", "m2"], ["oh2"])
                    tt("vector", dd[:], m2[:], m1[:], ALU.subtract, ["m1", "m2"], ["dd"])
                    act(dd[:], dd[:], AF.Exp, ["dd"], ["dd"])
                    ts("vector", dd[:], dd[:], 1.0, ALU.add, ["dd"], ["dd"])
                    p.add("vector", lambda e: e.reciprocal(out=dd[:], in_=dd[:]), ["dd"], ["dd"])
                    tt("vector", WTS[:, 0:NTm, 0], dd[:], gsum[:], ALU.mult, ["dd", "gsum"], ["WTS"])
                    tt("vector", WTS[:, 0:NTm, 1], gsum[:], WTS[:, 0:NTm, 0], ALU.subtract, ["gsum", "WTS"], ["WTS"])
                    tt("vector", Mb[:], oh1[:], oh2[:], ALU.add, ["oh1", "oh2"], ["Mb"])
                    Mf = Mb[:].rearrange("p t e -> p (t e)")
                    for c_ in range(NPS):
                        n_ = min(512, NC_ - c_ * 512)
                        mm(psR[:, c_ * 512:c_ * 512 + n_], ustr[:], Mf[:, c_ * 512:c_ * 512 + n_], True, True, ["ustr", "Mb"], ["psR"])
                    for t_ in range(NTm):
                        mm(psTot[0:32, t_:t_ + 1], Mb[:, t_, :], onesb[:], True, True, ["Mb", "onesb"], ["psTot"])
                    cp("vector", tot[:], psTot[0:32, 0:NTm], ["psTot"], ["tot"])
                    memset("vector", ones32[:], 1.0, ["ones32"])
                    p.add("vector", lambda e: e.tensor_tensor_scan(out=cum[:], data0=ones32[:], data1=tot[:], initial=0.0,
                                                                   op0=ALU.mult, op1=ALU.add), ["ones32", "tot"], ["cum"])
                    cp("vector", cnt[:, 0:1], cum[:, NTm - 1:NTm], ["cum"], ["cnt"])
                    ts("vector", cmpm[:], cm[0:32, o_mrow:o_mrow + MMAX], cnt[:, 0:1], ALU.is_lt, ["cm", "cnt"], ["cmpm"])
                    red(cnt[:, 1:2], cmpm[:], ALU.add, AX.X, ["cmpm"], ["cnt"])
                    ts("vector", cnt[:, 2:3], cnt[:, 1:2], float(BLK), ALU.mult, ["cnt"], ["cnt"])
                    mm(psSm[0:32, 0:1], cm[0:32, 0:32], cnt[:, 2:3], True, True, ["cm", "cnt"], ["psSm"])
                    tt("vector", cnt[:, 3:4], psSm[0:32, 0:1], cnt[:, 2:3], ALU.add, ["psSm", "cnt"], ["cnt"])
                    tt("vector", base[:], cum[:], tot[:], ALU.subtract, ["cum", "tot"], ["base"])
                    ts("vector", base[:], base[:], psSm[0:32, 0:1], ALU.add, ["base", "psSm"], ["base"])
                    bb = base[:]
                    i32 = cm[0:32, o_i32:o_i32 + 32]
                    p.add("vector", lambda e: e.tensor_tensor(
                        out=R[:], in0=bass.AP(tensor=bb.tensor, offset=bb.offset, ap=[list(bb.ap[0]), [1, NTm], [0, 32]]),
                        in1=bass.AP(tensor=i32.tensor, offset=i32.offset, ap=[list(i32.ap[0]), [0, NTm], [1, 32]]),
                        op=ALU.mult), ["base", "cm"], ["R"])
                    cp("vector", POS[:].rearrange("p t e -> p (t e)"), psR[:, 0:NC_], ["psR"], ["POS"])
                    Rf = R[:].rearrange("p t e -> p (t e)")
                    for c_ in range(NPS):
                        n_ = min(512, NC_ - c_ * 512)
                        mm(psR[:, c_ * 512:c_ * 512 + n_], onesf[:], Rf[:, c_ * 512:c_ * 512 + n_], True, True, ["onesf", "R", "POS"], ["psR"])
                    tt("vector", POS[:].rearrange("p t e -> p (t e)"), POS[:].rearrange("p t e -> p (t e)"), psR[:, 0:NC_],
                       ALU.add, ["psR", "POS"], ["POS"])
                    for k_, oh in enumerate((oh1, oh2)):
                        tt("vector", tmpP[:], oh[:], POS[:], ALU.mult, ["oh1", "oh2", "POS"], ["tmpP"])
                        red(dstf[:, :, k_], tmpP[:], ALU.add, AX.X, ["tmpP"], ["dstf"])
                    cp("vector", DEST[:, 0:NTm, :], dstf[:], ["dstf"], ["DEST"])
                    ts("vector", dg32[:], i32, cnt[:, 3:4], ALU.mult, ["cm", "cnt"], ["dg32"])
                    mm(psSm[:, 32:64], onesf[:], dg32[:], True, True, ["onesf", "dg32"], ["psSm"])
                    cp("vector", pendr[:], psSm[:, 32:64], ["psSm"], ["pendr"])
                    pr_ = pendr[:]
                    nrow = cm[:, o_nrow:o_nrow + NBLK]
                    p.add("vector", lambda e: e.tensor_tensor(
                        out=cmpb[:], in0=bass.AP(tensor=pr_.tensor, offset=pr_.offset, ap=[list(pr_.ap[0]), [0, NBLK], [1, 32]]),
                        in1=bass.AP(tensor=nrow.tensor, offset=nrow.offset, ap=[list(nrow.ap[0]), [1, NBLK], [0, 32]]),
                        op=ALU.is_le), ["pendr", "cm"], ["cmpb"])
                    red(be[:], cmpb[:], ALU.add, AX.X, ["cmpb"], ["be"])
                    ts("vector", be[:], be[:], 31.0, ALU.min, ["be"], ["be"], s2=float(l * NE), op1=ALU.add)
                    for (nk, o_k, idt, mul) in ((8, o_kp8, IDXG, float(D)), (4, o_kp4, IDXD, float(HID))):
                        kp = cm[:, o_k:o_k + nk]
                        bea = be[:]
                        p.add("vector", lambda e, nk=nk, kp=kp, mul=mul: e.scalar_tensor_tensor(
                            out=idf[:, :, 0:nk], in0=bass.AP(tensor=bea.tensor, offset=bea.offset, ap=[list(bea.ap[0]), [1, NBLK], [0, nk]]),
                            scalar=mul, in1=bass.AP(tensor=kp.tensor, offset=kp.offset, ap=[list(kp.ap[0]), [0, NBLK], [1, nk]]),
                            op0=ALU.mult, op1=ALU.add), ["be", "cm", "idf"], ["idf"])
                        cp("vector", idt[:, :, 0:nk], idf[:, :, 0:nk], ["idf"], ["IDX%d" % nk])
                    ph_end()

                with ExitStack() as ph:
                    hrs = Rot([sbt(ph, "hr%d" % i, [128, D], BF16) for i in range(3)], "hr")
                    for t_ in range(NTm):
                        hr, hk = hrs.next()
                        dma("sync", hr[:], H2d[t_ * 128:(t_ + 1) * 128, :], ["H2d"], [hk])
                        for k_ in range(2):
                            p.add("gpsimd", lambda e, hr=hr, t_=t_, k_=k_: e.indirect_dma_start(
                                out=XBUF[:, :], out_offset=bass.IndirectOffsetOnAxis(ap=DEST[:, t_, k_:k_ + 1], axis=0),
                                in_=hr[:], in_offset=None), [hk, "DEST"], ["XBUF"], dma=True)
                    ph_end()

                with ExitStack() as ph:
                    wgf = sbt(ph, "wgf", [128, 8, HID], F32)
                    wuf = sbt(ph, "wuf", [128, 8, HID], F32)
                    wdf = sbt(ph, "wdf", [128, 4, D], F32)
                    wgbs = Rot([sbt(ph, "wgb%d" % i, [128, 8, HID], BF16) for i in range(2)], "wgb")
                    wubs = Rot([sbt(ph, "wub%d" % i, [128, 8, HID], BF16) for i in range(2)], "wub")
                    wdbs = Rot([sbt(ph, "wdb%d" % i, [128, 4, D], BF16) for i in range(2)], "wdb")
                    xrs = Rot([sbt(ph, "xb%d" % i, [128, RB, D], BF16) for i in range(2)], "xb")
                    XT = sbt(ph, "XT", [128, 8, BLK], BF16)
                    sgt = sbt(ph, "sgt", [128, BLK], F32)
                    aT = sbt(ph, "aT", [128, 4, BLK], BF16)
                    yos = Rot([sbt(ph, "yo%d" % i, [128, D], BF16) for i in range(2)], "yo")
                    psX = pst(ph, "psX", [128, 8, 128], BF16)
                    psG = Rot([pst(ph, "psG%d" % i, [128, BLK]) for i in range(2)], "psG")
                    psU = Rot([pst(ph, "psU%d" % i, [128, BLK]) for i in range(2)], "psU")
                    psD = Rot([pst(ph, "psD%d" % i, [128, 512]) for i in range(2)], "psD")
                    for n in range(NBLK):
                        for kt in range(8):
                            p.add("gpsimd", lambda e, n=n, kt=kt: e.indirect_dma_start(
                                out=wgf[:, kt, :], out_offset=None, in_=ex_g[:, :],
                                in_offset=bass.IndirectOffsetOnAxis(ap=IDXG[:, n, kt:kt + 1], axis=0)), ["IDX8"], ["wgf"], dma=True)
                            p.add("gpsimd", lambda e, n=n, kt=kt: e.indirect_dma_start(
                                out=wuf[:, kt, :], out_offset=None, in_=ex_u[:, :],
                                in_offset=bass.IndirectOffsetOnAxis(ap=IDXG[:, n, kt:kt + 1], axis=0)), ["IDX8"], ["wuf"], dma=True)
                        for hc in range(4):
                            p.add("gpsimd", lambda e, n=n, hc=hc: e.indirect_dma_start(
                                out=wdf[:, hc, :], out_offset=None, in_=ex_d[:, :],
                                in_offset=bass.IndirectOffsetOnAxis(ap=IDXD[:, n, hc:hc + 1], axis=0)), ["IDX4"], ["wdf"], dma=True)
                        wgb, gk = wgbs.next()
                        wub, uk = wubs.next()
                        wdb, dk = wdbs.next()
                        cp("vector", wgb[:], wgf[:], ["wgf"], [gk])
                        cp("gpsimd", wub[:], wuf[:], ["wuf"], [uk])
                        cp("scalar", wdb[:], wdf[:], ["wdf"], [dk])
                        xb, xk = xrs.next()
                        dma("sync", xb[:], XBUF[n * BLK:(n + 1) * BLK, :].rearrange("(r p) d -> p r d", p=128), ["XBUF"], [xk])
                        for rb in range(RB):
                            for kt in range(8):
                                tr(psX[:, kt, :], xb[:, rb, kt * 128:(kt + 1) * 128], identb[:], [xk, "identb"], ["psX"])
                            cp("scalar" if rb % 2 else "vector", XT[:, :, rb * 128:(rb + 1) * 128], psX[:], ["psX"], ["XT"])
                        for hc in range(4):
                            pg, pgk = psG.next()
                            pu, puk = psU.next()
                            for kt in range(8):
                                mm(pg[:], wgb[:, kt, hc * 128:(hc + 1) * 128], XT[:, kt, :], kt == 0, kt == 7, [gk, "XT"], [pgk])
                            for kt in range(8):
                                mm(pu[:], wub[:, kt, hc * 128:(hc + 1) * 128], XT[:, kt, :], kt == 0, kt == 7, [uk, "XT"], [puk])
                            act(sgt[:], pg[:], AF.Silu, [pgk], ["sgt"])
                            tt("vector", aT[:, hc, :], sgt[:], pu[:], ALU.mult, ["sgt", puk], ["aT"])
                        for rb in range(RB):
                            yo, yk = yos.next()
                            for half in range(2):
                                pd, pdk = psD.next()
                                for hc in range(4):
                                    mm(pd[:], aT[:, hc, rb * 128:(rb + 1) * 128], wdb[:, hc, half * 512:(half + 1) * 512],
                                       hc == 0, hc == 3, [dk, "aT"], [pdk])
                                cp("scalar" if half else "vector", yo[:, half * 512:(half + 1) * 512], pd[:], [pdk], [yk])
                            r0 = n * BLK + rb * 128
                            dma("sync", YBUF[r0:r0 + 128, :], yo[:], [yk], ["YBUF"])
                    ph_end()

                with ExitStack() as ph:
                    r0s = Rot([sbt(ph, "r0_%d" % i, [128, D], BF16) for i in range(2)], "r0_")
                    r1s = Rot([sbt(ph, "r1_%d" % i, [128, D], BF16) for i in range(2)], "r1_")
                    xr = Rot([sbt(ph, "xq%d" % i, [128, D], F32) for i in range(2)], "xq")
                    yf = sbt(ph, "yf", [128, D], F32)
                    rr_ = sbt(ph, "rr2", [128, D], F32)
                    xo = Rot([sbt(ph, "xo%d" % i, [128, D], F32) for i in range(2)], "xo")
                    lg = sbt(ph, "lg2", [128, D], F32)
                    lb = sbt(ph, "lb2", [128, D], F32)
                    g2b = sbt(ph, "g2b", [128, D], F32)
                    lntmp = {"stats": sbt(ph, "lnst2", [128, 2, 6], F32), "mv": sbt(ph, "lnmv2", [128, 2], F32),
                             "rstd": sbt(ph, "lnrs2", [128, 1], F32)}
                    dma("sync", lg[:], bass.AP(tensor=ln2_g.tensor, offset=ln2_g.offset + l * D, ap=[[0, 128], [1, D]]), [], ["lnconst"])
                    dma("sync", lb[:], bass.AP(tensor=ln2_b.tensor, offset=ln2_b.offset + l * D, ap=[[0, 128], [1, D]]), [], ["lnconst"])
                    for (kind, b, tok0, Ls) in act_seqs:
                        mrow = b if kind == "lat" else NB
                        mo = MODS.offset + (l * (NB + 1) + mrow) * 6 * D
                        dma("sync", g2b[:], bass.AP(tensor=MODS.tensor, offset=mo + 5 * D, ap=[[0, 128], [1, D]]), ["MODS"], ["g2b"])
                        for tI in range(Ls // 128):
                            tk0 = tok0 + tI * 128
                            gt = tk0 // 128
                            r0, r0k = r0s.next()
                            r1, r1k = r1s.next()
                            for (rt, rk, k_) in ((r0, r0k, 0), (r1, r1k, 1)):
                                p.add("gpsimd", lambda e, rt=rt, gt=gt, k_=k_: e.indirect_dma_start(
                                    out=rt[:], out_offset=None, in_=YBUF[:, :],
                                    in_offset=bass.IndirectOffsetOnAxis(ap=DEST[:, gt, k_:k_ + 1], axis=0)), ["YBUF", "DEST"], [rk], dma=True)
                            xt, xk = xr.next()
                            dma("sync", xt[:], X1d[tk0:tk0 + 128, :], ["X1d"], [xk])
                            ts("vector", yf[:], r0[:], WTS[:, gt, 0:1], ALU.mult, [r0k, "WTS"], ["yf"])
                            stt(yf[:], r1[:], WTS[:, gt, 1:2], yf[:], ALU.mult, ALU.add, [r1k, "WTS", "yf"], ["yf"])
                            tt("gpsimd", yf[:], yf[:], g2b[:], ALU.mult, ["yf", "g2b"], ["yf"])
                            stt(rr_[:], xt[:], ALPHA, yf[:], ALU.mult, ALU.add, [xk, "yf"], ["rr2"])
                            xo_, xok = xo.next()
                            layer_norm_rows("ln2", rr_, "rr2", lg, lb, xo_, xok, lntmp)
                            if last:
                                dma("sync", out[tk0:tk0 + 128, :], xo_[:], [xok], ["out"])
                            else:
                                dma("sync", X2d[tk0:tk0 + 128, :], xo_[:], [xok], ["X2d"])
                    ph_end(final=last)
    return nc


def _const_tables(cfg):
    L, LC, BLK, NB = cfg.L, cfg.LC, cfg.BLK, cfg.NB
    f32 = np.float32
    c = {}
    c["c_ident"] = np.eye(128, dtype=f32)
    n_freq = 16
    t = np.arange(L)
    pos = np.stack([(t // GRID_W).astype(f32), (t % GRID_W).astype(f32)], 0)
    inv = (10000.0 ** (-np.arange(n_freq, dtype=f32) / n_freq)).astype(f32)
    rope = np.zeros((2, 128, L), f32)
    for n in range(128):
        a, r, f = (n // 32) % 2, (n // 16) % 2, n % 16
        ang = (pos[a] * inv[f]).astype(f32)
        rope[0, n] = np.cos(ang)
        rope[1, n] = np.sin(ang) * (-1.0 if r == 0 else 1.0)
    c["c_rope"] = rope

    def hy_tables(Lf):
        tt_ = np.linspace(0.0, 1.0, Lf, dtype=f32)
        w = (2.0 * math.pi * np.arange(Lf, dtype=f32) / Lf).astype(f32)
        fb = np.linspace(1e-4, HY_BANDS - 1, HY_BANDS, dtype=f32)
        emb = np.concatenate([tt_[:, None], np.cos(fb[None, :] * w[:, None]), -np.sin(fb[None, :] * w[:, None])], -1).astype(f32)
        max_decay = math.log(1e-2) / 0.3
        min_decay = math.log(1e-2) / 1.5
        deltas = np.abs(np.linspace(min_decay, max_decay, 256, dtype=f32))
        window = (np.exp(-tt_[:, None] * deltas[None, :]) + 0.05).astype(f32)
        pf = np.arange(Lf - 1, -1, -1)
        pb = np.concatenate([np.arange(1, Lf), [0]])
        e = np.stack([emb[pf].T, emb[pb].T], 0).astype(f32)
        wn = np.stack([window[pf].T, window[pb].T], 0).astype(f32)
        wn[1, :, Lf - 1] = 0.0
        return np.ascontiguousarray(e), np.ascontiguousarray(wn)

    c["c_emb_L"], c["c_win_L"] = hy_tables(L)
    c["c_emb_C"], c["c_win_C"] = hy_tables(LC)
    T = NB * (L + LC)
    NBLK = -(-(2 * T) // BLK) + NE
    MMAX = -(-(2 * T) // BLK) + 1
    cm = np.zeros((128, 128 + 8 + 4 + MMAX + NBLK + 32), f32)
    pp = np.arange(128)
    cm[:, 0:128] = (pp[:, None] < pp[None, :]).astype(f32)
    cm[:, 128:136] = np.arange(8)[None, :] * 128 + pp[:, None]
    cm[:, 136:140] = np.arange(4)[None, :] * 128 + pp[:, None]
    cm[:, 140:140 + MMAX] = (np.arange(MMAX) * BLK)[None, :]
    cm[:, 140 + MMAX:140 + MMAX + NBLK] = (np.arange(NBLK) * BLK)[None, :]
    cm[0:32, 140 + MMAX + NBLK:] = np.eye(32, dtype=f32)
    c["c_moe"] = cm
    return c


def _core_inputs(cfg, inp, core, consts):
    NB, L, LC = cfg.NB, cfg.L, cfg.LC
    f32 = np.float32
    bs = slice(core * NB, (core + 1) * NB)
    m = dict(consts)
    m["x"] = np.ascontiguousarray(inp["x"][bs].reshape(NB * L, D))
    m["ctx"] = np.ascontiguousarray(inp["ctx"][bs].reshape(NB * LC, D))
    cc = np.concatenate([inp["c"][bs], inp["c_ctx"][None, :]], 0)
    m["cT"] = np.ascontiguousarray(cc.T.reshape(8, 128, NB + 1).transpose(1, 0, 2))
    for k in ("ada_w", "ada_b", "w_in", "gm_ln_g", "gm_ln_b", "hy_f_w1", "hy_f_w2", "hy_f_w3", "da_norm_g",
              "p_a", "p_b", "p_c", "w_out", "ln1_g", "ln1_b", "ln2_g", "ln2_b"):
        m[k] = inp[k]
    m["gm_wsT"] = np.ascontiguousarray(inp["gm_ws"].transpose(0, 3, 1, 2))
    m["gm_bsT"] = np.ascontiguousarray(inp["gm_bs"].transpose(0, 2, 1))
    cw = np.concatenate([inp["hy_conv_w"], inp["hy_conv_b"][:, None, :]], 1)
    m["hy_cw"] = np.ascontiguousarray(cw.reshape(DEPTH, 4, 6, 128).transpose(0, 3, 2, 1))
    m["hy_f_b1"] = np.ascontiguousarray(inp["hy_f_b1"][:, :, None])
    m["hy_f_b2"] = np.ascontiguousarray(inp["hy_f_b2"][:, :, None])
    m["hy_b3T"] = np.ascontiguousarray(inp["hy_f_b3"].reshape(DEPTH, 4, 128).transpose(0, 2, 1))
    m["hy_skipT"] = np.ascontiguousarray(inp["hy_skip"].reshape(DEPTH, 2, 128).transpose(0, 2, 1))
    m["da_l"] = np.ascontiguousarray(np.stack([inp["da_lq1"], inp["da_lk1"], inp["da_lq2"], inp["da_lk2"]], 1))
    m["moe_wr"] = np.ascontiguousarray(np.concatenate([inp["moe_wg"], inp["moe_we"]], -1))
    m["moe_br"] = np.ascontiguousarray(np.concatenate([inp["moe_bg"], inp["moe_be"]], -1))
    m["ex_w_gate"] = inp["ex_w_gate"].reshape(DEPTH * NE * D, HID)
    m["ex_w_up"] = inp["ex_w_up"].reshape(DEPTH * NE * D, HID)
    m["ex_w_down"] = inp["ex_w_down"].reshape(DEPTH * NE * HID, D)
    return {k: np.ascontiguousarray(np.asarray(v, dtype=f32)) for k, v in m.items()}


def kernel(**inputs):
    cfg = Cfg()
    inp = {k: np.asarray(v) for k, v in inputs.items()}
    n_cores = inp["x"].shape[0] // cfg.NB
    nc = build(cfg)
    consts = _const_tables(cfg)
    in_maps = [_core_inputs(cfg, inp, c, consts) for c in range(n_cores)]
    res = run_bass_kernel_spmd(nc, in_maps, core_ids=list(range(n_cores)))
    outs = [np.asarray(r["out"]).reshape(cfg.NB, cfg.L, D) for r in res.results]
    return np.concatenate(outs, 0).astype(np.float32)
```

```python
import math
from contextlib import ExitStack

import numpy as np
import concourse.bass as bass
import concourse.mybir as mybir
from concourse.bass_utils import run_bass_kernel_spmd

F32 = mybir.dt.float32
BF16 = mybir.dt.bfloat16
I32 = mybir.dt.int32
AF = mybir.ActivationFunctionType
ALU = mybir.AluOpType
AX = mybir.AxisListType

D = 1024
DEPTH = 2
GRID_W = 64
N_IN = 5888
OFF_HY, OFF_Q, OFF_K, OFF_V, OFF_GATE = 512, 1280, 1792, 2304, 2816
NWB = N_IN + 1024
HY_EMB, HY_FFN, HY_BANDS = 33, 64, 16
NE, NG, EPG, HID = 32, 4, 8, 512
ALPHA = (2.0 * DEPTH) ** 0.25
EPS = 1e-5
BIG = 1.0e30

ENGINES = ("tensor", "vector", "scalar", "gpsimd", "sync")
N_DMA_SEMS = 40


class Prog:
    def __init__(self, nc, stack):
        self.nc = nc
        self.ops = []
        self.esem = {e: stack.enter_context(nc.semaphore("s_" + e)) for e in ENGINES}
        self.dsem = [stack.enter_context(nc.semaphore("d%d" % i)) for i in range(N_DMA_SEMS)]
        self.ecount = {e: 0 for e in ENGINES}
        self.dcount = [0] * N_DMA_SEMS
        self.dnext = 0
        self.lastw = {}
        self.readers = {}
        self.known = {}
        self.nops = 0

    def add(self, eng, fn, r=(), w=(), dma=False):
        self.ops.append((eng, fn, tuple(r), tuple(w), dma))

    def flush(self, final=False):
        nc = self.nc
        esem, dsem, ecount, dcount = self.esem, self.dsem, self.ecount, self.dcount
        lastw, readers, known = self.lastw, self.readers, self.known
        plan = {e: [] for e in ENGINES}
        fence = [(("d", i), dcount[i]) for i in range(N_DMA_SEMS) if dcount[i] > 0]
        fence += [(("e", e), ecount[e]) for e in ENGINES if ecount[e] > 0]
        for (eng, fn, r, w, dma) in self.ops:
            deps = []
            for k in r:
                t = lastw.get(k)
                if t is not None:
                    deps.append(t)
            for k in w:
                t = lastw.get(k)
                if t is not None:
                    deps.append(t)
                for tk, tv in readers.get(k, {}).items():
                    deps.append((tk[0], tk[1], tv))
            if dma:
                si = self.dnext
                self.dnext = (self.dnext + 1) % N_DMA_SEMS
                if dcount[si] > 0:
                    deps.append(("d", si, dcount[si]))
                dcount[si] += 16
                tok = ("d", si, dcount[si])
                inc = (dsem[si], 16)
            else:
                ecount[eng] += 1
                tok = ("e", eng, ecount[eng])
                inc = (esem[eng], 1)
            waits = {}
            for (kind, key, val) in deps:
                if kind == "e" and key == eng and eng == "tensor":
                    continue
                sk = (kind, key)
                if known.get((eng, sk), 0) >= val:
                    continue
                if waits.get(sk, 0) < val:
                    waits[sk] = val
            wl = []
            for sk, val in waits.items():
                known[(eng, sk)] = val
                wl.append((esem[sk[1]] if sk[0] == "e" else dsem[sk[1]], val))
            plan[eng].append((wl, fn, inc))
            for k in r:
                d = readers.setdefault(k, {})
                if d.get(tok[:2], 0) < tok[2]:
                    d[tok[:2]] = tok[2]
            for k in w:
                lastw[k] = tok
                readers[k] = {}
        self.nops += len(self.ops)
        self.ops = []
        endw = []
        if final:
            endw = [(dsem[i], dcount[i]) for i in range(N_DMA_SEMS) if dcount[i] > 0]
            endw += [(esem[e], ecount[e]) for e in ENGINES if ecount[e] > 0]

        def runner(ename):
            def body(eng):
                for sk, val in fence:
                    if sk == ("e", ename):
                        continue
                    if known.get((ename, sk), 0) >= val:
                        continue
                    known[(ename, sk)] = val
                    eng.wait_ge(esem[sk[1]] if sk[0] == "e" else dsem[sk[1]], val)
                for (wl, fn, inc) in plan[ename]:
                    for (sem, val) in wl:
                        eng.wait_ge(sem, val)
                    ins = fn(eng)
                    ins.then_inc(inc[0], inc[1])
                for (sem, val) in endw:
                    eng.wait_ge(sem, val)
            return body

        with nc.Block() as block:
            block.tensor(runner("tensor"))
            block.vector(runner("vector"))
            block.scalar(runner("scalar"))
            block.gpsimd(runner("gpsimd"))
            block.sync(runner("sync"))


class Cfg:
    def __init__(self, NB=4, L=2048, LC=256, BLK=512, stop=None):
        self.NB, self.L, self.LC, self.BLK, self.stop = NB, L, LC, BLK, stop


class _Stop(Exception):
    pass


def _ap(base, off, dims, part=None):
    p = list(base.ap[0]) if part is None else [base.ap[0][0], part]
    return bass.AP(tensor=base.tensor, offset=base.offset + off, ap=[p] + [list(d) for d in dims])


class Rot:
    def __init__(self, tiles, name):
        self.tiles, self.name, self.i = tiles, name, 0

    def next(self):
        t = self.tiles[self.i % len(self.tiles)]
        k = "%s%d" % (self.name, self.i % len(self.tiles))
        self.i += 1
        return t, k


def build(cfg, debug=False):
    holder = {}
    try:
        _build(cfg, debug, holder)
    except _Stop:
        pass
    return holder["nc"]


def _build(cfg, debug, holder):
    NB, L, LC, BLK = cfg.NB, cfg.L, cfg.LC, cfg.BLK
    nc = bass.Bass("TRN2", target_bir_lowering=False)
    holder["nc"] = nc
    T = NB * (L + LC)
    NT = T // 128
    NTL = NB * L // 128
    RB = BLK // 128

    def din(name, shape, dt=F32):
        return nc.dram_tensor(name, list(shape), dt, kind="ExternalInput").ap()

    def dscr(name, shape, dt):
        return nc.dram_tensor(name, list(shape), dt, kind="ExternalOutput" if debug else "Internal").ap()

    x_in = din("x", [NB * L, D])
    ctx_in = din("ctx", [NB * LC, D])
    cT_in = din("cT", [128, 8, NB + 1])
    ada_w = din("ada_w", [DEPTH, D, 6 * D])
    ada_b = din("ada_b", [DEPTH, 6 * D])
    w_in = din("w_in", [DEPTH, D, N_IN])
    gm_ln_g = din("gm_ln_g", [DEPTH, 256])
    gm_ln_b = din("gm_ln_b", [DEPTH, 256])
    gm_wsT = din("gm_wsT", [DEPTH, 128, 4, 128])
    gm_bsT = din("gm_bsT", [DEPTH, 128, 4])
    hy_cw = din("hy_cw", [DEPTH, 128, 6, 4])
    hy_w1 = din("hy_f_w1", [DEPTH, HY_EMB, HY_FFN])
    hy_b1 = din("hy_f_b1", [DEPTH, HY_FFN, 1])
    hy_w2 = din("hy_f_w2", [DEPTH, HY_FFN, HY_FFN])
    hy_b2 = din("hy_f_b2", [DEPTH, HY_FFN, 1])
    hy_w3 = din("hy_f_w3", [DEPTH, HY_FFN, 512])
    hy_b3T = din("hy_b3T", [DEPTH, 128, 4])
    hy_skipT = din("hy_skipT", [DEPTH, 128, 2])
    da_l = din("da_l", [DEPTH, 4, 64])
    da_g = din("da_norm_g", [DEPTH, 128])
    p_a = din("p_a", [DEPTH, 256, D])
    p_b = din("p_b", [DEPTH, 256, D])
    p_c = din("p_c", [DEPTH, 512, D])
    w_out = din("w_out", [DEPTH, D, D])
    ln1_g = din("ln1_g", [DEPTH, D])
    ln1_b = din("ln1_b", [DEPTH, D])
    moe_wr = din("moe_wr", [DEPTH, D, 36])
    moe_br = din("moe_br", [DEPTH, 36])
    ex_g = din("ex_w_gate", [DEPTH * NE * D, HID])
    ex_u = din("ex_w_up", [DEPTH * NE * D, HID])
    ex_d = din("ex_w_down", [DEPTH * NE * HID, D])
    ex_g8 = ex_g.rearrange("(r j) h -> r (j h)", j=8)
    ex_u8 = ex_u.rearrange("(r j) h -> r (j h)", j=8)
    ex_d4 = ex_d.rearrange("(r j) d -> r (j d)", j=4)
    ln2_g = din("ln2_g", [DEPTH, D])
    ln2_b = din("ln2_b", [DEPTH, D])
    ident_in = din("c_ident", [128, 128])
    rope_in = din("c_rope", [2, 128, L])
    emb_in = {L: din("c_emb_L", [2, HY_EMB, L]), LC: din("c_emb_C", [2, HY_EMB, LC])}
    win_in = {L: din("c_win_L", [2, 256, L]), LC: din("c_win_C", [2, 256, LC])}
    NBLK = -(-(2 * T) // BLK) + NE
    MMAX = -(-(2 * T) // BLK) + 1
    cmoe_in = din("c_moe", [128, 128 + 8 + 4 + MMAX + NBLK + 32])
    out = nc.dram_tensor("out", [NB * L, D], F32, kind="ExternalOutput").ap()

    Wb = dscr("s_wb", [8, 128, NWB], BF16)
    MODS = dscr("s_mods", [DEPTH, NB + 1, 6 * D], F32)
    KREV = {L: dscr("s_krevL", [256, 2 * L], BF16), LC: dscr("s_krevC", [256, 2 * LC], BF16)}
    QTd = dscr("s_qt", [4, 128, T], BF16)
    KTd = dscr("s_kt", [4, 128, T], BF16)
    Vd = dscr("s_v", [T, 512], BF16)
    Gd = dscr("s_g", [24, 128, T], BF16)
    YATd = dscr("s_yat", [2, 128, T], BF16)
    X0Td = dscr("s_x0t", [2, 128, T], BF16)
    YBTd = dscr("s_ybt", [2, 128, T], BF16)
    YCTd = dscr("s_yct", [4, 128, T], BF16)
    X1d = dscr("s_x1", [T, D], F32)
    X2d = dscr("s_x2", [T, D], F32)
    H2d = dscr("s_h2", [T, D], BF16)
    XBUF = dscr("s_xbuf", [NBLK * BLK, D], BF16)
    YBUF = dscr("s_ybuf", [NBLK * BLK, D], BF16)

    seqs = [("lat", b, b * L, L) for b in range(NB)] + [("ctx", b, NB * L + b * LC, LC) for b in range(NB)]

    with ExitStack() as top:
        p = Prog(nc, top)

        uniq = [0]

        def sbt(st, name, shape, dt):
            uniq[0] += 1
            return st.enter_context(nc.sbuf_tensor("%s_%d" % (name, uniq[0]), list(shape), dt))

        def pst(st, name, shape, dt=F32):
            uniq[0] += 1
            return st.enter_context(nc.psum_tensor("%s_%d" % (name, uniq[0]), list(shape), dt))

        def dma(eng, out_, in_, r, w, **kw):
            p.add(eng, lambda e: e.dma_start(out=out_, in_=in_, **kw), r, w, dma=True)

        def mm(out_, lhsT, rhs, start, stop, r, w):
            p.add("tensor", lambda e: e.matmul(out_, lhsT=lhsT, rhs=rhs, start=start, stop=stop,
                                               skip_group_check=True), r, w)

        def tr(out_, in_, ident, r, w):
            p.add("tensor", lambda e: e.transpose(out=out_, in_=in_, identity=ident), r, w)

        def act(out_, in_, func, r, w, **kw):
            p.add("scalar", lambda e: e.activation(out=out_, in_=in_, func=func, **kw), r, w)

        def tt(eng, out_, a, b, op, r, w):
            p.add(eng, lambda e: e.tensor_tensor(out=out_, in0=a, in1=b, op=op), r, w)

        def ts(eng, out_, a, s1, op0, r, w, s2=None, op1=None):
            if op1 is None:
                p.add(eng, lambda e: e.tensor_scalar(out=out_, in0=a, scalar1=s1, scalar2=None, op0=op0), r, w)
            else:
                p.add(eng, lambda e: e.tensor_scalar(out=out_, in0=a, scalar1=s1, scalar2=s2, op0=op0, op1=op1), r, w)

        def stt(out_, a, s, b, op0, op1, r, w):
            p.add("vector", lambda e: e.scalar_tensor_tensor(out=out_, in0=a, scalar=s, in1=b, op0=op0, op1=op1), r, w)

        def cp(eng, out_, in_, r, w):
            if eng == "scalar":
                p.add(eng, lambda e: e.copy(out=out_, in_=in_), r, w)
            else:
                p.add(eng, lambda e: e.tensor_copy(out=out_, in_=in_), r, w)

        def red(out_, in_, op, axis, r, w):
            p.add("vector", lambda e: e.tensor_reduce(out=out_, in_=in_, axis=axis, op=op), r, w)

        def memset(eng, ap_, val, w):
            p.add(eng, lambda e: e.memset(ap_, val), (), w)

        phase_no = [0]

        def ph_end(final=False):
            phase_no[0] += 1
            stop = cfg.stop is not None and phase_no[0] >= cfg.stop
            p.flush(final=final or stop)
            if stop and not final:
                raise _Stop()

        identf = sbt(top, "identf", [128, 128], F32)
        identb = sbt(top, "identb", [128, 128], BF16)
        ropeT = sbt(top, "ropeT", [128, 2, L], BF16)
        epsc = sbt(top, "epsc", [128, 1], F32)
        dma("sync", identf[:], ident_in[:, :], [], ["identf"])
        cp("vector", identb[:], identf[:], ["identf"], ["identb"])
        with ExitStack() as ph:
            ropeF = sbt(ph, "ropeF", [128, 2, L], F32)
            dma("sync", ropeF[:, 0, :], rope_in[0, :, :], [], ["ropeF"])
            dma("sync", ropeF[:, 1, :], rope_in[1, :, :], [], ["ropeF"])
            cp("vector", ropeT[:], ropeF[:], ["ropeF"], ["ropeT"])
            memset("vector", epsc[:], EPS, ["epsc"])
            ph_end()

        def layer_norm_rows(st_tag, r_t, rk, g_b, b_b, out_t, ok, tmp):
            stats, mv, rstd = tmp["stats"], tmp["mv"], tmp["rstd"]
            for hh in range(2):
                p.add("vector", lambda e, hh=hh: e.bn_stats(out=stats[:, hh, :], in_=r_t[:, hh * 512:(hh + 1) * 512]),
                      [rk], [st_tag + "stats"])
            p.add("vector", lambda e: e.bn_aggr(out=mv[:], in_=stats[:].rearrange("p a b -> p (a b)")),
                  [st_tag + "stats"], [st_tag + "mv"])
            act(rstd[:], mv[:, 1:2], AF.Sqrt, [st_tag + "mv", "epsc"], [st_tag + "rstd"], bias=epsc[:], scale=1.0)
            p.add("vector", lambda e: e.reciprocal(out=rstd[:], in_=rstd[:]), [st_tag + "rstd"], [st_tag + "rstd"])
            ts("vector", out_t[:], r_t[:], mv[:, 0:1], ALU.subtract, [rk, st_tag + "mv", st_tag + "rstd"], [ok],
               s2=rstd[:, 0:1], op1=ALU.mult)
            tt("gpsimd", out_t[:], out_t[:], g_b[:], ALU.mult, [ok, "lnconst"], [ok])
            tt("gpsimd", out_t[:], out_t[:], b_b[:], ALU.add, [ok, "lnconst"], [ok])

        for l in range(DEPTH):
            last = l == DEPTH - 1
            lam_init = 0.8 - 0.6 * math.exp(-0.3 * l)
            Xsrc = (lambda tok0, n: (x_in[tok0:tok0 + n, :] if tok0 < NB * L else ctx_in[tok0 - NB * L:tok0 - NB * L + n, :])) \
                if l == 0 else (lambda tok0, n: X2d[tok0:tok0 + n, :])
            act_seqs = [s for s in seqs if not (last and s[0] == "ctx")]
            NTm = (NTL if last else NT)
            with ExitStack() as lay:
                lamt = sbt(lay, "lamt", [128, 4], F32)
                gsc = sbt(lay, "gsc", [128, 128], F32)

                with ExitStack() as ph:
                    wf = [sbt(ph, "wf%d" % i, [128, N_IN], F32) for i in range(2)]
                    wbt = [sbt(ph, "wbt%d" % i, [128, NWB], BF16) for i in range(2)]
                    for kt in range(8):
                        a, b_ = wf[kt % 2], wbt[kt % 2]
                        ka, kb = "wf%d" % (kt % 2), "wbt%d" % (kt % 2)
                        dma("sync", a[:], w_in[l, kt * 128:(kt + 1) * 128, :], [], [ka])
                        cp("vector", b_[:, 0:2048], a[:, 0:2048], [ka], [kb + "a"])
                        cp("gpsimd", b_[:, 2048:4096], a[:, 2048:4096], [ka], [kb + "b"])
                        cp("scalar", b_[:, 4096:N_IN], a[:, 4096:N_IN], [ka], [kb + "c"])
                        for qi, off in enumerate((OFF_Q, OFF_K)):
                            for rr in range(2):
                                o_ = _ap(b_[:], N_IN + qi * 512 + rr * 16, [[32, 16], [1, 16]])
                                i_ = _ap(a[:], off + (1 - rr) * 16, [[32, 16], [1, 16]])
                                cp("vector", o_, i_, [ka], [kb + "d%d%d" % (qi, rr)])
                        dma("sync", Wb[kt, :, :], b_[:], [kb + "a", kb + "b", kb + "c", kb + "d00", kb + "d01", kb + "d10", kb + "d11"], ["Wb"])
                    dl = sbt(ph, "dl", [128, 4, 64], F32)
                    dg = sbt(ph, "dg", [128, 128], F32)
                    pr = sbt(ph, "pr", [128, 2, 64], F32)
                    dma("sync", dl[:], bass.AP(tensor=da_l.tensor, offset=da_l.offset + l * 256, ap=[[0, 128], [64, 4], [1, 64]]), [], ["dl"])
                    dma("sync", dg[:], bass.AP(tensor=da_g.tensor, offset=da_g.offset + l * 128, ap=[[0, 128], [1, 128]]), [], ["dg"])
                    tt("vector", pr[:, 0, :], dl[:, 0, :], dl[:, 1, :], ALU.mult, ["dl"], ["pr"])
                    tt("vector", pr[:, 1, :], dl[:, 2, :], dl[:, 3, :], ALU.mult, ["dl"], ["pr"])
                    red(lamt[:, 1:3], pr[:], ALU.add, AX.X, ["pr"], ["lamt"])
                    act(lamt[:, 1:3], lamt[:, 1:3], AF.Exp, ["lamt"], ["lamt"])
                    tt("vector", lamt[:, 0:1], lamt[:, 2:3], lamt[:, 1:2], ALU.subtract, ["lamt"], ["lamt"])
                    ts("vector", lamt[:, 0:1], lamt[:, 0:1], -lam_init, ALU.add, ["lamt"], ["lamt"])
                    ts("vector", gsc[:], dg[:], 1.0 - lam_init, ALU.mult, ["dg"], ["gsc"])
                    ph_end()

                with ExitStack() as ph:
                    cTt = sbt(ph, "cTt", [128, 8, NB + 1], F32)
                    sct = sbt(ph, "sct", [128, 8, NB + 1], F32)
                    adb = sbt(ph, "adb", [NB + 1, 6 * D], F32)
                    modt = sbt(ph, "modt", [NB + 1, 6 * D], F32)
                    awt = [sbt(ph, "awt%d" % i, [128, 3072], F32) for i in range(2)]
                    psm = pst(ph, "psm", [128, 3072])
                    dma("sync", cTt[:], cT_in[:, :, :], [], ["cTt"])
                    dma("sync", adb[:], bass.AP(tensor=ada_b.tensor, offset=ada_b.offset + l * 6 * D,
                                                ap=[[0, NB + 1], [1, 6 * D]]), [], ["adb"])
                    act(sct[:], cTt[:], AF.Silu, ["cTt"], ["sct"])
                    i = 0
                    for half in range(2):
                        for kt in range(8):
                            a, ka = awt[i % 2], "awt%d" % (i % 2)
                            i += 1
                            dma("sync" if kt % 2 == 0 else "gpsimd", a[:],
                                ada_w[l, kt * 128:(kt + 1) * 128, half * 3072:(half + 1) * 3072], [], [ka])
                            for ng in range(6):
                                mm(psm[0:NB + 1, ng * 512:(ng + 1) * 512], sct[:, kt, :], a[:, ng * 512:(ng + 1) * 512],
                                   kt == 0, kt == 7, [ka, "sct"], ["psm"])
                        tt("vector", modt[:, half * 3072:(half + 1) * 3072], psm[0:NB + 1, :],
                           adb[:, half * 3072:(half + 1) * 3072], ALU.add, ["psm", "adb"], ["modt"])
                    dma("sync", MODS[l, :, :], modt[:], ["modt"], ["MODS"])
                    ph_end()

                with ExitStack() as ph:
                    w1f = sbt(ph, "w1f", [HY_EMB, HY_FFN], F32)
                    w2f = sbt(ph, "w2f", [HY_FFN, HY_FFN], F32)
                    w3f = sbt(ph, "w3f", [HY_FFN, 512], F32)
                    b1t = sbt(ph, "b1t", [HY_FFN, 1], F32)
                    b2t = sbt(ph, "b2t", [HY_FFN, 1], F32)
                    b3t = sbt(ph, "b3t", [128, 4], F32)
                    skt = sbt(ph, "skt", [128, 2], F32)
                    dma("sync", w1f[:], hy_w1[l, :, :], [], ["hyw"])
                    dma("sync", w2f[:], hy_w2[l, :, :], [], ["hyw"])
                    dma("sync", w3f[:], hy_w3[l, :, :], [], ["hyw"])
                    dma("sync", b1t[:], hy_b1[l, :, :], [], ["hyw"])
                    dma("sync", b2t[:], hy_b2[l, :, :], [], ["hyw"])
                    dma("sync", b3t[:], hy_b3T[l, :, :], [], ["hyw"])
                    dma("sync", skt[:], hy_skipT[l, :, :], [], ["hyw"])
                    ps1 = pst(ph, "ps1", [128, 512])
                    ps2 = pst(ph, "ps2", [128, 512])
                    ps3 = pst(ph, "ps3", [128, 512])
                    for Lf in ([L] if last else [L, LC]):
                        with ExitStack() as ph2:
                            CHF = min(512, Lf)
                            embt = sbt(ph2, "embt", [HY_EMB, 2, Lf], F32)
                            wint = sbt(ph2, "wint", [128, 2, 2, Lf], F32)
                            krf = sbt(ph2, "krf", [128, 2, 2 * Lf], F32)
                            krb = sbt(ph2, "krb", [128, 2, 2 * Lf], BF16)
                            h1 = sbt(ph2, "h1", [HY_FFN, 512], F32)
                            h2 = sbt(ph2, "h2", [HY_FFN, 512], F32)
                            wr1 = sbt(ph2, "wr1", [HY_FFN, 512], F32)
                            wr2 = sbt(ph2, "wr2", [HY_FFN, 512], F32)
                            tg = "f%d" % Lf
                            for dr in range(2):
                                dma("sync", embt[:, dr, :], emb_in[Lf][dr, :, :], [], [tg + "emb"])
                                for cc in range(2):
                                    dma("gpsimd", wint[:, dr, cc, :], win_in[Lf][dr, cc * 128:(cc + 1) * 128, :], [], [tg + "win"])
                            memset("gpsimd", krf[:], 0.0, [tg + "krf"])
                            for dr in range(2):
                                for ch in range(Lf // CHF):
                                    cs = slice(ch * CHF, (ch + 1) * CHF)
                                    mm(ps1[0:HY_FFN, 0:CHF], w1f[:], embt[:, dr, cs], True, True, ["hyw", tg + "emb"], ["ps1"])
                                    ts("vector", h1[:, 0:CHF], ps1[0:HY_FFN, 0:CHF], b1t[:, 0:1], ALU.add, ["ps1", "hyw"], ["h1"])
                                    ts("vector", wr1[:, 0:CHF], h1[:, 0:CHF], math.pi, ALU.is_gt, ["h1"], ["wr1"], s2=-2 * math.pi, op1=ALU.mult)
                                    ts("vector", wr2[:, 0:CHF], h1[:, 0:CHF], -math.pi, ALU.is_lt, ["h1"], ["wr2"], s2=2 * math.pi, op1=ALU.mult)
                                    tt("vector", h1[:, 0:CHF], h1[:, 0:CHF], wr1[:, 0:CHF], ALU.add, ["h1", "wr1"], ["h1"])
                                    tt("vector", h1[:, 0:CHF], h1[:, 0:CHF], wr2[:, 0:CHF], ALU.add, ["h1", "wr2"], ["h1"])
                                    act(h1[:, 0:CHF], h1[:, 0:CHF], AF.Sin, ["h1"], ["h1"])
                                    mm(ps2[0:HY_FFN, 0:CHF], w2f[:], h1[:, 0:CHF], True, True, ["hyw", "h1"], ["ps2"])
                                    ts("vector", h2[:, 0:CHF], ps2[0:HY_FFN, 0:CHF], b2t[:, 0:1], ALU.add, ["ps2", "hyw"], ["h2"])
                                    ts("vector", wr1[:, 0:CHF], h2[:, 0:CHF], math.pi, ALU.is_gt, ["h2"], ["wr1"], s2=-2 * math.pi, op1=ALU.mult)
                                    ts("vector", wr2[:, 0:CHF], h2[:, 0:CHF], -math.pi, ALU.is_lt, ["h2"], ["wr2"], s2=2 * math.pi, op1=ALU.mult)
                                    tt("vector", h2[:, 0:CHF], h2[:, 0:CHF], wr1[:, 0:CHF], ALU.add, ["h2", "wr1"], ["h2"])
                                    tt("vector", h2[:, 0:CHF], h2[:, 0:CHF], wr2[:, 0:CHF], ALU.add, ["h2", "wr2"], ["h2"])
                                    act(h2[:, 0:CHF], h2[:, 0:CHF], AF.Sin, ["h2"], ["h2"])
                                    for cc in range(2):
                                        mm(ps3[:, 0:CHF], w3f[:, dr * 256 + cc * 128:dr * 256 + (cc + 1) * 128], h2[:, 0:CHF],
                                           True, True, ["hyw", "h2"], ["ps3"])
                                        o0 = dr * Lf + ch * CHF
                                        stt(krf[:, cc, o0:o0 + CHF], ps3[:, 0:CHF], b3t[:, dr * 2 + cc:dr * 2 + cc + 1],
                                            wint[:, dr, cc, cs], ALU.add, ALU.mult, ["ps3", "hyw", tg + "win", tg + "krf"], [tg + "krf"])
                            for cc in range(2):
                                ts("vector", krf[:, cc, Lf - 1:Lf], krf[:, cc, Lf - 1:Lf], skt[:, cc:cc + 1], ALU.add,
                                   [tg + "krf", "hyw"], [tg + "krf"])
                            cp("vector", krb[:, 0, :], krf[:, 0, :], [tg + "krf"], [tg + "krb"])
                            cp("gpsimd", krb[:, 1, :], krf[:, 1, :], [tg + "krf"], [tg + "krb"])
                            for cc in range(2):
                                dma("sync", KREV[Lf][cc * 128:(cc + 1) * 128, :], krb[:, cc, :], [tg + "krb"], ["KREV%d" % Lf])
                            ph_end()

                with ExitStack() as ph45:
                    nbL, nbC = L // 128, LC // 128
                    VXs = {"lat": sbt(ph45, "VXsL", [128, 256, nbL, NB], BF16)}
                    if not last:
                        VXs["ctx"] = sbt(ph45, "VXsC", [128, 256, nbC, NB], BF16)
                    with ExitStack() as ph:
                        LMAX = L
                        hT = sbt(ph, "hT", [128, 8, LMAX], BF16)
                        zhs = Rot([sbt(ph, "zh%d" % i, [128, LMAX + 2], F32) for i in range(2)], "zh")
                        VT2 = sbt(ph, "VT2", [128, 2, LMAX], F32)
                        tmpA = sbt(ph, "tmpA", [128, LMAX], F32)
                        x0bs = Rot([sbt(ph, "x0b%d" % i, [128, LMAX], BF16) for i in range(2)], "x0b")
                        wgs = Rot([sbt(ph, "wg%d" % i, [128, 8, 512], BF16) for i in range(3)], "wg")
                        scb = sbt(ph, "scb", [128, D], F32)
                        shb = sbt(ph, "shb", [128, D], F32)
                        xts = Rot([sbt(ph, "xt%d" % i, [128, D], F32) for i in range(2)], "xt")
                        hbs = Rot([sbt(ph, "hb%d" % i, [128, D], BF16) for i in range(2)], "hb")
                        lngb = sbt(ph, "lngb", [128, 256], F32)
                        lnbb = sbt(ph, "lnbb", [128, 256], F32)
                        wsf = sbt(ph, "wsf", [128, 4, 128], F32)
                        wsb = sbt(ph, "wsb", [128, 4, 128], BF16)
                        bst = sbt(ph, "bst", [128, 4], F32)
                        cwt = sbt(ph, "cwt", [128, 6, 4], F32)
                        gmf = sbt(ph, "gmf", [128, 512], F32)
                        vnb = sbt(ph, "vnb", [128, 256], BF16)
                        vnf = sbt(ph, "vnf", [128, 256], F32)
                        yab = sbt(ph, "yab", [128, 256], BF16)
                        yaT = sbt(ph, "yaT", [128, 2, 128], BF16)
                        gst = sbt(ph, "gst", [128, 6], F32)
                        gmv = sbt(ph, "gmv", [128, 2], F32)
                        grs = sbt(ph, "grs", [128, 1], F32)
                        vts = Rot([sbt(ph, "vt%d" % i, [128, 512], BF16) for i in range(2)], "vt")
                        fos = Rot([sbt(ph, "fo%d" % i, [128, 512], BF16) for i in range(3)], "fo")
                        t1s = Rot([sbt(ph, "t1_%d" % i, [128, 512], F32) for i in range(2)], "t1_")
                        t2s = Rot([sbt(ph, "t2_%d" % i, [128, 512], F32) for i in range(2)], "t2_")
                        VXT = sbt(ph, "VXT", [128, 2, LMAX], BF16)
                        psH = pst(ph, "psH", [128, 8, 128], BF16)
                        psFs = Rot([pst(ph, "psF%d" % i, [128, 512]) for i in range(2)], "psF")
                        psPs = Rot([pst(ph, "psP%d" % i, [128, 512]) for i in range(2)], "psP")
                        psT = pst(ph, "psT", [128, 512])
                        psS = pst(ph, "psS", [128, 256])
                        psYA = pst(ph, "psYA", [128, 2, 128], BF16)
                        dma("sync", lngb[:], bass.AP(tensor=gm_ln_g.tensor, offset=gm_ln_g.offset + l * 256, ap=[[0, 128], [1, 256]]), [], ["gmc"])
                        dma("sync", lnbb[:], bass.AP(tensor=gm_ln_b.tensor, offset=gm_ln_b.offset + l * 256, ap=[[0, 128], [1, 256]]), [], ["gmc"])
                        dma("sync", wsf[:], gm_wsT[l, :, :, :], [], ["wsf"])
                        cp("vector", wsb[:], wsf[:], ["wsf"], ["gmc"])
                        dma("sync", bst[:], gm_bsT[l, :, :], [], ["gmc"])
                        dma("sync", cwt[:], hy_cw[l, :, :, :], [], ["gmc"])
                        for zt in zhs.tiles:
                            memset("vector", zt[:, 0:1], 0.0, ["zh0", "zh1"])

                        for (kind, b, tok0, Ls) in seqs:
                            full = not (last and kind == "ctx")
                            CH = min(512, Ls)
                            nch = Ls // CH
                            ntile = Ls // 128
                            mrow = b if kind == "lat" else NB
                            rope = kind == "lat"
                            dma("sync", shb[:], bass.AP(tensor=MODS.tensor, offset=MODS.offset + (l * (NB + 1) + mrow) * 6 * D,
                                                        ap=[[0, 128], [1, D]]), ["MODS"], ["shb"])
                            dma("sync", scb[:], bass.AP(tensor=MODS.tensor, offset=MODS.offset + (l * (NB + 1) + mrow) * 6 * D + D,
                                                        ap=[[0, 128], [1, D]]), ["MODS"], ["scb"])
                            ts("gpsimd", scb[:], scb[:], 1.0, ALU.add, ["scb"], ["scb"])
                            for zt in zhs.tiles:
                                memset("vector", zt[:, Ls + 1:Ls + 2], 0.0, ["zh0", "zh1"])
                            for tI in range(ntile):
                                xt, xk = xts.next()
                                hb, hk = hbs.next()
                                dma("sync" if tI % 2 == 0 else "gpsimd", xt[:], Xsrc(tok0 + tI * 128, 128), ["X2d"] if l else [], [xk])
                                tt("vector", xt[:], xt[:], scb[:], ALU.mult, [xk, "scb"], [xk])
                                tt("gpsimd", hb[:], xt[:], shb[:], ALU.add, [xk, "shb"], [hk])
                                for kt in range(8):
                                    tr(psH[:, kt, :], hb[:, kt * 128:(kt + 1) * 128], identb[:], [hk, "identb"], ["psH"])
                                cp("scalar", hT[:, :, tI * 128:(tI + 1) * 128], psH[:], ["psH"], ["hT"])

                            def load_group(col0):
                                wg, wk = wgs.next()
                                dma("sync", wg[:], Wb[:, :, col0:col0 + 512].rearrange("k p c -> p k c"), ["Wb"], [wk])
                                return wg, wk

                            def fm_chunk(wg, wk, ci, tc, ps, pk):
                                for kt in range(8):
                                    mm(ps[:, 0:CH], wg[:, kt, ci * 128:(ci + 1) * 128], hT[:, kt, tc * CH:(tc + 1) * CH],
                                       kt == 0, kt == 7, [wk, "hT"], [pk])

                            if full:
                                wg, wk = load_group(0)
                                for tI in range(ntile):
                                    for kt in range(8):
                                        mm(psT[:], hT[:, kt, tI * 128:(tI + 1) * 128], wg[:, kt, :], kt == 0, kt == 7, [wk, "hT"], ["psT"])
                                    act(gmf[:], psT[:], AF.Gelu, ["psT"], ["gmf"])
                                    p.add("vector", lambda e: e.bn_stats(out=gst[:], in_=gmf[:, 256:512]), ["gmf"], ["gst"])
                                    p.add("vector", lambda e: e.bn_aggr(out=gmv[:], in_=gst[:]), ["gst"], ["gmv"])
                                    act(grs[:], gmv[:, 1:2], AF.Sqrt, ["gmv", "epsc"], ["grs"], bias=epsc[:], scale=1.0)
                                    p.add("vector", lambda e: e.reciprocal(out=grs[:], in_=grs[:]), ["grs"], ["grs"])
                                    ts("vector", vnf[:], gmf[:, 256:512], gmv[:, 0:1], ALU.subtract, ["gmf", "gmv", "grs"], ["vnf"],
                                       s2=grs[:, 0:1], op1=ALU.mult)
                                    tt("gpsimd", vnf[:], vnf[:], lngb[:], ALU.mult, ["vnf", "gmc"], ["vnf"])
                                    tt("gpsimd", vnb[:], vnf[:], lnbb[:], ALU.add, ["vnf", "gmc"], ["vnb"])
                                    for g in range(4):
                                        mm(psS[:, g * 64:(g + 1) * 64], wsb[:, g, :], vnb[:, g * 64:(g + 1) * 64], True, True, ["gmc", "vnb"], ["psS"])
                                    for g in range(4):
                                        stt(yab[:, g * 64:(g + 1) * 64], psS[:, g * 64:(g + 1) * 64], bst[:, g:g + 1],
                                            gmf[:, g * 64:(g + 1) * 64], ALU.add, ALU.mult, ["psS", "gmc", "gmf"], ["yab"])
                                    for cc in range(2):
                                        tr(psYA[:, cc, :], yab[:, cc * 128:(cc + 1) * 128], identb[:], ["yab", "identb"], ["psYA"])
                                    cp("scalar", yaT[:], psYA[:], ["psYA"], ["yaT"])
                                    t0 = tok0 + tI * 128
                                    dma("gpsimd", YATd[:, :, t0:t0 + 128].rearrange("c p t -> p c t"), yaT[:], ["yaT"], ["YATd"])
                                for (col0, cis) in ((OFF_HY + 512, (1, 0)), (OFF_HY, (3, 2, 1, 0))):
                                    wg, wk = load_group(col0)
                                    for ci in cis:
                                        r_ = (col0 - OFF_HY) // 128 + ci
                                        zh, zk = zhs.next()
                                        for tc in range(nch):
                                            ps, pk = psFs.next()
                                            fm_chunk(wg, wk, ci, tc, ps, pk)
                                            cp("scalar", zh[:, 1 + tc * CH:1 + (tc + 1) * CH], ps[:, 0:CH], [pk], [zk])
                                        if r_ >= 4:
                                            acc, ak = VT2[:, r_ - 4, 0:Ls], "VT2"
                                        else:
                                            acc, ak = tmpA[:, 0:Ls], "tmpA"
                                        ts("vector", acc, zh[:, 0:Ls], cwt[:, r_, 0:1], ALU.mult, [zk, "gmc"], [ak],
                                           s2=cwt[:, r_, 3:4], op1=ALU.add)
                                        stt(acc, zh[:, 1:Ls + 1], cwt[:, r_, 1:2], acc, ALU.mult, ALU.add, [zk, "gmc", ak], [ak])
                                        if r_ >= 4:
                                            stt(acc, zh[:, 2:Ls + 2], cwt[:, r_, 2:3], acc, ALU.mult, ALU.add, [zk, "gmc", ak], [ak])
                                        elif r_ >= 2:
                                            stt(acc, zh[:, 2:Ls + 2], cwt[:, r_, 2:3], acc, ALU.mult, ALU.add, [zk, "gmc", ak], [ak])
                                            tt("gpsimd", VXT[:, r_ - 2, 0:Ls], acc, VT2[:, r_ - 2, 0:Ls], ALU.mult, [ak, "VT2"], ["VXT"])
                                        else:
                                            x0b, x0k = x0bs.next()
                                            stt(x0b[:, 0:Ls], zh[:, 2:Ls + 2], cwt[:, r_, 2:3], acc, ALU.mult, ALU.add, [zk, "gmc", ak], [x0k])
                                            dma("gpsimd", X0Td[r_, :, tok0:tok0 + Ls], x0b[:, 0:Ls], [x0k], ["X0Td"])
                                vx = VXs[kind]
                                for tI in range(ntile):
                                    for cc in range(2):
                                        tr(psYA[:, cc, :], VXT[:, cc, tI * 128:(tI + 1) * 128], identb[:], ["VXT", "identb"], ["psYA"])
                                    cp("scalar", vx[:, :, tI, b], psYA[:].rearrange("p a b -> p (a b)"), ["psYA"], ["VXs" + kind])
                            for (nm, colA, colB, dst) in (("q", OFF_Q, N_IN, QTd), ("k", OFF_K, N_IN + 512, KTd)):
                                if nm == "q" and not full:
                                    continue
                                wgA, wkA = load_group(colA)
                                if rope:
                                    wgB, wkB = load_group(colB)
                                for h in range(4):
                                    for tc in range(nch):
                                        ps, pk = psFs.next()
                                        fm_chunk(wgA, wkA, h, tc, ps, pk)
                                        fo, fk = fos.next()
                                        if rope:
                                            ps2_, pk2 = psPs.next()
                                            fm_chunk(wgB, wkB, h, tc, ps2_, pk2)
                                            t1, k1 = t1s.next()
                                            t2, k2 = t2s.next()
                                            tt("vector", t1[:, 0:CH], ps[:, 0:CH], ropeT[:, 0, tc * CH:(tc + 1) * CH], ALU.mult, [pk, "ropeT"], [k1])
                                            tt("vector", t2[:, 0:CH], ps2_[:, 0:CH], ropeT[:, 1, tc * CH:(tc + 1) * CH], ALU.mult, [pk2, "ropeT"], [k2])
                                            tt("gpsimd", fo[:, 0:CH], t1[:, 0:CH], t2[:, 0:CH], ALU.add, [k1, k2], [fk])
                                        else:
                                            cp("scalar", fo[:, 0:CH], ps[:, 0:CH], [pk], [fk])
                                        t0 = tok0 + tc * CH
                                        dma("gpsimd", dst[h, :, t0:t0 + CH], fo[:, 0:CH], [fk], [nm + "Td"])
                            wg, wk = load_group(OFF_V)
                            for tI in range(ntile):
                                for kt in range(8):
                                    mm(psT[:], hT[:, kt, tI * 128:(tI + 1) * 128], wg[:, kt, :], kt == 0, kt == 7, [wk, "hT"], ["psT"])
                                vt, vk = vts.next()
                                cp("scalar", vt[:], psT[:], ["psT"], [vk])
                                t0 = tok0 + tI * 128
                                dma("gpsimd", Vd[t0:t0 + 128, :], vt[:], [vk], ["Vd"])
                            if full:
                                for gi in range(6):
                                    wg, wk = load_group(OFF_GATE + gi * 512)
                                    for ci in range(4):
                                        for tc in range(nch):
                                            ps, pk = psFs.next()
                                            fm_chunk(wg, wk, ci, tc, ps, pk)
                                            fo, fk = fos.next()
                                            act(fo[:, 0:CH], ps[:, 0:CH], AF.Sigmoid, [pk], [fk])
                                            t0 = tok0 + tc * CH
                                            dma("gpsimd", Gd[gi * 4 + ci, :, t0:t0 + CH], fo[:, 0:CH], [fk], ["Gd"])
                        ph_end()

                    with ExitStack() as ph:
                        hsk = Rot([sbt(ph, "hsk%d" % i, [128, 128 * (2 * nbL - 1)], BF16) for i in range(3)], "hsk")
                        psYs = Rot([pst(ph, "psY%d" % i, [128, 8, nbL * NB]) for i in range(2)], "psY")
                        psTt = pst(ph, "psTt", [128, 2, 128], BF16)
                        YBT = sbt(ph, "YBT", [128, 2, T], BF16)
                        x0ls = Rot([sbt(ph, "x0l%d" % i, [128, 2, L], BF16) for i in range(2)], "x0l")
                        for kind in (["lat"] if last else ["lat", "ctx"]):
                            Lf = L if kind == "lat" else LC
                            nb = Lf // 128
                            W = 128 * (2 * nb - 1)
                            with ExitStack() as ph2:
                                Yr = sbt(ph2, "Yr" + kind, [128, nb, NB, 256], BF16)
                                vx = VXs[kind]
                                kdr = KREV[Lf]
                                for cg in range(32):
                                    psY, pyk = psYs.next()
                                    for c8 in range(8):
                                        c = cg * 8 + c8
                                        hk_, hkk = hsk.next()
                                        dma("sync" if c % 2 == 0 else "gpsimd", hk_[:, 0:W],
                                            bass.AP(tensor=kdr.tensor, offset=kdr.offset + c * 2 * Lf, ap=[[1, 128], [1, W]]),
                                            ["KREV%d" % Lf], [hkk])
                                        lags = [0] + [d for d in range(-(nb - 1), nb) if d != 0]
                                        for di, d in enumerate(lags):
                                            j0, j1 = max(0, -d), min(nb, nb - d)
                                            m0 = 128 * (nb - 1 - d)
                                            o_ = _ap(psY[:], c8 * nbL * NB + (j0 + d) * NB, [[1, (j1 - j0) * NB]])
                                            mm(o_, hk_[:, m0:m0 + 128], vx[:, c, j0:j1, :].rearrange("p j b -> p (j b)"),
                                               di == 0, di == len(lags) - 1, [hkk, "VXs" + kind], [pyk])
                                    src = _ap(psY[:], 0, [[nbL * NB, 8], [NB, nb], [1, NB]])
                                    dst_ = _ap(Yr[:], cg * 8, [[1, 8], [NB * 256, nb], [256, NB]])
                                    cp("scalar" if cg % 2 == 0 else "vector", dst_, src, [pyk], ["Yr"])
                                for b in range(NB):
                                    tok0 = b * L if kind == "lat" else NB * L + b * LC
                                    x0l, x0lk = x0ls.next()
                                    dma("sync", x0l[:, :, 0:Lf], X0Td[:, :, tok0:tok0 + Lf].rearrange("c p t -> p c t"), ["X0Td"], [x0lk])
                                    for i_ in range(nb):
                                        for cc in range(2):
                                            tr(psTt[:, cc, :], Yr[:, i_, b, cc * 128:(cc + 1) * 128], identb[:], ["Yr", "identb"], ["psTt"])
                                        t0 = tok0 + i_ * 128
                                        for cc in range(2):
                                            rev = _ap(psTt[:], cc * 128 + 127, [[-1, 128]])
                                            tt("vector", YBT[:, cc, t0:t0 + 128], rev, x0l[:, cc, i_ * 128:(i_ + 1) * 128], ALU.mult,
                                               ["psTt", x0lk], ["YBT"])
                        ntok = NB * L if last else T
                        for cc in range(2):
                            dma("sync", YBTd[cc, :, 0:ntok], YBT[:, cc, 0:ntok], ["YBT"], ["YBTd"])
                        ph_end()

                with ExitStack() as ph:
                    NKmax = (L + LC) // 128
                    QT = sbt(ph, "QT", [128, 4, L], BF16)
                    KT = sbt(ph, "KT", [128, 4, L + LC], BF16)
                    Vone = sbt(ph, "Vone", [128, NKmax, 4, 129], BF16)
                    Es = Rot([sbt(ph, "E%d" % i, [128, 512], BF16) for i in range(3)], "E")
                    psSs = Rot([pst(ph, "psA%d" % i, [128, 512]) for i in range(2)], "psA")
                    acc = pst(ph, "acc", [128, 4, 2, 256])
                    psC = pst(ph, "psC", [128, 4, 128], BF16)
                    rec = sbt(ph, "rec", [128, 4, 2], F32)
                    o1 = sbt(ph, "o1", [128, 128], F32)
                    o2 = sbt(ph, "o2", [128, 128], F32)
                    sq = sbt(ph, "sq", [128, 128], F32)
                    ss = sbt(ph, "ss", [128, 1], F32)
                    YC = sbt(ph, "YC", [128, 4, 512], BF16)
                    ycts = Rot([sbt(ph, "yct%d" % i, [128, 4, 512], BF16) for i in range(2)], "yct")
                    memset("vector", Vone[:], 1.0, ["Vone"])
                    for (kind, b, tok0, Lq) in act_seqs:
                        if kind == "lat":
                            ksegs = [(tok0, L), (NB * L + b * LC, LC)]
                        else:
                            ksegs = [(tok0, LC)]
                        NK = sum(s[1] for s in ksegs) // 128
                        dma("sync", QT[:, :, 0:Lq], QTd[:, :, tok0:tok0 + Lq].rearrange("h p t -> p h t"), ["qTd"], ["QT"])
                        ko = 0
                        for (kt0, kl) in ksegs:
                            dma("gpsimd", KT[:, :, ko:ko + kl], KTd[:, :, kt0:kt0 + kl].rearrange("h p t -> p h t"), ["kTd"], ["KT"])
                            for j in range(kl // 128):
                                dma("sync" if j % 2 else "gpsimd", Vone[:, ko // 128 + j, :, 0:128],
                                    Vd[kt0 + j * 128:kt0 + (j + 1) * 128, :].rearrange("t (h e) -> t h e", h=4), ["Vd"], ["Vone"])
                            ko += kl
                        QC = min(512, Lq)
                        nsub = QC // 128
                        for qc in range(Lq // QC):
                            steps = [(h, m, kt) for h in range(4) for m in range(2) for kt in range(NK)]

                            def emit_S(st_):
                                h, m, kt = st_
                                ms = slice(m * 64, (m + 1) * 64)
                                psA, pak = psSs.next()
                                mm(psA[:, 0:QC], KT[ms, h, kt * 128:(kt + 1) * 128], QT[ms, h, qc * QC:(qc + 1) * QC],
                                   True, True, ["KT", "QT"], [pak])
                                return psA, pak

                            nxt = emit_S(steps[0])
                            for si, (h, m, kt) in enumerate(steps):
                                psA, pak = nxt
                                E, ek = Es.next()
                                act(E[:, 0:QC], psA[:, 0:QC], AF.Exp, [pak], [ek], scale=0.125)
                                if si + 1 < len(steps):
                                    nxt = emit_S(steps[si + 1])
                                for qs in range(nsub):
                                    mm(acc[:, qs, m, 0:129], E[:, qs * 128:(qs + 1) * 128], Vone[:, kt, h, :],
                                       kt == 0, kt == NK - 1, [ek, "Vone"], ["acc"])
                                if not (m == 1 and kt == NK - 1):
                                    continue
                                p.add("vector", lambda e, nsub=nsub: e.reciprocal(out=rec[:, 0:nsub, :], in_=acc[:, 0:nsub, :, 128]), ["acc"], ["rec"])
                                ts("vector", rec[:, 0:nsub, 1], rec[:, 0:nsub, 1], lamt[:, 0:1], ALU.mult, ["rec", "lamt"], ["rec"])
                                for qs in range(nsub):
                                    ts("vector", o1[:], acc[:, qs, 0, 0:128], rec[:, qs, 0:1], ALU.mult, ["acc", "rec"], ["o1"])
                                    stt(o2[:], acc[:, qs, 1, 0:128], rec[:, qs, 1:2], o1[:], ALU.mult, ALU.add, ["acc", "rec", "o1"], ["o2"])
                                    tt("gpsimd", sq[:], o2[:], o2[:], ALU.mult, ["o2"], ["sq"])
                                    red(ss[:], sq[:], ALU.add, AX.X, ["sq"], ["ss"])
                                    act(ss[:], ss[:], AF.Sqrt, ["ss", "epsc"], ["ss"], bias=epsc[:], scale=1.0 / 128.0)
                                    p.add("vector", lambda e: e.reciprocal(out=ss[:], in_=ss[:]), ["ss"], ["ss"])
                                    stt(YC[:, qs, h * 128:(h + 1) * 128], o2[:], ss[:, 0:1], gsc[:], ALU.mult, ALU.mult,
                                        ["o2", "ss", "gsc"], ["YC"])
                            yct, yk = ycts.next()
                            for qs in range(nsub):
                                for h in range(4):
                                    tr(psC[:, h, :], YC[:, qs, h * 128:(h + 1) * 128], identb[:], ["YC", "identb"], ["psC"])
                                cp("scalar", yct[:, :, qs * 128:(qs + 1) * 128], psC[:], ["psC"], [yk])
                            t0 = tok0 + qc * QC
                            dma("sync", YCTd[:, :, t0:t0 + QC].rearrange("h p t -> p h t"), yct[:, :, 0:QC], [yk], ["YCTd"])
                    ph_end()

                LOG = sbt(lay, "LOG", [128, NT, 36], F32)
                WTS = sbt(lay, "WTS", [128, NT, 2], F32)
                DEST = sbt(lay, "DEST", [128, NT, 2], I32)
                IDXE = sbt(lay, "IDXE", [128, NBLK], I32)
                with ExitStack() as ph:
                    pab = sbt(ph, "pab", [128, 2, D], BF16)
                    pbb = sbt(ph, "pbb", [128, 2, D], BF16)
                    pcb = sbt(ph, "pcb", [128, 4, D], BF16)
                    wob = sbt(ph, "wob", [128, 8, D], BF16)
                    wrb = sbt(ph, "wrb", [128, 8, 36], BF16)
                    wrf = sbt(ph, "wrf", [128, 8, 36], F32)
                    rbb = sbt(ph, "rbb", [128, 36], F32)
                    stg = Rot([sbt(ph, "stg%d" % i, [128, 2, D], F32) for i in range(2)], "stg")
                    lg = sbt(ph, "lg", [128, D], F32)
                    lb = sbt(ph, "lb", [128, D], F32)
                    g1b = sbt(ph, "g1b", [128, D], F32)
                    sc2b = sbt(ph, "sc2b", [128, D], F32)
                    sh2b = sbt(ph, "sh2b", [128, D], F32)
                    si = 0
                    for (src, nk, dstw) in ((p_a, 2, pab), (p_b, 2, pbb), (p_c, 4, pcb), (w_out, 8, wob)):
                        for k2 in range(0, nk, 2):
                            s_, sk_ = stg.next()
                            dma("sync", s_[:], src[l, k2 * 128:(k2 + 2) * 128, :].rearrange("(k p) d -> p k d", p=128), [], [sk_])
                            cp("vector" if si % 2 == 0 else "gpsimd", dstw[:, k2:k2 + 2, :], s_[:], [sk_], ["mw"])
                            si += 1
                    dma("sync", wrf[:], moe_wr[l, :, :].rearrange("(k p) e -> p k e", p=128), [], ["wrf"])
                    cp("vector", wrb[:], wrf[:], ["wrf"], ["mw"])
                    dma("sync", rbb[:], bass.AP(tensor=moe_br.tensor, offset=moe_br.offset + l * 36, ap=[[0, 128], [1, 36]]), [], ["mw"])
                    dma("sync", lg[:], bass.AP(tensor=ln1_g.tensor, offset=ln1_g.offset + l * D, ap=[[0, 128], [1, D]]), [], ["lnconst"])
                    dma("sync", lb[:], bass.AP(tensor=ln1_b.tensor, offset=ln1_b.offset + l * D, ap=[[0, 128], [1, D]]), [], ["lnconst"])
                    yaTs = sbt(ph, "yaTs", [128, 2, 512], BF16)
                    ybTs = sbt(ph, "ybTs", [128, 2, 512], BF16)
                    ycTs = sbt(ph, "ycTs", [128, 4, 512], BF16)
                    Gs = sbt(ph, "Gs", [128, 24, 512], BF16)
                    mT = sbt(ph, "mT", [128, 8, 512], BF16)
                    ta = sbt(ph, "ta", [128, 512], F32)
                    tb = sbt(ph, "tb", [128, 512], F32)
                    tcx = sbt(ph, "tcx", [128, 512], F32)
                    xr = Rot([sbt(ph, "xr%d" % i, [128, D], F32) for i in range(2)], "xr")
                    rr_ = sbt(ph, "rr_", [128, D], F32)
                    x1t = Rot([sbt(ph, "x1t%d" % i, [128, D], F32) for i in range(2)], "x1t")
                    h2f = sbt(ph, "h2f", [128, D], F32)
                    h2b = Rot([sbt(ph, "h2b%d" % i, [128, D], BF16) for i in range(2)], "h2b")
                    h2T = sbt(ph, "h2T", [128, 8, 128], BF16)
                    lntmp = {"stats": sbt(ph, "lnst", [128, 2, 6], F32), "mv": sbt(ph, "lnmv", [128, 2], F32),
                             "rstd": sbt(ph, "lnrs", [128, 1], F32)}
                    psa = pst(ph, "psa", [128, 512])
                    psb = pst(ph, "psb", [128, 512])
                    psc = pst(ph, "psc", [128, 512])
                    psO = Rot([pst(ph, "psO%d" % i, [128, 512]) for i in range(2)], "psO")
                    psh = pst(ph, "psh", [128, 8, 128], BF16)
                    psl = pst(ph, "psl", [128, 36])
                    for (kind, b, tok0, Ls) in act_seqs:
                        mrow = b if kind == "lat" else NB
                        mo = MODS.offset + (l * (NB + 1) + mrow) * 6 * D
                        dma("sync", g1b[:], bass.AP(tensor=MODS.tensor, offset=mo + 2 * D, ap=[[0, 128], [1, D]]), ["MODS"], ["g1b"])
                        dma("sync", sh2b[:], bass.AP(tensor=MODS.tensor, offset=mo + 3 * D, ap=[[0, 128], [1, D]]), ["MODS"], ["sh2b"])
                        dma("sync", sc2b[:], bass.AP(tensor=MODS.tensor, offset=mo + 4 * D, ap=[[0, 128], [1, D]]), ["MODS"], ["sc2b"])
                        ts("gpsimd", sc2b[:], sc2b[:], 1.0, ALU.add, ["sc2b"], ["sc2b"])
                        CH = min(512, Ls)
                        for tc in range(Ls // CH):
                            t0 = tok0 + tc * CH
                            dma("sync", yaTs[:, :, 0:CH], YATd[:, :, t0:t0 + CH].rearrange("c p t -> p c t"), ["YATd"], ["yaTs"])
                            dma("gpsimd", ybTs[:, :, 0:CH], YBTd[:, :, t0:t0 + CH].rearrange("c p t -> p c t"), ["YBTd"], ["ybTs"])
                            dma("sync", ycTs[:, :, 0:CH], YCTd[:, :, t0:t0 + CH].rearrange("c p t -> p c t"), ["YCTd"], ["ycTs"])
                            dma("gpsimd", Gs[:, :, 0:CH], Gd[:, :, t0:t0 + CH].rearrange("c p t -> p c t"), ["Gd"], ["Gs"])
                            for dc in range(8):
                                ds_ = slice(dc * 128, (dc + 1) * 128)
                                for kt in range(2):
                                    mm(psa[:, 0:CH], pab[:, kt, ds_], yaTs[:, kt, 0:CH], kt == 0, kt == 1, ["mw", "yaTs"], ["psa"])
                                for kt in range(2):
                                    mm(psb[:, 0:CH], pbb[:, kt, ds_], ybTs[:, kt, 0:CH], kt == 0, kt == 1, ["mw", "ybTs"], ["psb"])
                                for kt in range(4):
                                    mm(psc[:, 0:CH], pcb[:, kt, ds_], ycTs[:, kt, 0:CH], kt == 0, kt == 3, ["mw", "ycTs"], ["psc"])
                                tt("vector", ta[:, 0:CH], psa[:, 0:CH], Gs[:, dc, 0:CH], ALU.mult, ["psa", "Gs"], ["ta"])
                                tt("vector", tb[:, 0:CH], psb[:, 0:CH], Gs[:, 8 + dc, 0:CH], ALU.mult, ["psb", "Gs"], ["tb"])
                                tt("vector", tcx[:, 0:CH], psc[:, 0:CH], Gs[:, 16 + dc, 0:CH], ALU.mult, ["psc", "Gs"], ["tcx"])
                                tt("gpsimd", ta[:, 0:CH], ta[:, 0:CH], tb[:, 0:CH], ALU.add, ["ta", "tb"], ["ta"])
                                tt("gpsimd", mT[:, dc, 0:CH], ta[:, 0:CH], tcx[:, 0:CH], ALU.add, ["ta", "tcx"], ["mT"])
                            for tsb in range(CH // 128):
                                tk0 = t0 + tsb * 128
                                gt = tk0 // 128
                                xt, xk = xr.next()
                                dma("sync", xt[:], Xsrc(tk0, 128), ["X2d"] if l else [], [xk])
                                for half in range(2):
                                    hs = slice(half * 512, (half + 1) * 512)
                                    pso, pok = psO.next()
                                    for kt in range(8):
                                        mm(pso[:], mT[:, kt, tsb * 128:(tsb + 1) * 128], wob[:, kt, hs], kt == 0, kt == 7, ["mw", "mT"], [pok])
                                    tt("vector", rr_[:, hs], pso[:], g1b[:, hs], ALU.mult, [pok, "g1b"], ["rr_"])
                                stt(rr_[:], xt[:], ALPHA, rr_[:], ALU.mult, ALU.add, [xk, "rr_"], ["rr_"])
                                x1, x1k = x1t.next()
                                layer_norm_rows("ln1", rr_, "rr_", lg, lb, x1, x1k, lntmp)
                                dma("sync", X1d[tk0:tk0 + 128, :], x1[:], [x1k], ["X1d"])
                                tt("vector", h2f[:], x1[:], sc2b[:], ALU.mult, [x1k, "sc2b"], ["h2f"])
                                hb_, hbk = h2b.next()
                                tt("gpsimd", hb_[:], h2f[:], sh2b[:], ALU.add, ["h2f", "sh2b"], [hbk])
                                dma("gpsimd", H2d[tk0:tk0 + 128, :], hb_[:], [hbk], ["H2d"])
                                for kt in range(8):
                                    tr(psh[:, kt, :], hb_[:, kt * 128:(kt + 1) * 128], identb[:], [hbk, "identb"], ["psh"])
                                cp("scalar", h2T[:], psh[:], ["psh"], ["h2T"])
                                for kt in range(8):
                                    mm(psl[:], h2T[:, kt, :], wrb[:, kt, :], kt == 0, kt == 7, ["h2T", "mw"], ["psl"])
                                tt("vector", LOG[:, gt, :], psl[:], rbb[:], ALU.add, ["psl", "mw"], ["LOG"])
                    ph_end()

                with ExitStack() as ph:
                    NC_ = NTm * 32
                    cm = sbt(ph, "cm", [128, 128 + 8 + 4 + MMAX + NBLK + 32], F32)
                    dma("sync", cm[:], cmoe_in[:, :], [], ["cm"])
                    ustr = sbt(ph, "ustr", [128, 128], BF16)
                    cp("vector", ustr[:], cm[:, 0:128], ["cm"], ["ustr"])
                    o_kp8, o_kp4, o_mrow, o_nrow, o_i32 = 128, 136, 140, 140 + MMAX, 140 + MMAX + NBLK
                    onesb = sbt(ph, "onesb", [128, 1], BF16)
                    onesf = sbt(ph, "onesf", [32, 128], F32)
                    memset("vector", onesb[:], 1.0, ["onesb"])
                    memset("vector", onesf[:], 1.0, ["onesf"])
                    gmax = sbt(ph, "gmax", [128, NTm], F32)
                    goh = sbt(ph, "goh", [128, NTm, 4], F32)
                    gex = sbt(ph, "gex", [128, NTm, 4], F32)
                    gsum = sbt(ph, "gsum", [128, NTm], F32)
                    EM = sbt(ph, "EM", [128, NTm, 32], F32)
                    oh1 = sbt(ph, "oh1", [128, NTm, 32], F32)
                    oh2 = sbt(ph, "oh2", [128, NTm, 32], F32)
                    m1 = sbt(ph, "m1", [128, NTm], F32)
                    m2 = sbt(ph, "m2", [128, NTm], F32)
                    dd = sbt(ph, "dd", [128, NTm], F32)
                    Mb = sbt(ph, "Mb", [128, NTm, 32], BF16)
                    POS = sbt(ph, "POS", [128, NTm, 32], F32)
                    tmpP = sbt(ph, "tmpP", [128, NTm, 32], F32)
                    dstf = sbt(ph, "dstf", [128, NTm, 2], F32)
                    tot = sbt(ph, "tot", [32, NTm], F32)
                    cum = sbt(ph, "cum", [32, NTm], F32)
                    ones32 = sbt(ph, "ones32", [32, NTm], F32)
                    cnt = sbt(ph, "cnt", [32, 4], F32)
                    cmpm = sbt(ph, "cmpm", [32, MMAX], F32)
                    base = sbt(ph, "base", [32, NTm], F32)
                    R = sbt(ph, "R", [32, NTm, 32], F32)
                    dg32 = sbt(ph, "dg32", [32, 32], F32)
                    pendr = sbt(ph, "pendr", [128, 32], F32)
                    cmpb = sbt(ph, "cmpb", [128, NBLK, 32], F32)
                    be = sbt(ph, "be", [128, NBLK], F32)
                    idf = sbt(ph, "idf", [128, NBLK, 8], F32)
                    NPS = -(-NC_ // 512)
                    psR = pst(ph, "psR", [128, NPS * 512])
                    psTot = pst(ph, "psTot", [128, 512])
                    psSm = pst(ph, "psSm", [128, 512])
                    Lg = LOG[:, 0:NTm, :]
                    G4 = LOG[:, 0:NTm, 0:4]
                    E32 = LOG[:, 0:NTm, 4:36]

                    def bc_last(t2d, n):
                        a = t2d
                        return bass.AP(tensor=a.tensor, offset=a.offset, ap=[list(a.ap[0]), list(a.ap[1]), [0, n]])

                    red(gmax[:], G4, ALU.max, AX.X, ["LOG"], ["gmax"])
                    tt("vector", goh[:], G4, bc_last(gmax[:], 4), ALU.is_equal, ["LOG", "gmax"], ["goh"])
                    tt("vector", gex[:], G4, bc_last(gmax[:], 4), ALU.subtract, ["LOG", "gmax"], ["gex"])
                    act(gex[:], gex[:], AF.Exp, ["gex"], ["gex"])
                    red(gsum[:], gex[:], ALU.add, AX.X, ["gex"], ["gsum"])
                    p.add("vector", lambda e: e.reciprocal(out=gsum[:], in_=gsum[:]), ["gsum"], ["gsum"])
                    ts("vector", goh[:], goh[:], 1.0, ALU.subtract, ["goh"], ["goh"], s2=BIG, op1=ALU.mult)
                    gb = goh[:]
                    p.add("vector", lambda e: e.tensor_tensor(
                        out=bass.AP(tensor=EM[:].tensor, offset=EM[:].offset, ap=[list(EM[:].ap[0]), [32, NTm], [8, 4], [1, 8]]),
                        in0=bass.AP(tensor=LOG[:].tensor, offset=LOG[:].offset + 4, ap=[list(LOG[:].ap[0]), [36, NTm], [8, 4], [1, 8]]),
                        in1=bass.AP(tensor=gb.tensor, offset=gb.offset, ap=[list(gb.ap[0]), [4, NTm], [1, 4], [0, 8]]),
                        op=ALU.add), ["LOG", "goh"], ["EM"])
                    red(m1[:], EM[:], ALU.max, AX.X, ["EM"], ["m1"])
                    tt("vector", oh1[:], EM[:], bc_last(m1[:], 32), ALU.is_equal, ["EM", "m1"], ["oh1"])
                    stt(EM[:], oh1[:], -BIG, EM[:], ALU.mult, ALU.add, ["oh1", "EM"], ["EM"])
                    red(m2[:], EM[:], ALU.max, AX.X, ["EM"], ["m2"])
                    tt("vector", oh2[:], EM[:], bc_last(m2[:], 32), ALU.is_equal, ["EM", "m2"], ["oh2"])
                    tt("vector", dd[:], m2[:], m1[:], ALU.subtract, ["m1", "m2"], ["dd"])
                    act(dd[:], dd[:], AF.Exp, ["dd"], ["dd"])
                    ts("vector", dd[:], dd[:], 1.0, ALU.add, ["dd"], ["dd"])
                    p.add("vector", lambda e: e.reciprocal(out=dd[:], in_=dd[:]), ["dd"], ["dd"])
                    tt("vector", WTS[:, 0:NTm, 0], dd[:], gsum[:], ALU.mult, ["dd", "gsum"], ["WTS"])
                    tt("vector", WTS[:, 0:NTm, 1], gsum[:], WTS[:, 0:NTm, 0], ALU.subtract, ["gsum", "WTS"], ["WTS"])
                    tt("vector", Mb[:], oh1[:], oh2[:], ALU.add, ["oh1", "oh2"], ["Mb"])
                    Mf = Mb[:].rearrange("p t e -> p (t e)")
                    for c_ in range(NPS):
                        n_ = min(512, NC_ - c_ * 512)
                        mm(psR[:, c_ * 512:c_ * 512 + n_], ustr[:], Mf[:, c_ * 512:c_ * 512 + n_], True, True, ["ustr", "Mb"], ["psR"])
                    for t_ in range(NTm):
                        mm(psTot[0:32, t_:t_ + 1], Mb[:, t_, :], onesb[:], True, True, ["Mb", "onesb"], ["psTot"])
                    cp("vector", tot[:], psTot[0:32, 0:NTm], ["psTot"], ["tot"])
                    memset("vector", ones32[:], 1.0, ["ones32"])
                    p.add("vector", lambda e: e.tensor_tensor_scan(out=cum[:], data0=ones32[:], data1=tot[:], initial=0.0,
                                                                   op0=ALU.mult, op1=ALU.add), ["ones32", "tot"], ["cum"])
                    cp("vector", cnt[:, 0:1], cum[:, NTm - 1:NTm], ["cum"], ["cnt"])
                    ts("vector", cmpm[:], cm[0:32, o_mrow:o_mrow + MMAX], cnt[:, 0:1], ALU.is_lt, ["cm", "cnt"], ["cmpm"])
                    red(cnt[:, 1:2], cmpm[:], ALU.add, AX.X, ["cmpm"], ["cnt"])
                    ts("vector", cnt[:, 2:3], cnt[:, 1:2], float(BLK), ALU.mult, ["cnt"], ["cnt"])
                    mm(psSm[0:32, 0:1], cm[0:32, 0:32], cnt[:, 2:3], True, True, ["cm", "cnt"], ["psSm"])
                    tt("vector", cnt[:, 3:4], psSm[0:32, 0:1], cnt[:, 2:3], ALU.add, ["psSm", "cnt"], ["cnt"])
                    tt("vector", base[:], cum[:], tot[:], ALU.subtract, ["cum", "tot"], ["base"])
                    ts("vector", base[:], base[:], psSm[0:32, 0:1], ALU.add, ["base", "psSm"], ["base"])
                    bb = base[:]
                    i32 = cm[0:32, o_i32:o_i32 + 32]
                    p.add("vector", lambda e: e.tensor_tensor(
                        out=R[:], in0=bass.AP(tensor=bb.tensor, offset=bb.offset, ap=[list(bb.ap[0]), [1, NTm], [0, 32]]),
                        in1=bass.AP(tensor=i32.tensor, offset=i32.offset, ap=[list(i32.ap[0]), [0, NTm], [1, 32]]),
                        op=ALU.mult), ["base", "cm"], ["R"])
                    cp("vector", POS[:].rearrange("p t e -> p (t e)"), psR[:, 0:NC_], ["psR"], ["POS"])
                    Rf = R[:].rearrange("p t e -> p (t e)")
                    for c_ in range(NPS):
                        n_ = min(512, NC_ - c_ * 512)
                        mm(psR[:, c_ * 512:c_ * 512 + n_], onesf[:], Rf[:, c_ * 512:c_ * 512 + n_], True, True, ["onesf", "R", "POS"], ["psR"])
                    tt("vector", POS[:].rearrange("p t e -> p (t e)"), POS[:].rearrange("p t e -> p (t e)"), psR[:, 0:NC_],
                       ALU.add, ["psR", "POS"], ["POS"])
                    for k_, oh in enumerate((oh1, oh2)):
                        tt("vector", tmpP[:], oh[:], POS[:], ALU.mult, ["oh1", "oh2", "POS"], ["tmpP"])
                        red(dstf[:, :, k_], tmpP[:], ALU.add, AX.X, ["tmpP"], ["dstf"])
                    cp("vector", DEST[:, 0:NTm, :], dstf[:], ["dstf"], ["DEST"])
                    ts("vector", dg32[:], i32, cnt[:, 3:4], ALU.mult, ["cm", "cnt"], ["dg32"])
                    mm(psSm[:, 32:64], onesf[:], dg32[:], True, True, ["onesf", "dg32"], ["psSm"])
                    cp("vector", pendr[:], psSm[:, 32:64], ["psSm"], ["pendr"])
                    pr_ = pendr[:]
                    nrow = cm[:, o_nrow:o_nrow + NBLK]
                    p.add("vector", lambda e: e.tensor_tensor(
                        out=cmpb[:], in0=bass.AP(tensor=pr_.tensor, offset=pr_.offset, ap=[list(pr_.ap[0]), [0, NBLK], [1, 32]]),
                        in1=bass.AP(tensor=nrow.tensor, offset=nrow.offset, ap=[list(nrow.ap[0]), [1, NBLK], [0, 32]]),
                        op=ALU.is_le), ["pendr", "cm"], ["cmpb"])
                    red(be[:], cmpb[:], ALU.add, AX.X, ["cmpb"], ["be"])
                    ts("vector", be[:], be[:], 31.0, ALU.min, ["be"], ["be"], s2=float(l * NE), op1=ALU.add)
                    ts("vector", idf[:, :, 0], be[:], 128.0, ALU.mult, ["be", "idf"], ["idf"], s2=cm[:, o_kp8:o_kp8 + 1], op1=ALU.add)
                    cp("vector", IDXE[:], idf[:, :, 0], ["idf"], ["IDXE"])
                    ph_end()

                with ExitStack() as ph:
                    hrs = Rot([sbt(ph, "hr%d" % i, [128, D], BF16) for i in range(3)], "hr")
                    for t_ in range(NTm):
                        hr, hk = hrs.next()
                        dma("sync", hr[:], H2d[t_ * 128:(t_ + 1) * 128, :], ["H2d"], [hk])
                        for k_ in range(2):
                            p.add("gpsimd", lambda e, hr=hr, t_=t_, k_=k_: e.indirect_dma_start(
                                out=XBUF[:, :], out_offset=bass.IndirectOffsetOnAxis(ap=DEST[:, t_, k_:k_ + 1], axis=0),
                                in_=hr[:], in_offset=None), [hk, "DEST"], ["XBUF"], dma=True)
                    ph_end()

                with ExitStack() as ph:
                    wgf = sbt(ph, "wgf", [128, 8, HID], F32)
                    wuf = sbt(ph, "wuf", [128, 8, HID], F32)
                    wdf = sbt(ph, "wdf", [128, 4, D], F32)
                    wgbs = Rot([sbt(ph, "wgb%d" % i, [128, 8, HID], BF16) for i in range(2)], "wgb")
                    wubs = Rot([sbt(ph, "wub%d" % i, [128, 8, HID], BF16) for i in range(2)], "wub")
                    wdbs = Rot([sbt(ph, "wdb%d" % i, [128, 4, D], BF16) for i in range(2)], "wdb")
                    xrs = Rot([sbt(ph, "xb%d" % i, [128, RB, D], BF16) for i in range(2)], "xb")
                    XT = sbt(ph, "XT", [128, 8, BLK], BF16)
                    sgt = sbt(ph, "sgt", [128, BLK], F32)
                    aT = sbt(ph, "aT", [128, 4, BLK], BF16)
                    yos = Rot([sbt(ph, "yo%d" % i, [128, D], BF16) for i in range(2)], "yo")
                    psX = pst(ph, "psX", [128, 8, 128], BF16)
                    psG = Rot([pst(ph, "psG%d" % i, [128, BLK]) for i in range(2)], "psG")
                    psU = Rot([pst(ph, "psU%d" % i, [128, BLK]) for i in range(2)], "psU")
                    psD = Rot([pst(ph, "psD%d" % i, [128, 512]) for i in range(2)], "psD")
                    for n in range(NBLK):
                        for (wf_, src_, wk_) in ((wgf, ex_g8, "wgf"), (wuf, ex_u8, "wuf"), (wdf, ex_d4, "wdf")):
                            p.add("gpsimd", lambda e, n=n, wf_=wf_, src_=src_: e.indirect_dma_start(
                                out=wf_[:].rearrange("p a b -> p (a b)"), out_offset=None, in_=src_,
                                in_offset=bass.IndirectOffsetOnAxis(ap=IDXE[:, n:n + 1], axis=0)), ["IDXE"], [wk_], dma=True)
                        wgb, gk = wgbs.next()
                        wub, uk = wubs.next()
                        wdb, dk = wdbs.next()
                        cp("vector", wgb[:], wgf[:], ["wgf"], [gk])
                        cp("gpsimd", wub[:], wuf[:], ["wuf"], [uk])
                        cp("scalar", wdb[:], wdf[:], ["wdf"], [dk])
                        xb, xk = xrs.next()
                        dma("sync", xb[:], XBUF[n * BLK:(n + 1) * BLK, :].rearrange("(r p) d -> p r d", p=128), ["XBUF"], [xk])
                        for rb in range(RB):
                            for kt in range(8):
                                tr(psX[:, kt, :], _ap(xb[:], rb * D + kt, [[8, 128]]), identb[:], [xk, "identb"], ["psX"])
                            cp("scalar" if rb % 2 else "vector", XT[:, :, rb * 128:(rb + 1) * 128], psX[:], ["psX"], ["XT"])
                        for hc in range(4):
                            pg, pgk = psG.next()
                            pu, puk = psU.next()
                            for kt in range(8):
                                mm(pg[:], _ap(wgb[:], kt * HID + hc, [[4, 128]]), XT[:, kt, :], kt == 0, kt == 7, [gk, "XT"], [pgk])
                            for kt in range(8):
                                mm(pu[:], _ap(wub[:], kt * HID + hc, [[4, 128]]), XT[:, kt, :], kt == 0, kt == 7, [uk, "XT"], [puk])
                            act(sgt[:], pg[:], AF.Silu, [pgk], ["sgt"])
                            tt("vector", aT[:, hc, :], sgt[:], pu[:], ALU.mult, ["sgt", puk], ["aT"])
                        for rb in range(RB):
                            yo, yk = yos.next()
                            for half in range(2):
                                pd, pdk = psD.next()
                                for hc in range(4):
                                    mm(pd[:], aT[:, hc, rb * 128:(rb + 1) * 128], wdb[:, hc, half * 512:(half + 1) * 512],
                                       hc == 0, hc == 3, [dk, "aT"], [pdk])
                                cp("scalar" if half else "vector", yo[:, half * 512:(half + 1) * 512], pd[:], [pdk], [yk])
                            r0 = n * BLK + rb * 128
                            dma("sync", YBUF[r0:r0 + 128, :], yo[:], [yk], ["YBUF"])
                    ph_end()

                with ExitStack() as ph:
                    r0s = Rot([sbt(ph, "r0_%d" % i, [128, D], BF16) for i in range(2)], "r0_")
                    r1s = Rot([sbt(ph, "r1_%d" % i, [128, D], BF16) for i in range(2)], "r1_")
                    xr = Rot([sbt(ph, "xq%d" % i, [128, D], F32) for i in range(2)], "xq")
                    yf = sbt(ph, "yf", [128, D], F32)
                    rr_ = sbt(ph, "rr2", [128, D], F32)
                    xo = Rot([sbt(ph, "xo%d" % i, [128, D], F32) for i in range(2)], "xo")
                    lg = sbt(ph, "lg2", [128, D], F32)
                    lb = sbt(ph, "lb2", [128, D], F32)
                    g2b = sbt(ph, "g2b", [128, D], F32)
                    lntmp = {"stats": sbt(ph, "lnst2", [128, 2, 6], F32), "mv": sbt(ph, "lnmv2", [128, 2], F32),
                             "rstd": sbt(ph, "lnrs2", [128, 1], F32)}
                    dma("sync", lg[:], bass.AP(tensor=ln2_g.tensor, offset=ln2_g.offset + l * D, ap=[[0, 128], [1, D]]), [], ["lnconst"])
                    dma("sync", lb[:], bass.AP(tensor=ln2_b.tensor, offset=ln2_b.offset + l * D, ap=[[0, 128], [1, D]]), [], ["lnconst"])
                    for (kind, b, tok0, Ls) in act_seqs:
                        mrow = b if kind == "lat" else NB
                        mo = MODS.offset + (l * (NB + 1) + mrow) * 6 * D
                        dma("sync", g2b[:], bass.AP(tensor=MODS.tensor, offset=mo + 5 * D, ap=[[0, 128], [1, D]]), ["MODS"], ["g2b"])
                        for tI in range(Ls // 128):
                            tk0 = tok0 + tI * 128
                            gt = tk0 // 128
                            r0, r0k = r0s.next()
                            r1, r1k = r1s.next()
                            for (rt, rk, k_) in ((r0, r0k, 0), (r1, r1k, 1)):
                                p.add("gpsimd", lambda e, rt=rt, gt=gt, k_=k_: e.indirect_dma_start(
                                    out=rt[:], out_offset=None, in_=YBUF[:, :],
                                    in_offset=bass.IndirectOffsetOnAxis(ap=DEST[:, gt, k_:k_ + 1], axis=0)), ["YBUF", "DEST"], [rk], dma=True)
                            xt, xk = xr.next()
                            dma("sync", xt[:], X1d[tk0:tk0 + 128, :], ["X1d"], [xk])
                            ts("vector", yf[:], r0[:], WTS[:, gt, 0:1], ALU.mult, [r0k, "WTS"], ["yf"])
                            stt(yf[:], r1[:], WTS[:, gt, 1:2], yf[:], ALU.mult, ALU.add, [r1k, "WTS", "yf"], ["yf"])
                            tt("gpsimd", yf[:], yf[:], g2b[:], ALU.mult, ["yf", "g2b"], ["yf"])
                            stt(rr_[:], xt[:], ALPHA, yf[:], ALU.mult, ALU.add, [xk, "yf"], ["rr2"])
                            xo_, xok = xo.next()
                            layer_norm_rows("ln2", rr_, "rr2", lg, lb, xo_, xok, lntmp)
                            if last:
                                dma("sync", out[tk0:tk0 + 128, :], xo_[:], [xok], ["out"])
                            else:
                                dma("sync", X2d[tk0:tk0 + 128, :], xo_[:], [xok], ["X2d"])
                    ph_end(final=last)
    return nc


def _const_tables(cfg):
    L, LC, BLK, NB = cfg.L, cfg.LC, cfg.BLK, cfg.NB
    f32 = np.float32
    c = {}
    c["c_ident"] = np.eye(128, dtype=f32)
    n_freq = 16
    t = np.arange(L)
    pos = np.stack([(t // GRID_W).astype(f32), (t % GRID_W).astype(f32)], 0)
    inv = (10000.0 ** (-np.arange(n_freq, dtype=f32) / n_freq)).astype(f32)
    rope = np.zeros((2, 128, L), f32)
    for n in range(128):
        a, r, f = (n // 32) % 2, (n // 16) % 2, n % 16
        ang = (pos[a] * inv[f]).astype(f32)
        rope[0, n] = np.cos(ang)
        rope[1, n] = np.sin(ang) * (-1.0 if r == 0 else 1.0)
    c["c_rope"] = rope

    def hy_tables(Lf):
        tt_ = np.linspace(0.0, 1.0, Lf, dtype=f32)
        w = (2.0 * math.pi * np.arange(Lf, dtype=f32) / Lf).astype(f32)
        fb = np.linspace(1e-4, HY_BANDS - 1, HY_BANDS, dtype=f32)
        emb = np.concatenate([tt_[:, None], np.cos(fb[None, :] * w[:, None]), -np.sin(fb[None, :] * w[:, None])], -1).astype(f32)
        max_decay = math.log(1e-2) / 0.3
        min_decay = math.log(1e-2) / 1.5
        deltas = np.abs(np.linspace(min_decay, max_decay, 256, dtype=f32))
        window = (np.exp(-tt_[:, None] * deltas[None, :]) + 0.05).astype(f32)
        pf = np.arange(Lf - 1, -1, -1)
        pb = np.concatenate([np.arange(1, Lf), [0]])
        e = np.stack([emb[pf].T, emb[pb].T], 0).astype(f32)
        wn = np.stack([window[pf].T, window[pb].T], 0).astype(f32)
        wn[1, :, Lf - 1] = 0.0
        return np.ascontiguousarray(e), np.ascontiguousarray(wn)

    c["c_emb_L"], c["c_win_L"] = hy_tables(L)
    c["c_emb_C"], c["c_win_C"] = hy_tables(LC)
    T = NB * (L + LC)
    NBLK = -(-(2 * T) // BLK) + NE
    MMAX = -(-(2 * T) // BLK) + 1
    cm = np.zeros((128, 128 + 8 + 4 + MMAX + NBLK + 32), f32)
    pp = np.arange(128)
    cm[:, 0:128] = (pp[:, None] < pp[None, :]).astype(f32)
    cm[:, 128:136] = np.arange(8)[None, :] * 128 + pp[:, None]
    cm[:, 136:140] = np.arange(4)[None, :] * 128 + pp[:, None]
    cm[:, 140:140 + MMAX] = (np.arange(MMAX) * BLK)[None, :]
    cm[:, 140 + MMAX:140 + MMAX + NBLK] = (np.arange(NBLK) * BLK)[None, :]
    cm[0:32, 140 + MMAX + NBLK:] = np.eye(32, dtype=f32)
    c["c_moe"] = cm
    return c


def _core_inputs(cfg, inp, core, consts):
    NB, L, LC = cfg.NB, cfg.L, cfg.LC
    f32 = np.float32
    bs = slice(core * NB, (core + 1) * NB)
    m = dict(consts)
    m["x"] = np.ascontiguousarray(inp["x"][bs].reshape(NB * L, D))
    m["ctx"] = np.ascontiguousarray(inp["ctx"][bs].reshape(NB * LC, D))
    cc = np.concatenate([inp["c"][bs], inp["c_ctx"][None, :]], 0)
    m["cT"] = np.ascontiguousarray(cc.T.reshape(8, 128, NB + 1).transpose(1, 0, 2))
    for k in ("ada_w", "ada_b", "w_in", "gm_ln_g", "gm_ln_b", "hy_f_w1", "hy_f_w2", "hy_f_w3", "da_norm_g",
              "p_a", "p_b", "p_c", "w_out", "ln1_g", "ln1_b", "ln2_g", "ln2_b"):
        m[k] = inp[k]
    m["gm_wsT"] = np.ascontiguousarray(inp["gm_ws"].transpose(0, 3, 1, 2))
    m["gm_bsT"] = np.ascontiguousarray(inp["gm_bs"].transpose(0, 2, 1))
    cw = np.concatenate([inp["hy_conv_w"], inp["hy_conv_b"][:, None, :]], 1)
    m["hy_cw"] = np.ascontiguousarray(cw.reshape(DEPTH, 4, 6, 128).transpose(0, 3, 2, 1))
    m["hy_f_b1"] = np.ascontiguousarray(inp["hy_f_b1"][:, :, None])
    m["hy_f_b2"] = np.ascontiguousarray(inp["hy_f_b2"][:, :, None])
    m["hy_b3T"] = np.ascontiguousarray(inp["hy_f_b3"].reshape(DEPTH, 4, 128).transpose(0, 2, 1))
    m["hy_skipT"] = np.ascontiguousarray(inp["hy_skip"].reshape(DEPTH, 2, 128).transpose(0, 2, 1))
    m["da_l"] = np.ascontiguousarray(np.stack([inp["da_lq1"], inp["da_lk1"], inp["da_lq2"], inp["da_lk2"]], 1))
    m["moe_wr"] = np.ascontiguousarray(np.concatenate([inp["moe_wg"], inp["moe_we"]], -1))
    m["moe_br"] = np.ascontiguousarray(np.concatenate([inp["moe_bg"], inp["moe_be"]], -1))
    m["ex_w_gate"] = inp["ex_w_gate"].reshape(DEPTH * NE * D, HID)
    m["ex_w_up"] = inp["ex_w_up"].reshape(DEPTH * NE * D, HID)
    m["ex_w_down"] = inp["ex_w_down"].reshape(DEPTH * NE * HID, D)
    return {k: np.ascontiguousarray(np.asarray(v, dtype=f32)) for k, v in m.items()}


def kernel(**inputs):
    cfg = Cfg()
    inp = {k: np.asarray(v) for k, v in inputs.items()}
    n_cores = inp["x"].shape[0] // cfg.NB
    nc = build(cfg)
    consts = _const_tables(cfg)
    in_maps = [_core_inputs(cfg, inp, c, consts) for c in range(n_cores)]
    res = run_bass_kernel_spmd(nc, in_maps, core_ids=list(range(n_cores)))
    outs = [np.asarray(r["out"]).reshape(cfg.NB, cfg.L, D) for r in res.results]
    return np.concatenate(outs, 0).astype(np.float32)
```

```python
import math
from contextlib import ExitStack

import numpy as np
import concourse.bass as bass
import concourse.mybir as mybir
from concourse.bass_utils import run_bass_kernel_spmd

F32 = mybir.dt.float32
BF16 = mybir.dt.bfloat16
I32 = mybir.dt.int32
AF = mybir.ActivationFunctionType
ALU = mybir.AluOpType
AX = mybir.AxisListType

D = 1024
DEPTH = 2
GRID_W = 64
N_IN = 5888
OFF_HY, OFF_Q, OFF_K, OFF_V, OFF_GATE = 512, 1280, 1792, 2304, 2816
NWB = N_IN + 1024
HY_EMB, HY_FFN, HY_BANDS = 33, 64, 16
NE, NG, EPG, HID = 32, 4, 8, 512
ALPHA = (2.0 * DEPTH) ** 0.25
EPS = 1e-5
BIG = 1.0e30

ENGINES = ("tensor", "vector", "scalar", "gpsimd", "sync")
N_DMA_SEMS = 40


class Prog:
    def __init__(self, nc, stack):
        self.nc = nc
        self.ops = []
        self.esem = {e: stack.enter_context(nc.semaphore("s_" + e)) for e in ENGINES}
        self.dsem = [stack.enter_context(nc.semaphore("d%d" % i)) for i in range(N_DMA_SEMS)]
        self.ecount = {e: 0 for e in ENGINES}
        self.dcount = [0] * N_DMA_SEMS
        self.dnext = 0
        self.lastw = {}
        self.readers = {}
        self.known = {}
        self.nops = 0

    def add(self, eng, fn, r=(), w=(), dma=False):
        self.ops.append((eng, fn, tuple(r), tuple(w), dma))

    def flush(self, final=False):
        nc = self.nc
        esem, dsem, ecount, dcount = self.esem, self.dsem, self.ecount, self.dcount
        lastw, readers, known = self.lastw, self.readers, self.known
        plan = {e: [] for e in ENGINES}
        fence = [(("d", i), dcount[i]) for i in range(N_DMA_SEMS) if dcount[i] > 0]
        fence += [(("e", e), ecount[e]) for e in ENGINES if ecount[e] > 0]
        for (eng, fn, r, w, dma) in self.ops:
            deps = []
            for k in r:
                t = lastw.get(k)
                if t is not None:
                    deps.append(t)
            for k in w:
                t = lastw.get(k)
                if t is not None:
                    deps.append(t)
                for tk, tv in readers.get(k, {}).items():
                    deps.append((tk[0], tk[1], tv))
            if dma:
                si = self.dnext
                self.dnext = (self.dnext + 1) % N_DMA_SEMS
                if dcount[si] > 0:
                    deps.append(("d", si, dcount[si]))
                dcount[si] += 16
                tok = ("d", si, dcount[si])
                inc = (dsem[si], 16)
            else:
                ecount[eng] += 1
                tok = ("e", eng, ecount[eng])
                inc = (esem[eng], 1)
            waits = {}
            for (kind, key, val) in deps:
                if kind == "e" and key == eng and eng == "tensor":
                    continue
                sk = (kind, key)
                if known.get((eng, sk), 0) >= val:
                    continue
                if waits.get(sk, 0) < val:
                    waits[sk] = val
            wl = []
            for sk, val in waits.items():
                known[(eng, sk)] = val
                wl.append((esem[sk[1]] if sk[0] == "e" else dsem[sk[1]], val))
            plan[eng].append((wl, fn, inc))
            for k in r:
                d = readers.setdefault(k, {})
                if d.get(tok[:2], 0) < tok[2]:
                    d[tok[:2]] = tok[2]
            for k in w:
                lastw[k] = tok
                readers[k] = {}
        self.nops += len(self.ops)
        self.ops = []
        endw = []
        if final:
            endw = [(dsem[i], dcount[i]) for i in range(N_DMA_SEMS) if dcount[i] > 0]
            endw += [(esem[e], ecount[e]) for e in ENGINES if ecount[e] > 0]

        def runner(ename):
            def body(eng):
                for sk, val in fence:
                    if sk == ("e", ename):
                        continue
                    if known.get((ename, sk), 0) >= val:
                        continue
                    known[(ename, sk)] = val
                    eng.wait_ge(esem[sk[1]] if sk[0] == "e" else dsem[sk[1]], val)
                for (wl, fn, inc) in plan[ename]:
                    for (sem, val) in wl:
                        eng.wait_ge(sem, val)
                    ins = fn(eng)
                    ins.then_inc(inc[0], inc[1])
                for (sem, val) in endw:
                    eng.wait_ge(sem, val)
            return body

        with nc.Block() as block:
            block.tensor(runner("tensor"))
            block.vector(runner("vector"))
            block.scalar(runner("scalar"))
            block.gpsimd(runner("gpsimd"))
            block.sync(runner("sync"))


class Cfg:
    def __init__(self, NB=4, L=2048, LC=256, BLK=512, stop=None):
        self.NB, self.L, self.LC, self.BLK, self.stop = NB, L, LC, BLK, stop


class _Stop(Exception):
    pass


def _ap(base, off, dims, part=None):
    p = list(base.ap[0]) if part is None else [base.ap[0][0], part]
    return bass.AP(tensor=base.tensor, offset=base.offset + off, ap=[p] + [list(d) for d in dims])


class Rot:
    def __init__(self, tiles, name):
        self.tiles, self.name, self.i = tiles, name, 0

    def next(self):
        t = self.tiles[self.i % len(self.tiles)]
        k = "%s%d" % (self.name, self.i % len(self.tiles))
        self.i += 1
        return t, k


def build(cfg, debug=False):
    holder = {}
    try:
        _build(cfg, debug, holder)
    except _Stop:
        pass
    return holder["nc"]


def _build(cfg, debug, holder):
    NB, L, LC, BLK = cfg.NB, cfg.L, cfg.LC, cfg.BLK
    nc = bass.Bass("TRN2", target_bir_lowering=False)
    holder["nc"] = nc
    T = NB * (L + LC)
    NT = T // 128
    NTL = NB * L // 128
    RB = BLK // 128

    def din(name, shape, dt=F32):
        return nc.dram_tensor(name, list(shape), dt, kind="ExternalInput").ap()

    def dscr(name, shape, dt):
        return nc.dram_tensor(name, list(shape), dt, kind="ExternalOutput" if debug else "Internal").ap()

    x_in = din("x", [NB * L, D])
    ctx_in = din("ctx", [NB * LC, D])
    cT_in = din("cT", [128, 8, NB + 1])
    ada_w = din("ada_w", [DEPTH, D, 6 * D])
    ada_b = din("ada_b", [DEPTH, 6 * D])
    w_in = din("w_in", [DEPTH, D, N_IN])
    gm_ln_g = din("gm_ln_g", [DEPTH, 256])
    gm_ln_b = din("gm_ln_b", [DEPTH, 256])
    gm_wsT = din("gm_wsT", [DEPTH, 128, 4, 128])
    gm_bsT = din("gm_bsT", [DEPTH, 128, 4])
    hy_cw = din("hy_cw", [DEPTH, 128, 6, 4])
    hy_w1 = din("hy_f_w1", [DEPTH, HY_EMB, HY_FFN])
    hy_b1 = din("hy_f_b1", [DEPTH, HY_FFN, 1])
    hy_w2 = din("hy_f_w2", [DEPTH, HY_FFN, HY_FFN])
    hy_b2 = din("hy_f_b2", [DEPTH, HY_FFN, 1])
    hy_w3 = din("hy_f_w3", [DEPTH, HY_FFN, 512])
    hy_b3T = din("hy_b3T", [DEPTH, 128, 4])
    hy_skipT = din("hy_skipT", [DEPTH, 128, 2])
    da_l = din("da_l", [DEPTH, 4, 64])
    da_g = din("da_norm_g", [DEPTH, 128])
    p_a = din("p_a", [DEPTH, 256, D])
    p_b = din("p_b", [DEPTH, 256, D])
    p_c = din("p_c", [DEPTH, 512, D])
    w_out = din("w_out", [DEPTH, D, D])
    ln1_g = din("ln1_g", [DEPTH, D])
    ln1_b = din("ln1_b", [DEPTH, D])
    moe_wr = din("moe_wr", [DEPTH, D, 36])
    moe_br = din("moe_br", [DEPTH, 36])
    ex_g = din("ex_w_gate", [DEPTH * NE * D, HID])
    ex_u = din("ex_w_up", [DEPTH * NE * D, HID])
    ex_d = din("ex_w_down", [DEPTH * NE * HID, D])
    ex_g8 = ex_g.rearrange("(r j) h -> r (j h)", j=8)
    ex_u8 = ex_u.rearrange("(r j) h -> r (j h)", j=8)
    ex_d4 = ex_d.rearrange("(r j) d -> r (j d)", j=4)
    ln2_g = din("ln2_g", [DEPTH, D])
    ln2_b = din("ln2_b", [DEPTH, D])
    ident_in = din("c_ident", [128, 128])
    rope_in = din("c_rope", [2, 128, L])
    emb_in = {L: din("c_emb_L", [2, HY_EMB, L]), LC: din("c_emb_C", [2, HY_EMB, LC])}
    win_in = {L: din("c_win_L", [2, 256, L]), LC: din("c_win_C", [2, 256, LC])}
    NBLK = -(-(2 * T) // BLK) + NE
    MMAX = -(-(2 * T) // BLK) + 1
    cmoe_in = din("c_moe", [128, 128 + 8 + 4 + MMAX + NBLK + 32])
    out = nc.dram_tensor("out", [NB * L, D], F32, kind="ExternalOutput").ap()

    Wb = dscr("s_wb", [8, 128, NWB], BF16)
    MODS = dscr("s_mods", [DEPTH, NB + 1, 6 * D], F32)
    KREV = {L: dscr("s_krevL", [256, 2 * L], BF16), LC: dscr("s_krevC", [256, 2 * LC], BF16)}
    QTd = dscr("s_qt", [4, 128, T], BF16)
    KTd = dscr("s_kt", [4, 128, T], BF16)
    Vd = dscr("s_v", [T, 512], BF16)
    Gd = dscr("s_g", [24, 128, T], BF16)
    YATd = dscr("s_yat", [2, 128, T], BF16)
    X0Td = dscr("s_x0t", [2, 128, T], BF16)
    YBTd = dscr("s_ybt", [2, 128, T], BF16)
    YCTd = dscr("s_yct", [4, 128, T], BF16)
    X1d = dscr("s_x1", [T, D], F32)
    X2d = dscr("s_x2", [T, D], F32)
    H2d = dscr("s_h2", [T, D], BF16)
    XBUF = dscr("s_xbuf", [NBLK * BLK, D], BF16)
    YBUF = dscr("s_ybuf", [NBLK * BLK, D], BF16)

    seqs = [("lat", b, b * L, L) for b in range(NB)] + [("ctx", b, NB * L + b * LC, LC) for b in range(NB)]

    with ExitStack() as top:
        p = Prog(nc, top)

        uniq = [0]

        def sbt(st, name, shape, dt):
            uniq[0] += 1
            return st.enter_context(nc.sbuf_tensor("%s_%d" % (name, uniq[0]), list(shape), dt))

        def pst(st, name, shape, dt=F32):
            uniq[0] += 1
            return st.enter_context(nc.psum_tensor("%s_%d" % (name, uniq[0]), list(shape), dt))

        def dma(eng, out_, in_, r, w, **kw):
            p.add(eng, lambda e: e.dma_start(out=out_, in_=in_, **kw), r, w, dma=True)

        def mm(out_, lhsT, rhs, start, stop, r, w):
            p.add("tensor", lambda e: e.matmul(out_, lhsT=lhsT, rhs=rhs, start=start, stop=stop,
                                               skip_group_check=True), r, w)

        def tr(out_, in_, ident, r, w):
            p.add("tensor", lambda e: e.transpose(out=out_, in_=in_, identity=ident), r, w)

        def act(out_, in_, func, r, w, **kw):
            p.add("scalar", lambda e: e.activation(out=out_, in_=in_, func=func, **kw), r, w)

        def tt(eng, out_, a, b, op, r, w):
            p.add(eng, lambda e: e.tensor_tensor(out=out_, in0=a, in1=b, op=op), r, w)

        def ts(eng, out_, a, s1, op0, r, w, s2=None, op1=None):
            if op1 is None:
                p.add(eng, lambda e: e.tensor_scalar(out=out_, in0=a, scalar1=s1, scalar2=None, op0=op0), r, w)
            else:
                p.add(eng, lambda e: e.tensor_scalar(out=out_, in0=a, scalar1=s1, scalar2=s2, op0=op0, op1=op1), r, w)

        def stt(out_, a, s, b, op0, op1, r, w):
            p.add("vector", lambda e: e.scalar_tensor_tensor(out=out_, in0=a, scalar=s, in1=b, op0=op0, op1=op1), r, w)

        def cp(eng, out_, in_, r, w):
            if eng == "scalar":
                p.add(eng, lambda e: e.copy(out=out_, in_=in_), r, w)
            else:
                p.add(eng, lambda e: e.tensor_copy(out=out_, in_=in_), r, w)

        def red(out_, in_, op, axis, r, w):
            p.add("vector", lambda e: e.tensor_reduce(out=out_, in_=in_, axis=axis, op=op), r, w)

        def memset(eng, ap_, val, w):
            p.add(eng, lambda e: e.memset(ap_, val), (), w)

        phase_no = [0]

        def ph_end(final=False):
            phase_no[0] += 1
            stop = cfg.stop is not None and phase_no[0] >= cfg.stop
            p.flush(final=final or stop)
            if stop and not final:
                raise _Stop()

        identf = sbt(top, "identf", [128, 128], F32)
        identb = sbt(top, "identb", [128, 128], BF16)
        ropeT = sbt(top, "ropeT", [128, 2, L], BF16)
        epsc = sbt(top, "epsc", [128, 1], F32)
        dma("sync", identf[:], ident_in[:, :], [], ["identf"])
        cp("vector", identb[:], identf[:], ["identf"], ["identb"])
        with ExitStack() as ph:
            ropeF = sbt(ph, "ropeF", [128, 2, L], F32)
            dma("sync", ropeF[:, 0, :], rope_in[0, :, :], [], ["ropeF"])
            dma("sync", ropeF[:, 1, :], rope_in[1, :, :], [], ["ropeF"])
            cp("vector", ropeT[:], ropeF[:], ["ropeF"], ["ropeT"])
            memset("vector", epsc[:], EPS, ["epsc"])
            ph_end()

        def layer_norm_rows(st_tag, r_t, rk, g_b, b_b, out_t, ok, tmp):
            stats, mv, rstd = tmp["stats"], tmp["mv"], tmp["rstd"]
            for hh in range(2):
                p.add("vector", lambda e, hh=hh: e.bn_stats(out=stats[:, hh, :], in_=r_t[:, hh * 512:(hh + 1) * 512]),
                      [rk], [st_tag + "stats"])
            p.add("vector", lambda e: e.bn_aggr(out=mv[:], in_=stats[:].rearrange("p a b -> p (a b)")),
                  [st_tag + "stats"], [st_tag + "mv"])
            act(rstd[:], mv[:, 1:2], AF.Sqrt, [st_tag + "mv", "epsc"], [st_tag + "rstd"], bias=epsc[:], scale=1.0)
            p.add("vector", lambda e: e.reciprocal(out=rstd[:], in_=rstd[:]), [st_tag + "rstd"], [st_tag + "rstd"])
            ts("vector", out_t[:], r_t[:], mv[:, 0:1], ALU.subtract, [rk, st_tag + "mv", st_tag + "rstd"], [ok],
               s2=rstd[:, 0:1], op1=ALU.mult)
            tt("gpsimd", out_t[:], out_t[:], g_b[:], ALU.mult, [ok, "lnconst"], [ok])
            tt("gpsimd", out_t[:], out_t[:], b_b[:], ALU.add, [ok, "lnconst"], [ok])

        for l in range(DEPTH):
            last = l == DEPTH - 1
            lam_init = 0.8 - 0.6 * math.exp(-0.3 * l)
            Xsrc = (lambda tok0, n: (x_in[tok0:tok0 + n, :] if tok0 < NB * L else ctx_in[tok0 - NB * L:tok0 - NB * L + n, :])) \
                if l == 0 else (lambda tok0, n: X2d[tok0:tok0 + n, :])
            act_seqs = [s for s in seqs if not (last and s[0] == "ctx")]
            NTm = (NTL if last else NT)
            with ExitStack() as lay:
                lamt = sbt(lay, "lamt", [128, 4], F32)
                gsc = sbt(lay, "gsc", [128, 128], F32)

                with ExitStack() as ph:
                    wf = [sbt(ph, "wf%d" % i, [128, N_IN], F32) for i in range(2)]
                    wbt = [sbt(ph, "wbt%d" % i, [128, NWB], BF16) for i in range(2)]
                    for kt in range(8):
                        a, b_ = wf[kt % 2], wbt[kt % 2]
                        ka, kb = "wf%d" % (kt % 2), "wbt%d" % (kt % 2)
                        dma("sync", a[:], w_in[l, kt * 128:(kt + 1) * 128, :], [], [ka])
                        cp("vector", b_[:, 0:2048], a[:, 0:2048], [ka], [kb + "a"])
                        cp("gpsimd", b_[:, 2048:4096], a[:, 2048:4096], [ka], [kb + "b"])
                        cp("scalar", b_[:, 4096:N_IN], a[:, 4096:N_IN], [ka], [kb + "c"])
                        for qi, off in enumerate((OFF_Q, OFF_K)):
                            for rr in range(2):
                                o_ = _ap(b_[:], N_IN + qi * 512 + rr * 16, [[32, 16], [1, 16]])
                                i_ = _ap(a[:], off + (1 - rr) * 16, [[32, 16], [1, 16]])
                                cp("vector", o_, i_, [ka], [kb + "d%d%d" % (qi, rr)])
                        dma("sync", Wb[kt, :, :], b_[:], [kb + "a", kb + "b", kb + "c", kb + "d00", kb + "d01", kb + "d10", kb + "d11"], ["Wb"])
                    dl = sbt(ph, "dl", [128, 4, 64], F32)
                    dg = sbt(ph, "dg", [128, 128], F32)
                    pr = sbt(ph, "pr", [128, 2, 64], F32)
                    dma("sync", dl[:], bass.AP(tensor=da_l.tensor, offset=da_l.offset + l * 256, ap=[[0, 128], [64, 4], [1, 64]]), [], ["dl"])
                    dma("sync", dg[:], bass.AP(tensor=da_g.tensor, offset=da_g.offset + l * 128, ap=[[0, 128], [1, 128]]), [], ["dg"])
                    tt("vector", pr[:, 0, :], dl[:, 0, :], dl[:, 1, :], ALU.mult, ["dl"], ["pr"])
                    tt("vector", pr[:, 1, :], dl[:, 2, :], dl[:, 3, :], ALU.mult, ["dl"], ["pr"])
                    red(lamt[:, 1:3], pr[:], ALU.add, AX.X, ["pr"], ["lamt"])
                    act(lamt[:, 1:3], lamt[:, 1:3], AF.Exp, ["lamt"], ["lamt"])
                    tt("vector", lamt[:, 0:1], lamt[:, 2:3], lamt[:, 1:2], ALU.subtract, ["lamt"], ["lamt"])
                    ts("vector", lamt[:, 0:1], lamt[:, 0:1], -lam_init, ALU.add, ["lamt"], ["lamt"])
                    ts("vector", gsc[:], dg[:], 1.0 - lam_init, ALU.mult, ["dg"], ["gsc"])
                    ph_end()

                with ExitStack() as ph:
                    cTt = sbt(ph, "cTt", [128, 8, NB + 1], F32)
                    sct = sbt(ph, "sct", [128, 8, NB + 1], F32)
                    adb = sbt(ph, "adb", [NB + 1, 6 * D], F32)
                    modt = sbt(ph, "modt", [NB + 1, 6 * D], F32)
                    awt = [sbt(ph, "awt%d" % i, [128, 3072], F32) for i in range(2)]
                    psm = pst(ph, "psm", [128, 3072])
                    dma("sync", cTt[:], cT_in[:, :, :], [], ["cTt"])
                    dma("sync", adb[:], bass.AP(tensor=ada_b.tensor, offset=ada_b.offset + l * 6 * D,
                                                ap=[[0, NB + 1], [1, 6 * D]]), [], ["adb"])
                    act(sct[:], cTt[:], AF.Silu, ["cTt"], ["sct"])
                    i = 0
                    for half in range(2):
                        for kt in range(8):
                            a, ka = awt[i % 2], "awt%d" % (i % 2)
                            i += 1
                            dma("sync" if kt % 2 == 0 else "gpsimd", a[:],
                                ada_w[l, kt * 128:(kt + 1) * 128, half * 3072:(half + 1) * 3072], [], [ka])
                            for ng in range(6):
                                mm(psm[0:NB + 1, ng * 512:(ng + 1) * 512], sct[:, kt, :], a[:, ng * 512:(ng + 1) * 512],
                                   kt == 0, kt == 7, [ka, "sct"], ["psm"])
                        tt("vector", modt[:, half * 3072:(half + 1) * 3072], psm[0:NB + 1, :],
                           adb[:, half * 3072:(half + 1) * 3072], ALU.add, ["psm", "adb"], ["modt"])
                    dma("sync", MODS[l, :, :], modt[:], ["modt"], ["MODS"])
                    ph_end()

                with ExitStack() as ph:
                    w1f = sbt(ph, "w1f", [HY_EMB, HY_FFN], F32)
                    w2f = sbt(ph, "w2f", [HY_FFN, HY_FFN], F32)
                    w3f = sbt(ph, "w3f", [HY_FFN, 512], F32)
                    b1t = sbt(ph, "b1t", [HY_FFN, 1], F32)
                    b2t = sbt(ph, "b2t", [HY_FFN, 1], F32)
                    b3t = sbt(ph, "b3t", [128, 4], F32)
                    skt = sbt(ph, "skt", [128, 2], F32)
                    dma("sync", w1f[:], hy_w1[l, :, :], [], ["hyw"])
                    dma("sync", w2f[:], hy_w2[l, :, :], [], ["hyw"])
                    dma("sync", w3f[:], hy_w3[l, :, :], [], ["hyw"])
                    dma("sync", b1t[:], hy_b1[l, :, :], [], ["hyw"])
                    dma("sync", b2t[:], hy_b2[l, :, :], [], ["hyw"])
                    dma("sync", b3t[:], hy_b3T[l, :, :], [], ["hyw"])
                    dma("sync", skt[:], hy_skipT[l, :, :], [], ["hyw"])
                    ps1 = pst(ph, "ps1", [128, 512])
                    ps2 = pst(ph, "ps2", [128, 512])
                    ps3 = pst(ph, "ps3", [128, 512])
                    for Lf in ([L] if last else [L, LC]):
                        with ExitStack() as ph2:
                            CHF = min(512, Lf)
                            embt = sbt(ph2, "embt", [HY_EMB, 2, Lf], F32)
                            wint = sbt(ph2, "wint", [128, 2, 2, Lf], F32)
                            krf = sbt(ph2, "krf", [128, 2, 2 * Lf], F32)
                            krb = sbt(ph2, "krb", [128, 2, 2 * Lf], BF16)
                            h1 = sbt(ph2, "h1", [HY_FFN, 512], F32)
                            h2 = sbt(ph2, "h2", [HY_FFN, 512], F32)
                            wr1 = sbt(ph2, "wr1", [HY_FFN, 512], F32)
                            wr2 = sbt(ph2, "wr2", [HY_FFN, 512], F32)
                            tg = "f%d" % Lf
                            for dr in range(2):
                                dma("sync", embt[:, dr, :], emb_in[Lf][dr, :, :], [], [tg + "emb"])
                                for cc in range(2):
                                    dma("gpsimd", wint[:, dr, cc, :], win_in[Lf][dr, cc * 128:(cc + 1) * 128, :], [], [tg + "win"])
                            memset("gpsimd", krf[:], 0.0, [tg + "krf"])
                            for dr in range(2):
                                for ch in range(Lf // CHF):
                                    cs = slice(ch * CHF, (ch + 1) * CHF)
                                    mm(ps1[0:HY_FFN, 0:CHF], w1f[:], embt[:, dr, cs], True, True, ["hyw", tg + "emb"], ["ps1"])
                                    ts("vector", h1[:, 0:CHF], ps1[0:HY_FFN, 0:CHF], b1t[:, 0:1], ALU.add, ["ps1", "hyw"], ["h1"])
                                    ts("vector", wr1[:, 0:CHF], h1[:, 0:CHF], math.pi, ALU.is_gt, ["h1"], ["wr1"], s2=-2 * math.pi, op1=ALU.mult)
                                    ts("vector", wr2[:, 0:CHF], h1[:, 0:CHF], -math.pi, ALU.is_lt, ["h1"], ["wr2"], s2=2 * math.pi, op1=ALU.mult)
                                    tt("vector", h1[:, 0:CHF], h1[:, 0:CHF], wr1[:, 0:CHF], ALU.add, ["h1", "wr1"], ["h1"])
                                    tt("vector", h1[:, 0:CHF], h1[:, 0:CHF], wr2[:, 0:CHF], ALU.add, ["h1", "wr2"], ["h1"])
                                    act(h1[:, 0:CHF], h1[:, 0:CHF], AF.Sin, ["h1"], ["h1"])
                                    mm(ps2[0:HY_FFN, 0:CHF], w2f[:], h1[:, 0:CHF], True, True, ["hyw", "h1"], ["ps2"])
                                    ts("vector", h2[:, 0:CHF], ps2[0:HY_FFN, 0:CHF], b2t[:, 0:1], ALU.add, ["ps2", "hyw"], ["h2"])
                                    ts("vector", wr1[:, 0:CHF], h2[:, 0:CHF], math.pi, ALU.is_gt, ["h2"], ["wr1"], s2=-2 * math.pi, op1=ALU.mult)
                                    ts("vector", wr2[:, 0:CHF], h2[:, 0:CHF], -math.pi, ALU.is_lt, ["h2"], ["wr2"], s2=2 * math.pi, op1=ALU.mult)
                                    tt("vector", h2[:, 0:CHF], h2[:, 0:CHF], wr1[:, 0:CHF], ALU.add, ["h2", "wr1"], ["h2"])
                                    tt("vector", h2[:, 0:CHF], h2[:, 0:CHF], wr2[:, 0:CHF], ALU.add, ["h2", "wr2"], ["h2"])
                                    act(h2[:, 0:CHF], h2[:, 0:CHF], AF.Sin, ["h2"], ["h2"])
                                    for cc in range(2):
                                        mm(ps3[:, 0:CHF], w3f[:, dr * 256 + cc * 128:dr * 256 + (cc + 1) * 128], h2[:, 0:CHF],
                                           True, True, ["hyw", "h2"], ["ps3"])
                                        o0 = dr * Lf + ch * CHF
                                        stt(krf[:, cc, o0:o0 + CHF], ps3[:, 0:CHF], b3t[:, dr * 2 + cc:dr * 2 + cc + 1],
                                            wint[:, dr, cc, cs], ALU.add, ALU.mult, ["ps3", "hyw", tg + "win", tg + "krf"], [tg + "krf"])
                            for cc in range(2):
                                ts("vector", krf[:, cc, Lf - 1:Lf], krf[:, cc, Lf - 1:Lf], skt[:, cc:cc + 1], ALU.add,
                                   [tg + "krf", "hyw"], [tg + "krf"])
                            cp("vector", krb[:, 0, :], krf[:, 0, :], [tg + "krf"], [tg + "krb"])
                            cp("gpsimd", krb[:, 1, :], krf[:, 1, :], [tg + "krf"], [tg + "krb"])
                            for cc in range(2):
                                dma("sync", KREV[Lf][cc * 128:(cc + 1) * 128, :], krb[:, cc, :], [tg + "krb"], ["KREV%d" % Lf])
                            ph_end()

                with ExitStack() as ph45:
                    nbL, nbC = L // 128, LC // 128
                    VXs = {"lat": sbt(ph45, "VXsL", [128, 256, nbL, NB], BF16)}
                    if not last:
                        VXs["ctx"] = sbt(ph45, "VXsC", [128, 256, nbC, NB], BF16)
                    with ExitStack() as ph:
                        LMAX = L
                        hT = sbt(ph, "hT", [128, 8, LMAX], BF16)
                        zhs = Rot([sbt(ph, "zh%d" % i, [128, LMAX + 2], F32) for i in range(2)], "zh")
                        VT2 = sbt(ph, "VT2", [128, 2, LMAX], F32)
                        tmpA = sbt(ph, "tmpA", [128, LMAX], F32)
                        x0bs = Rot([sbt(ph, "x0b%d" % i, [128, LMAX], BF16) for i in range(2)], "x0b")
                        wgs = Rot([sbt(ph, "wg%d" % i, [128, 8, 512], BF16) for i in range(3)], "wg")
                        scb = sbt(ph, "scb", [128, D], F32)
                        shb = sbt(ph, "shb", [128, D], F32)
                        xts = Rot([sbt(ph, "xt%d" % i, [128, D], F32) for i in range(2)], "xt")
                        hbs = Rot([sbt(ph, "hb%d" % i, [128, D], BF16) for i in range(2)], "hb")
                        lngb = sbt(ph, "lngb", [128, 256], F32)
                        lnbb = sbt(ph, "lnbb", [128, 256], F32)
                        wsf = sbt(ph, "wsf", [128, 4, 128], F32)
                        wsb = sbt(ph, "wsb", [128, 4, 128], BF16)
                        bst = sbt(ph, "bst", [128, 4], F32)
                        cwt = sbt(ph, "cwt", [128, 6, 4], F32)
                        gmf = sbt(ph, "gmf", [128, 512], F32)
                        vnb = sbt(ph, "vnb", [128, 256], BF16)
                        vnf = sbt(ph, "vnf", [128, 256], F32)
                        yab = sbt(ph, "yab", [128, 256], BF16)
                        yaT = sbt(ph, "yaT", [128, 2, 128], BF16)
                        gst = sbt(ph, "gst", [128, 6], F32)
                        gmv = sbt(ph, "gmv", [128, 2], F32)
                        grs = sbt(ph, "grs", [128, 1], F32)
                        vts = Rot([sbt(ph, "vt%d" % i, [128, 512], BF16) for i in range(2)], "vt")
                        fos = Rot([sbt(ph, "fo%d" % i, [128, 512], BF16) for i in range(3)], "fo")
                        t1s = Rot([sbt(ph, "t1_%d" % i, [128, 512], F32) for i in range(2)], "t1_")
                        t2s = Rot([sbt(ph, "t2_%d" % i, [128, 512], F32) for i in range(2)], "t2_")
                        VXT = sbt(ph, "VXT", [128, 2, LMAX], BF16)
                        psH = pst(ph, "psH", [128, 8, 128], BF16)
                        psFs = Rot([pst(ph, "psF%d" % i, [128, 512]) for i in range(2)], "psF")
                        psPs = Rot([pst(ph, "psP%d" % i, [128, 512]) for i in range(2)], "psP")
                        psT = pst(ph, "psT", [128, 512])
                        psS = pst(ph, "psS", [128, 256])
                        psYA = pst(ph, "psYA", [128, 2, 128], BF16)
                        dma("sync", lngb[:], bass.AP(tensor=gm_ln_g.tensor, offset=gm_ln_g.offset + l * 256, ap=[[0, 128], [1, 256]]), [], ["gmc"])
                        dma("sync", lnbb[:], bass.AP(tensor=gm_ln_b.tensor, offset=gm_ln_b.offset + l * 256, ap=[[0, 128], [1, 256]]), [], ["gmc"])
                        dma("sync", wsf[:], gm_wsT[l, :, :, :], [], ["wsf"])
                        cp("vector", wsb[:], wsf[:], ["wsf"], ["gmc"])
                        dma("sync", bst[:], gm_bsT[l, :, :], [], ["gmc"])
                        dma("sync", cwt[:], hy_cw[l, :, :, :], [], ["gmc"])
                        for zt in zhs.tiles:
                            memset("vector", zt[:, 0:1], 0.0, ["zh0", "zh1"])

                        for (kind, b, tok0, Ls) in seqs:
                            full = not (last and kind == "ctx")
                            CH = min(512, Ls)
                            nch = Ls // CH
                            ntile = Ls // 128
                            mrow = b if kind == "lat" else NB
                            rope = kind == "lat"
                            dma("sync", shb[:], bass.AP(tensor=MODS.tensor, offset=MODS.offset + (l * (NB + 1) + mrow) * 6 * D,
                                                        ap=[[0, 128], [1, D]]), ["MODS"], ["shb"])
                            dma("sync", scb[:], bass.AP(tensor=MODS.tensor, offset=MODS.offset + (l * (NB + 1) + mrow) * 6 * D + D,
                                                        ap=[[0, 128], [1, D]]), ["MODS"], ["scb"])
                            ts("gpsimd", scb[:], scb[:], 1.0, ALU.add, ["scb"], ["scb"])
                            for zt in zhs.tiles:
                                memset("vector", zt[:, Ls + 1:Ls + 2], 0.0, ["zh0", "zh1"])
                            for tI in range(ntile):
                                xt, xk = xts.next()
                                hb, hk = hbs.next()
                                dma("sync" if tI % 2 == 0 else "gpsimd", xt[:], Xsrc(tok0 + tI * 128, 128), ["X2d"] if l else [], [xk])
                                tt("vector", xt[:], xt[:], scb[:], ALU.mult, [xk, "scb"], [xk])
                                tt("gpsimd", hb[:], xt[:], shb[:], ALU.add, [xk, "shb"], [hk])
                                for kt in range(8):
                                    tr(psH[:, kt, :], hb[:, kt * 128:(kt + 1) * 128], identb[:], [hk, "identb"], ["psH"])
                                cp("scalar", hT[:, :, tI * 128:(tI + 1) * 128], psH[:], ["psH"], ["hT"])

                            def load_group(col0):
                                wg, wk = wgs.next()
                                dma("sync", wg[:], Wb[:, :, col0:col0 + 512].rearrange("k p c -> p k c"), ["Wb"], [wk])
                                return wg, wk

                            def fm_chunk(wg, wk, ci, tc, ps, pk):
                                for kt in range(8):
                                    mm(ps[:, 0:CH], wg[:, kt, ci * 128:(ci + 1) * 128], hT[:, kt, tc * CH:(tc + 1) * CH],
                                       kt == 0, kt == 7, [wk, "hT"], [pk])

                            if full:
                                wg, wk = load_group(0)
                                for tI in range(ntile):
                                    for kt in range(8):
                                        mm(psT[:], hT[:, kt, tI * 128:(tI + 1) * 128], wg[:, kt, :], kt == 0, kt == 7, [wk, "hT"], ["psT"])
                                    act(gmf[:], psT[:], AF.Gelu, ["psT"], ["gmf"])
                                    p.add("vector", lambda e: e.bn_stats(out=gst[:], in_=gmf[:, 256:512]), ["gmf"], ["gst"])
                                    p.add("vector", lambda e: e.bn_aggr(out=gmv[:], in_=gst[:]), ["gst"], ["gmv"])
                                    act(grs[:], gmv[:, 1:2], AF.Sqrt, ["gmv", "epsc"], ["grs"], bias=epsc[:], scale=1.0)
                                    p.add("vector", lambda e: e.reciprocal(out=grs[:], in_=grs[:]), ["grs"], ["grs"])
                                    ts("vector", vnf[:], gmf[:, 256:512], gmv[:, 0:1], ALU.subtract, ["gmf", "gmv", "grs"], ["vnf"],
                                       s2=grs[:, 0:1], op1=ALU.mult)
                                    tt("gpsimd", vnf[:], vnf[:], lngb[:], ALU.mult, ["vnf", "gmc"], ["vnf"])
                                    tt("gpsimd", vnb[:], vnf[:], lnbb[:], ALU.add, ["vnf", "gmc"], ["vnb"])
                                    for g in range(4):
                                        mm(psS[:, g * 64:(g + 1) * 64], wsb[:, g, :], vnb[:, g * 64:(g + 1) * 64], True, True, ["gmc", "vnb"], ["psS"])
                                    for g in range(4):
                                        stt(yab[:, g * 64:(g + 1) * 64], psS[:, g * 64:(g + 1) * 64], bst[:, g:g + 1],
                                            gmf[:, g * 64:(g + 1) * 64], ALU.add, ALU.mult, ["psS", "gmc", "gmf"], ["yab"])
                                    for cc in range(2):
                                        tr(psYA[:, cc, :], yab[:, cc * 128:(cc + 1) * 128], identb[:], ["yab", "identb"], ["psYA"])
                                    cp("scalar", yaT[:], psYA[:], ["psYA"], ["yaT"])
                                    t0 = tok0 + tI * 128
                                    dma("gpsimd", YATd[:, :, t0:t0 + 128].rearrange("c p t -> p c t"), yaT[:], ["yaT"], ["YATd"])
                                for (col0, cis) in ((OFF_HY + 512, (1, 0)), (OFF_HY, (3, 2, 1, 0))):
                                    wg, wk = load_group(col0)
                                    for ci in cis:
                                        r_ = (col0 - OFF_HY) // 128 + ci
                                        zh, zk = zhs.next()
                                        for tc in range(nch):
                                            ps, pk = psFs.next()
                                            fm_chunk(wg, wk, ci, tc, ps, pk)
                                            cp("scalar", zh[:, 1 + tc * CH:1 + (tc + 1) * CH], ps[:, 0:CH], [pk], [zk])
                                        if r_ >= 4:
                                            acc, ak = VT2[:, r_ - 4, 0:Ls], "VT2"
                                        else:
                                            acc, ak = tmpA[:, 0:Ls], "tmpA"
                                        ts("vector", acc, zh[:, 0:Ls], cwt[:, r_, 0:1], ALU.mult, [zk, "gmc"], [ak],
                                           s2=cwt[:, r_, 3:4], op1=ALU.add)
                                        stt(acc, zh[:, 1:Ls + 1], cwt[:, r_, 1:2], acc, ALU.mult, ALU.add, [zk, "gmc", ak], [ak])
                                        if r_ >= 4:
                                            stt(acc, zh[:, 2:Ls + 2], cwt[:, r_, 2:3], acc, ALU.mult, ALU.add, [zk, "gmc", ak], [ak])
                                        elif r_ >= 2:
                                            stt(acc, zh[:, 2:Ls + 2], cwt[:, r_, 2:3], acc, ALU.mult, ALU.add, [zk, "gmc", ak], [ak])
                                            tt("gpsimd", VXT[:, r_ - 2, 0:Ls], acc, VT2[:, r_ - 2, 0:Ls], ALU.mult, [ak, "VT2"], ["VXT"])
                                        else:
                                            x0b, x0k = x0bs.next()
                                            stt(x0b[:, 0:Ls], zh[:, 2:Ls + 2], cwt[:, r_, 2:3], acc, ALU.mult, ALU.add, [zk, "gmc", ak], [x0k])
                                            dma("gpsimd", X0Td[r_, :, tok0:tok0 + Ls], x0b[:, 0:Ls], [x0k], ["X0Td"])
                                vx = VXs[kind]
                                for tI in range(ntile):
                                    for cc in range(2):
                                        tr(psYA[:, cc, :], VXT[:, cc, tI * 128:(tI + 1) * 128], identb[:], ["VXT", "identb"], ["psYA"])
                                    cp("scalar", vx[:, :, tI, b], psYA[:].rearrange("p a b -> p (a b)"), ["psYA"], ["VXs" + kind])
                            for (nm, colA, colB, dst) in (("q", OFF_Q, N_IN, QTd), ("k", OFF_K, N_IN + 512, KTd)):
                                if nm == "q" and not full:
                                    continue
                                wgA, wkA = load_group(colA)
                                if rope:
                                    wgB, wkB = load_group(colB)
                                for h in range(4):
                                    for tc in range(nch):
                                        ps, pk = psFs.next()
                                        fm_chunk(wgA, wkA, h, tc, ps, pk)
                                        fo, fk = fos.next()
                                        if rope:
                                            ps2_, pk2 = psPs.next()
                                            fm_chunk(wgB, wkB, h, tc, ps2_, pk2)
                                            t1, k1 = t1s.next()
                                            t2, k2 = t2s.next()
                                            tt("vector", t1[:, 0:CH], ps[:, 0:CH], ropeT[:, 0, tc * CH:(tc + 1) * CH], ALU.mult, [pk, "ropeT"], [k1])
                                            tt("vector", t2[:, 0:CH], ps2_[:, 0:CH], ropeT[:, 1, tc * CH:(tc + 1) * CH], ALU.mult, [pk2, "ropeT"], [k2])
                                            tt("gpsimd", fo[:, 0:CH], t1[:, 0:CH], t2[:, 0:CH], ALU.add, [k1, k2], [fk])
                                        else:
                                            cp("scalar", fo[:, 0:CH], ps[:, 0:CH], [pk], [fk])
                                        t0 = tok0 + tc * CH
                                        dma("gpsimd", dst[h, :, t0:t0 + CH], fo[:, 0:CH], [fk], [nm + "Td"])
                            wg, wk = load_group(OFF_V)
                            for tI in range(ntile):
                                for kt in range(8):
                                    mm(psT[:], hT[:, kt, tI * 128:(tI + 1) * 128], wg[:, kt, :], kt == 0, kt == 7, [wk, "hT"], ["psT"])
                                vt, vk = vts.next()
                                cp("scalar", vt[:], psT[:], ["psT"], [vk])
                                t0 = tok0 + tI * 128
                                dma("gpsimd", Vd[t0:t0 + 128, :], vt[:], [vk], ["Vd"])
                            if full:
                                for gi in range(6):
                                    wg, wk = load_group(OFF_GATE + gi * 512)
                                    for ci in range(4):
                                        for tc in range(nch):
                                            ps, pk = psFs.next()
                                            fm_chunk(wg, wk, ci, tc, ps, pk)
                                            fo, fk = fos.next()
                                            act(fo[:, 0:CH], ps[:, 0:CH], AF.Sigmoid, [pk], [fk])
                                            t0 = tok0 + tc * CH
                                            dma("gpsimd", Gd[gi * 4 + ci, :, t0:t0 + CH], fo[:, 0:CH], [fk], ["Gd"])
                        ph_end()

                    with ExitStack() as ph:
                        hsk = Rot([sbt(ph, "hsk%d" % i, [128, 128 * (2 * nbL - 1)], BF16) for i in range(3)], "hsk")
                        psYs = Rot([pst(ph, "psY%d" % i, [128, 8, nbL * NB]) for i in range(2)], "psY")
                        psTt = pst(ph, "psTt", [128, 2, 128], BF16)
                        YBT = sbt(ph, "YBT", [128, 2, T], BF16)
                        x0ls = Rot([sbt(ph, "x0l%d" % i, [128, 2, L], BF16) for i in range(2)], "x0l")
                        for kind in (["lat"] if last else ["lat", "ctx"]):
                            Lf = L if kind == "lat" else LC
                            nb = Lf // 128
                            W = 128 * (2 * nb - 1)
                            with ExitStack() as ph2:
                                Yr = sbt(ph2, "Yr" + kind, [128, nb, NB, 256], BF16)
                                vx = VXs[kind]
                                kdr = KREV[Lf]
                                for cg in range(32):
                                    psY, pyk = psYs.next()
                                    for c8 in range(8):
                                        c = cg * 8 + c8
                                        hk_, hkk = hsk.next()
                                        dma("sync" if c % 2 == 0 else "gpsimd", hk_[:, 0:W],
                                            bass.AP(tensor=kdr.tensor, offset=kdr.offset + c * 2 * Lf, ap=[[1, 128], [1, W]]),
                                            ["KREV%d" % Lf], [hkk])
                                        lags = [0] + [d for d in range(-(nb - 1), nb) if d != 0]
                                        for di, d in enumerate(lags):
                                            j0, j1 = max(0, -d), min(nb, nb - d)
                                            m0 = 128 * (nb - 1 - d)
                                            o_ = _ap(psY[:], c8 * nbL * NB + (j0 + d) * NB, [[1, (j1 - j0) * NB]])
                                            mm(o_, hk_[:, m0:m0 + 128], vx[:, c, j0:j1, :].rearrange("p j b -> p (j b)"),
                                               di == 0, di == len(lags) - 1, [hkk, "VXs" + kind], [pyk])
                                    src = _ap(psY[:], 0, [[nbL * NB, 8], [NB, nb], [1, NB]])
                                    dst_ = _ap(Yr[:], cg * 8, [[1, 8], [NB * 256, nb], [256, NB]])
                                    cp("scalar" if cg % 2 == 0 else "vector", dst_, src, [pyk], ["Yr"])
                                for b in range(NB):
                                    tok0 = b * L if kind == "lat" else NB * L + b * LC
                                    x0l, x0lk = x0ls.next()
                                    dma("sync", x0l[:, :, 0:Lf], X0Td[:, :, tok0:tok0 + Lf].rearrange("c p t -> p c t"), ["X0Td"], [x0lk])
                                    for i_ in range(nb):
                                        for cc in range(2):
                                            tr(psTt[:, cc, :], Yr[:, i_, b, cc * 128:(cc + 1) * 128], identb[:], ["Yr", "identb"], ["psTt"])
                                        t0 = tok0 + i_ * 128
                                        for cc in range(2):
                                            rev = _ap(psTt[:], cc * 128 + 127, [[-1, 128]])
                                            tt("vector", YBT[:, cc, t0:t0 + 128], rev, x0l[:, cc, i_ * 128:(i_ + 1) * 128], ALU.mult,
                                               ["psTt", x0lk], ["YBT"])
                        ntok = NB * L if last else T
                        for cc in range(2):
                            dma("sync", YBTd[cc, :, 0:ntok], YBT[:, cc, 0:ntok], ["YBT"], ["YBTd"])
                        ph_end()

                with ExitStack() as ph:
                    NKmax = (L + LC) // 128
                    QT = sbt(ph, "QT", [128, 4, 2, L], BF16)
                    KT = sbt(ph, "KT", [128, 4, L + LC], BF16)
                    Vone = sbt(ph, "Vone", [128, NKmax, 4, 129], BF16)
                    Es = Rot([sbt(ph, "E%d" % i, [128, 512], BF16) for i in range(3)], "E")
                    psSs = Rot([pst(ph, "psA%d" % i, [128, 512]) for i in range(2)], "psA")
                    acc = pst(ph, "acc", [128, 4, 2, 256])
                    psC = pst(ph, "psC", [128, 4, 128], BF16)
                    rec = sbt(ph, "rec", [128, 4, 2], F32)
                    o1 = sbt(ph, "o1", [128, 128], F32)
                    o2 = sbt(ph, "o2", [128, 128], F32)
                    sq = sbt(ph, "sq", [128, 128], F32)
                    ss = sbt(ph, "ss", [128, 1], F32)
                    YC = sbt(ph, "YC", [128, 4, 512], BF16)
                    ycts = Rot([sbt(ph, "yct%d" % i, [128, 4, 512], BF16) for i in range(2)], "yct")
                    memset("vector", Vone[:], 1.0, ["Vone"])
                    memset("gpsimd", QT[:], 0.0, ["QT"])
                    for (kind, b, tok0, Lq) in act_seqs:
                        if kind == "lat":
                            ksegs = [(tok0, L), (NB * L + b * LC, LC)]
                        else:
                            ksegs = [(tok0, LC)]
                        NK = sum(s[1] for s in ksegs) // 128
                        for m in range(2):
                            dma("sync", QT[m * 64:(m + 1) * 64, :, m, 0:Lq],
                                QTd[:, m * 64:(m + 1) * 64, tok0:tok0 + Lq].rearrange("h p t -> p h t"), ["qTd"], ["QT"])
                        ko = 0
                        for (kt0, kl) in ksegs:
                            dma("gpsimd", KT[:, :, ko:ko + kl], KTd[:, :, kt0:kt0 + kl].rearrange("h p t -> p h t"), ["kTd"], ["KT"])
                            for j in range(kl // 128):
                                dma("sync" if j % 2 else "gpsimd", Vone[:, ko // 128 + j, :, 0:128],
                                    Vd[kt0 + j * 128:kt0 + (j + 1) * 128, :].rearrange("t (h e) -> t h e", h=4), ["Vd"], ["Vone"])
                            ko += kl
                        QC = min(512, Lq)
                        nsub = QC // 128
                        for qc in range(Lq // QC):
                            steps = [(h, m, kt) for h in range(4) for m in range(2) for kt in range(NK)]

                            def emit_S(st_):
                                h, m, kt = st_
                                ms = slice(m * 64, (m + 1) * 64)
                                psA, pak = psSs.next()
                                mm(psA[:, 0:QC], KT[:, h, kt * 128:(kt + 1) * 128], QT[:, h, m, qc * QC:(qc + 1) * QC],
                                   True, True, ["KT", "QT"], [pak])
                                return psA, pak

                            nxt = emit_S(steps[0])
                            for si, (h, m, kt) in enumerate(steps):
                                psA, pak = nxt
                                E, ek = Es.next()
                                act(E[:, 0:QC], psA[:, 0:QC], AF.Exp, [pak], [ek], scale=0.125)
                                if si + 1 < len(steps):
                                    nxt = emit_S(steps[si + 1])
                                for qs in range(nsub):
                                    mm(acc[:, qs, m, 0:129], E[:, qs * 128:(qs + 1) * 128], Vone[:, kt, h, :],
                                       kt == 0, kt == NK - 1, [ek, "Vone"], ["acc"])
                                if not (m == 1 and kt == NK - 1):
                                    continue
                                p.add("vector", lambda e, nsub=nsub: e.reciprocal(out=rec[:, 0:nsub, :], in_=acc[:, 0:nsub, :, 128]), ["acc"], ["rec"])
                                ts("vector", rec[:, 0:nsub, 1], rec[:, 0:nsub, 1], lamt[:, 0:1], ALU.mult, ["rec", "lamt"], ["rec"])
                                for qs in range(nsub):
                                    ts("vector", o1[:], acc[:, qs, 0, 0:128], rec[:, qs, 0:1], ALU.mult, ["acc", "rec"], ["o1"])
                                    stt(o2[:], acc[:, qs, 1, 0:128], rec[:, qs, 1:2], o1[:], ALU.mult, ALU.add, ["acc", "rec", "o1"], ["o2"])
                                    tt("gpsimd", sq[:], o2[:], o2[:], ALU.mult, ["o2"], ["sq"])
                                    red(ss[:], sq[:], ALU.add, AX.X, ["sq"], ["ss"])
                                    act(ss[:], ss[:], AF.Sqrt, ["ss", "epsc"], ["ss"], bias=epsc[:], scale=1.0 / 128.0)
                                    p.add("vector", lambda e: e.reciprocal(out=ss[:], in_=ss[:]), ["ss"], ["ss"])
                                    stt(YC[:, qs, h * 128:(h + 1) * 128], o2[:], ss[:, 0:1], gsc[:], ALU.mult, ALU.mult,
                                        ["o2", "ss", "gsc"], ["YC"])
                            yct, yk = ycts.next()
                            for qs in range(nsub):
                                for h in range(4):
                                    tr(psC[:, h, :], YC[:, qs, h * 128:(h + 1) * 128], identb[:], ["YC", "identb"], ["psC"])
                                cp("scalar", yct[:, :, qs * 128:(qs + 1) * 128], psC[:], ["psC"], [yk])
                            t0 = tok0 + qc * QC
                            dma("sync", YCTd[:, :, t0:t0 + QC].rearrange("h p t -> p h t"), yct[:, :, 0:QC], [yk], ["YCTd"])
                    ph_end()

                LOG = sbt(lay, "LOG", [128, NT, 36], F32)
                WTS = sbt(lay, "WTS", [128, NT, 2], F32)
                DEST = sbt(lay, "DEST", [128, NT, 2], I32)
                IDXE = sbt(lay, "IDXE", [128, NBLK], I32)
                with ExitStack() as ph:
                    pab = sbt(ph, "pab", [128, 2, D], BF16)
                    pbb = sbt(ph, "pbb", [128, 2, D], BF16)
                    pcb = sbt(ph, "pcb", [128, 4, D], BF16)
                    wob = sbt(ph, "wob", [128, 8, D], BF16)
                    wrb = sbt(ph, "wrb", [128, 8, 36], BF16)
                    wrf = sbt(ph, "wrf", [128, 8, 36], F32)
                    rbb = sbt(ph, "rbb", [128, 36], F32)
                    stg = Rot([sbt(ph, "stg%d" % i, [128, 2, D], F32) for i in range(2)], "stg")
                    lg = sbt(ph, "lg", [128, D], F32)
                    lb = sbt(ph, "lb", [128, D], F32)
                    g1b = sbt(ph, "g1b", [128, D], F32)
                    sc2b = sbt(ph, "sc2b", [128, D], F32)
                    sh2b = sbt(ph, "sh2b", [128, D], F32)
                    si = 0
                    for (src, nk, dstw) in ((p_a, 2, pab), (p_b, 2, pbb), (p_c, 4, pcb), (w_out, 8, wob)):
                        for k2 in range(0, nk, 2):
                            s_, sk_ = stg.next()
                            dma("sync", s_[:], src[l, k2 * 128:(k2 + 2) * 128, :].rearrange("(k p) d -> p k d", p=128), [], [sk_])
                            cp("vector" if si % 2 == 0 else "gpsimd", dstw[:, k2:k2 + 2, :], s_[:], [sk_], ["mw"])
                            si += 1
                    dma("sync", wrf[:], moe_wr[l, :, :].rearrange("(k p) e -> p k e", p=128), [], ["wrf"])
                    cp("vector", wrb[:], wrf[:], ["wrf"], ["mw"])
                    dma("sync", rbb[:], bass.AP(tensor=moe_br.tensor, offset=moe_br.offset + l * 36, ap=[[0, 128], [1, 36]]), [], ["mw"])
                    dma("sync", lg[:], bass.AP(tensor=ln1_g.tensor, offset=ln1_g.offset + l * D, ap=[[0, 128], [1, D]]), [], ["lnconst"])
                    dma("sync", lb[:], bass.AP(tensor=ln1_b.tensor, offset=ln1_b.offset + l * D, ap=[[0, 128], [1, D]]), [], ["lnconst"])
                    yaTs = sbt(ph, "yaTs", [128, 2, 512], BF16)
                    ybTs = sbt(ph, "ybTs", [128, 2, 512], BF16)
                    ycTs = sbt(ph, "ycTs", [128, 4, 512], BF16)
                    Gs = sbt(ph, "Gs", [128, 24, 512], BF16)
                    mT = sbt(ph, "mT", [128, 8, 512], BF16)
                    ta = sbt(ph, "ta", [128, 512], F32)
                    tb = sbt(ph, "tb", [128, 512], F32)
                    tcx = sbt(ph, "tcx", [128, 512], F32)
                    xr = Rot([sbt(ph, "xr%d" % i, [128, D], F32) for i in range(2)], "xr")
                    rr_ = sbt(ph, "rr_", [128, D], F32)
                    x1t = Rot([sbt(ph, "x1t%d" % i, [128, D], F32) for i in range(2)], "x1t")
                    h2f = sbt(ph, "h2f", [128, D], F32)
                    h2b = Rot([sbt(ph, "h2b%d" % i, [128, D], BF16) for i in range(2)], "h2b")
                    h2T = sbt(ph, "h2T", [128, 8, 128], BF16)
                    lntmp = {"stats": sbt(ph, "lnst", [128, 2, 6], F32), "mv": sbt(ph, "lnmv", [128, 2], F32),
                             "rstd": sbt(ph, "lnrs", [128, 1], F32)}
                    psa = pst(ph, "psa", [128, 512])
                    psb = pst(ph, "psb", [128, 512])
                    psc = pst(ph, "psc", [128, 512])
                    psO = Rot([pst(ph, "psO%d" % i, [128, 512]) for i in range(2)], "psO")
                    psh = pst(ph, "psh", [128, 8, 128], BF16)
                    psl = pst(ph, "psl", [128, 36])
                    pend_router = []
                    for (kind, b, tok0, Ls) in act_seqs:
                        mrow = b if kind == "lat" else NB
                        mo = MODS.offset + (l * (NB + 1) + mrow) * 6 * D
                        dma("sync", g1b[:], bass.AP(tensor=MODS.tensor, offset=mo + 2 * D, ap=[[0, 128], [1, D]]), ["MODS"], ["g1b"])
                        dma("sync", sh2b[:], bass.AP(tensor=MODS.tensor, offset=mo + 3 * D, ap=[[0, 128], [1, D]]), ["MODS"], ["sh2b"])
                        dma("sync", sc2b[:], bass.AP(tensor=MODS.tensor, offset=mo + 4 * D, ap=[[0, 128], [1, D]]), ["MODS"], ["sc2b"])
                        ts("gpsimd", sc2b[:], sc2b[:], 1.0, ALU.add, ["sc2b"], ["sc2b"])
                        CH = min(512, Ls)
                        for tc in range(Ls // CH):
                            t0 = tok0 + tc * CH
                            dma("sync", yaTs[:, :, 0:CH], YATd[:, :, t0:t0 + CH].rearrange("c p t -> p c t"), ["YATd"], ["yaTs"])
                            dma("gpsimd", ybTs[:, :, 0:CH], YBTd[:, :, t0:t0 + CH].rearrange("c p t -> p c t"), ["YBTd"], ["ybTs"])
                            dma("sync", ycTs[:, :, 0:CH], YCTd[:, :, t0:t0 + CH].rearrange("c p t -> p c t"), ["YCTd"], ["ycTs"])
                            dma("gpsimd", Gs[:, :, 0:CH], Gd[:, :, t0:t0 + CH].rearrange("c p t -> p c t"), ["Gd"], ["Gs"])
                            for dc in range(8):
                                ds_ = slice(dc * 128, (dc + 1) * 128)
                                for kt in range(2):
                                    mm(psa[:, 0:CH], pab[:, kt, ds_], yaTs[:, kt, 0:CH], kt == 0, kt == 1, ["mw", "yaTs"], ["psa"])
                                for kt in range(2):
                                    mm(psb[:, 0:CH], pbb[:, kt, ds_], ybTs[:, kt, 0:CH], kt == 0, kt == 1, ["mw", "ybTs"], ["psb"])
                                for kt in range(4):
                                    mm(psc[:, 0:CH], pcb[:, kt, ds_], ycTs[:, kt, 0:CH], kt == 0, kt == 3, ["mw", "ycTs"], ["psc"])
                                tt("vector", ta[:, 0:CH], psa[:, 0:CH], Gs[:, dc, 0:CH], ALU.mult, ["psa", "Gs"], ["ta"])
                                tt("vector", tb[:, 0:CH], psb[:, 0:CH], Gs[:, 8 + dc, 0:CH], ALU.mult, ["psb", "Gs"], ["tb"])
                                tt("vector", tcx[:, 0:CH], psc[:, 0:CH], Gs[:, 16 + dc, 0:CH], ALU.mult, ["psc", "Gs"], ["tcx"])
                                tt("gpsimd", ta[:, 0:CH], ta[:, 0:CH], tb[:, 0:CH], ALU.add, ["ta", "tb"], ["ta"])
                                tt("gpsimd", mT[:, dc, 0:CH], ta[:, 0:CH], tcx[:, 0:CH], ALU.add, ["ta", "tcx"], ["mT"])
                            for tsb in range(CH // 128):
                                tk0 = t0 + tsb * 128
                                gt = tk0 // 128
                                xt, xk = xr.next()
                                dma("sync", xt[:], Xsrc(tk0, 128), ["X2d"] if l else [], [xk])
                                for half in range(2):
                                    hs = slice(half * 512, (half + 1) * 512)
                                    pso, pok = psO.next()
                                    for kt in range(8):
                                        mm(pso[:], mT[:, kt, tsb * 128:(tsb + 1) * 128], wob[:, kt, hs], kt == 0, kt == 7, ["mw", "mT"], [pok])
                                    tt("vector", rr_[:, hs], pso[:], g1b[:, hs], ALU.mult, [pok, "g1b"], ["rr_"])
                                while pend_router:
                                    pend_router.pop(0)()
                                stt(rr_[:], xt[:], ALPHA, rr_[:], ALU.mult, ALU.add, [xk, "rr_"], ["rr_"])
                                x1, x1k = x1t.next()
                                layer_norm_rows("ln1", rr_, "rr_", lg, lb, x1, x1k, lntmp)
                                dma("sync", X1d[tk0:tk0 + 128, :], x1[:], [x1k], ["X1d"])
                                tt("vector", h2f[:], x1[:], sc2b[:], ALU.mult, [x1k, "sc2b"], ["h2f"])
                                hb_, hbk = h2b.next()
                                tt("gpsimd", hb_[:], h2f[:], sh2b[:], ALU.add, ["h2f", "sh2b"], [hbk])
                                dma("gpsimd", H2d[tk0:tk0 + 128, :], hb_[:], [hbk], ["H2d"])
                                def router(hb_=hb_, hbk=hbk, gt=gt):
                                    for kt in range(8):
                                        tr(psh[:, kt, :], hb_[:, kt * 128:(kt + 1) * 128], identb[:], [hbk, "identb"], ["psh"])
                                    cp("scalar", h2T[:], psh[:], ["psh"], ["h2T"])
                                    for kt in range(8):
                                        mm(psl[:], h2T[:, kt, :], wrb[:, kt, :], kt == 0, kt == 7, ["h2T", "mw"], ["psl"])
                                    tt("vector", LOG[:, gt, :], psl[:], rbb[:], ALU.add, ["psl", "mw"], ["LOG"])
                                pend_router.append(router)
                    while pend_router:
                        pend_router.pop(0)()
                    ph_end()

                with ExitStack() as ph:
                    NC_ = NTm * 32
                    cm = sbt(ph, "cm", [128, 128 + 8 + 4 + MMAX + NBLK + 32], F32)
                    dma("sync", cm[:], cmoe_in[:, :], [], ["cm"])
                    ustr = sbt(ph, "ustr", [128, 128], BF16)
                    cp("vector", ustr[:], cm[:, 0:128], ["cm"], ["ustr"])
                    o_kp8, o_kp4, o_mrow, o_nrow, o_i32 = 128, 136, 140, 140 + MMAX, 140 + MMAX + NBLK
                    onesb = sbt(ph, "onesb", [128, 1], BF16)
                    onesf = sbt(ph, "onesf", [32, 128], F32)
                    memset("vector", onesb[:], 1.0, ["onesb"])
                    memset("vector", onesf[:], 1.0, ["onesf"])
                    gmax = sbt(ph, "gmax", [128, NTm], F32)
                    goh = sbt(ph, "goh", [128, NTm, 4], F32)
                    gex = sbt(ph, "gex", [128, NTm, 4], F32)
                    gsum = sbt(ph, "gsum", [128, NTm], F32)
                    EM = sbt(ph, "EM", [128, NTm, 32], F32)
                    oh1 = sbt(ph, "oh1", [128, NTm, 32], F32)
                    oh2 = sbt(ph, "oh2", [128, NTm, 32], F32)
                    m1 = sbt(ph, "m1", [128, NTm], F32)
                    m2 = sbt(ph, "m2", [128, NTm], F32)
                    dd = sbt(ph, "dd", [128, NTm], F32)
                    Mb = sbt(ph, "Mb", [128, NTm, 32], BF16)
                    POS = sbt(ph, "POS", [128, NTm, 32], F32)
                    tmpP = sbt(ph, "tmpP", [128, NTm, 32], F32)
                    dstf = sbt(ph, "dstf", [128, NTm, 2], F32)
                    tot = sbt(ph, "tot", [32, NTm], F32)
                    cum = sbt(ph, "cum", [32, NTm], F32)
                    ones32 = sbt(ph, "ones32", [32, NTm], F32)
                    cnt = sbt(ph, "cnt", [32, 4], F32)
                    cmpm = sbt(ph, "cmpm", [32, MMAX], F32)
                    base = sbt(ph, "base", [32, NTm], F32)
                    R = sbt(ph, "R", [32, NTm, 32], F32)
                    dg32 = sbt(ph, "dg32", [32, 32], F32)
                    pendr = sbt(ph, "pendr", [128, 32], F32)
                    cmpb = sbt(ph, "cmpb", [128, NBLK, 32], F32)
                    be = sbt(ph, "be", [128, NBLK], F32)
                    idf = sbt(ph, "idf", [128, NBLK, 8], F32)
                    NPS = -(-NC_ // 512)
                    psR = pst(ph, "psR", [128, NPS * 512])
                    psTot = pst(ph, "psTot", [128, 512])
                    psSm = pst(ph, "psSm", [128, 512])
                    Lg = LOG[:, 0:NTm, :]
                    G4 = LOG[:, 0:NTm, 0:4]
                    E32 = LOG[:, 0:NTm, 4:36]

                    def bc_last(t2d, n):
                        a = t2d
                        return bass.AP(tensor=a.tensor, offset=a.offset, ap=[list(a.ap[0]), list(a.ap[1]), [0, n]])

                    red(gmax[:], G4, ALU.max, AX.X, ["LOG"], ["gmax"])
                    tt("vector", goh[:], G4, bc_last(gmax[:], 4), ALU.is_equal, ["LOG", "gmax"], ["goh"])
                    tt("vector", gex[:], G4, bc_last(gmax[:], 4), ALU.subtract, ["LOG", "gmax"], ["gex"])
                    act(gex[:], gex[:], AF.Exp, ["gex"], ["gex"])
                    red(gsum[:], gex[:], ALU.add, AX.X, ["gex"], ["gsum"])
                    p.add("vector", lambda e: e.reciprocal(out=gsum[:], in_=gsum[:]), ["gsum"], ["gsum"])
                    ts("vector", goh[:], goh[:], 1.0, ALU.subtract, ["goh"], ["goh"], s2=BIG, op1=ALU.mult)
                    gb = goh[:]
                    p.add("vector", lambda e: e.tensor_tensor(
                        out=bass.AP(tensor=EM[:].tensor, offset=EM[:].offset, ap=[list(EM[:].ap[0]), [32, NTm], [8, 4], [1, 8]]),
                        in0=bass.AP(tensor=LOG[:].tensor, offset=LOG[:].offset + 4, ap=[list(LOG[:].ap[0]), [36, NTm], [8, 4], [1, 8]]),
                        in1=bass.AP(tensor=gb.tensor, offset=gb.offset, ap=[list(gb.ap[0]), [4, NTm], [1, 4], [0, 8]]),
                        op=ALU.add), ["LOG", "goh"], ["EM"])
                    red(m1[:], EM[:], ALU.max, AX.X, ["EM"], ["m1"])
                    tt("vector", oh1[:], EM[:], bc_last(m1[:], 32), ALU.is_equal, ["EM", "m1"], ["oh1"])
                    stt(EM[:], oh1[:], -BIG, EM[:], ALU.mult, ALU.add, ["oh1", "EM"], ["EM"])
                    red(m2[:], EM[:], ALU.max, AX.X, ["EM"], ["m2"])
                    tt("vector", oh2[:], EM[:], bc_last(m2[:], 32), ALU.is_equal, ["EM", "m2"], ["oh2"])
                    tt("vector", dd[:], m2[:], m1[:], ALU.subtract, ["m1", "m2"], ["dd"])
                    act(dd[:], dd[:], AF.Exp, ["dd"], ["dd"])
                    ts("vector", dd[:], dd[:], 1.0, ALU.add, ["dd"], ["dd"])
                    p.add("vector", lambda e: e.reciprocal(out=dd[:], in_=dd[:]), ["dd"], ["dd"])
                    tt("vector", WTS[:, 0:NTm, 0], dd[:], gsum[:], ALU.mult, ["dd", "gsum"], ["WTS"])
                    tt("vector", WTS[:, 0:NTm, 1], gsum[:], WTS[:, 0:NTm, 0], ALU.subtract, ["gsum", "WTS"], ["WTS"])
                    tt("vector", Mb[:], oh1[:], oh2[:], ALU.add, ["oh1", "oh2"], ["Mb"])
                    Mf = Mb[:].rearrange("p t e -> p (t e)")
                    for c_ in range(NPS):
                        n_ = min(512, NC_ - c_ * 512)
                        mm(psR[:, c_ * 512:c_ * 512 + n_], ustr[:], Mf[:, c_ * 512:c_ * 512 + n_], True, True, ["ustr", "Mb"], ["psR"])
                    for t_ in range(NTm):
                        mm(psTot[0:32, t_:t_ + 1], Mb[:, t_, :], onesb[:], True, True, ["Mb", "onesb"], ["psTot"])
                    cp("vector", tot[:], psTot[0:32, 0:NTm], ["psTot"], ["tot"])
                    memset("vector", ones32[:], 1.0, ["ones32"])
                    p.add("vector", lambda e: e.tensor_tensor_scan(out=cum[:], data0=ones32[:], data1=tot[:], initial=0.0,
                                                                   op0=ALU.mult, op1=ALU.add), ["ones32", "tot"], ["cum"])
                    cp("vector", cnt[:, 0:1], cum[:, NTm - 1:NTm], ["cum"], ["cnt"])
                    ts("vector", cmpm[:], cm[0:32, o_mrow:o_mrow + MMAX], cnt[:, 0:1], ALU.is_lt, ["cm", "cnt"], ["cmpm"])
                    red(cnt[:, 1:2], cmpm[:], ALU.add, AX.X, ["cmpm"], ["cnt"])
                    ts("vector", cnt[:, 2:3], cnt[:, 1:2], float(BLK), ALU.mult, ["cnt"], ["cnt"])
                    mm(psSm[0:32, 0:1], cm[0:32, 0:32], cnt[:, 2:3], True, True, ["cm", "cnt"], ["psSm"])
                    tt("vector", cnt[:, 3:4], psSm[0:32, 0:1], cnt[:, 2:3], ALU.add, ["psSm", "cnt"], ["cnt"])
                    tt("vector", base[:], cum[:], tot[:], ALU.subtract, ["cum", "tot"], ["base"])
                    ts("vector", base[:], base[:], psSm[0:32, 0:1], ALU.add, ["base", "psSm"], ["base"])
                    bb = base[:]
                    i32 = cm[0:32, o_i32:o_i32 + 32]
                    p.add("vector", lambda e: e.tensor_tensor(
                        out=R[:], in0=bass.AP(tensor=bb.tensor, offset=bb.offset, ap=[list(bb.ap[0]), [1, NTm], [0, 32]]),
                        in1=bass.AP(tensor=i32.tensor, offset=i32.offset, ap=[list(i32.ap[0]), [0, NTm], [1, 32]]),
                        op=ALU.mult), ["base", "cm"], ["R"])
                    cp("vector", POS[:].rearrange("p t e -> p (t e)"), psR[:, 0:NC_], ["psR"], ["POS"])
                    Rf = R[:].rearrange("p t e -> p (t e)")
                    for c_ in range(NPS):
                        n_ = min(512, NC_ - c_ * 512)
                        mm(psR[:, c_ * 512:c_ * 512 + n_], onesf[:], Rf[:, c_ * 512:c_ * 512 + n_], True, True, ["onesf", "R", "POS"], ["psR"])
                    tt("vector", POS[:].rearrange("p t e -> p (t e)"), POS[:].rearrange("p t e -> p (t e)"), psR[:, 0:NC_],
                       ALU.add, ["psR", "POS"], ["POS"])
                    for k_, oh in enumerate((oh1, oh2)):
                        tt("vector", tmpP[:], oh[:], POS[:], ALU.mult, ["oh1", "oh2", "POS"], ["tmpP"])
                        red(dstf[:, :, k_], tmpP[:], ALU.add, AX.X, ["tmpP"], ["dstf"])
                    cp("vector", DEST[:, 0:NTm, :], dstf[:], ["dstf"], ["DEST"])
                    ts("vector", dg32[:], i32, cnt[:, 3:4], ALU.mult, ["cm", "cnt"], ["dg32"])
                    mm(psSm[:, 32:64], onesf[:], dg32[:], True, True, ["onesf", "dg32"], ["psSm"])
                    cp("vector", pendr[:], psSm[:, 32:64], ["psSm"], ["pendr"])
                    pr_ = pendr[:]
                    nrow = cm[:, o_nrow:o_nrow + NBLK]
                    p.add("vector", lambda e: e.tensor_tensor(
                        out=cmpb[:], in0=bass.AP(tensor=pr_.tensor, offset=pr_.offset, ap=[list(pr_.ap[0]), [0, NBLK], [1, 32]]),
                        in1=bass.AP(tensor=nrow.tensor, offset=nrow.offset, ap=[list(nrow.ap[0]), [1, NBLK], [0, 32]]),
                        op=ALU.is_le), ["pendr", "cm"], ["cmpb"])
                    red(be[:], cmpb[:], ALU.add, AX.X, ["cmpb"], ["be"])
                    ts("vector", be[:], be[:], 31.0, ALU.min, ["be"], ["be"], s2=float(l * NE), op1=ALU.add)
                    ts("vector", idf[:, :, 0], be[:], 128.0, ALU.mult, ["be", "idf"], ["idf"], s2=cm[:, o_kp8:o_kp8 + 1], op1=ALU.add)
                    cp("vector", IDXE[:], idf[:, :, 0], ["idf"], ["IDXE"])
                    ph_end()

                with ExitStack() as ph:
                    hrs = Rot([sbt(ph, "hr%d" % i, [128, D], BF16) for i in range(3)], "hr")
                    for t_ in range(NTm):
                        hr, hk = hrs.next()
                        dma("sync", hr[:], H2d[t_ * 128:(t_ + 1) * 128, :], ["H2d"], [hk])
                        for k_ in range(2):
                            p.add("gpsimd", lambda e, hr=hr, t_=t_, k_=k_: e.indirect_dma_start(
                                out=XBUF[:, :], out_offset=bass.IndirectOffsetOnAxis(ap=DEST[:, t_, k_:k_ + 1], axis=0),
                                in_=hr[:], in_offset=None), [hk, "DEST"], ["XBUF"], dma=True)
                    ph_end()

                with ExitStack() as ph:
                    wgf = sbt(ph, "wgf", [128, 8, HID], F32)
                    wuf = sbt(ph, "wuf", [128, 8, HID], F32)
                    wdf = sbt(ph, "wdf", [128, 4, D], F32)
                    wgbs = Rot([sbt(ph, "wgb%d" % i, [128, 8, HID], BF16) for i in range(2)], "wgb")
                    wubs = Rot([sbt(ph, "wub%d" % i, [128, 8, HID], BF16) for i in range(2)], "wub")
                    wdbs = Rot([sbt(ph, "wdb%d" % i, [128, 4, D], BF16) for i in range(2)], "wdb")
                    xrs = Rot([sbt(ph, "xb%d" % i, [128, RB, D], BF16) for i in range(2)], "xb")
                    XT = sbt(ph, "XT", [128, 8, BLK], BF16)
                    sgt = sbt(ph, "sgt", [128, BLK], F32)
                    aT = sbt(ph, "aT", [128, 4, BLK], BF16)
                    yos = Rot([sbt(ph, "yo%d" % i, [128, D], BF16) for i in range(2)], "yo")
                    psX = pst(ph, "psX", [128, 8, 128], BF16)
                    psG = Rot([pst(ph, "psG%d" % i, [128, BLK]) for i in range(2)], "psG")
                    psU = Rot([pst(ph, "psU%d" % i, [128, BLK]) for i in range(2)], "psU")
                    psD = Rot([pst(ph, "psD%d" % i, [128, 512]) for i in range(2)], "psD")
                    for n in range(NBLK):
                        for (wf_, src_, wk_) in ((wgf, ex_g8, "wgf"), (wuf, ex_u8, "wuf"), (wdf, ex_d4, "wdf")):
                            p.add("gpsimd", lambda e, n=n, wf_=wf_, src_=src_: e.indirect_dma_start(
                                out=wf_[:].rearrange("p a b -> p (a b)"), out_offset=None, in_=src_,
                                in_offset=bass.IndirectOffsetOnAxis(ap=IDXE[:, n:n + 1], axis=0)), ["IDXE"], [wk_], dma=True)
                        wgb, gk = wgbs.next()
                        wub, uk = wubs.next()
                        wdb, dk = wdbs.next()
                        cp("vector", wgb[:], wgf[:], ["wgf"], [gk])
                        cp("gpsimd", wub[:], wuf[:], ["wuf"], [uk])
                        cp("scalar", wdb[:], wdf[:], ["wdf"], [dk])
                        xb, xk = xrs.next()
                        dma("sync", xb[:], XBUF[n * BLK:(n + 1) * BLK, :].rearrange("(r p) d -> p r d", p=128), ["XBUF"], [xk])
                        for rb in range(RB):
                            for kt in range(8):
                                tr(psX[:, kt, :], _ap(xb[:], rb * D + kt, [[8, 128]]), identb[:], [xk, "identb"], ["psX"])
                            cp("scalar" if rb % 2 else "vector", XT[:, :, rb * 128:(rb + 1) * 128], psX[:], ["psX"], ["XT"])
                        for hc in range(4):
                            pg, pgk = psG.next()
                            pu, puk = psU.next()
                            for kt in range(8):
                                mm(pg[:], _ap(wgb[:], kt * HID + hc, [[4, 128]]), XT[:, kt, :], kt == 0, kt == 7, [gk, "XT"], [pgk])
                            for kt in range(8):
                                mm(pu[:], _ap(wub[:], kt * HID + hc, [[4, 128]]), XT[:, kt, :], kt == 0, kt == 7, [uk, "XT"], [puk])
                            act(sgt[:], pg[:], AF.Silu, [pgk], ["sgt"])
                            tt("vector", aT[:, hc, :], sgt[:], pu[:], ALU.mult, ["sgt", puk], ["aT"])
                        for rb in range(RB):
                            yo, yk = yos.next()
                            for half in range(2):
                                pd, pdk = psD.next()
                                for hc in range(4):
                                    mm(pd[:], aT[:, hc, rb * 128:(rb + 1) * 128], wdb[:, hc, half * 512:(half + 1) * 512],
                                       hc == 0, hc == 3, [dk, "aT"], [pdk])
                                cp("scalar" if half else "vector", yo[:, half * 512:(half + 1) * 512], pd[:], [pdk], [yk])
                            r0 = n * BLK + rb * 128
                            dma("sync", YBUF[r0:r0 + 128, :], yo[:], [yk], ["YBUF"])
                    ph_end()

                with ExitStack() as ph:
                    r0s = Rot([sbt(ph, "r0_%d" % i, [128, D], BF16) for i in range(2)], "r0_")
                    r1s = Rot([sbt(ph, "r1_%d" % i, [128, D], BF16) for i in range(2)], "r1_")
                    xr = Rot([sbt(ph, "xq%d" % i, [128, D], F32) for i in range(2)], "xq")
                    yf = sbt(ph, "yf", [128, D], F32)
                    rr_ = sbt(ph, "rr2", [128, D], F32)
                    xo = Rot([sbt(ph, "xo%d" % i, [128, D], F32) for i in range(2)], "xo")
                    lg = sbt(ph, "lg2", [128, D], F32)
                    lb = sbt(ph, "lb2", [128, D], F32)
                    g2b = sbt(ph, "g2b", [128, D], F32)
                    lntmp = {"stats": sbt(ph, "lnst2", [128, 2, 6], F32), "mv": sbt(ph, "lnmv2", [128, 2], F32),
                             "rstd": sbt(ph, "lnrs2", [128, 1], F32)}
                    dma("sync", lg[:], bass.AP(tensor=ln2_g.tensor, offset=ln2_g.offset + l * D, ap=[[0, 128], [1, D]]), [], ["lnconst"])
                    dma("sync", lb[:], bass.AP(tensor=ln2_b.tensor, offset=ln2_b.offset + l * D, ap=[[0, 128], [1, D]]), [], ["lnconst"])
                    for (kind, b, tok0, Ls) in act_seqs:
                        mrow = b if kind == "lat" else NB
                        mo = MODS.offset + (l * (NB + 1) + mrow) * 6 * D
                        dma("sync", g2b[:], bass.AP(tensor=MODS.tensor, offset=mo + 5 * D, ap=[[0, 128], [1, D]]), ["MODS"], ["g2b"])
                        for tI in range(Ls // 128):
                            tk0 = tok0 + tI * 128
                            gt = tk0 // 128
                            r0, r0k = r0s.next()
                            r1, r1k = r1s.next()
                            for (rt, rk, k_) in ((r0, r0k, 0), (r1, r1k, 1)):
                                p.add("gpsimd", lambda e, rt=rt, gt=gt, k_=k_: e.indirect_dma_start(
                                    out=rt[:], out_offset=None, in_=YBUF[:, :],
                                    in_offset=bass.IndirectOffsetOnAxis(ap=DEST[:, gt, k_:k_ + 1], axis=0)), ["YBUF", "DEST"], [rk], dma=True)
                            xt, xk = xr.next()
                            dma("sync", xt[:], X1d[tk0:tk0 + 128, :], ["X1d"], [xk])
                            ts("vector", yf[:], r0[:], WTS[:, gt, 0:1], ALU.mult, [r0k, "WTS"], ["yf"])
                            stt(yf[:], r1[:], WTS[:, gt, 1:2], yf[:], ALU.mult, ALU.add, [r1k, "WTS", "yf"], ["yf"])
                            tt("gpsimd", yf[:], yf[:], g2b[:], ALU.mult, ["yf", "g2b"], ["yf"])
                            stt(rr_[:], xt[:], ALPHA, yf[:], ALU.mult, ALU.add, [xk, "yf"], ["rr2"])
                            xo_, xok = xo.next()
                            layer_norm_rows("ln2", rr_, "rr2", lg, lb, xo_, xok, lntmp)
                            if last:
                                dma("sync", out[tk0:tk0 + 128, :], xo_[:], [xok], ["out"])
                            else:
                                dma("sync", X2d[tk0:tk0 + 128, :], xo_[:], [xok], ["X2d"])
                    ph_end(final=last)
    return nc


def _const_tables(cfg):
    L, LC, BLK, NB = cfg.L, cfg.LC, cfg.BLK, cfg.NB
    f32 = np.float32
    c = {}
    c["c_ident"] = np.eye(128, dtype=f32)
    n_freq = 16
    t = np.arange(L)
    pos = np.stack([(t // GRID_W).astype(f32), (t % GRID_W).astype(f32)], 0)
    inv = (10000.0 ** (-np.arange(n_freq, dtype=f32) / n_freq)).astype(f32)
    rope = np.zeros((2, 128, L), f32)
    for n in range(128):
        a, r, f = (n // 32) % 2, (n // 16) % 2, n % 16
        ang = (pos[a] * inv[f]).astype(f32)
        rope[0, n] = np.cos(ang)
        rope[1, n] = np.sin(ang) * (-1.0 if r == 0 else 1.0)
    c["c_rope"] = rope

    def hy_tables(Lf):
        tt_ = np.linspace(0.0, 1.0, Lf, dtype=f32)
        w = (2.0 * math.pi * np.arange(Lf, dtype=f32) / Lf).astype(f32)
        fb = np.linspace(1e-4, HY_BANDS - 1, HY_BANDS, dtype=f32)
        emb = np.concatenate([tt_[:, None], np.cos(fb[None, :] * w[:, None]), -np.sin(fb[None, :] * w[:, None])], -1).astype(f32)
        max_decay = math.log(1e-2) / 0.3
        min_decay = math.log(1e-2) / 1.5
        deltas = np.abs(np.linspace(min_decay, max_decay, 256, dtype=f32))
        window = (np.exp(-tt_[:, None] * deltas[None, :]) + 0.05).astype(f32)
        pf = np.arange(Lf - 1, -1, -1)
        pb = np.concatenate([np.arange(1, Lf), [0]])
        e = np.stack([emb[pf].T, emb[pb].T], 0).astype(f32)
        wn = np.stack([window[pf].T, window[pb].T], 0).astype(f32)
        wn[1, :, Lf - 1] = 0.0
        return np.ascontiguousarray(e), np.ascontiguousarray(wn)

    c["c_emb_L"], c["c_win_L"] = hy_tables(L)
    c["c_emb_C"], c["c_win_C"] = hy_tables(LC)
    T = NB * (L + LC)
    NBLK = -(-(2 * T) // BLK) + NE
    MMAX = -(-(2 * T) // BLK) + 1
    cm = np.zeros((128, 128 + 8 + 4 + MMAX + NBLK + 32), f32)
    pp = np.arange(128)
    cm[:, 0:128] = (pp[:, None] < pp[None, :]).astype(f32)
    cm[:, 128:136] = np.arange(8)[None, :] * 128 + pp[:, None]
    cm[:, 136:140] = np.arange(4)[None, :] * 128 + pp[:, None]
    cm[:, 140:140 + MMAX] = (np.arange(MMAX) * BLK)[None, :]
    cm[:, 140 + MMAX:140 + MMAX + NBLK] = (np.arange(NBLK) * BLK)[None, :]
    cm[0:32, 140 + MMAX + NBLK:] = np.eye(32, dtype=f32)
    c["c_moe"] = cm
    return c


def _core_inputs(cfg, inp, core, consts):
    NB, L, LC = cfg.NB, cfg.L, cfg.LC
    f32 = np.float32
    bs = slice(core * NB, (core + 1) * NB)
    m = dict(consts)
    m["x"] = np.ascontiguousarray(inp["x"][bs].reshape(NB * L, D))
    m["ctx"] = np.ascontiguousarray(inp["ctx"][bs].reshape(NB * LC, D))
    cc = np.concatenate([inp["c"][bs], inp["c_ctx"][None, :]], 0)
    m["cT"] = np.ascontiguousarray(cc.T.reshape(8, 128, NB + 1).transpose(1, 0, 2))
    for k in ("ada_w", "ada_b", "w_in", "gm_ln_g", "gm_ln_b", "hy_f_w1", "hy_f_w2", "hy_f_w3", "da_norm_g",
              "p_a", "p_b", "p_c", "w_out", "ln1_g", "ln1_b", "ln2_g", "ln2_b"):
        m[k] = inp[k]
    m["gm_wsT"] = np.ascontiguousarray(inp["gm_ws"].transpose(0, 3, 1, 2))
    m["gm_bsT"] = np.ascontiguousarray(inp["gm_bs"].transpose(0, 2, 1))
    cw = np.concatenate([inp["hy_conv_w"], inp["hy_conv_b"][:, None, :]], 1)
    m["hy_cw"] = np.ascontiguousarray(cw.reshape(DEPTH, 4, 6, 128).transpose(0, 3, 2, 1))
    m["hy_f_b1"] = np.ascontiguousarray(inp["hy_f_b1"][:, :, None])
    m["hy_f_b2"] = np.ascontiguousarray(inp["hy_f_b2"][:, :, None])
    m["hy_b3T"] = np.ascontiguousarray(inp["hy_f_b3"].reshape(DEPTH, 4, 128).transpose(0, 2, 1))
    m["hy_skipT"] = np.ascontiguousarray(inp["hy_skip"].reshape(DEPTH, 2, 128).transpose(0, 2, 1))
    m["da_l"] = np.ascontiguousarray(np.stack([inp["da_lq1"], inp["da_lk1"], inp["da_lq2"], inp["da_lk2"]], 1))
    m["moe_wr"] = np.ascontiguousarray(np.concatenate([inp["moe_wg"], inp["moe_we"]], -1))
    m["moe_br"] = np.ascontiguousarray(np.concatenate([inp["moe_bg"], inp["moe_be"]], -1))
    m["ex_w_gate"] = inp["ex_w_gate"].reshape(DEPTH * NE * D, HID)
    m["ex_w_up"] = inp["ex_w_up"].reshape(DEPTH * NE * D, HID)
    m["ex_w_down"] = inp["ex_w_down"].reshape(DEPTH * NE * HID, D)
    return {k: np.ascontiguousarray(np.asarray(v, dtype=f32)) for k, v in m.items()}


def kernel(**inputs):
    cfg = Cfg()
    inp = {k: np.asarray(v) for k, v in inputs.items()}
    n_cores = inp["x"].shape[0] // cfg.NB
    nc = build(cfg)
    consts = _const_tables(cfg)
    in_maps = [_core_inputs(cfg, inp, c, consts) for c in range(n_cores)]
    res = run_bass_kernel_spmd(nc, in_maps, core_ids=list(range(n_cores)))
    outs = [np.asarray(r["out"]).reshape(cfg.NB, cfg.L, D) for r in res.results]
    return np.concatenate(outs, 0).astype(np.float32)
```

```python
import math
from contextlib import ExitStack

import numpy as np
import concourse.bass as bass
import concourse.mybir as mybir
from concourse.bass_utils import run_bass_kernel_spmd

F32 = mybir.dt.float32
BF16 = mybir.dt.bfloat16
I32 = mybir.dt.int32
AF = mybir.ActivationFunctionType
ALU = mybir.AluOpType
AX = mybir.AxisListType

D = 1024
DEPTH = 2
GRID_W = 64
N_IN = 5888
OFF_HY, OFF_Q, OFF_K, OFF_V, OFF_GATE = 512, 1280, 1792, 2304, 2816
NWB = N_IN + 1024
HY_EMB, HY_FFN, HY_BANDS = 33, 64, 16
NE, NG, EPG, HID = 32, 4, 8, 512
ALPHA = (2.0 * DEPTH) ** 0.25
EPS = 1e-5
BIG = 1.0e30

ENGINES = ("tensor", "vector", "scalar", "gpsimd", "sync")
N_DMA_SEMS = 40


class Prog:
    def __init__(self, nc, stack):
        self.nc = nc
        self.ops = []
        self.esem = {e: stack.enter_context(nc.semaphore("s_" + e)) for e in ENGINES}
        self.dsem = [stack.enter_context(nc.semaphore("d%d" % i)) for i in range(N_DMA_SEMS)]
        self.ecount = {e: 0 for e in ENGINES}
        self.dcount = [0] * N_DMA_SEMS
        self.dnext = 0
        self.lastw = {}
        self.readers = {}
        self.known = {}
        self.nops = 0

    def add(self, eng, fn, r=(), w=(), dma=False):
        self.ops.append((eng, fn, tuple(r), tuple(w), dma))

    def flush(self, final=False):
        nc = self.nc
        esem, dsem, ecount, dcount = self.esem, self.dsem, self.ecount, self.dcount
        lastw, readers, known = self.lastw, self.readers, self.known
        plan = {e: [] for e in ENGINES}
        fence = [(("d", i), dcount[i]) for i in range(N_DMA_SEMS) if dcount[i] > 0]
        fence += [(("e", e), ecount[e]) for e in ENGINES if ecount[e] > 0]
        for (eng, fn, r, w, dma) in self.ops:
            deps = []
            for k in r:
                t = lastw.get(k)
                if t is not None:
                    deps.append(t)
            for k in w:
                t = lastw.get(k)
                if t is not None:
                    deps.append(t)
                for tk, tv in readers.get(k, {}).items():
                    deps.append((tk[0], tk[1], tv))
            if dma:
                si = self.dnext
                self.dnext = (self.dnext + 1) % N_DMA_SEMS
                if dcount[si] > 0:
                    deps.append(("d", si, dcount[si]))
                dcount[si] += 16
                tok = ("d", si, dcount[si])
                inc = (dsem[si], 16)
            else:
                ecount[eng] += 1
                tok = ("e", eng, ecount[eng])
                inc = (esem[eng], 1)
            waits = {}
            for (kind, key, val) in deps:
                if kind == "e" and key == eng and eng == "tensor":
                    continue
                sk = (kind, key)
                if known.get((eng, sk), 0) >= val:
                    continue
                if waits.get(sk, 0) < val:
                    waits[sk] = val
            wl = []
            for sk, val in waits.items():
                known[(eng, sk)] = val
                wl.append((esem[sk[1]] if sk[0] == "e" else dsem[sk[1]], val))
            plan[eng].append((wl, fn, inc))
            for k in r:
                d = readers.setdefault(k, {})
                if d.get(tok[:2], 0) < tok[2]:
                    d[tok[:2]] = tok[2]
            for k in w:
                lastw[k] = tok
                readers[k] = {}
        self.nops += len(self.ops)
        self.ops = []
        endw = []
        if final:
            endw = [(dsem[i], dcount[i]) for i in range(N_DMA_SEMS) if dcount[i] > 0]
            endw += [(esem[e], ecount[e]) for e in ENGINES if ecount[e] > 0]

        def runner(ename):
            def body(eng):
                for sk, val in fence:
                    if sk == ("e", ename):
                        continue
                    if known.get((ename, sk), 0) >= val:
                        continue
                    known[(ename, sk)] = val
                    eng.wait_ge(esem[sk[1]] if sk[0] == "e" else dsem[sk[1]], val)
                for (wl, fn, inc) in plan[ename]:
                    for (sem, val) in wl:
                        eng.wait_ge(sem, val)
                    ins = fn(eng)
                    ins.then_inc(inc[0], inc[1])
                for (sem, val) in endw:
                    eng.wait_ge(sem, val)
            return body

        with nc.Block() as block:
            block.tensor(runner("tensor"))
            block.vector(runner("vector"))
            block.scalar(runner("scalar"))
            block.gpsimd(runner("gpsimd"))
            block.sync(runner("sync"))


class Cfg:
    def __init__(self, NB=4, L=2048, LC=256, BLK=512, stop=None):
        self.NB, self.L, self.LC, self.BLK, self.stop = NB, L, LC, BLK, stop


class _Stop(Exception):
    pass


def _ap(base, off, dims, part=None):
    p = list(base.ap[0]) if part is None else [base.ap[0][0], part]
    return bass.AP(tensor=base.tensor, offset=base.offset + off, ap=[p] + [list(d) for d in dims])


class Rot:
    def __init__(self, tiles, name):
        self.tiles, self.name, self.i = tiles, name, 0

    def next(self):
        t = self.tiles[self.i % len(self.tiles)]
        k = "%s%d" % (self.name, self.i % len(self.tiles))
        self.i += 1
        return t, k


def build(cfg, debug=False):
    holder = {}
    try:
        _build(cfg, debug, holder)
    except _Stop:
        pass
    return holder["nc"]


def _build(cfg, debug, holder):
    NB, L, LC, BLK = cfg.NB, cfg.L, cfg.LC, cfg.BLK
    nc = bass.Bass("TRN2", target_bir_lowering=False)
    holder["nc"] = nc
    T = NB * (L + LC)
    NT = T // 128
    NTL = NB * L // 128
    RB = BLK // 128

    def din(name, shape, dt=F32):
        return nc.dram_tensor(name, list(shape), dt, kind="ExternalInput").ap()

    def dscr(name, shape, dt):
        return nc.dram_tensor(name, list(shape), dt, kind="ExternalOutput" if debug else "Internal").ap()

    x_in = din("x", [NB * L, D])
    ctx_in = din("ctx", [NB * LC, D])
    cT_in = din("cT", [128, 8, NB + 1])
    ada_w = din("ada_w", [DEPTH, D, 6 * D])
    ada_b = din("ada_b", [DEPTH, 6 * D])
    w_in = din("w_in", [DEPTH, D, N_IN])
    gm_ln_g = din("gm_ln_g", [DEPTH, 256])
    gm_ln_b = din("gm_ln_b", [DEPTH, 256])
    gm_wsT = din("gm_wsT", [DEPTH, 128, 4, 128])
    gm_bsT = din("gm_bsT", [DEPTH, 128, 4])
    hy_cw = din("hy_cw", [DEPTH, 128, 6, 4])
    hy_w1 = din("hy_f_w1", [DEPTH, HY_EMB, HY_FFN])
    hy_b1 = din("hy_f_b1", [DEPTH, HY_FFN, 1])
    hy_w2 = din("hy_f_w2", [DEPTH, HY_FFN, HY_FFN])
    hy_b2 = din("hy_f_b2", [DEPTH, HY_FFN, 1])
    hy_w3 = din("hy_f_w3", [DEPTH, HY_FFN, 512])
    hy_b3T = din("hy_b3T", [DEPTH, 128, 4])
    hy_skipT = din("hy_skipT", [DEPTH, 128, 2])
    da_l = din("da_l", [DEPTH, 4, 64])
    da_g = din("da_norm_g", [DEPTH, 128])
    p_a = din("p_a", [DEPTH, 256, D])
    p_b = din("p_b", [DEPTH, 256, D])
    p_c = din("p_c", [DEPTH, 512, D])
    w_out = din("w_out", [DEPTH, D, D])
    ln1_g = din("ln1_g", [DEPTH, D])
    ln1_b = din("ln1_b", [DEPTH, D])
    moe_wr = din("moe_wr", [DEPTH, D, 36])
    moe_br = din("moe_br", [DEPTH, 36])
    ex_g = din("ex_w_gate", [DEPTH * NE * D, HID])
    ex_u = din("ex_w_up", [DEPTH * NE * D, HID])
    ex_d = din("ex_w_down", [DEPTH * NE * HID, D])
    ex_g8 = ex_g.rearrange("(r j) h -> r (j h)", j=8)
    ex_u8 = ex_u.rearrange("(r j) h -> r (j h)", j=8)
    ex_d4 = ex_d.rearrange("(r j) d -> r (j d)", j=4)
    ln2_g = din("ln2_g", [DEPTH, D])
    ln2_b = din("ln2_b", [DEPTH, D])
    ident_in = din("c_ident", [128, 128])
    rope_in = din("c_rope", [2, 128, L])
    emb_in = {L: din("c_emb_L", [2, HY_EMB, L]), LC: din("c_emb_C", [2, HY_EMB, LC])}
    win_in = {L: din("c_win_L", [2, 256, L]), LC: din("c_win_C", [2, 256, LC])}
    NBLK = -(-(2 * T) // BLK) + NE
    MMAX = -(-(2 * T) // BLK) + 1
    cmoe_in = din("c_moe", [128, 128 + 8 + 4 + MMAX + NBLK + 32])
    out = nc.dram_tensor("out", [NB * L, D], F32, kind="ExternalOutput").ap()

    Wb = dscr("s_wb", [8, 128, NWB], BF16)
    MODS = dscr("s_mods", [DEPTH, NB + 1, 6 * D], F32)
    KREV = {L: dscr("s_krevL", [256, 2 * L], BF16), LC: dscr("s_krevC", [256, 2 * LC], BF16)}
    QTd = dscr("s_qt", [4, 128, T], BF16)
    KTd = dscr("s_kt", [4, 128, T], BF16)
    Vd = dscr("s_v", [T, 512], BF16)
    Gd = dscr("s_g", [24, 128, T], BF16)
    YATd = dscr("s_yat", [2, 128, T], BF16)
    X0Td = dscr("s_x0t", [2, 128, T], BF16)
    YBTd = dscr("s_ybt", [2, 128, T], BF16)
    YCTd = dscr("s_yct", [4, 128, T], BF16)
    X1d = dscr("s_x1", [T, D], F32)
    X2d = dscr("s_x2", [T, D], F32)
    H2d = dscr("s_h2", [T, D], BF16)
    XBUF = dscr("s_xbuf", [NBLK * BLK, D], BF16)
    YBUF = dscr("s_ybuf", [NBLK * BLK, D], BF16)

    seqs = [("lat", b, b * L, L) for b in range(NB)] + [("ctx", b, NB * L + b * LC, LC) for b in range(NB)]

    with ExitStack() as top:
        p = Prog(nc, top)

        uniq = [0]

        def sbt(st, name, shape, dt):
            uniq[0] += 1
            return st.enter_context(nc.sbuf_tensor("%s_%d" % (name, uniq[0]), list(shape), dt))

        def pst(st, name, shape, dt=F32):
            uniq[0] += 1
            return st.enter_context(nc.psum_tensor("%s_%d" % (name, uniq[0]), list(shape), dt))

        def dma(eng, out_, in_, r, w, **kw):
            p.add(eng, lambda e: e.dma_start(out=out_, in_=in_, **kw), r, w, dma=True)

        def mm(out_, lhsT, rhs, start, stop, r, w):
            p.add("tensor", lambda e: e.matmul(out_, lhsT=lhsT, rhs=rhs, start=start, stop=stop,
                                               skip_group_check=True), r, w)

        def tr(out_, in_, ident, r, w):
            p.add("tensor", lambda e: e.transpose(out=out_, in_=in_, identity=ident), r, w)

        def act(out_, in_, func, r, w, **kw):
            p.add("scalar", lambda e: e.activation(out=out_, in_=in_, func=func, **kw), r, w)

        def tt(eng, out_, a, b, op, r, w):
            p.add(eng, lambda e: e.tensor_tensor(out=out_, in0=a, in1=b, op=op), r, w)

        def ts(eng, out_, a, s1, op0, r, w, s2=None, op1=None):
            if op1 is None:
                p.add(eng, lambda e: e.tensor_scalar(out=out_, in0=a, scalar1=s1, scalar2=None, op0=op0), r, w)
            else:
                p.add(eng, lambda e: e.tensor_scalar(out=out_, in0=a, scalar1=s1, scalar2=s2, op0=op0, op1=op1), r, w)

        def stt(out_, a, s, b, op0, op1, r, w):
            p.add("vector", lambda e: e.scalar_tensor_tensor(out=out_, in0=a, scalar=s, in1=b, op0=op0, op1=op1), r, w)

        def cp(eng, out_, in_, r, w):
            if eng == "scalar":
                p.add(eng, lambda e: e.copy(out=out_, in_=in_), r, w)
            else:
                p.add(eng, lambda e: e.tensor_copy(out=out_, in_=in_), r, w)

        def red(out_, in_, op, axis, r, w):
            p.add("vector", lambda e: e.tensor_reduce(out=out_, in_=in_, axis=axis, op=op), r, w)

        def memset(eng, ap_, val, w):
            p.add(eng, lambda e: e.memset(ap_, val), (), w)

        phase_no = [0]

        def ph_end(final=False):
            phase_no[0] += 1
            stop = cfg.stop is not None and phase_no[0] >= cfg.stop
            p.flush(final=final or stop)
            if stop and not final:
                raise _Stop()

        identf = sbt(top, "identf", [128, 128], F32)
        identb = sbt(top, "identb", [128, 128], BF16)
        ropeT = sbt(top, "ropeT", [128, 2, L], BF16)
        epsc = sbt(top, "epsc", [128, 1], F32)
        dma("sync", identf[:], ident_in[:, :], [], ["identf"])
        cp("vector", identb[:], identf[:], ["identf"], ["identb"])
        with ExitStack() as ph:
            ropeF = sbt(ph, "ropeF", [128, 2, L], F32)
            dma("sync", ropeF[:, 0, :], rope_in[0, :, :], [], ["ropeF"])
            dma("sync", ropeF[:, 1, :], rope_in[1, :, :], [], ["ropeF"])
            cp("vector", ropeT[:], ropeF[:], ["ropeF"], ["ropeT"])
            memset("vector", epsc[:], EPS, ["epsc"])
            ph_end()

        def layer_norm_rows(st_tag, r_t, rk, g_b, b_b, out_t, ok, tmp):
            stats, mv, rstd = tmp["stats"], tmp["mv"], tmp["rstd"]
            for hh in range(2):
                p.add("vector", lambda e, hh=hh: e.bn_stats(out=stats[:, hh, :], in_=r_t[:, hh * 512:(hh + 1) * 512]),
                      [rk], [st_tag + "stats"])
            p.add("vector", lambda e: e.bn_aggr(out=mv[:], in_=stats[:].rearrange("p a b -> p (a b)")),
                  [st_tag + "stats"], [st_tag + "mv"])
            act(rstd[:], mv[:, 1:2], AF.Sqrt, [st_tag + "mv", "epsc"], [st_tag + "rstd"], bias=epsc[:], scale=1.0)
            p.add("vector", lambda e: e.reciprocal(out=rstd[:], in_=rstd[:]), [st_tag + "rstd"], [st_tag + "rstd"])
            ts("vector", out_t[:], r_t[:], mv[:, 0:1], ALU.subtract, [rk, st_tag + "mv", st_tag + "rstd"], [ok],
               s2=rstd[:, 0:1], op1=ALU.mult)
            tt("gpsimd", out_t[:], out_t[:], g_b[:], ALU.mult, [ok, "lnconst"], [ok])
            tt("gpsimd", out_t[:], out_t[:], b_b[:], ALU.add, [ok, "lnconst"], [ok])

        for l in range(DEPTH):
            last = l == DEPTH - 1
            lam_init = 0.8 - 0.6 * math.exp(-0.3 * l)
            Xsrc = (lambda tok0, n: (x_in[tok0:tok0 + n, :] if tok0 < NB * L else ctx_in[tok0 - NB * L:tok0 - NB * L + n, :])) \
                if l == 0 else (lambda tok0, n: X2d[tok0:tok0 + n, :])
            act_seqs = [s for s in seqs if not (last and s[0] == "ctx")]
            NTm = (NTL if last else NT)
            with ExitStack() as lay:
                lamt = sbt(lay, "lamt", [128, 4], F32)
                gsc = sbt(lay, "gsc", [128, 128], F32)

                with ExitStack() as ph:
                    wf = [sbt(ph, "wf%d" % i, [128, N_IN], F32) for i in range(2)]
                    wbt = [sbt(ph, "wbt%d" % i, [128, NWB], BF16) for i in range(2)]
                    for kt in range(8):
                        a, b_ = wf[kt % 2], wbt[kt % 2]
                        ka, kb = "wf%d" % (kt % 2), "wbt%d" % (kt % 2)
                        dma("sync", a[:], w_in[l, kt * 128:(kt + 1) * 128, :], [], [ka])
                        cp("vector", b_[:, 0:2048], a[:, 0:2048], [ka], [kb + "a"])
                        cp("gpsimd", b_[:, 2048:4096], a[:, 2048:4096], [ka], [kb + "b"])
                        cp("scalar", b_[:, 4096:N_IN], a[:, 4096:N_IN], [ka], [kb + "c"])
                        for qi, off in enumerate((OFF_Q, OFF_K)):
                            for rr in range(2):
                                o_ = _ap(b_[:], N_IN + qi * 512 + rr * 16, [[32, 16], [1, 16]])
                                i_ = _ap(a[:], off + (1 - rr) * 16, [[32, 16], [1, 16]])
                                cp("vector", o_, i_, [ka], [kb + "d%d%d" % (qi, rr)])
                        dma("sync", Wb[kt, :, :], b_[:], [kb + "a", kb + "b", kb + "c", kb + "d00", kb + "d01", kb + "d10", kb + "d11"], ["Wb"])
                    dl = sbt(ph, "dl", [128, 4, 64], F32)
                    dg = sbt(ph, "dg", [128, 128], F32)
                    pr = sbt(ph, "pr", [128, 2, 64], F32)
                    dma("sync", dl[:], bass.AP(tensor=da_l.tensor, offset=da_l.offset + l * 256, ap=[[0, 128], [64, 4], [1, 64]]), [], ["dl"])
                    dma("sync", dg[:], bass.AP(tensor=da_g.tensor, offset=da_g.offset + l * 128, ap=[[0, 128], [1, 128]]), [], ["dg"])
                    tt("vector", pr[:, 0, :], dl[:, 0, :], dl[:, 1, :], ALU.mult, ["dl"], ["pr"])
                    tt("vector", pr[:, 1, :], dl[:, 2, :], dl[:, 3, :], ALU.mult, ["dl"], ["pr"])
                    red(lamt[:, 1:3], pr[:], ALU.add, AX.X, ["pr"], ["lamt"])
                    act(lamt[:, 1:3], lamt[:, 1:3], AF.Exp, ["lamt"], ["lamt"])
                    tt("vector", lamt[:, 0:1], lamt[:, 2:3], lamt[:, 1:2], ALU.subtract, ["lamt"], ["lamt"])
                    ts("vector", lamt[:, 0:1], lamt[:, 0:1], -lam_init, ALU.add, ["lamt"], ["lamt"])
                    ts("vector", gsc[:], dg[:], 1.0 - lam_init, ALU.mult, ["dg"], ["gsc"])
                    ph_end()

                with ExitStack() as ph:
                    cTt = sbt(ph, "cTt", [128, 8, NB + 1], F32)
                    sct = sbt(ph, "sct", [128, 8, NB + 1], F32)
                    adb = sbt(ph, "adb", [NB + 1, 6 * D], F32)
                    modt = sbt(ph, "modt", [NB + 1, 6 * D], F32)
                    awt = [sbt(ph, "awt%d" % i, [128, 3072], F32) for i in range(2)]
                    psm = pst(ph, "psm", [128, 3072])
                    dma("sync", cTt[:], cT_in[:, :, :], [], ["cTt"])
                    dma("sync", adb[:], bass.AP(tensor=ada_b.tensor, offset=ada_b.offset + l * 6 * D,
                                                ap=[[0, NB + 1], [1, 6 * D]]), [], ["adb"])
                    act(sct[:], cTt[:], AF.Silu, ["cTt"], ["sct"])
                    i = 0
                    for half in range(2):
                        for kt in range(8):
                            a, ka = awt[i % 2], "awt%d" % (i % 2)
                            i += 1
                            dma("sync" if kt % 2 == 0 else "gpsimd", a[:],
                                ada_w[l, kt * 128:(kt + 1) * 128, half * 3072:(half + 1) * 3072], [], [ka])
                            for ng in range(6):
                                mm(psm[0:NB + 1, ng * 512:(ng + 1) * 512], sct[:, kt, :], a[:, ng * 512:(ng + 1) * 512],
                                   kt == 0, kt == 7, [ka, "sct"], ["psm"])
                        tt("vector", modt[:, half * 3072:(half + 1) * 3072], psm[0:NB + 1, :],
                           adb[:, half * 3072:(half + 1) * 3072], ALU.add, ["psm", "adb"], ["modt"])
                    dma("sync", MODS[l, :, :], modt[:], ["modt"], ["MODS"])
                    ph_end()

                with ExitStack() as ph:
                    w1f = sbt(ph, "w1f", [HY_EMB, HY_FFN], F32)
                    w2f = sbt(ph, "w2f", [HY_FFN, HY_FFN], F32)
                    w3f = sbt(ph, "w3f", [HY_FFN, 512], F32)
                    b1t = sbt(ph, "b1t", [HY_FFN, 1], F32)
                    b2t = sbt(ph, "b2t", [HY_FFN, 1], F32)
                    b3t = sbt(ph, "b3t", [128, 4], F32)
                    skt = sbt(ph, "skt", [128, 2], F32)
                    dma("sync", w1f[:], hy_w1[l, :, :], [], ["hyw"])
                    dma("sync", w2f[:], hy_w2[l, :, :], [], ["hyw"])
                    dma("sync", w3f[:], hy_w3[l, :, :], [], ["hyw"])
                    dma("sync", b1t[:], hy_b1[l, :, :], [], ["hyw"])
                    dma("sync", b2t[:], hy_b2[l, :, :], [], ["hyw"])
                    dma("sync", b3t[:], hy_b3T[l, :, :], [], ["hyw"])
                    dma("sync", skt[:], hy_skipT[l, :, :], [], ["hyw"])
                    ps1 = pst(ph, "ps1", [128, 512])
                    ps2 = pst(ph, "ps2", [128, 512])
                    ps3 = pst(ph, "ps3", [128, 512])
                    for Lf in ([L] if last else [L, LC]):
                        with ExitStack() as ph2:
                            CHF = min(512, Lf)
                            embt = sbt(ph2, "embt", [HY_EMB, 2, Lf], F32)
                            wint = sbt(ph2, "wint", [128, 2, 2, Lf], F32)
                            krf = sbt(ph2, "krf", [128, 2, 2 * Lf], F32)
                            krb = sbt(ph2, "krb", [128, 2, 2 * Lf], BF16)
                            h1 = sbt(ph2, "h1", [HY_FFN, 512], F32)
                            h2 = sbt(ph2, "h2", [HY_FFN, 512], F32)
                            wr1 = sbt(ph2, "wr1", [HY_FFN, 512], F32)
                            wr2 = sbt(ph2, "wr2", [HY_FFN, 512], F32)
                            tg = "f%d" % Lf
                            for dr in range(2):
                                dma("sync", embt[:, dr, :], emb_in[Lf][dr, :, :], [], [tg + "emb"])
                                for cc in range(2):
                                    dma("gpsimd", wint[:, dr, cc, :], win_in[Lf][dr, cc * 128:(cc + 1) * 128, :], [], [tg + "win"])
                            memset("gpsimd", krf[:], 0.0, [tg + "krf"])
                            for dr in range(2):
                                for ch in range(Lf // CHF):
                                    cs = slice(ch * CHF, (ch + 1) * CHF)
                                    mm(ps1[0:HY_FFN, 0:CHF], w1f[:], embt[:, dr, cs], True, True, ["hyw", tg + "emb"], ["ps1"])
                                    ts("vector", h1[:, 0:CHF], ps1[0:HY_FFN, 0:CHF], b1t[:, 0:1], ALU.add, ["ps1", "hyw"], ["h1"])
                                    ts("vector", wr1[:, 0:CHF], h1[:, 0:CHF], math.pi, ALU.is_gt, ["h1"], ["wr1"], s2=-2 * math.pi, op1=ALU.mult)
                                    ts("vector", wr2[:, 0:CHF], h1[:, 0:CHF], -math.pi, ALU.is_lt, ["h1"], ["wr2"], s2=2 * math.pi, op1=ALU.mult)
                                    tt("vector", h1[:, 0:CHF], h1[:, 0:CHF], wr1[:, 0:CHF], ALU.add, ["h1", "wr1"], ["h1"])
                                    tt("vector", h1[:, 0:CHF], h1[:, 0:CHF], wr2[:, 0:CHF], ALU.add, ["h1", "wr2"], ["h1"])
                                    act(h1[:, 0:CHF], h1[:, 0:CHF], AF.Sin, ["h1"], ["h1"])
                                    mm(ps2[0:HY_FFN, 0:CHF], w2f[:], h1[:, 0:CHF], True, True, ["hyw", "h1"], ["ps2"])
                                    ts("vector", h2[:, 0:CHF], ps2[0:HY_FFN, 0:CHF], b2t[:, 0:1], ALU.add, ["ps2", "hyw"], ["h2"])
                                    ts("vector", wr1[:, 0:CHF], h2[:, 0:CHF], math.pi, ALU.is_gt, ["h2"], ["wr1"], s2=-2 * math.pi, op1=ALU.mult)
                                    ts("vector", wr2[:, 0:CHF], h2[:, 0:CHF], -math.pi, ALU.is_lt, ["h2"], ["wr2"], s2=2 * math.pi, op1=ALU.mult)
                                    tt("vector", h2[:, 0:CHF], h2[:, 0:CHF], wr1[:, 0:CHF], ALU.add, ["h2", "wr1"], ["h2"])
                                    tt("vector", h2[:, 0:CHF], h2[:, 0:CHF], wr2[:, 0:CHF], ALU.add, ["h2", "wr2"], ["h2"])
                                    act(h2[:, 0:CHF], h2[:, 0:CHF], AF.Sin, ["h2"], ["h2"])
                                    for cc in range(2):
                                        mm(ps3[:, 0:CHF], w3f[:, dr * 256 + cc * 128:dr * 256 + (cc + 1) * 128], h2[:, 0:CHF],
                                           True, True, ["hyw", "h2"], ["ps3"])
                                        o0 = dr * Lf + ch * CHF
                                        stt(krf[:, cc, o0:o0 + CHF], ps3[:, 0:CHF], b3t[:, dr * 2 + cc:dr * 2 + cc + 1],
                                            wint[:, dr, cc, cs], ALU.add, ALU.mult, ["ps3", "hyw", tg + "win", tg + "krf"], [tg + "krf"])
                            for cc in range(2):
                                ts("vector", krf[:, cc, Lf - 1:Lf], krf[:, cc, Lf - 1:Lf], skt[:, cc:cc + 1], ALU.add,
                                   [tg + "krf", "hyw"], [tg + "krf"])
                            cp("vector", krb[:, 0, :], krf[:, 0, :], [tg + "krf"], [tg + "krb"])
                            cp("gpsimd", krb[:, 1, :], krf[:, 1, :], [tg + "krf"], [tg + "krb"])
                            for cc in range(2):
                                dma("sync", KREV[Lf][cc * 128:(cc + 1) * 128, :], krb[:, cc, :], [tg + "krb"], ["KREV%d" % Lf])
                            ph_end()

                with ExitStack() as ph45:
                    nbL, nbC = L // 128, LC // 128
                    VXs = {"lat": sbt(ph45, "VXsL", [128, 256, nbL, NB], BF16)}
                    if not last:
                        VXs["ctx"] = sbt(ph45, "VXsC", [128, 256, nbC, NB], BF16)
                    with ExitStack() as ph:
                        LMAX = L
                        hT = sbt(ph, "hT", [128, 8, LMAX], BF16)
                        zhs = Rot([sbt(ph, "zh%d" % i, [128, LMAX + 2], F32) for i in range(2)], "zh")
                        VT2 = sbt(ph, "VT2", [128, 2, LMAX], F32)
                        tmpA = sbt(ph, "tmpA", [128, LMAX], F32)
                        x0bs = Rot([sbt(ph, "x0b%d" % i, [128, LMAX], BF16) for i in range(2)], "x0b")
                        wgs = Rot([sbt(ph, "wg%d" % i, [128, 8, 512], BF16) for i in range(3)], "wg")
                        scb = sbt(ph, "scb", [128, D], F32)
                        shb = sbt(ph, "shb", [128, D], F32)
                        xts = Rot([sbt(ph, "xt%d" % i, [128, D], F32) for i in range(2)], "xt")
                        hbs = Rot([sbt(ph, "hb%d" % i, [128, D], BF16) for i in range(2)], "hb")
                        lngb = sbt(ph, "lngb", [128, 256], F32)
                        lnbb = sbt(ph, "lnbb", [128, 256], F32)
                        wsf = sbt(ph, "wsf", [128, 4, 128], F32)
                        wsb = sbt(ph, "wsb", [128, 4, 128], BF16)
                        bst = sbt(ph, "bst", [128, 4], F32)
                        cwt = sbt(ph, "cwt", [128, 6, 4], F32)
                        gmf = sbt(ph, "gmf", [128, 512], F32)
                        vnb = sbt(ph, "vnb", [128, 256], BF16)
                        vnf = sbt(ph, "vnf", [128, 256], F32)
                        yab = sbt(ph, "yab", [128, 256], BF16)
                        yaT = sbt(ph, "yaT", [128, 2, 128], BF16)
                        gst = sbt(ph, "gst", [128, 6], F32)
                        gmv = sbt(ph, "gmv", [128, 2], F32)
                        grs = sbt(ph, "grs", [128, 1], F32)
                        vts = Rot([sbt(ph, "vt%d" % i, [128, 512], BF16) for i in range(2)], "vt")
                        fos = Rot([sbt(ph, "fo%d" % i, [128, 512], BF16) for i in range(3)], "fo")
                        t1s = Rot([sbt(ph, "t1_%d" % i, [128, 512], F32) for i in range(2)], "t1_")
                        t2s = Rot([sbt(ph, "t2_%d" % i, [128, 512], F32) for i in range(2)], "t2_")
                        VXT = sbt(ph, "VXT", [128, 2, LMAX], BF16)
                        psH = pst(ph, "psH", [128, 8, 128], BF16)
                        psFs = Rot([pst(ph, "psF%d" % i, [128, 512]) for i in range(2)], "psF")
                        psPs = Rot([pst(ph, "psP%d" % i, [128, 512]) for i in range(2)], "psP")
                        psT = pst(ph, "psT", [128, 512])
                        psS = pst(ph, "psS", [128, 256])
                        psYA = pst(ph, "psYA", [128, 2, 128], BF16)
                        dma("sync", lngb[:], bass.AP(tensor=gm_ln_g.tensor, offset=gm_ln_g.offset + l * 256, ap=[[0, 128], [1, 256]]), [], ["gmc"])
                        dma("sync", lnbb[:], bass.AP(tensor=gm_ln_b.tensor, offset=gm_ln_b.offset + l * 256, ap=[[0, 128], [1, 256]]), [], ["gmc"])
                        dma("sync", wsf[:], gm_wsT[l, :, :, :], [], ["wsf"])
                        cp("vector", wsb[:], wsf[:], ["wsf"], ["gmc"])
                        dma("sync", bst[:], gm_bsT[l, :, :], [], ["gmc"])
                        dma("sync", cwt[:], hy_cw[l, :, :, :], [], ["gmc"])
                        for zt in zhs.tiles:
                            memset("vector", zt[:, 0:1], 0.0, ["zh0", "zh1"])

                        for (kind, b, tok0, Ls) in seqs:
                            full = not (last and kind == "ctx")
                            CH = min(512, Ls)
                            nch = Ls // CH
                            ntile = Ls // 128
                            mrow = b if kind == "lat" else NB
                            rope = kind == "lat"
                            dma("sync", shb[:], bass.AP(tensor=MODS.tensor, offset=MODS.offset + (l * (NB + 1) + mrow) * 6 * D,
                                                        ap=[[0, 128], [1, D]]), ["MODS"], ["shb"])
                            dma("sync", scb[:], bass.AP(tensor=MODS.tensor, offset=MODS.offset + (l * (NB + 1) + mrow) * 6 * D + D,
                                                        ap=[[0, 128], [1, D]]), ["MODS"], ["scb"])
                            ts("gpsimd", scb[:], scb[:], 1.0, ALU.add, ["scb"], ["scb"])
                            for zt in zhs.tiles:
                                memset("vector", zt[:, Ls + 1:Ls + 2], 0.0, ["zh0", "zh1"])
                            for tI in range(ntile):
                                xt, xk = xts.next()
                                hb, hk = hbs.next()
                                dma("sync" if tI % 2 == 0 else "gpsimd", xt[:], Xsrc(tok0 + tI * 128, 128), ["X2d"] if l else [], [xk])
                                tt("vector", xt[:], xt[:], scb[:], ALU.mult, [xk, "scb"], [xk])
                                tt("gpsimd", hb[:], xt[:], shb[:], ALU.add, [xk, "shb"], [hk])
                                for kt in range(8):
                                    tr(psH[:, kt, :], hb[:, kt * 128:(kt + 1) * 128], identb[:], [hk, "identb"], ["psH"])
                                cp("scalar", hT[:, :, tI * 128:(tI + 1) * 128], psH[:], ["psH"], ["hT"])

                            def load_group(col0):
                                wg, wk = wgs.next()
                                dma("sync", wg[:], Wb[:, :, col0:col0 + 512].rearrange("k p c -> p k c"), ["Wb"], [wk])
                                return wg, wk

                            def fm_chunk(wg, wk, ci, tc, ps, pk):
                                for kt in range(8):
                                    mm(ps[:, 0:CH], wg[:, kt, ci * 128:(ci + 1) * 128], hT[:, kt, tc * CH:(tc + 1) * CH],
                                       kt == 0, kt == 7, [wk, "hT"], [pk])

                            if full:
                                wg, wk = load_group(0)
                                for tI in range(ntile):
                                    for kt in range(8):
                                        mm(psT[:], hT[:, kt, tI * 128:(tI + 1) * 128], wg[:, kt, :], kt == 0, kt == 7, [wk, "hT"], ["psT"])
                                    act(gmf[:], psT[:], AF.Gelu, ["psT"], ["gmf"])
                                    p.add("vector", lambda e: e.bn_stats(out=gst[:], in_=gmf[:, 256:512]), ["gmf"], ["gst"])
                                    p.add("vector", lambda e: e.bn_aggr(out=gmv[:], in_=gst[:]), ["gst"], ["gmv"])
                                    act(grs[:], gmv[:, 1:2], AF.Sqrt, ["gmv", "epsc"], ["grs"], bias=epsc[:], scale=1.0)
                                    p.add("vector", lambda e: e.reciprocal(out=grs[:], in_=grs[:]), ["grs"], ["grs"])
                                    ts("vector", vnf[:], gmf[:, 256:512], gmv[:, 0:1], ALU.subtract, ["gmf", "gmv", "grs"], ["vnf"],
                                       s2=grs[:, 0:1], op1=ALU.mult)
                                    tt("gpsimd", vnf[:], vnf[:], lngb[:], ALU.mult, ["vnf", "gmc"], ["vnf"])
                                    tt("gpsimd", vnb[:], vnf[:], lnbb[:], ALU.add, ["vnf", "gmc"], ["vnb"])
                                    for g in range(4):
                                        mm(psS[:, g * 64:(g + 1) * 64], wsb[:, g, :], vnb[:, g * 64:(g + 1) * 64], True, True, ["gmc", "vnb"], ["psS"])
                                    for g in range(4):
                                        stt(yab[:, g * 64:(g + 1) * 64], psS[:, g * 64:(g + 1) * 64], bst[:, g:g + 1],
                                            gmf[:, g * 64:(g + 1) * 64], ALU.add, ALU.mult, ["psS", "gmc", "gmf"], ["yab"])
                                    for cc in range(2):
                                        tr(psYA[:, cc, :], yab[:, cc * 128:(cc + 1) * 128], identb[:], ["yab", "identb"], ["psYA"])
                                    cp("scalar", yaT[:], psYA[:], ["psYA"], ["yaT"])
                                    t0 = tok0 + tI * 128
                                    dma("gpsimd", YATd[:, :, t0:t0 + 128].rearrange("c p t -> p c t"), yaT[:], ["yaT"], ["YATd"])
                                for (col0, cis) in ((OFF_HY + 512, (1, 0)), (OFF_HY, (3, 2, 1, 0))):
                                    wg, wk = load_group(col0)
                                    for ci in cis:
                                        r_ = (col0 - OFF_HY) // 128 + ci
                                        zh, zk = zhs.next()
                                        for tc in range(nch):
                                            ps, pk = psFs.next()
                                            fm_chunk(wg, wk, ci, tc, ps, pk)
                                            cp("scalar", zh[:, 1 + tc * CH:1 + (tc + 1) * CH], ps[:, 0:CH], [pk], [zk])
                                        if r_ >= 4:
                                            acc, ak = VT2[:, r_ - 4, 0:Ls], "VT2"
                                        else:
                                            acc, ak = tmpA[:, 0:Ls], "tmpA"
                                        ts("vector", acc, zh[:, 0:Ls], cwt[:, r_, 0:1], ALU.mult, [zk, "gmc"], [ak],
                                           s2=cwt[:, r_, 3:4], op1=ALU.add)
                                        stt(acc, zh[:, 1:Ls + 1], cwt[:, r_, 1:2], acc, ALU.mult, ALU.add, [zk, "gmc", ak], [ak])
                                        if r_ >= 4:
                                            stt(acc, zh[:, 2:Ls + 2], cwt[:, r_, 2:3], acc, ALU.mult, ALU.add, [zk, "gmc", ak], [ak])
                                        elif r_ >= 2:
                                            stt(acc, zh[:, 2:Ls + 2], cwt[:, r_, 2:3], acc, ALU.mult, ALU.add, [zk, "gmc", ak], [ak])
                                            tt("gpsimd", VXT[:, r_ - 2, 0:Ls], acc, VT2[:, r_ - 2, 0:Ls], ALU.mult, [ak, "VT2"], ["VXT"])
                                        else:
                                            x0b, x0k = x0bs.next()
                                            stt(x0b[:, 0:Ls], zh[:, 2:Ls + 2], cwt[:, r_, 2:3], acc, ALU.mult, ALU.add, [zk, "gmc", ak], [x0k])
                                            dma("gpsimd", X0Td[r_, :, tok0:tok0 + Ls], x0b[:, 0:Ls], [x0k], ["X0Td"])
                                vx = VXs[kind]
                                for tI in range(ntile):
                                    for cc in range(2):
                                        tr(psYA[:, cc, :], VXT[:, cc, tI * 128:(tI + 1) * 128], identb[:], ["VXT", "identb"], ["psYA"])
                                    cp("scalar", vx[:, :, tI, b], psYA[:].rearrange("p a b -> p (a b)"), ["psYA"], ["VXs" + kind])
                            for (nm, colA, colB, dst) in (("q", OFF_Q, N_IN, QTd), ("k", OFF_K, N_IN + 512, KTd)):
                                if nm == "q" and not full:
                                    continue
                                wgA, wkA = load_group(colA)
                                if rope:
                                    wgB, wkB = load_group(colB)
                                for h in range(4):
                                    for tc in range(nch):
                                        ps, pk = psFs.next()
                                        fm_chunk(wgA, wkA, h, tc, ps, pk)
                                        fo, fk = fos.next()
                                        if rope:
                                            ps2_, pk2 = psPs.next()
                                            fm_chunk(wgB, wkB, h, tc, ps2_, pk2)
                                            t1, k1 = t1s.next()
                                            t2, k2 = t2s.next()
                                            tt("vector", t1[:, 0:CH], ps[:, 0:CH], ropeT[:, 0, tc * CH:(tc + 1) * CH], ALU.mult, [pk, "ropeT"], [k1])
                                            tt("vector", t2[:, 0:CH], ps2_[:, 0:CH], ropeT[:, 1, tc * CH:(tc + 1) * CH], ALU.mult, [pk2, "ropeT"], [k2])
                                            tt("gpsimd", fo[:, 0:CH], t1[:, 0:CH], t2[:, 0:CH], ALU.add, [k1, k2], [fk])
                                        else:
                                            cp("scalar", fo[:, 0:CH], ps[:, 0:CH], [pk], [fk])
                                        t0 = tok0 + tc * CH
                                        dma("gpsimd", dst[h, :, t0:t0 + CH], fo[:, 0:CH], [fk], [nm + "Td"])
                            wg, wk = load_group(OFF_V)
                            for tI in range(ntile):
                                for kt in range(8):
                                    mm(psT[:], hT[:, kt, tI * 128:(tI + 1) * 128], wg[:, kt, :], kt == 0, kt == 7, [wk, "hT"], ["psT"])
                                vt, vk = vts.next()
                                cp("scalar", vt[:], psT[:], ["psT"], [vk])
                                t0 = tok0 + tI * 128
                                dma("gpsimd", Vd[t0:t0 + 128, :], vt[:], [vk], ["Vd"])
                            if full:
                                for gi in range(6):
                                    wg, wk = load_group(OFF_GATE + gi * 512)
                                    for ci in range(4):
                                        for tc in range(nch):
                                            ps, pk = psFs.next()
                                            fm_chunk(wg, wk, ci, tc, ps, pk)
                                            fo, fk = fos.next()
                                            act(fo[:, 0:CH], ps[:, 0:CH], AF.Sigmoid, [pk], [fk])
                                            t0 = tok0 + tc * CH
                                            dma("gpsimd", Gd[gi * 4 + ci, :, t0:t0 + CH], fo[:, 0:CH], [fk], ["Gd"])
                        ph_end()

                    with ExitStack() as ph:
                        hsk = Rot([sbt(ph, "hsk%d" % i, [128, 128 * (2 * nbL - 1)], BF16) for i in range(3)], "hsk")
                        psYs = Rot([pst(ph, "psY%d" % i, [128, 8, nbL * NB]) for i in range(2)], "psY")
                        psTt = pst(ph, "psTt", [128, 2, 128], BF16)
                        YBT = sbt(ph, "YBT", [128, 2, T], BF16)
                        x0ls = Rot([sbt(ph, "x0l%d" % i, [128, 2, L], BF16) for i in range(2)], "x0l")
                        for kind in (["lat"] if last else ["lat", "ctx"]):
                            Lf = L if kind == "lat" else LC
                            nb = Lf // 128
                            W = 128 * (2 * nb - 1)
                            with ExitStack() as ph2:
                                Yr = sbt(ph2, "Yr" + kind, [128, nb, NB, 256], BF16)
                                vx = VXs[kind]
                                kdr = KREV[Lf]
                                for cg in range(32):
                                    psY, pyk = psYs.next()
                                    for c8 in range(8):
                                        c = cg * 8 + c8
                                        hk_, hkk = hsk.next()
                                        dma("sync" if c % 2 == 0 else "gpsimd", hk_[:, 0:W],
                                            bass.AP(tensor=kdr.tensor, offset=kdr.offset + c * 2 * Lf, ap=[[1, 128], [1, W]]),
                                            ["KREV%d" % Lf], [hkk])
                                        lags = [0] + [d for d in range(-(nb - 1), nb) if d != 0]
                                        for di, d in enumerate(lags):
                                            j0, j1 = max(0, -d), min(nb, nb - d)
                                            m0 = 128 * (nb - 1 - d)
                                            o_ = _ap(psY[:], c8 * nbL * NB + (j0 + d) * NB, [[1, (j1 - j0) * NB]])
                                            mm(o_, hk_[:, m0:m0 + 128], vx[:, c, j0:j1, :].rearrange("p j b -> p (j b)"),
                                               di == 0, di == len(lags) - 1, [hkk, "VXs" + kind], [pyk])
                                    src = _ap(psY[:], 0, [[nbL * NB, 8], [NB, nb], [1, NB]])
                                    dst_ = _ap(Yr[:], cg * 8, [[1, 8], [NB * 256, nb], [256, NB]])
                                    cp("scalar" if cg % 2 == 0 else "vector", dst_, src, [pyk], ["Yr"])
                                for b in range(NB):
                                    tok0 = b * L if kind == "lat" else NB * L + b * LC
                                    x0l, x0lk = x0ls.next()
                                    dma("sync", x0l[:, :, 0:Lf], X0Td[:, :, tok0:tok0 + Lf].rearrange("c p t -> p c t"), ["X0Td"], [x0lk])
                                    for i_ in range(nb):
                                        for cc in range(2):
                                            tr(psTt[:, cc, :], Yr[:, i_, b, cc * 128:(cc + 1) * 128], identb[:], ["Yr", "identb"], ["psTt"])
                                        t0 = tok0 + i_ * 128
                                        for cc in range(2):
                                            rev = _ap(psTt[:], cc * 128 + 127, [[-1, 128]])
                                            tt("vector", YBT[:, cc, t0:t0 + 128], rev, x0l[:, cc, i_ * 128:(i_ + 1) * 128], ALU.mult,
                                               ["psTt", x0lk], ["YBT"])
                        ntok = NB * L if last else T
                        for cc in range(2):
                            dma("sync", YBTd[cc, :, 0:ntok], YBT[:, cc, 0:ntok], ["YBT"], ["YBTd"])
                        ph_end()

                with ExitStack() as ph:
                    NKmax = (L + LC) // 128
                    QT = sbt(ph, "QT", [128, 4, 2, L], BF16)
                    KT = sbt(ph, "KT", [128, 4, L + LC], BF16)
                    Vone = sbt(ph, "Vone", [128, NKmax, 4, 129], BF16)
                    Es = Rot([sbt(ph, "E%d" % i, [128, 512], BF16) for i in range(3)], "E")
                    psSs = Rot([pst(ph, "psA%d" % i, [128, 512]) for i in range(2)], "psA")
                    acc = pst(ph, "acc", [128, 4, 2, 256])
                    psC = pst(ph, "psC", [128, 4, 128], BF16)
                    rec = sbt(ph, "rec", [128, 4, 2], F32)
                    o1 = sbt(ph, "o1", [128, 128], F32)
                    o2 = sbt(ph, "o2", [128, 128], F32)
                    sq = sbt(ph, "sq", [128, 128], F32)
                    ss = sbt(ph, "ss", [128, 1], F32)
                    YC = sbt(ph, "YC", [128, 4, 512], BF16)
                    ycts = Rot([sbt(ph, "yct%d" % i, [128, 4, 512], BF16) for i in range(2)], "yct")
                    memset("vector", Vone[:], 1.0, ["Vone"])
                    memset("gpsimd", QT[:], 0.0, ["QT"])
                    for (kind, b, tok0, Lq) in act_seqs:
                        if kind == "lat":
                            ksegs = [(tok0, L), (NB * L + b * LC, LC)]
                        else:
                            ksegs = [(tok0, LC)]
                        NK = sum(s[1] for s in ksegs) // 128
                        for m in range(2):
                            dma("sync", QT[m * 64:(m + 1) * 64, :, m, 0:Lq],
                                QTd[:, m * 64:(m + 1) * 64, tok0:tok0 + Lq].rearrange("h p t -> p h t"), ["qTd"], ["QT"])
                        ko = 0
                        for (kt0, kl) in ksegs:
                            dma("gpsimd", KT[:, :, ko:ko + kl], KTd[:, :, kt0:kt0 + kl].rearrange("h p t -> p h t"), ["kTd"], ["KT"])
                            for j in range(kl // 128):
                                dma("sync" if j % 2 else "gpsimd", Vone[:, ko // 128 + j, :, 0:128],
                                    Vd[kt0 + j * 128:kt0 + (j + 1) * 128, :].rearrange("t (h e) -> t h e", h=4), ["Vd"], ["Vone"])
                            ko += kl
                        QC = min(512, Lq)
                        nsub = QC // 128
                        for qc in range(Lq // QC):
                            steps = [(h, m, kt) for h in range(4) for m in range(2) for kt in range(NK)]

                            def emit_S(st_):
                                h, m, kt = st_
                                ms = slice(m * 64, (m + 1) * 64)
                                psA, pak = psSs.next()
                                mm(psA[:, 0:QC], KT[:, h, kt * 128:(kt + 1) * 128], QT[:, h, m, qc * QC:(qc + 1) * QC],
                                   True, True, ["KT", "QT"], [pak])
                                return psA, pak

                            nxt = emit_S(steps[0])
                            for si, (h, m, kt) in enumerate(steps):
                                psA, pak = nxt
                                E, ek = Es.next()
                                act(E[:, 0:QC], psA[:, 0:QC], AF.Exp, [pak], [ek], scale=0.125)
                                if si + 1 < len(steps):
                                    nxt = emit_S(steps[si + 1])
                                for qs in range(nsub):
                                    mm(acc[:, qs, m, 0:129], E[:, qs * 128:(qs + 1) * 128], Vone[:, kt, h, :],
                                       kt == 0, kt == NK - 1, [ek, "Vone"], ["acc"])
                                if not (m == 1 and kt == NK - 1):
                                    continue
                                p.add("vector", lambda e, nsub=nsub: e.reciprocal(out=rec[:, 0:nsub, :], in_=acc[:, 0:nsub, :, 128]), ["acc"], ["rec"])
                                ts("vector", rec[:, 0:nsub, 1], rec[:, 0:nsub, 1], lamt[:, 0:1], ALU.mult, ["rec", "lamt"], ["rec"])
                                for qs in range(nsub):
                                    ts("vector", o1[:], acc[:, qs, 0, 0:128], rec[:, qs, 0:1], ALU.mult, ["acc", "rec"], ["o1"])
                                    stt(o2[:], acc[:, qs, 1, 0:128], rec[:, qs, 1:2], o1[:], ALU.mult, ALU.add, ["acc", "rec", "o1"], ["o2"])
                                    tt("gpsimd", sq[:], o2[:], o2[:], ALU.mult, ["o2"], ["sq"])
                                    red(ss[:], sq[:], ALU.add, AX.X, ["sq"], ["ss"])
                                    act(ss[:], ss[:], AF.Sqrt, ["ss", "epsc"], ["ss"], bias=epsc[:], scale=1.0 / 128.0)
                                    p.add("vector", lambda e: e.reciprocal(out=ss[:], in_=ss[:]), ["ss"], ["ss"])
                                    stt(YC[:, qs, h * 128:(h + 1) * 128], o2[:], ss[:, 0:1], gsc[:], ALU.mult, ALU.mult,
                                        ["o2", "ss", "gsc"], ["YC"])
                            yct, yk = ycts.next()
                            for qs in range(nsub):
                                for h in range(4):
                                    tr(psC[:, h, :], YC[:, qs, h * 128:(h + 1) * 128], identb[:], ["YC", "identb"], ["psC"])
                                cp("scalar", yct[:, :, qs * 128:(qs + 1) * 128], psC[:], ["psC"], [yk])
                            t0 = tok0 + qc * QC
                            dma("sync", YCTd[:, :, t0:t0 + QC].rearrange("h p t -> p h t"), yct[:, :, 0:QC], [yk], ["YCTd"])
                    ph_end()

                LOG = sbt(lay, "LOG", [128, NT, 36], F32)
                WTS = sbt(lay, "WTS", [128, NT, 2], F32)
                DEST = sbt(lay, "DEST", [128, NT, 2], I32)
                IDXE = sbt(lay, "IDXE", [128, NBLK], I32)
                with ExitStack() as ph:
                    pab = sbt(ph, "pab", [128, 2, D], BF16)
                    pbb = sbt(ph, "pbb", [128, 2, D], BF16)
                    pcb = sbt(ph, "pcb", [128, 4, D], BF16)
                    wob = sbt(ph, "wob", [128, 8, D], BF16)
                    wrb = sbt(ph, "wrb", [128, 8, 36], BF16)
                    wrf = sbt(ph, "wrf", [128, 8, 36], F32)
                    rbb = sbt(ph, "rbb", [128, 36], F32)
                    stg = Rot([sbt(ph, "stg%d" % i, [128, 2, D], F32) for i in range(2)], "stg")
                    lg = sbt(ph, "lg", [128, D], F32)
                    lb = sbt(ph, "lb", [128, D], F32)
                    g1b = sbt(ph, "g1b", [128, D], F32)
                    sc2b = sbt(ph, "sc2b", [128, D], F32)
                    sh2b = sbt(ph, "sh2b", [128, D], F32)
                    si = 0
                    for (src, nk, dstw) in ((p_a, 2, pab), (p_b, 2, pbb), (p_c, 4, pcb), (w_out, 8, wob)):
                        for k2 in range(0, nk, 2):
                            s_, sk_ = stg.next()
                            dma("sync", s_[:], src[l, k2 * 128:(k2 + 2) * 128, :].rearrange("(k p) d -> p k d", p=128), [], [sk_])
                            cp("vector" if si % 2 == 0 else "gpsimd", dstw[:, k2:k2 + 2, :], s_[:], [sk_], ["mw"])
                            si += 1
                    dma("sync", wrf[:], moe_wr[l, :, :].rearrange("(k p) e -> p k e", p=128), [], ["wrf"])
                    cp("vector", wrb[:], wrf[:], ["wrf"], ["mw"])
                    dma("sync", rbb[:], bass.AP(tensor=moe_br.tensor, offset=moe_br.offset + l * 36, ap=[[0, 128], [1, 36]]), [], ["mw"])
                    dma("sync", lg[:], bass.AP(tensor=ln1_g.tensor, offset=ln1_g.offset + l * D, ap=[[0, 128], [1, D]]), [], ["lnconst"])
                    dma("sync", lb[:], bass.AP(tensor=ln1_b.tensor, offset=ln1_b.offset + l * D, ap=[[0, 128], [1, D]]), [], ["lnconst"])
                    yaTs = sbt(ph, "yaTs", [128, 2, 512], BF16)
                    ybTs = sbt(ph, "ybTs", [128, 2, 512], BF16)
                    ycTs = sbt(ph, "ycTs", [128, 4, 512], BF16)
                    Gs = sbt(ph, "Gs", [128, 24, 512], BF16)
                    mT = sbt(ph, "mT", [128, 8, 512], BF16)
                    ta = sbt(ph, "ta", [128, 512], F32)
                    tb = sbt(ph, "tb", [128, 512], F32)
                    tcx = sbt(ph, "tcx", [128, 512], F32)
                    xr = Rot([sbt(ph, "xr%d" % i, [128, D], F32) for i in range(2)], "xr")
                    rr1s = Rot([sbt(ph, "rr1_%d" % i, [128, D], F32) for i in range(2)], "rr1_")
                    x1t = Rot([sbt(ph, "x1t%d" % i, [128, D], F32) for i in range(2)], "x1t")
                    h2fs = Rot([sbt(ph, "h2f%d" % i, [128, D], F32) for i in range(2)], "h2f")
                    h2b = Rot([sbt(ph, "h2b%d" % i, [128, D], BF16) for i in range(2)], "h2b")
                    h2T = sbt(ph, "h2T", [128, 8, 128], BF16)
                    lntmps = [{"stats": sbt(ph, "lnst", [128, 2, 6], F32), "mv": sbt(ph, "lnmv", [128, 2], F32),
                               "rstd": sbt(ph, "lnrs", [128, 1], F32)} for _ in range(2)]
                    lni = [0]
                    psa = pst(ph, "psa", [128, 512])
                    psb = pst(ph, "psb", [128, 512])
                    psc = pst(ph, "psc", [128, 512])
                    psO = Rot([pst(ph, "psO%d" % i, [128, 512]) for i in range(2)], "psO")
                    psh = pst(ph, "psh", [128, 8, 128], BF16)
                    psl = pst(ph, "psl", [128, 36])
                    pend_router = []
                    for (kind, b, tok0, Ls) in act_seqs:
                        mrow = b if kind == "lat" else NB
                        mo = MODS.offset + (l * (NB + 1) + mrow) * 6 * D
                        dma("sync", g1b[:], bass.AP(tensor=MODS.tensor, offset=mo + 2 * D, ap=[[0, 128], [1, D]]), ["MODS"], ["g1b"])
                        dma("sync", sh2b[:], bass.AP(tensor=MODS.tensor, offset=mo + 3 * D, ap=[[0, 128], [1, D]]), ["MODS"], ["sh2b"])
                        dma("sync", sc2b[:], bass.AP(tensor=MODS.tensor, offset=mo + 4 * D, ap=[[0, 128], [1, D]]), ["MODS"], ["sc2b"])
                        ts("gpsimd", sc2b[:], sc2b[:], 1.0, ALU.add, ["sc2b"], ["sc2b"])
                        CH = min(512, Ls)
                        for tc in range(Ls // CH):
                            t0 = tok0 + tc * CH
                            dma("sync", yaTs[:, :, 0:CH], YATd[:, :, t0:t0 + CH].rearrange("c p t -> p c t"), ["YATd"], ["yaTs"])
                            dma("gpsimd", ybTs[:, :, 0:CH], YBTd[:, :, t0:t0 + CH].rearrange("c p t -> p c t"), ["YBTd"], ["ybTs"])
                            dma("sync", ycTs[:, :, 0:CH], YCTd[:, :, t0:t0 + CH].rearrange("c p t -> p c t"), ["YCTd"], ["ycTs"])
                            dma("gpsimd", Gs[:, :, 0:CH], Gd[:, :, t0:t0 + CH].rearrange("c p t -> p c t"), ["Gd"], ["Gs"])
                            for dc in range(8):
                                ds_ = slice(dc * 128, (dc + 1) * 128)
                                for kt in range(2):
                                    mm(psa[:, 0:CH], pab[:, kt, ds_], yaTs[:, kt, 0:CH], kt == 0, kt == 1, ["mw", "yaTs"], ["psa"])
                                for kt in range(2):
                                    mm(psb[:, 0:CH], pbb[:, kt, ds_], ybTs[:, kt, 0:CH], kt == 0, kt == 1, ["mw", "ybTs"], ["psb"])
                                for kt in range(4):
                                    mm(psc[:, 0:CH], pcb[:, kt, ds_], ycTs[:, kt, 0:CH], kt == 0, kt == 3, ["mw", "ycTs"], ["psc"])
                                tt("vector", ta[:, 0:CH], psa[:, 0:CH], Gs[:, dc, 0:CH], ALU.mult, ["psa", "Gs"], ["ta"])
                                tt("vector", tb[:, 0:CH], psb[:, 0:CH], Gs[:, 8 + dc, 0:CH], ALU.mult, ["psb", "Gs"], ["tb"])
                                tt("vector", tcx[:, 0:CH], psc[:, 0:CH], Gs[:, 16 + dc, 0:CH], ALU.mult, ["psc", "Gs"], ["tcx"])
                                tt("gpsimd", ta[:, 0:CH], ta[:, 0:CH], tb[:, 0:CH], ALU.add, ["ta", "tb"], ["ta"])
                                tt("gpsimd", mT[:, dc, 0:CH], ta[:, 0:CH], tcx[:, 0:CH], ALU.add, ["ta", "tcx"], ["mT"])
                            for tsb in range(CH // 128):
                                tk0 = t0 + tsb * 128
                                gt = tk0 // 128
                                xt, xk = xr.next()
                                rr_, rrk = rr1s.next()
                                h2f, h2fk = h2fs.next()
                                dma("sync", xt[:], Xsrc(tk0, 128), ["X2d"] if l else [], [xk])
                                for half in range(2):
                                    hs = slice(half * 512, (half + 1) * 512)
                                    pso, pok = psO.next()
                                    for kt in range(8):
                                        mm(pso[:], mT[:, kt, tsb * 128:(tsb + 1) * 128], wob[:, kt, hs], kt == 0, kt == 7, ["mw", "mT"], [pok])
                                    tt("vector", rr_[:, hs], pso[:], g1b[:, hs], ALU.mult, [pok, "g1b"], [rrk])
                                while pend_router:
                                    pend_router.pop(0)()
                                stt(rr_[:], xt[:], ALPHA, rr_[:], ALU.mult, ALU.add, [xk, rrk], [rrk])
                                x1, x1k = x1t.next()
                                lni[0] += 1
                                layer_norm_rows("ln1%d" % (lni[0] % 2), rr_, rrk, lg, lb, x1, x1k, lntmps[lni[0] % 2])
                                dma("sync", X1d[tk0:tk0 + 128, :], x1[:], [x1k], ["X1d"])
                                tt("vector", h2f[:], x1[:], sc2b[:], ALU.mult, [x1k, "sc2b"], [h2fk])
                                hb_, hbk = h2b.next()
                                tt("gpsimd", hb_[:], h2f[:], sh2b[:], ALU.add, [h2fk, "sh2b"], [hbk])
                                dma("gpsimd", H2d[tk0:tk0 + 128, :], hb_[:], [hbk], ["H2d"])
                                def router(hb_=hb_, hbk=hbk, gt=gt):
                                    for kt in range(8):
                                        tr(psh[:, kt, :], hb_[:, kt * 128:(kt + 1) * 128], identb[:], [hbk, "identb"], ["psh"])
                                    cp("scalar", h2T[:], psh[:], ["psh"], ["h2T"])
                                    for kt in range(8):
                                        mm(psl[:], h2T[:, kt, :], wrb[:, kt, :], kt == 0, kt == 7, ["h2T", "mw"], ["psl"])
                                    tt("vector", LOG[:, gt, :], psl[:], rbb[:], ALU.add, ["psl", "mw"], ["LOG"])
                                pend_router.append(router)
                    while pend_router:
                        pend_router.pop(0)()
                    ph_end()

                with ExitStack() as ph:
                    NC_ = NTm * 32
                    cm = sbt(ph, "cm", [128, 128 + 8 + 4 + MMAX + NBLK + 32], F32)
                    dma("sync", cm[:], cmoe_in[:, :], [], ["cm"])
                    ustr = sbt(ph, "ustr", [128, 128], BF16)
                    cp("vector", ustr[:], cm[:, 0:128], ["cm"], ["ustr"])
                    o_kp8, o_kp4, o_mrow, o_nrow, o_i32 = 128, 136, 140, 140 + MMAX, 140 + MMAX + NBLK
                    onesb = sbt(ph, "onesb", [128, 1], BF16)
                    onesf = sbt(ph, "onesf", [32, 128], F32)
                    memset("vector", onesb[:], 1.0, ["onesb"])
                    memset("vector", onesf[:], 1.0, ["onesf"])
                    gmax = sbt(ph, "gmax", [128, NTm], F32)
                    goh = sbt(ph, "goh", [128, NTm, 4], F32)
                    gex = sbt(ph, "gex", [128, NTm, 4], F32)
                    gsum = sbt(ph, "gsum", [128, NTm], F32)
                    EM = sbt(ph, "EM", [128, NTm, 32], F32)
                    oh1 = sbt(ph, "oh1", [128, NTm, 32], F32)
                    oh2 = sbt(ph, "oh2", [128, NTm, 32], F32)
                    m1 = sbt(ph, "m1", [128, NTm], F32)
                    m2 = sbt(ph, "m2", [128, NTm], F32)
                    dd = sbt(ph, "dd", [128, NTm], F32)
                    Mb = sbt(ph, "Mb", [128, NTm, 32], BF16)
                    POS = sbt(ph, "POS", [128, NTm, 32], F32)
                    tmpP = sbt(ph, "tmpP", [128, NTm, 32], F32)
                    dstf = sbt(ph, "dstf", [128, NTm, 2], F32)
                    tot = sbt(ph, "tot", [32, NTm], F32)
                    cum = sbt(ph, "cum", [32, NTm], F32)
                    ones32 = sbt(ph, "ones32", [32, NTm], F32)
                    cnt = sbt(ph, "cnt", [32, 4], F32)
                    cmpm = sbt(ph, "cmpm", [32, MMAX], F32)
                    base = sbt(ph, "base", [32, NTm], F32)
                    R = sbt(ph, "R", [32, NTm, 32], F32)
                    dg32 = sbt(ph, "dg32", [32, 32], F32)
                    pendr = sbt(ph, "pendr", [128, 32], F32)
                    cmpb = sbt(ph, "cmpb", [128, NBLK, 32], F32)
                    be = sbt(ph, "be", [128, NBLK], F32)
                    idf = sbt(ph, "idf", [128, NBLK, 8], F32)
                    NPS = -(-NC_ // 512)
                    psR = pst(ph, "psR", [128, NPS * 512])
                    psTot = pst(ph, "psTot", [128, 512])
                    psSm = pst(ph, "psSm", [128, 512])
                    Lg = LOG[:, 0:NTm, :]
                    G4 = LOG[:, 0:NTm, 0:4]
                    E32 = LOG[:, 0:NTm, 4:36]

                    def bc_last(t2d, n):
                        a = t2d
                        return bass.AP(tensor=a.tensor, offset=a.offset, ap=[list(a.ap[0]), list(a.ap[1]), [0, n]])

                    red(gmax[:], G4, ALU.max, AX.X, ["LOG"], ["gmax"])
                    tt("vector", goh[:], G4, bc_last(gmax[:], 4), ALU.is_equal, ["LOG", "gmax"], ["goh"])
                    tt("vector", gex[:], G4, bc_last(gmax[:], 4), ALU.subtract, ["LOG", "gmax"], ["gex"])
                    act(gex[:], gex[:], AF.Exp, ["gex"], ["gex"])
                    red(gsum[:], gex[:], ALU.add, AX.X, ["gex"], ["gsum"])
                    p.add("vector", lambda e: e.reciprocal(out=gsum[:], in_=gsum[:]), ["gsum"], ["gsum"])
                    ts("vector", goh[:], goh[:], 1.0, ALU.subtract, ["goh"], ["goh"], s2=BIG, op1=ALU.mult)
                    gb = goh[:]
                    p.add("vector", lambda e: e.tensor_tensor(
                        out=bass.AP(tensor=EM[:].tensor, offset=EM[:].offset, ap=[list(EM[:].ap[0]), [32, NTm], [8, 4], [1, 8]]),
                        in0=bass.AP(tensor=LOG[:].tensor, offset=LOG[:].offset + 4, ap=[list(LOG[:].ap[0]), [36, NTm], [8, 4], [1, 8]]),
                        in1=bass.AP(tensor=gb.tensor, offset=gb.offset, ap=[list(gb.ap[0]), [4, NTm], [1, 4], [0, 8]]),
                        op=ALU.add), ["LOG", "goh"], ["EM"])
                    red(m1[:], EM[:], ALU.max, AX.X, ["EM"], ["m1"])
                    tt("vector", oh1[:], EM[:], bc_last(m1[:], 32), ALU.is_equal, ["EM", "m1"], ["oh1"])
                    stt(EM[:], oh1[:], -BIG, EM[:], ALU.mult, ALU.add, ["oh1", "EM"], ["EM"])
                    red(m2[:], EM[:], ALU.max, AX.X, ["EM"], ["m2"])
                    tt("vector", oh2[:], EM[:], bc_last(m2[:], 32), ALU.is_equal, ["EM", "m2"], ["oh2"])
                    tt("vector", dd[:], m2[:], m1[:], ALU.subtract, ["m1", "m2"], ["dd"])
                    act(dd[:], dd[:], AF.Exp, ["dd"], ["dd"])
                    ts("vector", dd[:], dd[:], 1.0, ALU.add, ["dd"], ["dd"])
                    p.add("vector", lambda e: e.reciprocal(out=dd[:], in_=dd[:]), ["dd"], ["dd"])
                    tt("vector", WTS[:, 0:NTm, 0], dd[:], gsum[:], ALU.mult, ["dd", "gsum"], ["WTS"])
                    tt("vector", WTS[:, 0:NTm, 1], gsum[:], WTS[:, 0:NTm, 0], ALU.subtract, ["gsum", "WTS"], ["WTS"])
                    tt("vector", Mb[:], oh1[:], oh2[:], ALU.add, ["oh1", "oh2"], ["Mb"])
                    Mf = Mb[:].rearrange("p t e -> p (t e)")
                    for c_ in range(NPS):
                        n_ = min(512, NC_ - c_ * 512)
                        mm(psR[:, c_ * 512:c_ * 512 + n_], ustr[:], Mf[:, c_ * 512:c_ * 512 + n_], True, True, ["ustr", "Mb"], ["psR"])
                    for t_ in range(NTm):
                        mm(psTot[0:32, t_:t_ + 1], Mb[:, t_, :], onesb[:], True, True, ["Mb", "onesb"], ["psTot"])
                    cp("vector", tot[:], psTot[0:32, 0:NTm], ["psTot"], ["tot"])
                    memset("vector", ones32[:], 1.0, ["ones32"])
                    p.add("vector", lambda e: e.tensor_tensor_scan(out=cum[:], data0=ones32[:], data1=tot[:], initial=0.0,
                                                                   op0=ALU.mult, op1=ALU.add), ["ones32", "tot"], ["cum"])
                    cp("vector", cnt[:, 0:1], cum[:, NTm - 1:NTm], ["cum"], ["cnt"])
                    ts("vector", cmpm[:], cm[0:32, o_mrow:o_mrow + MMAX], cnt[:, 0:1], ALU.is_lt, ["cm", "cnt"], ["cmpm"])
                    red(cnt[:, 1:2], cmpm[:], ALU.add, AX.X, ["cmpm"], ["cnt"])
                    ts("vector", cnt[:, 2:3], cnt[:, 1:2], float(BLK), ALU.mult, ["cnt"], ["cnt"])
                    mm(psSm[0:32, 0:1], cm[0:32, 0:32], cnt[:, 2:3], True, True, ["cm", "cnt"], ["psSm"])
                    tt("vector", cnt[:, 3:4], psSm[0:32, 0:1], cnt[:, 2:3], ALU.add, ["psSm", "cnt"], ["cnt"])
                    tt("vector", base[:], cum[:], tot[:], ALU.subtract, ["cum", "tot"], ["base"])
                    ts("vector", base[:], base[:], psSm[0:32, 0:1], ALU.add, ["base", "psSm"], ["base"])
                    bb = base[:]
                    i32 = cm[0:32, o_i32:o_i32 + 32]
                    p.add("vector", lambda e: e.tensor_tensor(
                        out=R[:], in0=bass.AP(tensor=bb.tensor, offset=bb.offset, ap=[list(bb.ap[0]), [1, NTm], [0, 32]]),
                        in1=bass.AP(tensor=i32.tensor, offset=i32.offset, ap=[list(i32.ap[0]), [0, NTm], [1, 32]]),
                        op=ALU.mult), ["base", "cm"], ["R"])
                    cp("vector", POS[:].rearrange("p t e -> p (t e)"), psR[:, 0:NC_], ["psR"], ["POS"])
                    Rf = R[:].rearrange("p t e -> p (t e)")
                    for c_ in range(NPS):
                        n_ = min(512, NC_ - c_ * 512)
                        mm(psR[:, c_ * 512:c_ * 512 + n_], onesf[:], Rf[:, c_ * 512:c_ * 512 + n_], True, True, ["onesf", "R", "POS"], ["psR"])
                    tt("vector", POS[:].rearrange("p t e -> p (t e)"), POS[:].rearrange("p t e -> p (t e)"), psR[:, 0:NC_],
                       ALU.add, ["psR", "POS"], ["POS"])
                    for k_, oh in enumerate((oh1, oh2)):
                        tt("vector", tmpP[:], oh[:], POS[:], ALU.mult, ["oh1", "oh2", "POS"], ["tmpP"])
                        red(dstf[:, :, k_], tmpP[:], ALU.add, AX.X, ["tmpP"], ["dstf"])
                    cp("vector", DEST[:, 0:NTm, :], dstf[:], ["dstf"], ["DEST"])
                    ts("vector", dg32[:], i32, cnt[:, 3:4], ALU.mult, ["cm", "cnt"], ["dg32"])
                    mm(psSm[:, 32:64], onesf[:], dg32[:], True, True, ["onesf", "dg32"], ["psSm"])
                    cp("vector", pendr[:], psSm[:, 32:64], ["psSm"], ["pendr"])
                    pr_ = pendr[:]
                    nrow = cm[:, o_nrow:o_nrow + NBLK]
                    p.add("vector", lambda e: e.tensor_tensor(
                        out=cmpb[:], in0=bass.AP(tensor=pr_.tensor, offset=pr_.offset, ap=[list(pr_.ap[0]), [0, NBLK], [1, 32]]),
                        in1=bass.AP(tensor=nrow.tensor, offset=nrow.offset, ap=[list(nrow.ap[0]), [1, NBLK], [0, 32]]),
                        op=ALU.is_le), ["pendr", "cm"], ["cmpb"])
                    red(be[:], cmpb[:], ALU.add, AX.X, ["cmpb"], ["be"])
                    ts("vector", be[:], be[:], 31.0, ALU.min, ["be"], ["be"], s2=float(l * NE), op1=ALU.add)
                    ts("vector", idf[:, :, 0], be[:], 128.0, ALU.mult, ["be", "idf"], ["idf"], s2=cm[:, o_kp8:o_kp8 + 1], op1=ALU.add)
                    cp("vector", IDXE[:], idf[:, :, 0], ["idf"], ["IDXE"])
                    ph_end()

                with ExitStack() as ph:
                    hrs = Rot([sbt(ph, "hr%d" % i, [128, D], BF16) for i in range(3)], "hr")
                    for t_ in range(NTm):
                        hr, hk = hrs.next()
                        dma("sync", hr[:], H2d[t_ * 128:(t_ + 1) * 128, :], ["H2d"], [hk])
                        for k_ in range(2):
                            p.add("gpsimd", lambda e, hr=hr, t_=t_, k_=k_: e.indirect_dma_start(
                                out=XBUF[:, :], out_offset=bass.IndirectOffsetOnAxis(ap=DEST[:, t_, k_:k_ + 1], axis=0),
                                in_=hr[:], in_offset=None), [hk, "DEST"], ["XBUF"], dma=True)
                    ph_end()

                with ExitStack() as ph:
                    wgf = sbt(ph, "wgf", [128, 8, HID], F32)
                    wuf = sbt(ph, "wuf", [128, 8, HID], F32)
                    wdf = sbt(ph, "wdf", [128, 4, D], F32)
                    wgbs = Rot([sbt(ph, "wgb%d" % i, [128, 8, HID], BF16) for i in range(2)], "wgb")
                    wubs = Rot([sbt(ph, "wub%d" % i, [128, 8, HID], BF16) for i in range(2)], "wub")
                    wdbs = Rot([sbt(ph, "wdb%d" % i, [128, 4, D], BF16) for i in range(2)], "wdb")
                    xrs = Rot([sbt(ph, "xb%d" % i, [128, RB, D], BF16) for i in range(2)], "xb")
                    XT = sbt(ph, "XT", [128, 8, BLK], BF16)
                    sgt = sbt(ph, "sgt", [128, BLK], F32)
                    aT = sbt(ph, "aT", [128, 4, BLK], BF16)
                    yos = Rot([sbt(ph, "yo%d" % i, [128, D], BF16) for i in range(2)], "yo")
                    psX = pst(ph, "psX", [128, 8, 128], BF16)
                    psG = Rot([pst(ph, "psG%d" % i, [128, BLK]) for i in range(2)], "psG")
                    psU = Rot([pst(ph, "psU%d" % i, [128, BLK]) for i in range(2)], "psU")
                    psD = Rot([pst(ph, "psD%d" % i, [128, 512]) for i in range(2)], "psD")
                    for n in range(-(-(2 * NTm * 128) // BLK) + NE):
                        for (wf_, src_, wk_) in ((wgf, ex_g8, "wgf"), (wuf, ex_u8, "wuf"), (wdf, ex_d4, "wdf")):
                            p.add("gpsimd", lambda e, n=n, wf_=wf_, src_=src_: e.indirect_dma_start(
                                out=wf_[:].rearrange("p a b -> p (a b)"), out_offset=None, in_=src_,
                                in_offset=bass.IndirectOffsetOnAxis(ap=IDXE[:, n:n + 1], axis=0)), ["IDXE"], [wk_], dma=True)
                        wgb, gk = wgbs.next()
                        wub, uk = wubs.next()
                        wdb, dk = wdbs.next()
                        cp("vector", wgb[:], wgf[:], ["wgf"], [gk])
                        cp("gpsimd", wub[:], wuf[:], ["wuf"], [uk])
                        cp("scalar", wdb[:], wdf[:], ["wdf"], [dk])
                        xb, xk = xrs.next()
                        dma("sync", xb[:], XBUF[n * BLK:(n + 1) * BLK, :].rearrange("(r p) d -> p r d", p=128), ["XBUF"], [xk])
                        for rb in range(RB):
                            for kt in range(8):
                                tr(psX[:, kt, :], _ap(xb[:], rb * D + kt, [[8, 128]]), identb[:], [xk, "identb"], ["psX"])
                            cp("scalar" if rb % 2 else "vector", XT[:, :, rb * 128:(rb + 1) * 128], psX[:], ["psX"], ["XT"])
                        for hc in range(4):
                            pg, pgk = psG.next()
                            pu, puk = psU.next()
                            for kt in range(8):
                                mm(pg[:], _ap(wgb[:], kt * HID + hc, [[4, 128]]), XT[:, kt, :], kt == 0, kt == 7, [gk, "XT"], [pgk])
                            for kt in range(8):
                                mm(pu[:], _ap(wub[:], kt * HID + hc, [[4, 128]]), XT[:, kt, :], kt == 0, kt == 7, [uk, "XT"], [puk])
                            act(sgt[:], pg[:], AF.Silu, [pgk], ["sgt"])
                            tt("vector", aT[:, hc, :], sgt[:], pu[:], ALU.mult, ["sgt", puk], ["aT"])
                        for rb in range(RB):
                            yo, yk = yos.next()
                            for half in range(2):
                                pd, pdk = psD.next()
                                for hc in range(4):
                                    mm(pd[:], aT[:, hc, rb * 128:(rb + 1) * 128], wdb[:, hc, half * 512:(half + 1) * 512],
                                       hc == 0, hc == 3, [dk, "aT"], [pdk])
                                cp("scalar" if half else "vector", yo[:, half * 512:(half + 1) * 512], pd[:], [pdk], [yk])
                            r0 = n * BLK + rb * 128
                            dma("sync", YBUF[r0:r0 + 128, :], yo[:], [yk], ["YBUF"])
                    ph_end()

                with ExitStack() as ph:
                    r0s = Rot([sbt(ph, "r0_%d" % i, [128, D], BF16) for i in range(2)], "r0_")
                    r1s = Rot([sbt(ph, "r1_%d" % i, [128, D], BF16) for i in range(2)], "r1_")
                    xr = Rot([sbt(ph, "xq%d" % i, [128, D], F32) for i in range(2)], "xq")
                    yfs = Rot([sbt(ph, "yf%d" % i, [128, D], F32) for i in range(2)], "yf")
                    rr2s = Rot([sbt(ph, "rr2_%d" % i, [128, D], F32) for i in range(2)], "rr2_")
                    xo = Rot([sbt(ph, "xo%d" % i, [128, D], F32) for i in range(2)], "xo")
                    lg = sbt(ph, "lg2", [128, D], F32)
                    lb = sbt(ph, "lb2", [128, D], F32)
                    g2b = sbt(ph, "g2b", [128, D], F32)
                    lntmps = [{"stats": sbt(ph, "lnst2", [128, 2, 6], F32), "mv": sbt(ph, "lnmv2", [128, 2], F32),
                               "rstd": sbt(ph, "lnrs2", [128, 1], F32)} for _ in range(2)]
                    lni = [0]
                    dma("sync", lg[:], bass.AP(tensor=ln2_g.tensor, offset=ln2_g.offset + l * D, ap=[[0, 128], [1, D]]), [], ["lnconst"])
                    dma("sync", lb[:], bass.AP(tensor=ln2_b.tensor, offset=ln2_b.offset + l * D, ap=[[0, 128], [1, D]]), [], ["lnconst"])
                    for (kind, b, tok0, Ls) in act_seqs:
                        mrow = b if kind == "lat" else NB
                        mo = MODS.offset + (l * (NB + 1) + mrow) * 6 * D
                        dma("sync", g2b[:], bass.AP(tensor=MODS.tensor, offset=mo + 5 * D, ap=[[0, 128], [1, D]]), ["MODS"], ["g2b"])
                        for tI in range(Ls // 128):
                            tk0 = tok0 + tI * 128
                            gt = tk0 // 128
                            r0, r0k = r0s.next()
                            r1, r1k = r1s.next()
                            for (rt, rk, k_) in ((r0, r0k, 0), (r1, r1k, 1)):
                                p.add("gpsimd", lambda e, rt=rt, gt=gt, k_=k_: e.indirect_dma_start(
                                    out=rt[:], out_offset=None, in_=YBUF[:, :],
                                    in_offset=bass.IndirectOffsetOnAxis(ap=DEST[:, gt, k_:k_ + 1], axis=0)), ["YBUF", "DEST"], [rk], dma=True)
                            xt, xk = xr.next()
                            dma("sync", xt[:], X1d[tk0:tk0 + 128, :], ["X1d"], [xk])
                            yf, yfk = yfs.next()
                            rr_, rrk = rr2s.next()
                            ts("vector", yf[:], r0[:], WTS[:, gt, 0:1], ALU.mult, [r0k, "WTS"], [yfk])
                            stt(yf[:], r1[:], WTS[:, gt, 1:2], yf[:], ALU.mult, ALU.add, [r1k, "WTS", yfk], [yfk])
                            tt("gpsimd", yf[:], yf[:], g2b[:], ALU.mult, [yfk, "g2b"], [yfk])
                            stt(rr_[:], xt[:], ALPHA, yf[:], ALU.mult, ALU.add, [xk, yfk], [rrk])
                            xo_, xok = xo.next()
                            lni[0] += 1
                            layer_norm_rows("ln2%d" % (lni[0] % 2), rr_, rrk, lg, lb, xo_, xok, lntmps[lni[0] % 2])
                            if last:
                                dma("sync", out[tk0:tk0 + 128, :], xo_[:], [xok], ["out"])
                            else:
                                dma("sync", X2d[tk0:tk0 + 128, :], xo_[:], [xok], ["X2d"])
                    ph_end(final=last)
    return nc


def _const_tables(cfg):
    L, LC, BLK, NB = cfg.L, cfg.LC, cfg.BLK, cfg.NB
    f32 = np.float32
    c = {}
    c["c_ident"] = np.eye(128, dtype=f32)
    n_freq = 16
    t = np.arange(L)
    pos = np.stack([(t // GRID_W).astype(f32), (t % GRID_W).astype(f32)], 0)
    inv = (10000.0 ** (-np.arange(n_freq, dtype=f32) / n_freq)).astype(f32)
    rope = np.zeros((2, 128, L), f32)
    for n in range(128):
        a, r, f = (n // 32) % 2, (n // 16) % 2, n % 16
        ang = (pos[a] * inv[f]).astype(f32)
        rope[0, n] = np.cos(ang)
        rope[1, n] = np.sin(ang) * (-1.0 if r == 0 else 1.0)
    c["c_rope"] = rope

    def hy_tables(Lf):
        tt_ = np.linspace(0.0, 1.0, Lf, dtype=f32)
        w = (2.0 * math.pi * np.arange(Lf, dtype=f32) / Lf).astype(f32)
        fb = np.linspace(1e-4, HY_BANDS - 1, HY_BANDS, dtype=f32)
        emb = np.concatenate([tt_[:, None], np.cos(fb[None, :] * w[:, None]), -np.sin(fb[None, :] * w[:, None])], -1).astype(f32)
        max_decay = math.log(1e-2) / 0.3
        min_decay = math.log(1e-2) / 1.5
        deltas = np.abs(np.linspace(min_decay, max_decay, 256, dtype=f32))
        window = (np.exp(-tt_[:, None] * deltas[None, :]) + 0.05).astype(f32)
        pf = np.arange(Lf - 1, -1, -1)
        pb = np.concatenate([np.arange(1, Lf), [0]])
        e = np.stack([emb[pf].T, emb[pb].T], 0).astype(f32)
        wn = np.stack([window[pf].T, window[pb].T], 0).astype(f32)
        wn[1, :, Lf - 1] = 0.0
        return np.ascontiguousarray(e), np.ascontiguousarray(wn)

    c["c_emb_L"], c["c_win_L"] = hy_tables(L)
    c["c_emb_C"], c["c_win_C"] = hy_tables(LC)
    T = NB * (L + LC)
    NBLK = -(-(2 * T) // BLK) + NE
    MMAX = -(-(2 * T) // BLK) + 1
    cm = np.zeros((128, 128 + 8 + 4 + MMAX + NBLK + 32), f32)
    pp = np.arange(128)
    cm[:, 0:128] = (pp[:, None] < pp[None, :]).astype(f32)
    cm[:, 128:136] = np.arange(8)[None, :] * 128 + pp[:, None]
    cm[:, 136:140] = np.arange(4)[None, :] * 128 + pp[:, None]
    cm[:, 140:140 + MMAX] = (np.arange(MMAX) * BLK)[None, :]
    cm[:, 140 + MMAX:140 + MMAX + NBLK] = (np.arange(NBLK) * BLK)[None, :]
    cm[0:32, 140 + MMAX + NBLK:] = np.eye(32, dtype=f32)
    c["c_moe"] = cm
    return c


def _core_inputs(cfg, inp, core, consts):
    NB, L, LC = cfg.NB, cfg.L, cfg.LC
    f32 = np.float32
    bs = slice(core * NB, (core + 1) * NB)
    m = dict(consts)
    m["x"] = np.ascontiguousarray(inp["x"][bs].reshape(NB * L, D))
    m["ctx"] = np.ascontiguousarray(inp["ctx"][bs].reshape(NB * LC, D))
    cc = np.concatenate([inp["c"][bs], inp["c_ctx"][None, :]], 0)
    m["cT"] = np.ascontiguousarray(cc.T.reshape(8, 128, NB + 1).transpose(1, 0, 2))
    for k in ("ada_w", "ada_b", "w_in", "gm_ln_g", "gm_ln_b", "hy_f_w1", "hy_f_w2", "hy_f_w3", "da_norm_g",
              "p_a", "p_b", "p_c", "w_out", "ln1_g", "ln1_b", "ln2_g", "ln2_b"):
        m[k] = inp[k]
    m["gm_wsT"] = np.ascontiguousarray(inp["gm_ws"].transpose(0, 3, 1, 2))
    m["gm_bsT"] = np.ascontiguousarray(inp["gm_bs"].transpose(0, 2, 1))
    cw = np.concatenate([inp["hy_conv_w"], inp["hy_conv_b"][:, None, :]], 1)
    m["hy_cw"] = np.ascontiguousarray(cw.reshape(DEPTH, 4, 6, 128).transpose(0, 3, 2, 1))
    m["hy_f_b1"] = np.ascontiguousarray(inp["hy_f_b1"][:, :, None])
    m["hy_f_b2"] = np.ascontiguousarray(inp["hy_f_b2"][:, :, None])
    m["hy_b3T"] = np.ascontiguousarray(inp["hy_f_b3"].reshape(DEPTH, 4, 128).transpose(0, 2, 1))
    m["hy_skipT"] = np.ascontiguousarray(inp["hy_skip"].reshape(DEPTH, 2, 128).transpose(0, 2, 1))
    m["da_l"] = np.ascontiguousarray(np.stack([inp["da_lq1"], inp["da_lk1"], inp["da_lq2"], inp["da_lk2"]], 1))
    m["moe_wr"] = np.ascontiguousarray(np.concatenate([inp["moe_wg"], inp["moe_we"]], -1))
    m["moe_br"] = np.ascontiguousarray(np.concatenate([inp["moe_bg"], inp["moe_be"]], -1))
    m["ex_w_gate"] = inp["ex_w_gate"].reshape(DEPTH * NE * D, HID)
    m["ex_w_up"] = inp["ex_w_up"].reshape(DEPTH * NE * D, HID)
    m["ex_w_down"] = inp["ex_w_down"].reshape(DEPTH * NE * HID, D)
    return {k: np.ascontiguousarray(np.asarray(v, dtype=f32)) for k, v in m.items()}


def kernel(**inputs):
    cfg = Cfg()
    inp = {k: np.asarray(v) for k, v in inputs.items()}
    n_cores = inp["x"].shape[0] // cfg.NB
    nc = build(cfg)
    consts = _const_tables(cfg)
    in_maps = [_core_inputs(cfg, inp, c, consts) for c in range(n_cores)]
    res = run_bass_kernel_spmd(nc, in_maps, core_ids=list(range(n_cores)))
    outs = [np.asarray(r["out"]).reshape(cfg.NB, cfg.L, D) for r in res.results]
    return np.concatenate(outs, 0).astype(np.float32)
```

```python
import math
from contextlib import ExitStack

import numpy as np
import concourse.bass as bass
import concourse.mybir as mybir
from concourse.bass_utils import run_bass_kernel_spmd

F32 = mybir.dt.float32
BF16 = mybir.dt.bfloat16
I32 = mybir.dt.int32
AF = mybir.ActivationFunctionType
ALU = mybir.AluOpType
AX = mybir.AxisListType

D = 1024
DEPTH = 2
GRID_W = 64
N_IN = 5888
OFF_HY, OFF_Q, OFF_K, OFF_V, OFF_GATE = 512, 1280, 1792, 2304, 2816
NWB = N_IN + 1024
HY_EMB, HY_FFN, HY_BANDS = 33, 64, 16
NE, NG, EPG, HID = 32, 4, 8, 512
ALPHA = (2.0 * DEPTH) ** 0.25
EPS = 1e-5
BIG = 1.0e30

ENGINES = ("tensor", "vector", "scalar", "gpsimd", "sync")
N_DMA_SEMS = 40


class Prog:
    def __init__(self, nc, stack):
        self.nc = nc
        self.ops = []
        self.esem = {e: stack.enter_context(nc.semaphore("s_" + e)) for e in ENGINES}
        self.dsem = [stack.enter_context(nc.semaphore("d%d" % i)) for i in range(N_DMA_SEMS)]
        self.ecount = {e: 0 for e in ENGINES}
        self.dcount = [0] * N_DMA_SEMS
        self.dnext = 0
        self.lastw = {}
        self.readers = {}
        self.known = {}
        self.nops = 0

    def add(self, eng, fn, r=(), w=(), dma=False):
        self.ops.append((eng, fn, tuple(r), tuple(w), dma))

    def flush(self, final=False):
        nc = self.nc
        esem, dsem, ecount, dcount = self.esem, self.dsem, self.ecount, self.dcount
        lastw, readers, known = self.lastw, self.readers, self.known
        plan = {e: [] for e in ENGINES}
        fence = [(("d", i), dcount[i]) for i in range(N_DMA_SEMS) if dcount[i] > 0]
        fence += [(("e", e), ecount[e]) for e in ENGINES if ecount[e] > 0]
        for (eng, fn, r, w, dma) in self.ops:
            deps = []
            for k in r:
                t = lastw.get(k)
                if t is not None:
                    deps.append(t)
            for k in w:
                t = lastw.get(k)
                if t is not None:
                    deps.append(t)
                for tk, tv in readers.get(k, {}).items():
                    deps.append((tk[0], tk[1], tv))
            if dma:
                si = self.dnext
                self.dnext = (self.dnext + 1) % N_DMA_SEMS
                if dcount[si] > 0:
                    deps.append(("d", si, dcount[si]))
                dcount[si] += 16
                tok = ("d", si, dcount[si])
                inc = (dsem[si], 16)
            else:
                ecount[eng] += 1
                tok = ("e", eng, ecount[eng])
                inc = (esem[eng], 1)
            waits = {}
            for (kind, key, val) in deps:
                if kind == "e" and key == eng and eng == "tensor":
                    continue
                sk = (kind, key)
                if known.get((eng, sk), 0) >= val:
                    continue
                if waits.get(sk, 0) < val:
                    waits[sk] = val
            wl = []
            for sk, val in waits.items():
                known[(eng, sk)] = val
                wl.append((esem[sk[1]] if sk[0] == "e" else dsem[sk[1]], val))
            plan[eng].append((wl, fn, inc))
            for k in r:
                d = readers.setdefault(k, {})
                if d.get(tok[:2], 0) < tok[2]:
                    d[tok[:2]] = tok[2]
            for k in w:
                lastw[k] = tok
                readers[k] = {}
        self.nops += len(self.ops)
        self.ops = []
        endw = []
        if final:
            endw = [(dsem[i], dcount[i]) for i in range(N_DMA_SEMS) if dcount[i] > 0]
            endw += [(esem[e], ecount[e]) for e in ENGINES if ecount[e] > 0]

        def runner(ename):
            def body(eng):
                for sk, val in fence:
                    if sk == ("e", ename):
                        continue
                    if known.get((ename, sk), 0) >= val:
                        continue
                    known[(ename, sk)] = val
                    eng.wait_ge(esem[sk[1]] if sk[0] == "e" else dsem[sk[1]], val)
                for (wl, fn, inc) in plan[ename]:
                    for (sem, val) in wl:
                        eng.wait_ge(sem, val)
                    ins = fn(eng)
                    ins.then_inc(inc[0], inc[1])
                for (sem, val) in endw:
                    eng.wait_ge(sem, val)
            return body

        with nc.Block() as block:
            block.tensor(runner("tensor"))
            block.vector(runner("vector"))
            block.scalar(runner("scalar"))
            block.gpsimd(runner("gpsimd"))
            block.sync(runner("sync"))


class Cfg:
    def __init__(self, NB=4, L=2048, LC=256, BLK=512, stop=None):
        self.NB, self.L, self.LC, self.BLK, self.stop = NB, L, LC, BLK, stop


class _Stop(Exception):
    pass


def _ap(base, off, dims, part=None):
    p = list(base.ap[0]) if part is None else [base.ap[0][0], part]
    return bass.AP(tensor=base.tensor, offset=base.offset + off, ap=[p] + [list(d) for d in dims])


class Rot:
    def __init__(self, tiles, name):
        self.tiles, self.name, self.i = tiles, name, 0

    def next(self):
        t = self.tiles[self.i % len(self.tiles)]
        k = "%s%d" % (self.name, self.i % len(self.tiles))
        self.i += 1
        return t, k


def build(cfg, debug=False):
    holder = {}
    try:
        _build(cfg, debug, holder)
    except _Stop:
        pass
    return holder["nc"]


def _build(cfg, debug, holder):
    NB, L, LC, BLK = cfg.NB, cfg.L, cfg.LC, cfg.BLK
    nc = bass.Bass("TRN2", target_bir_lowering=False)
    holder["nc"] = nc
    T = NB * (L + LC)
    NT = T // 128
    NTL = NB * L // 128
    RB = BLK // 128

    def din(name, shape, dt=F32):
        return nc.dram_tensor(name, list(shape), dt, kind="ExternalInput").ap()

    def dscr(name, shape, dt):
        return nc.dram_tensor(name, list(shape), dt, kind="ExternalOutput" if debug else "Internal").ap()

    x_in = din("x", [NB * L, D])
    ctx_in = din("ctx", [NB * LC, D])
    cT_in = din("cT", [128, 8, NB + 1])
    ada_w = din("ada_w", [DEPTH, D, 6 * D])
    ada_b = din("ada_b", [DEPTH, 6 * D])
    w_in = din("w_in", [DEPTH, D, N_IN])
    gm_ln_g = din("gm_ln_g", [DEPTH, 256])
    gm_ln_b = din("gm_ln_b", [DEPTH, 256])
    gm_wsT = din("gm_wsT", [DEPTH, 128, 4, 128])
    gm_bsT = din("gm_bsT", [DEPTH, 128, 4])
    hy_cw = din("hy_cw", [DEPTH, 128, 6, 4])
    hy_w1 = din("hy_f_w1", [DEPTH, HY_EMB, HY_FFN])
    hy_b1 = din("hy_f_b1", [DEPTH, HY_FFN, 1])
    hy_w2 = din("hy_f_w2", [DEPTH, HY_FFN, HY_FFN])
    hy_b2 = din("hy_f_b2", [DEPTH, HY_FFN, 1])
    hy_w3 = din("hy_f_w3", [DEPTH, HY_FFN, 512])
    hy_b3T = din("hy_b3T", [DEPTH, 128, 4])
    hy_skipT = din("hy_skipT", [DEPTH, 128, 2])
    da_l = din("da_l", [DEPTH, 4, 64])
    da_g = din("da_norm_g", [DEPTH, 128])
    p_a = din("p_a", [DEPTH, 256, D])
    p_b = din("p_b", [DEPTH, 256, D])
    p_c = din("p_c", [DEPTH, 512, D])
    w_out = din("w_out", [DEPTH, D, D])
    ln1_g = din("ln1_g", [DEPTH, D])
    ln1_b = din("ln1_b", [DEPTH, D])
    moe_wr = din("moe_wr", [DEPTH, D, 36])
    moe_br = din("moe_br", [DEPTH, 36])
    ex_g = din("ex_w_gate", [DEPTH * NE * D, HID])
    ex_u = din("ex_w_up", [DEPTH * NE * D, HID])
    ex_d = din("ex_w_down", [DEPTH * NE * HID, D])
    ex_g8 = ex_g.rearrange("(r j) h -> r (j h)", j=8)
    ex_u8 = ex_u.rearrange("(r j) h -> r (j h)", j=8)
    ex_d4 = ex_d.rearrange("(r j) d -> r (j d)", j=4)
    ln2_g = din("ln2_g", [DEPTH, D])
    ln2_b = din("ln2_b", [DEPTH, D])
    ident_in = din("c_ident", [128, 128])
    rope_in = din("c_rope", [2, 128, L])
    emb_in = {L: din("c_emb_L", [2, HY_EMB, L]), LC: din("c_emb_C", [2, HY_EMB, LC])}
    win_in = {L: din("c_win_L", [2, 256, L]), LC: din("c_win_C", [2, 256, LC])}
    NBLK = -(-(2 * T) // BLK) + NE
    MMAX = -(-(2 * T) // BLK) + 1
    cmoe_in = din("c_moe", [128, 128 + 8 + 4 + MMAX + NBLK + 32])
    out = nc.dram_tensor("out", [NB * L, D], F32, kind="ExternalOutput").ap()

    Wb = dscr("s_wb", [8, 128, NWB], BF16)
    MODS = dscr("s_mods", [DEPTH, NB + 1, 6 * D], F32)
    KREV = {L: dscr("s_krevL", [256, 2 * L], BF16), LC: dscr("s_krevC", [256, 2 * LC], BF16)}
    QTd = dscr("s_qt", [4, 128, T], BF16)
    KTd = dscr("s_kt", [4, 128, T], BF16)
    Vd = dscr("s_v", [T, 512], BF16)
    Gd = dscr("s_g", [24, 128, T], BF16)
    YATd = dscr("s_yat", [2, 128, T], BF16)
    X0Td = dscr("s_x0t", [2, 128, T], BF16)
    YBTd = dscr("s_ybt", [2, 128, T], BF16)
    YCTd = dscr("s_yct", [4, 128, T], BF16)
    X1d = dscr("s_x1", [T, D], F32)
    X2d = dscr("s_x2", [T, D], F32)
    H2d = dscr("s_h2", [T, D], BF16)
    XBUF = dscr("s_xbuf", [NBLK * BLK, D], BF16)
    YBUF = dscr("s_ybuf", [NBLK * BLK, D], BF16)

    seqs = [("lat", b, b * L, L) for b in range(NB)] + [("ctx", b, NB * L + b * LC, LC) for b in range(NB)]

    with ExitStack() as top:
        p = Prog(nc, top)

        uniq = [0]

        def sbt(st, name, shape, dt):
            uniq[0] += 1
            return st.enter_context(nc.sbuf_tensor("%s_%d" % (name, uniq[0]), list(shape), dt))

        def pst(st, name, shape, dt=F32):
            uniq[0] += 1
            return st.enter_context(nc.psum_tensor("%s_%d" % (name, uniq[0]), list(shape), dt))

        def dma(eng, out_, in_, r, w, **kw):
            p.add(eng, lambda e: e.dma_start(out=out_, in_=in_, **kw), r, w, dma=True)

        def mm(out_, lhsT, rhs, start, stop, r, w):
            p.add("tensor", lambda e: e.matmul(out_, lhsT=lhsT, rhs=rhs, start=start, stop=stop,
                                               skip_group_check=True), r, w)

        def tr(out_, in_, ident, r, w):
            p.add("tensor", lambda e: e.transpose(out=out_, in_=in_, identity=ident), r, w)

        def act(out_, in_, func, r, w, **kw):
            p.add("scalar", lambda e: e.activation(out=out_, in_=in_, func=func, **kw), r, w)

        def tt(eng, out_, a, b, op, r, w):
            p.add(eng, lambda e: e.tensor_tensor(out=out_, in0=a, in1=b, op=op), r, w)

        def ts(eng, out_, a, s1, op0, r, w, s2=None, op1=None):
            if op1 is None:
                p.add(eng, lambda e: e.tensor_scalar(out=out_, in0=a, scalar1=s1, scalar2=None, op0=op0), r, w)
            else:
                p.add(eng, lambda e: e.tensor_scalar(out=out_, in0=a, scalar1=s1, scalar2=s2, op0=op0, op1=op1), r, w)

        def stt(out_, a, s, b, op0, op1, r, w):
            p.add("vector", lambda e: e.scalar_tensor_tensor(out=out_, in0=a, scalar=s, in1=b, op0=op0, op1=op1), r, w)

        def cp(eng, out_, in_, r, w):
            if eng == "scalar":
                p.add(eng, lambda e: e.copy(out=out_, in_=in_), r, w)
            else:
                p.add(eng, lambda e: e.tensor_copy(out=out_, in_=in_), r, w)

        def red(out_, in_, op, axis, r, w):
            p.add("vector", lambda e: e.tensor_reduce(out=out_, in_=in_, axis=axis, op=op), r, w)

        def memset(eng, ap_, val, w):
            p.add(eng, lambda e: e.memset(ap_, val), (), w)

        phase_no = [0]

        def ph_end(final=False):
            phase_no[0] += 1
            stop = cfg.stop is not None and phase_no[0] >= cfg.stop
            p.flush(final=final or stop)
            if stop and not final:
                raise _Stop()

        identf = sbt(top, "identf", [128, 128], F32)
        identb = sbt(top, "identb", [128, 128], BF16)
        ropeT = sbt(top, "ropeT", [128, 2, L], BF16)
        epsc = sbt(top, "epsc", [128, 1], F32)
        dma("sync", identf[:], ident_in[:, :], [], ["identf"])
        cp("vector", identb[:], identf[:], ["identf"], ["identb"])
        with ExitStack() as ph:
            ropeF = sbt(ph, "ropeF", [128, 2, L], F32)
            dma("sync", ropeF[:, 0, :], rope_in[0, :, :], [], ["ropeF"])
            dma("sync", ropeF[:, 1, :], rope_in[1, :, :], [], ["ropeF"])
            cp("vector", ropeT[:], ropeF[:], ["ropeF"], ["ropeT"])
            memset("vector", epsc[:], EPS, ["epsc"])
            ph_end()

        def layer_norm_rows(st_tag, r_t, rk, g_b, b_b, out_t, ok, tmp):
            stats, mv, rstd = tmp["stats"], tmp["mv"], tmp["rstd"]
            for hh in range(2):
                p.add("vector", lambda e, hh=hh: e.bn_stats(out=stats[:, hh, :], in_=r_t[:, hh * 512:(hh + 1) * 512]),
                      [rk], [st_tag + "stats"])
            p.add("vector", lambda e: e.bn_aggr(out=mv[:], in_=stats[:].rearrange("p a b -> p (a b)")),
                  [st_tag + "stats"], [st_tag + "mv"])
            act(rstd[:], mv[:, 1:2], AF.Sqrt, [st_tag + "mv", "epsc"], [st_tag + "rstd"], bias=epsc[:], scale=1.0)
            p.add("vector", lambda e: e.reciprocal(out=rstd[:], in_=rstd[:]), [st_tag + "rstd"], [st_tag + "rstd"])
            ts("vector", out_t[:], r_t[:], mv[:, 0:1], ALU.subtract, [rk, st_tag + "mv", st_tag + "rstd"], [ok],
               s2=rstd[:, 0:1], op1=ALU.mult)
            tt("gpsimd", out_t[:], out_t[:], g_b[:], ALU.mult, [ok, "lnconst"], [ok])
            tt("gpsimd", out_t[:], out_t[:], b_b[:], ALU.add, [ok, "lnconst"], [ok])

        for l in range(DEPTH):
            last = l == DEPTH - 1
            lam_init = 0.8 - 0.6 * math.exp(-0.3 * l)
            Xsrc = (lambda tok0, n: (x_in[tok0:tok0 + n, :] if tok0 < NB * L else ctx_in[tok0 - NB * L:tok0 - NB * L + n, :])) \
                if l == 0 else (lambda tok0, n: X2d[tok0:tok0 + n, :])
            act_seqs = [s for s in seqs if not (last and s[0] == "ctx")]
            NTm = (NTL if last else NT)
            with ExitStack() as lay:
                lamt = sbt(lay, "lamt", [128, 4], F32)
                gsc = sbt(lay, "gsc", [128, 128], F32)

                with ExitStack() as ph:
                    wf = [sbt(ph, "wf%d" % i, [128, N_IN], F32) for i in range(2)]
                    wbt = [sbt(ph, "wbt%d" % i, [128, NWB], BF16) for i in range(2)]
                    for kt in range(8):
                        a, b_ = wf[kt % 2], wbt[kt % 2]
                        ka, kb = "wf%d" % (kt % 2), "wbt%d" % (kt % 2)
                        dma("sync", a[:], w_in[l, kt * 128:(kt + 1) * 128, :], [], [ka])
                        cp("vector", b_[:, 0:2048], a[:, 0:2048], [ka], [kb + "a"])
                        cp("gpsimd", b_[:, 2048:4096], a[:, 2048:4096], [ka], [kb + "b"])
                        cp("scalar", b_[:, 4096:N_IN], a[:, 4096:N_IN], [ka], [kb + "c"])
                        for qi, off in enumerate((OFF_Q, OFF_K)):
                            for rr in range(2):
                                o_ = _ap(b_[:], N_IN + qi * 512 + rr * 16, [[32, 16], [1, 16]])
                                i_ = _ap(a[:], off + (1 - rr) * 16, [[32, 16], [1, 16]])
                                cp("vector", o_, i_, [ka], [kb + "d%d%d" % (qi, rr)])
                        dma("sync", Wb[kt, :, :], b_[:], [kb + "a", kb + "b", kb + "c", kb + "d00", kb + "d01", kb + "d10", kb + "d11"], ["Wb"])
                    dl = sbt(ph, "dl", [128, 4, 64], F32)
                    dg = sbt(ph, "dg", [128, 128], F32)
                    pr = sbt(ph, "pr", [128, 2, 64], F32)
                    dma("sync", dl[:], bass.AP(tensor=da_l.tensor, offset=da_l.offset + l * 256, ap=[[0, 128], [64, 4], [1, 64]]), [], ["dl"])
                    dma("sync", dg[:], bass.AP(tensor=da_g.tensor, offset=da_g.offset + l * 128, ap=[[0, 128], [1, 128]]), [], ["dg"])
                    tt("vector", pr[:, 0, :], dl[:, 0, :], dl[:, 1, :], ALU.mult, ["dl"], ["pr"])
                    tt("vector", pr[:, 1, :], dl[:, 2, :], dl[:, 3, :], ALU.mult, ["dl"], ["pr"])
                    red(lamt[:, 1:3], pr[:], ALU.add, AX.X, ["pr"], ["lamt"])
                    act(lamt[:, 1:3], lamt[:, 1:3], AF.Exp, ["lamt"], ["lamt"])
                    tt("vector", lamt[:, 0:1], lamt[:, 2:3], lamt[:, 1:2], ALU.subtract, ["lamt"], ["lamt"])
                    ts("vector", lamt[:, 0:1], lamt[:, 0:1], -lam_init, ALU.add, ["lamt"], ["lamt"])
                    ts("vector", gsc[:], dg[:], 1.0 - lam_init, ALU.mult, ["dg"], ["gsc"])
                    ph_end()

                with ExitStack() as ph:
                    cTt = sbt(ph, "cTt", [128, 8, NB + 1], F32)
                    sct = sbt(ph, "sct", [128, 8, NB + 1], F32)
                    adb = sbt(ph, "adb", [NB + 1, 6 * D], F32)
                    modt = sbt(ph, "modt", [NB + 1, 6 * D], F32)
                    awt = [sbt(ph, "awt%d" % i, [128, 3072], F32) for i in range(2)]
                    psm = pst(ph, "psm", [128, 3072])
                    dma("sync", cTt[:], cT_in[:, :, :], [], ["cTt"])
                    dma("sync", adb[:], bass.AP(tensor=ada_b.tensor, offset=ada_b.offset + l * 6 * D,
                                                ap=[[0, NB + 1], [1, 6 * D]]), [], ["adb"])
                    act(sct[:], cTt[:], AF.Silu, ["cTt"], ["sct"])
                    i = 0
                    for half in range(2):
                        for kt in range(8):
                            a, ka = awt[i % 2], "awt%d" % (i % 2)
                            i += 1
                            dma("sync" if kt % 2 == 0 else "gpsimd", a[:],
                                ada_w[l, kt * 128:(kt + 1) * 128, half * 3072:(half + 1) * 3072], [], [ka])
                            for ng in range(6):
                                mm(psm[0:NB + 1, ng * 512:(ng + 1) * 512], sct[:, kt, :], a[:, ng * 512:(ng + 1) * 512],
                                   kt == 0, kt == 7, [ka, "sct"], ["psm"])
                        tt("vector", modt[:, half * 3072:(half + 1) * 3072], psm[0:NB + 1, :],
                           adb[:, half * 3072:(half + 1) * 3072], ALU.add, ["psm", "adb"], ["modt"])
                    dma("sync", MODS[l, :, :], modt[:], ["modt"], ["MODS"])
                    ph_end()

                with ExitStack() as ph:
                    w1f = sbt(ph, "w1f", [HY_EMB, HY_FFN], F32)
                    w2f = sbt(ph, "w2f", [HY_FFN, HY_FFN], F32)
                    w3f = sbt(ph, "w3f", [HY_FFN, 512], F32)
                    b1t = sbt(ph, "b1t", [HY_FFN, 1], F32)
                    b2t = sbt(ph, "b2t", [HY_FFN, 1], F32)
                    b3t = sbt(ph, "b3t", [128, 4], F32)
                    skt = sbt(ph, "skt", [128, 2], F32)
                    dma("sync", w1f[:], hy_w1[l, :, :], [], ["hyw"])
                    dma("sync", w2f[:], hy_w2[l, :, :], [], ["hyw"])
                    dma("sync", w3f[:], hy_w3[l, :, :], [], ["hyw"])
                    dma("sync", b1t[:], hy_b1[l, :, :], [], ["hyw"])
                    dma("sync", b2t[:], hy_b2[l, :, :], [], ["hyw"])
                    dma("sync", b3t[:], hy_b3T[l, :, :], [], ["hyw"])
                    dma("sync", skt[:], hy_skipT[l, :, :], [], ["hyw"])
                    ps1 = pst(ph, "ps1", [128, 512])
                    ps2 = pst(ph, "ps2", [128, 512])
                    ps3 = pst(ph, "ps3", [128, 512])
                    for Lf in ([L] if last else [L, LC]):
                        with ExitStack() as ph2:
                            CHF = min(512, Lf)
                            embt = sbt(ph2, "embt", [HY_EMB, 2, Lf], F32)
                            wint = sbt(ph2, "wint", [128, 2, 2, Lf], F32)
                            krf = sbt(ph2, "krf", [128, 2, 2 * Lf], F32)
                            krb = sbt(ph2, "krb", [128, 2, 2 * Lf], BF16)
                            h1 = sbt(ph2, "h1", [HY_FFN, 512], F32)
                            h2 = sbt(ph2, "h2", [HY_FFN, 512], F32)
                            wr1 = sbt(ph2, "wr1", [HY_FFN, 512], F32)
                            wr2 = sbt(ph2, "wr2", [HY_FFN, 512], F32)
                            tg = "f%d" % Lf
                            for dr in range(2):
                                dma("sync", embt[:, dr, :], emb_in[Lf][dr, :, :], [], [tg + "emb"])
                                for cc in range(2):
                                    dma("gpsimd", wint[:, dr, cc, :], win_in[Lf][dr, cc * 128:(cc + 1) * 128, :], [], [tg + "win"])
                            memset("gpsimd", krf[:], 0.0, [tg + "krf"])
                            for dr in range(2):
                                for ch in range(Lf // CHF):
                                    cs = slice(ch * CHF, (ch + 1) * CHF)
                                    mm(ps1[0:HY_FFN, 0:CHF], w1f[:], embt[:, dr, cs], True, True, ["hyw", tg + "emb"], ["ps1"])
                                    ts("vector", h1[:, 0:CHF], ps1[0:HY_FFN, 0:CHF], b1t[:, 0:1], ALU.add, ["ps1", "hyw"], ["h1"])
                                    ts("vector", wr1[:, 0:CHF], h1[:, 0:CHF], math.pi, ALU.is_gt, ["h1"], ["wr1"], s2=-2 * math.pi, op1=ALU.mult)
                                    ts("vector", wr2[:, 0:CHF], h1[:, 0:CHF], -math.pi, ALU.is_lt, ["h1"], ["wr2"], s2=2 * math.pi, op1=ALU.mult)
                                    tt("vector", h1[:, 0:CHF], h1[:, 0:CHF], wr1[:, 0:CHF], ALU.add, ["h1", "wr1"], ["h1"])
                                    tt("vector", h1[:, 0:CHF], h1[:, 0:CHF], wr2[:, 0:CHF], ALU.add, ["h1", "wr2"], ["h1"])
                                    act(h1[:, 0:CHF], h1[:, 0:CHF], AF.Sin, ["h1"], ["h1"])
                                    mm(ps2[0:HY_FFN, 0:CHF], w2f[:], h1[:, 0:CHF], True, True, ["hyw", "h1"], ["ps2"])
                                    ts("vector", h2[:, 0:CHF], ps2[0:HY_FFN, 0:CHF], b2t[:, 0:1], ALU.add, ["ps2", "hyw"], ["h2"])
                                    ts("vector", wr1[:, 0:CHF], h2[:, 0:CHF], math.pi, ALU.is_gt, ["h2"], ["wr1"], s2=-2 * math.pi, op1=ALU.mult)
                                    ts("vector", wr2[:, 0:CHF], h2[:, 0:CHF], -math.pi, ALU.is_lt, ["h2"], ["wr2"], s2=2 * math.pi, op1=ALU.mult)
                                    tt("vector", h2[:, 0:CHF], h2[:, 0:CHF], wr1[:, 0:CHF], ALU.add, ["h2", "wr1"], ["h2"])
                                    tt("vector", h2[:, 0:CHF], h2[:, 0:CHF], wr2[:, 0:CHF], ALU.add, ["h2", "wr2"], ["h2"])
                                    act(h2[:, 0:CHF], h2[:, 0:CHF], AF.Sin, ["h2"], ["h2"])
                                    for cc in range(2):
                                        mm(ps3[:, 0:CHF], w3f[:, dr * 256 + cc * 128:dr * 256 + (cc + 1) * 128], h2[:, 0:CHF],
                                           True, True, ["hyw", "h2"], ["ps3"])
                                        o0 = dr * Lf + ch * CHF
                                        stt(krf[:, cc, o0:o0 + CHF], ps3[:, 0:CHF], b3t[:, dr * 2 + cc:dr * 2 + cc + 1],
                                            wint[:, dr, cc, cs], ALU.add, ALU.mult, ["ps3", "hyw", tg + "win", tg + "krf"], [tg + "krf"])
                            for cc in range(2):
                                ts("vector", krf[:, cc, Lf - 1:Lf], krf[:, cc, Lf - 1:Lf], skt[:, cc:cc + 1], ALU.add,
                                   [tg + "krf", "hyw"], [tg + "krf"])
                            cp("vector", krb[:, 0, :], krf[:, 0, :], [tg + "krf"], [tg + "krb"])
                            cp("gpsimd", krb[:, 1, :], krf[:, 1, :], [tg + "krf"], [tg + "krb"])
                            for cc in range(2):
                                dma("sync", KREV[Lf][cc * 128:(cc + 1) * 128, :], krb[:, cc, :], [tg + "krb"], ["KREV%d" % Lf])
                            ph_end()

                with ExitStack() as ph45:
                    nbL, nbC = L // 128, LC // 128
                    VXs = {"lat": sbt(ph45, "VXsL", [128, 256, nbL, NB], BF16)}
                    if not last:
                        VXs["ctx"] = sbt(ph45, "VXsC", [128, 256, nbC, NB], BF16)
                    with ExitStack() as ph:
                        LMAX = L
                        hT = sbt(ph, "hT", [128, 8, LMAX], BF16)
                        zhs = Rot([sbt(ph, "zh%d" % i, [128, LMAX + 2], F32) for i in range(2)], "zh")
                        VT2 = sbt(ph, "VT2", [128, 2, LMAX], F32)
                        tmpA = sbt(ph, "tmpA", [128, LMAX], F32)
                        x0bs = Rot([sbt(ph, "x0b%d" % i, [128, LMAX], BF16) for i in range(2)], "x0b")
                        wgs = Rot([sbt(ph, "wg%d" % i, [128, 8, 512], BF16) for i in range(3)], "wg")
                        scb = sbt(ph, "scb", [128, D], F32)
                        shb = sbt(ph, "shb", [128, D], F32)
                        xts = Rot([sbt(ph, "xt%d" % i, [128, D], F32) for i in range(2)], "xt")
                        hbs = Rot([sbt(ph, "hb%d" % i, [128, D], BF16) for i in range(2)], "hb")
                        lngb = sbt(ph, "lngb", [128, 256], F32)
                        lnbb = sbt(ph, "lnbb", [128, 256], F32)
                        wsf = sbt(ph, "wsf", [128, 4, 128], F32)
                        wsb = sbt(ph, "wsb", [128, 4, 128], BF16)
                        bst = sbt(ph, "bst", [128, 4], F32)
                        cwt = sbt(ph, "cwt", [128, 6, 4], F32)
                        gmf = sbt(ph, "gmf", [128, 512], F32)
                        vnb = sbt(ph, "vnb", [128, 256], BF16)
                        vnf = sbt(ph, "vnf", [128, 256], F32)
                        yab = sbt(ph, "yab", [128, 256], BF16)
                        yaT = sbt(ph, "yaT", [128, 2, 128], BF16)
                        gst = sbt(ph, "gst", [128, 6], F32)
                        gmv = sbt(ph, "gmv", [128, 2], F32)
                        grs = sbt(ph, "grs", [128, 1], F32)
                        vts = Rot([sbt(ph, "vt%d" % i, [128, 512], BF16) for i in range(2)], "vt")
                        fos = Rot([sbt(ph, "fo%d" % i, [128, 512], BF16) for i in range(3)], "fo")
                        t1s = Rot([sbt(ph, "t1_%d" % i, [128, 512], F32) for i in range(2)], "t1_")
                        t2s = Rot([sbt(ph, "t2_%d" % i, [128, 512], F32) for i in range(2)], "t2_")
                        VXT = sbt(ph, "VXT", [128, 2, LMAX], BF16)
                        psH = pst(ph, "psH", [128, 8, 128], BF16)
                        psFs = Rot([pst(ph, "psF%d" % i, [128, 512]) for i in range(2)], "psF")
                        psPs = Rot([pst(ph, "psP%d" % i, [128, 512]) for i in range(2)], "psP")
                        psT = pst(ph, "psT", [128, 512])
                        psS = pst(ph, "psS", [128, 256])
                        psYA = pst(ph, "psYA", [128, 2, 128], BF16)
                        dma("sync", lngb[:], bass.AP(tensor=gm_ln_g.tensor, offset=gm_ln_g.offset + l * 256, ap=[[0, 128], [1, 256]]), [], ["gmc"])
                        dma("sync", lnbb[:], bass.AP(tensor=gm_ln_b.tensor, offset=gm_ln_b.offset + l * 256, ap=[[0, 128], [1, 256]]), [], ["gmc"])
                        dma("sync", wsf[:], gm_wsT[l, :, :, :], [], ["wsf"])
                        cp("vector", wsb[:], wsf[:], ["wsf"], ["gmc"])
                        dma("sync", bst[:], gm_bsT[l, :, :], [], ["gmc"])
                        dma("sync", cwt[:], hy_cw[l, :, :, :], [], ["gmc"])
                        for zt in zhs.tiles:
                            memset("vector", zt[:, 0:1], 0.0, ["zh0", "zh1"])

                        for (kind, b, tok0, Ls) in seqs:
                            full = not (last and kind == "ctx")
                            CH = min(512, Ls)
                            nch = Ls // CH
                            ntile = Ls // 128
                            mrow = b if kind == "lat" else NB
                            rope = kind == "lat"
                            dma("sync", shb[:], bass.AP(tensor=MODS.tensor, offset=MODS.offset + (l * (NB + 1) + mrow) * 6 * D,
                                                        ap=[[0, 128], [1, D]]), ["MODS"], ["shb"])
                            dma("sync", scb[:], bass.AP(tensor=MODS.tensor, offset=MODS.offset + (l * (NB + 1) + mrow) * 6 * D + D,
                                                        ap=[[0, 128], [1, D]]), ["MODS"], ["scb"])
                            ts("gpsimd", scb[:], scb[:], 1.0, ALU.add, ["scb"], ["scb"])
                            for zt in zhs.tiles:
                                memset("vector", zt[:, Ls + 1:Ls + 2], 0.0, ["zh0", "zh1"])
                            for tI in range(ntile):
                                xt, xk = xts.next()
                                hb, hk = hbs.next()
                                dma("sync" if tI % 2 == 0 else "gpsimd", xt[:], Xsrc(tok0 + tI * 128, 128), ["X2d"] if l else [], [xk])
                                tt("vector", xt[:], xt[:], scb[:], ALU.mult, [xk, "scb"], [xk])
                                tt("gpsimd", hb[:], xt[:], shb[:], ALU.add, [xk, "shb"], [hk])
                                for kt in range(8):
                                    tr(psH[:, kt, :], hb[:, kt * 128:(kt + 1) * 128], identb[:], [hk, "identb"], ["psH"])
                                cp("scalar", hT[:, :, tI * 128:(tI + 1) * 128], psH[:], ["psH"], ["hT"])

                            def load_group(col0):
                                wg, wk = wgs.next()
                                dma("sync", wg[:], Wb[:, :, col0:col0 + 512].rearrange("k p c -> p k c"), ["Wb"], [wk])
                                return wg, wk

                            def fm_chunk(wg, wk, ci, tc, ps, pk):
                                for kt in range(8):
                                    mm(ps[:, 0:CH], wg[:, kt, ci * 128:(ci + 1) * 128], hT[:, kt, tc * CH:(tc + 1) * CH],
                                       kt == 0, kt == 7, [wk, "hT"], [pk])

                            if full:
                                wg, wk = load_group(0)
                                for tI in range(ntile):
                                    for kt in range(8):
                                        mm(psT[:], hT[:, kt, tI * 128:(tI + 1) * 128], wg[:, kt, :], kt == 0, kt == 7, [wk, "hT"], ["psT"])
                                    act(gmf[:], psT[:], AF.Gelu, ["psT"], ["gmf"])
                                    p.add("vector", lambda e: e.bn_stats(out=gst[:], in_=gmf[:, 256:512]), ["gmf"], ["gst"])
                                    p.add("vector", lambda e: e.bn_aggr(out=gmv[:], in_=gst[:]), ["gst"], ["gmv"])
                                    act(grs[:], gmv[:, 1:2], AF.Sqrt, ["gmv", "epsc"], ["grs"], bias=epsc[:], scale=1.0)
                                    p.add("vector", lambda e: e.reciprocal(out=grs[:], in_=grs[:]), ["grs"], ["grs"])
                                    ts("vector", vnf[:], gmf[:, 256:512], gmv[:, 0:1], ALU.subtract, ["gmf", "gmv", "grs"], ["vnf"],
                                       s2=grs[:, 0:1], op1=ALU.mult)
                                    tt("gpsimd", vnf[:], vnf[:], lngb[:], ALU.mult, ["vnf", "gmc"], ["vnf"])
                                    tt("gpsimd", vnb[:], vnf[:], lnbb[:], ALU.add, ["vnf", "gmc"], ["vnb"])
                                    for g in range(4):
                                        mm(psS[:, g * 64:(g + 1) * 64], wsb[:, g, :], vnb[:, g * 64:(g + 1) * 64], True, True, ["gmc", "vnb"], ["psS"])
                                    for g in range(4):
                                        stt(yab[:, g * 64:(g + 1) * 64], psS[:, g * 64:(g + 1) * 64], bst[:, g:g + 1],
                                            gmf[:, g * 64:(g + 1) * 64], ALU.add, ALU.mult, ["psS", "gmc", "gmf"], ["yab"])
                                    for cc in range(2):
                                        tr(psYA[:, cc, :], yab[:, cc * 128:(cc + 1) * 128], identb[:], ["yab", "identb"], ["psYA"])
                                    cp("scalar", yaT[:], psYA[:], ["psYA"], ["yaT"])
                                    t0 = tok0 + tI * 128
                                    dma("gpsimd", YATd[:, :, t0:t0 + 128].rearrange("c p t -> p c t"), yaT[:], ["yaT"], ["YATd"])
                                for (col0, cis) in ((OFF_HY + 512, (1, 0)), (OFF_HY, (3, 2, 1, 0))):
                                    wg, wk = load_group(col0)
                                    for ci in cis:
                                        r_ = (col0 - OFF_HY) // 128 + ci
                                        zh, zk = zhs.next()
                                        for tc in range(nch):
                                            ps, pk = psFs.next()
                                            fm_chunk(wg, wk, ci, tc, ps, pk)
                                            cp("scalar", zh[:, 1 + tc * CH:1 + (tc + 1) * CH], ps[:, 0:CH], [pk], [zk])
                                        if r_ >= 4:
                                            acc, ak = VT2[:, r_ - 4, 0:Ls], "VT2"
                                        else:
                                            acc, ak = tmpA[:, 0:Ls], "tmpA"
                                        ts("vector", acc, zh[:, 0:Ls], cwt[:, r_, 0:1], ALU.mult, [zk, "gmc"], [ak],
                                           s2=cwt[:, r_, 3:4], op1=ALU.add)
                                        stt(acc, zh[:, 1:Ls + 1], cwt[:, r_, 1:2], acc, ALU.mult, ALU.add, [zk, "gmc", ak], [ak])
                                        if r_ >= 4:
                                            stt(acc, zh[:, 2:Ls + 2], cwt[:, r_, 2:3], acc, ALU.mult, ALU.add, [zk, "gmc", ak], [ak])
                                        elif r_ >= 2:
                                            stt(acc, zh[:, 2:Ls + 2], cwt[:, r_, 2:3], acc, ALU.mult, ALU.add, [zk, "gmc", ak], [ak])
                                            tt("gpsimd", VXT[:, r_ - 2, 0:Ls], acc, VT2[:, r_ - 2, 0:Ls], ALU.mult, [ak, "VT2"], ["VXT"])
                                        else:
                                            x0b, x0k = x0bs.next()
                                            stt(x0b[:, 0:Ls], zh[:, 2:Ls + 2], cwt[:, r_, 2:3], acc, ALU.mult, ALU.add, [zk, "gmc", ak], [x0k])
                                            dma("gpsimd", X0Td[r_, :, tok0:tok0 + Ls], x0b[:, 0:Ls], [x0k], ["X0Td"])
                                vx = VXs[kind]
                                for tI in range(ntile):
                                    for cc in range(2):
                                        tr(psYA[:, cc, :], VXT[:, cc, tI * 128:(tI + 1) * 128], identb[:], ["VXT", "identb"], ["psYA"])
                                    cp("scalar", vx[:, :, tI, b], psYA[:].rearrange("p a b -> p (a b)"), ["psYA"], ["VXs" + kind])
                            for (nm, colA, colB, dst) in (("q", OFF_Q, N_IN, QTd), ("k", OFF_K, N_IN + 512, KTd)):
                                if nm == "q" and not full:
                                    continue
                                wgA, wkA = load_group(colA)
                                if rope:
                                    wgB, wkB = load_group(colB)
                                for h in range(4):
                                    for tc in range(nch):
                                        ps, pk = psFs.next()
                                        fm_chunk(wgA, wkA, h, tc, ps, pk)
                                        fo, fk = fos.next()
                                        if rope:
                                            ps2_, pk2 = psPs.next()
                                            fm_chunk(wgB, wkB, h, tc, ps2_, pk2)
                                            t1, k1 = t1s.next()
                                            t2, k2 = t2s.next()
                                            tt("vector", t1[:, 0:CH], ps[:, 0:CH], ropeT[:, 0, tc * CH:(tc + 1) * CH], ALU.mult, [pk, "ropeT"], [k1])
                                            tt("vector", t2[:, 0:CH], ps2_[:, 0:CH], ropeT[:, 1, tc * CH:(tc + 1) * CH], ALU.mult, [pk2, "ropeT"], [k2])
                                            tt("gpsimd", fo[:, 0:CH], t1[:, 0:CH], t2[:, 0:CH], ALU.add, [k1, k2], [fk])
                                        else:
                                            cp("scalar", fo[:, 0:CH], ps[:, 0:CH], [pk], [fk])
                                        t0 = tok0 + tc * CH
                                        dma("gpsimd", dst[h, :, t0:t0 + CH], fo[:, 0:CH], [fk], [nm + "Td"])
                            wg, wk = load_group(OFF_V)
                            for tI in range(ntile):
                                for kt in range(8):
                                    mm(psT[:], hT[:, kt, tI * 128:(tI + 1) * 128], wg[:, kt, :], kt == 0, kt == 7, [wk, "hT"], ["psT"])
                                vt, vk = vts.next()
                                cp("scalar", vt[:], psT[:], ["psT"], [vk])
                                t0 = tok0 + tI * 128
                                dma("gpsimd", Vd[t0:t0 + 128, :], vt[:], [vk], ["Vd"])
                            if full:
                                for gi in range(6):
                                    wg, wk = load_group(OFF_GATE + gi * 512)
                                    for ci in range(4):
                                        for tc in range(nch):
                                            ps, pk = psFs.next()
                                            fm_chunk(wg, wk, ci, tc, ps, pk)
                                            fo, fk = fos.next()
                                            act(fo[:, 0:CH], ps[:, 0:CH], AF.Sigmoid, [pk], [fk])
                                            t0 = tok0 + tc * CH
                                            dma("gpsimd", Gd[gi * 4 + ci, :, t0:t0 + CH], fo[:, 0:CH], [fk], ["Gd"])
                        ph_end()

                    with ExitStack() as ph:
                        hsk = Rot([sbt(ph, "hsk%d" % i, [128, 128 * (2 * nbL - 1)], BF16) for i in range(3)], "hsk")
                        psYs = Rot([pst(ph, "psY%d" % i, [128, 8, nbL * NB]) for i in range(2)], "psY")
                        psTt = pst(ph, "psTt", [128, 2, 128], BF16)
                        YBT = sbt(ph, "YBT", [128, 2, T], BF16)
                        x0ls = Rot([sbt(ph, "x0l%d" % i, [128, 2, L], BF16) for i in range(2)], "x0l")
                        for kind in (["lat"] if last else ["lat", "ctx"]):
                            Lf = L if kind == "lat" else LC
                            nb = Lf // 128
                            W = 128 * (2 * nb - 1)
                            with ExitStack() as ph2:
                                Yr = sbt(ph2, "Yr" + kind, [128, nb, NB, 256], BF16)
                                vx = VXs[kind]
                                kdr = KREV[Lf]
                                for cg in range(32):
                                    psY, pyk = psYs.next()
                                    for c8 in range(8):
                                        c = cg * 8 + c8
                                        hk_, hkk = hsk.next()
                                        dma("sync" if c % 2 == 0 else "gpsimd", hk_[:, 0:W],
                                            bass.AP(tensor=kdr.tensor, offset=kdr.offset + c * 2 * Lf, ap=[[1, 128], [1, W]]),
                                            ["KREV%d" % Lf], [hkk])
                                        lags = [0] + [d for d in range(-(nb - 1), nb) if d != 0]
                                        for di, d in enumerate(lags):
                                            j0, j1 = max(0, -d), min(nb, nb - d)
                                            m0 = 128 * (nb - 1 - d)
                                            o_ = _ap(psY[:], c8 * nbL * NB + (j0 + d) * NB, [[1, (j1 - j0) * NB]])
                                            mm(o_, hk_[:, m0:m0 + 128], vx[:, c, j0:j1, :].rearrange("p j b -> p (j b)"),
                                               di == 0, di == len(lags) - 1, [hkk, "VXs" + kind], [pyk])
                                    src = _ap(psY[:], 0, [[nbL * NB, 8], [NB, nb], [1, NB]])
                                    dst_ = _ap(Yr[:], cg * 8, [[1, 8], [NB * 256, nb], [256, NB]])
                                    cp("scalar" if cg % 2 == 0 else "vector", dst_, src, [pyk], ["Yr"])
                                for b in range(NB):
                                    tok0 = b * L if kind == "lat" else NB * L + b * LC
                                    x0l, x0lk = x0ls.next()
                                    dma("sync", x0l[:, :, 0:Lf], X0Td[:, :, tok0:tok0 + Lf].rearrange("c p t -> p c t"), ["X0Td"], [x0lk])
                                    for i_ in range(nb):
                                        for cc in range(2):
                                            tr(psTt[:, cc, :], Yr[:, i_, b, cc * 128:(cc + 1) * 128], identb[:], ["Yr", "identb"], ["psTt"])
                                        t0 = tok0 + i_ * 128
                                        for cc in range(2):
                                            rev = _ap(psTt[:], cc * 128 + 127, [[-1, 128]])
                                            tt("vector", YBT[:, cc, t0:t0 + 128], rev, x0l[:, cc, i_ * 128:(i_ + 1) * 128], ALU.mult,
                                               ["psTt", x0lk], ["YBT"])
                        ntok = NB * L if last else T
                        for cc in range(2):
                            dma("sync", YBTd[cc, :, 0:ntok], YBT[:, cc, 0:ntok], ["YBT"], ["YBTd"])
                        ph_end()

                with ExitStack() as ph:
                    NKmax = (L + LC) // 128
                    QT = sbt(ph, "QT", [128, 4, 2, L], BF16)
                    KT = sbt(ph, "KT", [128, 4, L + LC], BF16)
                    Vone = sbt(ph, "Vone", [128, NKmax, 4, 129], BF16)
                    Es = Rot([sbt(ph, "E%d" % i, [128, 512], BF16) for i in range(3)], "E")
                    psSs = Rot([pst(ph, "psA%d" % i, [128, 512]) for i in range(2)], "psA")
                    acc = pst(ph, "acc", [128, 4, 2, 256])
                    psC = pst(ph, "psC", [128, 4, 128], BF16)
                    rec = sbt(ph, "rec", [128, 4, 2], F32)
                    o1 = sbt(ph, "o1", [128, 128], F32)
                    o2 = sbt(ph, "o2", [128, 128], F32)
                    sq = sbt(ph, "sq", [128, 128], F32)
                    ss = sbt(ph, "ss", [128, 1], F32)
                    YC = sbt(ph, "YC", [128, 4, 512], BF16)
                    ycts = Rot([sbt(ph, "yct%d" % i, [128, 4, 512], BF16) for i in range(2)], "yct")
                    memset("vector", Vone[:], 1.0, ["Vone"])
                    memset("gpsimd", QT[:], 0.0, ["QT"])
                    for (kind, b, tok0, Lq) in act_seqs:
                        if kind == "lat":
                            ksegs = [(tok0, L), (NB * L + b * LC, LC)]
                        else:
                            ksegs = [(tok0, LC)]
                        NK = sum(s[1] for s in ksegs) // 128
                        for m in range(2):
                            dma("sync", QT[m * 64:(m + 1) * 64, :, m, 0:Lq],
                                QTd[:, m * 64:(m + 1) * 64, tok0:tok0 + Lq].rearrange("h p t -> p h t"), ["qTd"], ["QT"])
                        ko = 0
                        for (kt0, kl) in ksegs:
                            dma("gpsimd", KT[:, :, ko:ko + kl], KTd[:, :, kt0:kt0 + kl].rearrange("h p t -> p h t"), ["kTd"], ["KT"])
                            for j in range(kl // 128):
                                dma("sync" if j % 2 else "gpsimd", Vone[:, ko // 128 + j, :, 0:128],
                                    Vd[kt0 + j * 128:kt0 + (j + 1) * 128, :].rearrange("t (h e) -> t h e", h=4), ["Vd"], ["Vone"])
                            ko += kl
                        QC = min(512, Lq)
                        nsub = QC // 128
                        for qc in range(Lq // QC):
                            steps = [(h, m, kt) for h in range(4) for m in range(2) for kt in range(NK)]

                            def emit_S(st_):
                                h, m, kt = st_
                                ms = slice(m * 64, (m + 1) * 64)
                                psA, pak = psSs.next()
                                mm(psA[:, 0:QC], KT[:, h, kt * 128:(kt + 1) * 128], QT[:, h, m, qc * QC:(qc + 1) * QC],
                                   True, True, ["KT", "QT"], [pak])
                                return psA, pak

                            nxt = emit_S(steps[0])
                            for si, (h, m, kt) in enumerate(steps):
                                psA, pak = nxt
                                E, ek = Es.next()
                                act(E[:, 0:QC], psA[:, 0:QC], AF.Exp, [pak], [ek], scale=0.125)
                                if si + 1 < len(steps):
                                    nxt = emit_S(steps[si + 1])
                                for qs in range(nsub):
                                    mm(acc[:, qs, m, 0:129], E[:, qs * 128:(qs + 1) * 128], Vone[:, kt, h, :],
                                       kt == 0, kt == NK - 1, [ek, "Vone"], ["acc"])
                                if not (m == 1 and kt == NK - 1):
                                    continue
                                p.add("vector", lambda e, nsub=nsub: e.reciprocal(out=rec[:, 0:nsub, :], in_=acc[:, 0:nsub, :, 128]), ["acc"], ["rec"])
                                ts("vector", rec[:, 0:nsub, 1], rec[:, 0:nsub, 1], lamt[:, 0:1], ALU.mult, ["rec", "lamt"], ["rec"])
                                for qs in range(nsub):
                                    ts("vector", o1[:], acc[:, qs, 0, 0:128], rec[:, qs, 0:1], ALU.mult, ["acc", "rec"], ["o1"])
                                    stt(o2[:], acc[:, qs, 1, 0:128], rec[:, qs, 1:2], o1[:], ALU.mult, ALU.add, ["acc", "rec", "o1"], ["o2"])
                                    tt("gpsimd", sq[:], o2[:], o2[:], ALU.mult, ["o2"], ["sq"])
                                    red(ss[:], sq[:], ALU.add, AX.X, ["sq"], ["ss"])
                                    act(ss[:], ss[:], AF.Sqrt, ["ss", "epsc"], ["ss"], bias=epsc[:], scale=1.0 / 128.0)
                                    p.add("vector", lambda e: e.reciprocal(out=ss[:], in_=ss[:]), ["ss"], ["ss"])
                                    stt(YC[:, qs, h * 128:(h + 1) * 128], o2[:], ss[:, 0:1], gsc[:], ALU.mult, ALU.mult,
                                        ["o2", "ss", "gsc"], ["YC"])
                            yct, yk = ycts.next()
                            for qs in range(nsub):
                                for h in range(4):
                                    tr(psC[:, h, :], YC[:, qs, h * 128:(h + 1) * 128], identb[:], ["YC", "identb"], ["psC"])
                                cp("scalar", yct[:, :, qs * 128:(qs + 1) * 128], psC[:], ["psC"], [yk])
                            t0 = tok0 + qc * QC
                            dma("sync", YCTd[:, :, t0:t0 + QC].rearrange("h p t -> p h t"), yct[:, :, 0:QC], [yk], ["YCTd"])
                    ph_end()

                LOG = sbt(lay, "LOG", [128, NT, 36], F32)
                WTS = sbt(lay, "WTS", [128, NT, 2], F32)
                DEST = sbt(lay, "DEST", [128, NT, 2], I32)
                IDXE = sbt(lay, "IDXE", [128, NBLK], I32)
                with ExitStack() as ph:
                    pab = sbt(ph, "pab", [128, 2, D], BF16)
                    pbb = sbt(ph, "pbb", [128, 2, D], BF16)
                    pcb = sbt(ph, "pcb", [128, 4, D], BF16)
                    wob = sbt(ph, "wob", [128, 8, D], BF16)
                    wrb = sbt(ph, "wrb", [128, 8, 36], BF16)
                    wrf = sbt(ph, "wrf", [128, 8, 36], F32)
                    rbb = sbt(ph, "rbb", [128, 36], F32)
                    stg = Rot([sbt(ph, "stg%d" % i, [128, 2, D], F32) for i in range(2)], "stg")
                    lg = sbt(ph, "lg", [128, D], F32)
                    lb = sbt(ph, "lb", [128, D], F32)
                    g1b = sbt(ph, "g1b", [128, D], F32)
                    sc2b = sbt(ph, "sc2b", [128, D], F32)
                    sh2b = sbt(ph, "sh2b", [128, D], F32)
                    si = 0
                    for (src, nk, dstw) in ((p_a, 2, pab), (p_b, 2, pbb), (p_c, 4, pcb), (w_out, 8, wob)):
                        for k2 in range(0, nk, 2):
                            s_, sk_ = stg.next()
                            dma("sync", s_[:], src[l, k2 * 128:(k2 + 2) * 128, :].rearrange("(k p) d -> p k d", p=128), [], [sk_])
                            cp("vector" if si % 2 == 0 else "gpsimd", dstw[:, k2:k2 + 2, :], s_[:], [sk_], ["mw"])
                            si += 1
                    dma("sync", wrf[:], moe_wr[l, :, :].rearrange("(k p) e -> p k e", p=128), [], ["wrf"])
                    cp("vector", wrb[:], wrf[:], ["wrf"], ["mw"])
                    dma("sync", rbb[:], bass.AP(tensor=moe_br.tensor, offset=moe_br.offset + l * 36, ap=[[0, 128], [1, 36]]), [], ["mw"])
                    dma("sync", lg[:], bass.AP(tensor=ln1_g.tensor, offset=ln1_g.offset + l * D, ap=[[0, 128], [1, D]]), [], ["lnconst"])
                    dma("sync", lb[:], bass.AP(tensor=ln1_b.tensor, offset=ln1_b.offset + l * D, ap=[[0, 128], [1, D]]), [], ["lnconst"])
                    yaTs = sbt(ph, "yaTs", [128, 2, 512], BF16)
                    ybTs = sbt(ph, "ybTs", [128, 2, 512], BF16)
                    ycTs = sbt(ph, "ycTs", [128, 4, 512], BF16)
                    Gs = sbt(ph, "Gs", [128, 24, 512], BF16)
                    mT = sbt(ph, "mT", [128, 8, 512], BF16)
                    ta = sbt(ph, "ta", [128, 512], F32)
                    tb = sbt(ph, "tb", [128, 512], F32)
                    tcx = sbt(ph, "tcx", [128, 512], F32)
                    xr = Rot([sbt(ph, "xr%d" % i, [128, D], F32) for i in range(2)], "xr")
                    rr1s = Rot([sbt(ph, "rr1_%d" % i, [128, D], F32) for i in range(2)], "rr1_")
                    x1t = Rot([sbt(ph, "x1t%d" % i, [128, D], F32) for i in range(2)], "x1t")
                    h2fs = Rot([sbt(ph, "h2f%d" % i, [128, D], F32) for i in range(2)], "h2f")
                    h2b = Rot([sbt(ph, "h2b%d" % i, [128, D], BF16) for i in range(2)], "h2b")
                    h2T = sbt(ph, "h2T", [128, 8, 128], BF16)
                    lntmps = [{"stats": sbt(ph, "lnst", [128, 2, 6], F32), "mv": sbt(ph, "lnmv", [128, 2], F32),
                               "rstd": sbt(ph, "lnrs", [128, 1], F32)} for _ in range(2)]
                    lni = [0]
                    psa = pst(ph, "psa", [128, 512])
                    psb = pst(ph, "psb", [128, 512])
                    psc = pst(ph, "psc", [128, 512])
                    psO = Rot([pst(ph, "psO%d" % i, [128, 512]) for i in range(2)], "psO")
                    psh = pst(ph, "psh", [128, 8, 128], BF16)
                    psl = pst(ph, "psl", [128, 36])
                    pend_router = []
                    for (kind, b, tok0, Ls) in act_seqs:
                        mrow = b if kind == "lat" else NB
                        mo = MODS.offset + (l * (NB + 1) + mrow) * 6 * D
                        dma("sync", g1b[:], bass.AP(tensor=MODS.tensor, offset=mo + 2 * D, ap=[[0, 128], [1, D]]), ["MODS"], ["g1b"])
                        dma("sync", sh2b[:], bass.AP(tensor=MODS.tensor, offset=mo + 3 * D, ap=[[0, 128], [1, D]]), ["MODS"], ["sh2b"])
                        dma("sync", sc2b[:], bass.AP(tensor=MODS.tensor, offset=mo + 4 * D, ap=[[0, 128], [1, D]]), ["MODS"], ["sc2b"])
                        ts("gpsimd", sc2b[:], sc2b[:], 1.0, ALU.add, ["sc2b"], ["sc2b"])
                        CH = min(512, Ls)
                        for tc in range(Ls // CH):
                            t0 = tok0 + tc * CH
                            dma("sync", yaTs[:, :, 0:CH], YATd[:, :, t0:t0 + CH].rearrange("c p t -> p c t"), ["YATd"], ["yaTs"])
                            dma("gpsimd", ybTs[:, :, 0:CH], YBTd[:, :, t0:t0 + CH].rearrange("c p t -> p c t"), ["YBTd"], ["ybTs"])
                            dma("sync", ycTs[:, :, 0:CH], YCTd[:, :, t0:t0 + CH].rearrange("c p t -> p c t"), ["YCTd"], ["ycTs"])
                            dma("gpsimd", Gs[:, :, 0:CH], Gd[:, :, t0:t0 + CH].rearrange("c p t -> p c t"), ["Gd"], ["Gs"])
                            for dc in range(8):
                                ds_ = slice(dc * 128, (dc + 1) * 128)
                                for kt in range(2):
                                    mm(psa[:, 0:CH], pab[:, kt, ds_], yaTs[:, kt, 0:CH], kt == 0, kt == 1, ["mw", "yaTs"], ["psa"])
                                for kt in range(2):
                                    mm(psb[:, 0:CH], pbb[:, kt, ds_], ybTs[:, kt, 0:CH], kt == 0, kt == 1, ["mw", "ybTs"], ["psb"])
                                for kt in range(4):
                                    mm(psc[:, 0:CH], pcb[:, kt, ds_], ycTs[:, kt, 0:CH], kt == 0, kt == 3, ["mw", "ycTs"], ["psc"])
                                tt("vector", ta[:, 0:CH], psa[:, 0:CH], Gs[:, dc, 0:CH], ALU.mult, ["psa", "Gs"], ["ta"])
                                tt("vector", tb[:, 0:CH], psb[:, 0:CH], Gs[:, 8 + dc, 0:CH], ALU.mult, ["psb", "Gs"], ["tb"])
                                tt("vector", tcx[:, 0:CH], psc[:, 0:CH], Gs[:, 16 + dc, 0:CH], ALU.mult, ["psc", "Gs"], ["tcx"])
                                tt("gpsimd", ta[:, 0:CH], ta[:, 0:CH], tb[:, 0:CH], ALU.add, ["ta", "tb"], ["ta"])
                                tt("gpsimd", mT[:, dc, 0:CH], ta[:, 0:CH], tcx[:, 0:CH], ALU.add, ["ta", "tcx"], ["mT"])
                            for tsb in range(CH // 128):
                                tk0 = t0 + tsb * 128
                                gt = tk0 // 128
                                xt, xk = xr.next()
                                rr_, rrk = rr1s.next()
                                h2f, h2fk = h2fs.next()
                                dma("sync", xt[:], Xsrc(tk0, 128), ["X2d"] if l else [], [xk])
                                for half in range(2):
                                    hs = slice(half * 512, (half + 1) * 512)
                                    pso, pok = psO.next()
                                    for kt in range(8):
                                        mm(pso[:], mT[:, kt, tsb * 128:(tsb + 1) * 128], wob[:, kt, hs], kt == 0, kt == 7, ["mw", "mT"], [pok])
                                    tt("vector", rr_[:, hs], pso[:], g1b[:, hs], ALU.mult, [pok, "g1b"], [rrk])
                                while pend_router:
                                    pend_router.pop(0)()
                                stt(rr_[:], xt[:], ALPHA, rr_[:], ALU.mult, ALU.add, [xk, rrk], [rrk])
                                x1, x1k = x1t.next()
                                lni[0] += 1
                                layer_norm_rows("ln1%d" % (lni[0] % 2), rr_, rrk, lg, lb, x1, x1k, lntmps[lni[0] % 2])
                                dma("sync", X1d[tk0:tk0 + 128, :], x1[:], [x1k], ["X1d"])
                                tt("vector", h2f[:], x1[:], sc2b[:], ALU.mult, [x1k, "sc2b"], [h2fk])
                                hb_, hbk = h2b.next()
                                tt("gpsimd", hb_[:], h2f[:], sh2b[:], ALU.add, [h2fk, "sh2b"], [hbk])
                                dma("gpsimd", H2d[tk0:tk0 + 128, :], hb_[:], [hbk], ["H2d"])
                                def router(hb_=hb_, hbk=hbk, gt=gt):
                                    for kt in range(8):
                                        tr(psh[:, kt, :], hb_[:, kt * 128:(kt + 1) * 128], identb[:], [hbk, "identb"], ["psh"])
                                    cp("scalar", h2T[:], psh[:], ["psh"], ["h2T"])
                                    for kt in range(8):
                                        mm(psl[:], h2T[:, kt, :], wrb[:, kt, :], kt == 0, kt == 7, ["h2T", "mw"], ["psl"])
                                    tt("vector", LOG[:, gt, :], psl[:], rbb[:], ALU.add, ["psl", "mw"], ["LOG"])
                                pend_router.append(router)
                    while pend_router:
                        pend_router.pop(0)()
                    ph_end()

                with ExitStack() as ph:
                    NC_ = NTm * 32
                    cm = sbt(ph, "cm", [128, 128 + 8 + 4 + MMAX + NBLK + 32], F32)
                    dma("sync", cm[:], cmoe_in[:, :], [], ["cm"])
                    ustr = sbt(ph, "ustr", [128, 128], BF16)
                    cp("vector", ustr[:], cm[:, 0:128], ["cm"], ["ustr"])
                    o_kp8, o_kp4, o_mrow, o_nrow, o_i32 = 128, 136, 140, 140 + MMAX, 140 + MMAX + NBLK
                    onesb = sbt(ph, "onesb", [128, 1], BF16)
                    onesf = sbt(ph, "onesf", [32, 128], F32)
                    memset("vector", onesb[:], 1.0, ["onesb"])
                    memset("vector", onesf[:], 1.0, ["onesf"])
                    gmax = sbt(ph, "gmax", [128, NTm], F32)
                    goh = sbt(ph, "goh", [128, NTm, 4], F32)
                    gex = sbt(ph, "gex", [128, NTm, 4], F32)
                    gsum = sbt(ph, "gsum", [128, NTm], F32)
                    EM = sbt(ph, "EM", [128, NTm, 32], F32)
                    oh1 = sbt(ph, "oh1", [128, NTm, 32], F32)
                    oh2 = sbt(ph, "oh2", [128, NTm, 32], F32)
                    m1 = sbt(ph, "m1", [128, NTm], F32)
                    m2 = sbt(ph, "m2", [128, NTm], F32)
                    dd = sbt(ph, "dd", [128, NTm], F32)
                    Mb = sbt(ph, "Mb", [128, NTm, 32], BF16)
                    POS = sbt(ph, "POS", [128, NTm, 32], F32)
                    tmpP = sbt(ph, "tmpP", [128, NTm, 32], F32)
                    dstf = sbt(ph, "dstf", [128, NTm, 2], F32)
                    tot = sbt(ph, "tot", [32, NTm], F32)
                    cum = sbt(ph, "cum", [32, NTm], F32)
                    ones32 = sbt(ph, "ones32", [32, NTm], F32)
                    cnt = sbt(ph, "cnt", [32, 4], F32)
                    cmpm = sbt(ph, "cmpm", [32, MMAX], F32)
                    base = sbt(ph, "base", [32, NTm], F32)
                    R = sbt(ph, "R", [32, NTm, 32], F32)
                    dg32 = sbt(ph, "dg32", [32, 32], F32)
                    pendr = sbt(ph, "pendr", [128, 32], F32)
                    cmpb = sbt(ph, "cmpb", [128, NBLK, 32], F32)
                    be = sbt(ph, "be", [128, NBLK], F32)
                    idf = sbt(ph, "idf", [128, NBLK, 8], F32)
                    NPS = -(-NC_ // 512)
                    psR = pst(ph, "psR", [128, NPS * 512])
                    psTot = pst(ph, "psTot", [128, 512])
                    psSm = pst(ph, "psSm", [128, 512])
                    Lg = LOG[:, 0:NTm, :]
                    G4 = LOG[:, 0:NTm, 0:4]
                    E32 = LOG[:, 0:NTm, 4:36]

                    def bc_last(t2d, n):
                        a = t2d
                        return bass.AP(tensor=a.tensor, offset=a.offset, ap=[list(a.ap[0]), list(a.ap[1]), [0, n]])

                    red(gmax[:], G4, ALU.max, AX.X, ["LOG"], ["gmax"])
                    tt("vector", goh[:], G4, bc_last(gmax[:], 4), ALU.is_equal, ["LOG", "gmax"], ["goh"])
                    tt("vector", gex[:], G4, bc_last(gmax[:], 4), ALU.subtract, ["LOG", "gmax"], ["gex"])
                    act(gex[:], gex[:], AF.Exp, ["gex"], ["gex"])
                    red(gsum[:], gex[:], ALU.add, AX.X, ["gex"], ["gsum"])
                    p.add("vector", lambda e: e.reciprocal(out=gsum[:], in_=gsum[:]), ["gsum"], ["gsum"])
                    ts("vector", goh[:], goh[:], 1.0, ALU.subtract, ["goh"], ["goh"], s2=BIG, op1=ALU.mult)
                    gb = goh[:]
                    p.add("vector", lambda e: e.tensor_tensor(
                        out=bass.AP(tensor=EM[:].tensor, offset=EM[:].offset, ap=[list(EM[:].ap[0]), [32, NTm], [8, 4], [1, 8]]),
                        in0=bass.AP(tensor=LOG[:].tensor, offset=LOG[:].offset + 4, ap=[list(LOG[:].ap[0]), [36, NTm], [8, 4], [1, 8]]),
                        in1=bass.AP(tensor=gb.tensor, offset=gb.offset, ap=[list(gb.ap[0]), [4, NTm], [1, 4], [0, 8]]),
                        op=ALU.add), ["LOG", "goh"], ["EM"])
                    red(m1[:], EM[:], ALU.max, AX.X, ["EM"], ["m1"])
                    tt("vector", oh1[:], EM[:], bc_last(m1[:], 32), ALU.is_equal, ["EM", "m1"], ["oh1"])
                    stt(EM[:], oh1[:], -BIG, EM[:], ALU.mult, ALU.add, ["oh1", "EM"], ["EM"])
                    red(m2[:], EM[:], ALU.max, AX.X, ["EM"], ["m2"])
                    tt("vector", oh2[:], EM[:], bc_last(m2[:], 32), ALU.is_equal, ["EM", "m2"], ["oh2"])
                    tt("vector", dd[:], m2[:], m1[:], ALU.subtract, ["m1", "m2"], ["dd"])
                    act(dd[:], dd[:], AF.Exp, ["dd"], ["dd"])
                    ts("vector", dd[:], dd[:], 1.0, ALU.add, ["dd"], ["dd"])
                    p.add("vector", lambda e: e.reciprocal(out=dd[:], in_=dd[:]), ["dd"], ["dd"])
                    tt("vector", WTS[:, 0:NTm, 0], dd[:], gsum[:], ALU.mult, ["dd", "gsum"], ["WTS"])
                    tt("vector", WTS[:, 0:NTm, 1], gsum[:], WTS[:, 0:NTm, 0], ALU.subtract, ["gsum", "WTS"], ["WTS"])
                    tt("vector", Mb[:], oh1[:], oh2[:], ALU.add, ["oh1", "oh2"], ["Mb"])
                    Mf = Mb[:].rearrange("p t e -> p (t e)")
                    for c_ in range(NPS):
                        n_ = min(512, NC_ - c_ * 512)
                        mm(psR[:, c_ * 512:c_ * 512 + n_], ustr[:], Mf[:, c_ * 512:c_ * 512 + n_], True, True, ["ustr", "Mb"], ["psR"])
                    for t_ in range(NTm):
                        mm(psTot[0:32, t_:t_ + 1], Mb[:, t_, :], onesb[:], True, True, ["Mb", "onesb"], ["psTot"])
                    cp("vector", tot[:], psTot[0:32, 0:NTm], ["psTot"], ["tot"])
                    memset("vector", ones32[:], 1.0, ["ones32"])
                    p.add("vector", lambda e: e.tensor_tensor_scan(out=cum[:], data0=ones32[:], data1=tot[:], initial=0.0,
                                                                   op0=ALU.mult, op1=ALU.add), ["ones32", "tot"], ["cum"])
                    cp("vector", cnt[:, 0:1], cum[:, NTm - 1:NTm], ["cum"], ["cnt"])
                    ts("vector", cmpm[:], cm[0:32, o_mrow:o_mrow + MMAX], cnt[:, 0:1], ALU.is_lt, ["cm", "cnt"], ["cmpm"])
                    red(cnt[:, 1:2], cmpm[:], ALU.add, AX.X, ["cmpm"], ["cnt"])
                    ts("vector", cnt[:, 2:3], cnt[:, 1:2], float(BLK), ALU.mult, ["cnt"], ["cnt"])
                    mm(psSm[0:32, 0:1], cm[0:32, 0:32], cnt[:, 2:3], True, True, ["cm", "cnt"], ["psSm"])
                    tt("vector", cnt[:, 3:4], psSm[0:32, 0:1], cnt[:, 2:3], ALU.add, ["psSm", "cnt"], ["cnt"])
                    tt("vector", base[:], cum[:], tot[:], ALU.subtract, ["cum", "tot"], ["base"])
                    ts("vector", base[:], base[:], psSm[0:32, 0:1], ALU.add, ["base", "psSm"], ["base"])
                    bb = base[:]
                    i32 = cm[0:32, o_i32:o_i32 + 32]
                    p.add("vector", lambda e: e.tensor_tensor(
                        out=R[:], in0=bass.AP(tensor=bb.tensor, offset=bb.offset, ap=[list(bb.ap[0]), [1, NTm], [0, 32]]),
                        in1=bass.AP(tensor=i32.tensor, offset=i32.offset, ap=[list(i32.ap[0]), [0, NTm], [1, 32]]),
                        op=ALU.mult), ["base", "cm"], ["R"])
                    cp("vector", POS[:].rearrange("p t e -> p (t e)"), psR[:, 0:NC_], ["psR"], ["POS"])
                    Rf = R[:].rearrange("p t e -> p (t e)")
                    for c_ in range(NPS):
                        n_ = min(512, NC_ - c_ * 512)
                        mm(psR[:, c_ * 512:c_ * 512 + n_], onesf[:], Rf[:, c_ * 512:c_ * 512 + n_], True, True, ["onesf", "R", "POS"], ["psR"])
                    tt("vector", POS[:].rearrange("p t e -> p (t e)"), POS[:].rearrange("p t e -> p (t e)"), psR[:, 0:NC_],
                       ALU.add, ["psR", "POS"], ["POS"])
                    for k_, oh in enumerate((oh1, oh2)):
                        tt("vector", tmpP[:], oh[:], POS[:], ALU.mult, ["oh1", "oh2", "POS"], ["tmpP"])
                        red(dstf[:, :, k_], tmpP[:], ALU.add, AX.X, ["tmpP"], ["dstf"])
                    cp("vector", DEST[:, 0:NTm, :], dstf[:], ["dstf"], ["DEST"])
                    ts("vector", dg32[:], i32, cnt[:, 3:4], ALU.mult, ["cm", "cnt"], ["dg32"])
                    mm(psSm[:, 32:64], onesf[:], dg32[:], True, True, ["onesf", "dg32"], ["psSm"])
                    cp("vector", pendr[:], psSm[:, 32:64], ["psSm"], ["pendr"])
                    pr_ = pendr[:]
                    nrow = cm[:, o_nrow:o_nrow + NBLK]
                    p.add("vector", lambda e: e.tensor_tensor(
                        out=cmpb[:], in0=bass.AP(tensor=pr_.tensor, offset=pr_.offset, ap=[list(pr_.ap[0]), [0, NBLK], [1, 32]]),
                        in1=bass.AP(tensor=nrow.tensor, offset=nrow.offset, ap=[list(nrow.ap[0]), [1, NBLK], [0, 32]]),
                        op=ALU.is_le), ["pendr", "cm"], ["cmpb"])
                    red(be[:], cmpb[:], ALU.add, AX.X, ["cmpb"], ["be"])
                    ts("vector", be[:], be[:], 31.0, ALU.min, ["be"], ["be"], s2=float(l * NE), op1=ALU.add)
                    ts("vector", idf[:, :, 0], be[:], 128.0, ALU.mult, ["be", "idf"], ["idf"], s2=cm[:, o_kp8:o_kp8 + 1], op1=ALU.add)
                    cp("vector", IDXE[:], idf[:, :, 0], ["idf"], ["IDXE"])
                    ph_end()

                with ExitStack() as ph:
                    hrs = Rot([sbt(ph, "hr%d" % i, [128, D], BF16) for i in range(3)], "hr")
                    for t_ in range(NTm):
                        hr, hk = hrs.next()
                        dma("sync", hr[:], H2d[t_ * 128:(t_ + 1) * 128, :], ["H2d"], [hk])
                        for k_ in range(2):
                            p.add("gpsimd", lambda e, hr=hr, t_=t_, k_=k_: e.indirect_dma_start(
                                out=XBUF[:, :], out_offset=bass.IndirectOffsetOnAxis(ap=DEST[:, t_, k_:k_ + 1], axis=0),
                                in_=hr[:], in_offset=None), [hk, "DEST"], ["XBUF"], dma=True)
                    ph_end()

                with ExitStack() as ph:
                    wgf = sbt(ph, "wgf", [128, 8, HID], F32)
                    wuf = sbt(ph, "wuf", [128, 8, HID], F32)
                    wdf = sbt(ph, "wdf", [128, 4, D], F32)
                    wgbs = Rot([sbt(ph, "wgb%d" % i, [128, 8, HID], BF16) for i in range(2)], "wgb")
                    wubs = Rot([sbt(ph, "wub%d" % i, [128, 8, HID], BF16) for i in range(2)], "wub")
                    wdbs = Rot([sbt(ph, "wdb%d" % i, [128, 4, D], BF16) for i in range(2)], "wdb")
                    xrs = Rot([sbt(ph, "xb%d" % i, [128, RB, D], BF16) for i in range(2)], "xb")
                    XT = sbt(ph, "XT", [128, 8, BLK], BF16)
                    sgt = sbt(ph, "sgt", [128, BLK], F32)
                    aT = sbt(ph, "aT", [128, 4, BLK], BF16)
                    yos = Rot([sbt(ph, "yo%d" % i, [128, D], BF16) for i in range(2)], "yo")
                    psX = pst(ph, "psX", [128, 8, 128], BF16)
                    psG = Rot([pst(ph, "psG%d" % i, [128, BLK]) for i in range(2)], "psG")
                    psU = Rot([pst(ph, "psU%d" % i, [128, BLK]) for i in range(2)], "psU")
                    psD = Rot([pst(ph, "psD%d" % i, [128, 512]) for i in range(2)], "psD")
                    for n in range(-(-(2 * NTm * 128) // BLK) + NE):
                        for (wf_, src_, wk_) in ((wgf, ex_g8, "wgf"), (wuf, ex_u8, "wuf"), (wdf, ex_d4, "wdf")):
                            p.add("gpsimd", lambda e, n=n, wf_=wf_, src_=src_: e.indirect_dma_start(
                                out=wf_[:].rearrange("p a b -> p (a b)"), out_offset=None, in_=src_,
                                in_offset=bass.IndirectOffsetOnAxis(ap=IDXE[:, n:n + 1], axis=0)), ["IDXE"], [wk_], dma=True)
                        wgb, gk = wgbs.next()
                        wub, uk = wubs.next()
                        wdb, dk = wdbs.next()
                        cp("vector", wgb[:], wgf[:], ["wgf"], [gk])
                        cp("gpsimd", wub[:], wuf[:], ["wuf"], [uk])
                        cp("scalar", wdb[:], wdf[:], ["wdf"], [dk])
                        xb, xk = xrs.next()
                        dma("sync", xb[:], XBUF[n * BLK:(n + 1) * BLK, :].rearrange("(r p) d -> p r d", p=128), ["XBUF"], [xk])
                        for rb in range(RB):
                            for kt in range(8):
                                tr(psX[:, kt, :], _ap(xb[:], rb * D + kt, [[8, 128]]), identb[:], [xk, "identb"], ["psX"])
                            cp("scalar" if rb % 2 else "vector", XT[:, :, rb * 128:(rb + 1) * 128], psX[:], ["psX"], ["XT"])
                        for hc in range(4):
                            pg, pgk = psG.next()
                            pu, puk = psU.next()
                            for kt in range(8):
                                mm(pg[:], _ap(wgb[:], kt * HID + hc, [[4, 128]]), XT[:, kt, :], kt == 0, kt == 7, [gk, "XT"], [pgk])
                            for kt in range(8):
                                mm(pu[:], _ap(wub[:], kt * HID + hc, [[4, 128]]), XT[:, kt, :], kt == 0, kt == 7, [uk, "XT"], [puk])
                            act(sgt[:], pg[:], AF.Silu, [pgk], ["sgt"])
                            tt("vector", aT[:, hc, :], sgt[:], pu[:], ALU.mult, ["sgt", puk], ["aT"])
                        for rb in range(RB):
                            yo, yk = yos.next()
                            for half in range(2):
                                pd, pdk = psD.next()
                                for hc in range(4):
                                    mm(pd[:], aT[:, hc, rb * 128:(rb + 1) * 128], wdb[:, hc, half * 512:(half + 1) * 512],
                                       hc == 0, hc == 3, [dk, "aT"], [pdk])
                                cp("scalar" if half else "vector", yo[:, half * 512:(half + 1) * 512], pd[:], [pdk], [yk])
                            r0 = n * BLK + rb * 128
                            dma("sync", YBUF[r0:r0 + 128, :], yo[:], [yk], ["YBUF"])
                    ph_end()

                with ExitStack() as ph:
                    r0s = Rot([sbt(ph, "r0_%d" % i, [128, D], BF16) for i in range(4)], "r0_")
                    r1s = Rot([sbt(ph, "r1_%d" % i, [128, D], BF16) for i in range(4)], "r1_")
                    xr = Rot([sbt(ph, "xq%d" % i, [128, D], F32) for i in range(2)], "xq")
                    yfs = Rot([sbt(ph, "yf%d" % i, [128, D], F32) for i in range(2)], "yf")
                    rr2s = Rot([sbt(ph, "rr2_%d" % i, [128, D], F32) for i in range(2)], "rr2_")
                    xo = Rot([sbt(ph, "xo%d" % i, [128, D], F32) for i in range(2)], "xo")
                    lg = sbt(ph, "lg2", [128, D], F32)
                    lb = sbt(ph, "lb2", [128, D], F32)
                    g2b = sbt(ph, "g2b", [128, D], F32)
                    lntmps = [{"stats": sbt(ph, "lnst2", [128, 2, 6], F32), "mv": sbt(ph, "lnmv2", [128, 2], F32),
                               "rstd": sbt(ph, "lnrs2", [128, 1], F32)} for _ in range(2)]
                    lni = [0]
                    dma("sync", lg[:], bass.AP(tensor=ln2_g.tensor, offset=ln2_g.offset + l * D, ap=[[0, 128], [1, D]]), [], ["lnconst"])
                    dma("sync", lb[:], bass.AP(tensor=ln2_b.tensor, offset=ln2_b.offset + l * D, ap=[[0, 128], [1, D]]), [], ["lnconst"])
                    items = [(kind, b, tok0, tI) for (kind, b, tok0, Ls) in act_seqs for tI in range(Ls // 128)]

                    def emit_gather(it):
                        gt_ = (it[2] + it[3] * 128) // 128
                        r0, r0k = r0s.next()
                        r1, r1k = r1s.next()
                        for (rt, rk, k_) in ((r0, r0k, 0), (r1, r1k, 1)):
                            p.add("gpsimd", lambda e, rt=rt, gt_=gt_, k_=k_: e.indirect_dma_start(
                                out=rt[:], out_offset=None, in_=YBUF[:, :],
                                in_offset=bass.IndirectOffsetOnAxis(ap=DEST[:, gt_, k_:k_ + 1], axis=0)), ["YBUF", "DEST"], [rk], dma=True)
                        return (r0, r0k, r1, r1k)

                    LOOK = 2
                    pre = [emit_gather(it) for it in items[:LOOK]]
                    for ii, (kind, b, tok0, tI) in enumerate(items):
                        if tI == 0:
                            mrow = b if kind == "lat" else NB
                            mo = MODS.offset + (l * (NB + 1) + mrow) * 6 * D
                            dma("sync", g2b[:], bass.AP(tensor=MODS.tensor, offset=mo + 5 * D, ap=[[0, 128], [1, D]]), ["MODS"], ["g2b"])
                        if ii + LOOK < len(items):
                            pre.append(emit_gather(items[ii + LOOK]))
                        r0, r0k, r1, r1k = pre.pop(0)
                        if True:
                            tk0 = tok0 + tI * 128
                            gt = tk0 // 128
                            xt, xk = xr.next()
                            dma("sync", xt[:], X1d[tk0:tk0 + 128, :], ["X1d"], [xk])
                            yf, yfk = yfs.next()
                            rr_, rrk = rr2s.next()
                            ts("vector", yf[:], r0[:], WTS[:, gt, 0:1], ALU.mult, [r0k, "WTS"], [yfk])
                            stt(yf[:], r1[:], WTS[:, gt, 1:2], yf[:], ALU.mult, ALU.add, [r1k, "WTS", yfk], [yfk])
                            tt("gpsimd", yf[:], yf[:], g2b[:], ALU.mult, [yfk, "g2b"], [yfk])
                            stt(rr_[:], xt[:], ALPHA, yf[:], ALU.mult, ALU.add, [xk, yfk], [rrk])
                            xo_, xok = xo.next()
                            lni[0] += 1
                            layer_norm_rows("ln2%d" % (lni[0] % 2), rr_, rrk, lg, lb, xo_, xok, lntmps[lni[0] % 2])
                            if last:
                                dma("sync", out[tk0:tk0 + 128, :], xo_[:], [xok], ["out"])
                            else:
                                dma("sync", X2d[tk0:tk0 + 128, :], xo_[:], [xok], ["X2d"])
                    ph_end(final=last)
    return nc


def _const_tables(cfg):
    L, LC, BLK, NB = cfg.L, cfg.LC, cfg.BLK, cfg.NB
    f32 = np.float32
    c = {}
    c["c_ident"] = np.eye(128, dtype=f32)
    n_freq = 16
    t = np.arange(L)
    pos = np.stack([(t // GRID_W).astype(f32), (t % GRID_W).astype(f32)], 0)
    inv = (10000.0 ** (-np.arange(n_freq, dtype=f32) / n_freq)).astype(f32)
    rope = np.zeros((2, 128, L), f32)
    for n in range(128):
        a, r, f = (n // 32) % 2, (n // 16) % 2, n % 16
        ang = (pos[a] * inv[f]).astype(f32)
        rope[0, n] = np.cos(ang)
        rope[1, n] = np.sin(ang) * (-1.0 if r == 0 else 1.0)
    c["c_rope"] = rope

    def hy_tables(Lf):
        tt_ = np.linspace(0.0, 1.0, Lf, dtype=f32)
        w = (2.0 * math.pi * np.arange(Lf, dtype=f32) / Lf).astype(f32)
        fb = np.linspace(1e-4, HY_BANDS - 1, HY_BANDS, dtype=f32)
        emb = np.concatenate([tt_[:, None], np.cos(fb[None, :] * w[:, None]), -np.sin(fb[None, :] * w[:, None])], -1).astype(f32)
        max_decay = math.log(1e-2) / 0.3
        min_decay = math.log(1e-2) / 1.5
        deltas = np.abs(np.linspace(min_decay, max_decay, 256, dtype=f32))
        window = (np.exp(-tt_[:, None] * deltas[None, :]) + 0.05).astype(f32)
        pf = np.arange(Lf - 1, -1, -1)
        pb = np.concatenate([np.arange(1, Lf), [0]])
        e = np.stack([emb[pf].T, emb[pb].T], 0).astype(f32)
        wn = np.stack([window[pf].T, window[pb].T], 0).astype(f32)
        wn[1, :, Lf - 1] = 0.0
        return np.ascontiguousarray(e), np.ascontiguousarray(wn)

    c["c_emb_L"], c["c_win_L"] = hy_tables(L)
    c["c_emb_C"], c["c_win_C"] = hy_tables(LC)
    T = NB * (L + LC)
    NBLK = -(-(2 * T) // BLK) + NE
    MMAX = -(-(2 * T) // BLK) + 1
    cm = np.zeros((128, 128 + 8 + 4 + MMAX + NBLK + 32), f32)
    pp = np.arange(128)
    cm[:, 0:128] = (pp[:, None] < pp[None, :]).astype(f32)
    cm[:, 128:136] = np.arange(8)[None, :] * 128 + pp[:, None]
    cm[:, 136:140] = np.arange(4)[None, :] * 128 + pp[:, None]
    cm[:, 140:140 + MMAX] = (np.arange(MMAX) * BLK)[None, :]
    cm[:, 140 + MMAX:140 + MMAX + NBLK] = (np.arange(NBLK) * BLK)[None, :]
    cm[0:32, 140 + MMAX + NBLK:] = np.eye(32, dtype=f32)
    c["c_moe"] = cm
    return c


def _core_inputs(cfg, inp, core, consts):
    NB, L, LC = cfg.NB, cfg.L, cfg.LC
    f32 = np.float32
    bs = slice(core * NB, (core + 1) * NB)
    m = dict(consts)
    m["x"] = np.ascontiguousarray(inp["x"][bs].reshape(NB * L, D))
    m["ctx"] = np.ascontiguousarray(inp["ctx"][bs].reshape(NB * LC, D))
    cc = np.concatenate([inp["c"][bs], inp["c_ctx"][None, :]], 0)
    m["cT"] = np.ascontiguousarray(cc.T.reshape(8, 128, NB + 1).transpose(1, 0, 2))
    for k in ("ada_w", "ada_b", "w_in", "gm_ln_g", "gm_ln_b", "hy_f_w1", "hy_f_w2", "hy_f_w3", "da_norm_g",
              "p_a", "p_b", "p_c", "w_out", "ln1_g", "ln1_b", "ln2_g", "ln2_b"):
        m[k] = inp[k]
    m["gm_wsT"] = np.ascontiguousarray(inp["gm_ws"].transpose(0, 3, 1, 2))
    m["gm_bsT"] = np.ascontiguousarray(inp["gm_bs"].transpose(0, 2, 1))
    cw = np.concatenate([inp["hy_conv_w"], inp["hy_conv_b"][:, None, :]], 1)
    m["hy_cw"] = np.ascontiguousarray(cw.reshape(DEPTH, 4, 6, 128).transpose(0, 3, 2, 1))
    m["hy_f_b1"] = np.ascontiguousarray(inp["hy_f_b1"][:, :, None])
    m["hy_f_b2"] = np.ascontiguousarray(inp["hy_f_b2"][:, :, None])
    m["hy_b3T"] = np.ascontiguousarray(inp["hy_f_b3"].reshape(DEPTH, 4, 128).transpose(0, 2, 1))
    m["hy_skipT"] = np.ascontiguousarray(inp["hy_skip"].reshape(DEPTH, 2, 128).transpose(0, 2, 1))
    m["da_l"] = np.ascontiguousarray(np.stack([inp["da_lq1"], inp["da_lk1"], inp["da_lq2"], inp["da_lk2"]], 1))
    m["moe_wr"] = np.ascontiguousarray(np.concatenate([inp["moe_wg"], inp["moe_we"]], -1))
    m["moe_br"] = np.ascontiguousarray(np.concatenate([inp["moe_bg"], inp["moe_be"]], -1))
    m["ex_w_gate"] = inp["ex_w_gate"].reshape(DEPTH * NE * D, HID)
    m["ex_w_up"] = inp["ex_w_up"].reshape(DEPTH * NE * D, HID)
    m["ex_w_down"] = inp["ex_w_down"].reshape(DEPTH * NE * HID, D)
    return {k: np.ascontiguousarray(np.asarray(v, dtype=f32)) for k, v in m.items()}


def kernel(**inputs):
    cfg = Cfg()
    inp = {k: np.asarray(v) for k, v in inputs.items()}
    n_cores = inp["x"].shape[0] // cfg.NB
    nc = build(cfg)
    consts = _const_tables(cfg)
    in_maps = [_core_inputs(cfg, inp, c, consts) for c in range(n_cores)]
    res = run_bass_kernel_spmd(nc, in_maps, core_ids=list(range(n_cores)))
    outs = [np.asarray(r["out"]).reshape(cfg.NB, cfg.L, D) for r in res.results]
    return np.concatenate(outs, 0).astype(np.float32)
```
